# Optimizing a Trainium2 kernel written in Bass

```python
import math, functools
import jax, jax.numpy as jnp
from jax import lax
import numpy as np

D_MODEL = 1024
BATCH = 16
SEQ = 2048
DEPTH = 2

GRID_W = 64
CTX_LEN = 256
D_MIX = D_MODEL
MIX_W = D_MIX // 4
EPS = 1e-6
GLA_HEADS = 4
GLA_DK = MIX_W // GLA_HEADS // 2
GLA_DV = MIX_W // GLA_HEADS
GLA_RANK = 16
GLA_GATE_NORM = 16.0
GLA_CHUNK = 16
RET_HEADS = 4
RET_DK = MIX_W // RET_HEADS
RET_DV = MIX_W // RET_HEADS
RET_CHUNK = 64
ROPE_BASE = 10000.0
S5_GROUP_CH = 16
S5_GROUPS = MIX_W // S5_GROUP_CH
S5_STATE = 64
DN_HEADS = 4
DN_DK = MIX_W // DN_HEADS
DN_DV = MIX_W // DN_HEADS
DN_CONV = 5
DN_CHUNK = 64
N_EXPERTS = 16
N_GROUPS = 4
EXPERTS_PER_GROUP = N_EXPERTS // N_GROUPS
TOP_K = 2
D_FF_EXPERT = D_MODEL // 2
N_MOD = 6
IN_SIZES = (
    GLA_HEADS * GLA_DK, GLA_HEADS * GLA_DK, GLA_HEADS * GLA_DV, GLA_HEADS * GLA_DV, GLA_RANK, GLA_RANK,
    RET_HEADS * RET_DK, RET_HEADS * RET_DK, RET_HEADS * RET_DV, RET_HEADS * RET_DV,
    MIX_W,
    DN_HEADS * DN_DK, DN_HEADS * DN_DK, DN_HEADS * DN_DV, DN_HEADS * DN_DV, 2 * DN_HEADS, 2 * DN_HEADS,
)
D_IN = sum(IN_SIZES)
F32 = jnp.float32

kernel_name = 'hybrid_dit_gla_ret_s5_gdn_moe'


def _rmsnorm(x, g=None):
    xf = x.astype(F32)
    y = xf * lax.rsqrt(jnp.mean(xf * xf, axis=-1, keepdims=True) + EPS)
    if g is not None:
        y = y * g.astype(F32)
    return y.astype(x.dtype)


def _groupnorm(x):
    xf = x.astype(F32)
    mu = jnp.mean(xf, axis=-1, keepdims=True)
    var = jnp.mean(jnp.square(xf - mu), axis=-1, keepdims=True)
    return (xf - mu) * lax.rsqrt(var + EPS)


def _l2norm(x):
    xf = x.astype(F32)
    return xf * lax.rsqrt(jnp.sum(xf * xf, axis=-1, keepdims=True) + EPS)


def _modulate(t, g, shift, scale):
    return _rmsnorm(t, g) * (1.0 + scale) + shift


def _split_seq(ts, lc):
    return tuple(t[:, :lc] for t in ts), tuple(t[:, lc:] for t in ts)


def _flip(ts):
    return tuple(jnp.flip(t, axis=1) for t in ts)


def _prefix_scan(run, ctx_args, lat_args, s0):
    o_ctx, s_ctx = run(*ctx_args, s0)
    o_lat, _ = run(*lat_args, s_ctx)
    return o_ctx, o_lat


def _bidirectional(run_f, run_b, ctx_f, lat_f, ctx_b, lat_b, s0):
    of_c, of_l = _prefix_scan(run_f, ctx_f, lat_f, s0)
    ob_c, ob_l = _prefix_scan(run_b, _flip(ctx_b), _flip(lat_b), s0)
    return jnp.concatenate([of_c + jnp.flip(ob_c, 1), of_l + jnp.flip(ob_l, 1)], axis=1)


def _chunks(t, c):
    b, l, h, d = t.shape
    return t.astype(F32).reshape(b, l // c, c, h, d).transpose(0, 1, 3, 2, 4)


def _unchunk(t):
    b, n, h, c, d = t.shape
    return t.transpose(0, 1, 3, 2, 4).reshape(b, n * c, h, d)


def _gla_chunk(q, k, v, log_a, s0):
    C = GLA_CHUNK
    qc, kc, vc, lac = (_chunks(t, C) for t in (q, k, v, log_a))
    b = jnp.cumsum(lac, axis=3)
    incl = jnp.tril(jnp.ones((C, C), bool))[:, :, None]
    rel = b[:, :, :, :, None, :] - b[:, :, :, None, :, :]
    dec = jnp.exp(jnp.where(incl, rel, -jnp.inf))
    scores = jnp.einsum('bnhrd,bnhsd,bnhrsd->bnhrs', qc, kc, dec)
    o_intra = jnp.einsum('bnhrs,bnhsv->bnhrv', scores, vc)
    b_last = b[:, :, :, -1:]
    q_in = qc * jnp.exp(b)
    k_out = kc * jnp.exp(b_last - b)

    def step(S, xs):
        qi, ki, vi, bl = xs
        o = jnp.einsum('bhrd,bhdv->bhrv', qi, S)
        S = jnp.exp(bl[:, :, 0, :, None]) * S + jnp.einsum('bhsd,bhsv->bhdv', ki, vi)
        return S, o

    xs = tuple(jnp.moveaxis(t, 1, 0) for t in (q_in, k_out, vc, b_last))
    s_fin, o_inter = lax.scan(step, s0, xs)
    return _unchunk(o_intra + jnp.moveaxis(o_inter, 0, 1)), s_fin


def _ret_chunk(q, k, v, s0, log_gamma):
    C = RET_CHUNK
    qc, kc, vc = (_chunks(t, C) for t in (q, k, v))
    lg = log_gamma.astype(F32)[:, None, None]
    pos = jnp.arange(C, dtype=F32)
    rel = pos[:, None] - pos[None, :]
    dmat = jnp.exp(jnp.where(rel >= 0, rel * lg, -jnp.inf))
    scores = jnp.einsum('bnhrd,bnhsd->bnhrs', qc, kc) * dmat
    o_intra = jnp.einsum('bnhrs,bnhsv->bnhrv', scores, vc)
    q_in = qc * jnp.exp((pos + 1.0)[:, None] * lg)
    k_out = kc * jnp.exp((C - 1.0 - pos)[:, None] * lg)
    chunk_dec = jnp.exp(C * lg)

    def step(S, xs):
        qi, ki, vi = xs
        o = jnp.einsum('bhrd,bhdv->bhrv', qi, S)
        S = chunk_dec * S + jnp.einsum('bhsd,bhsv->bhdv', ki, vi)
        return S, o

    xs = tuple(jnp.moveaxis(t, 1, 0) for t in (q_in, k_out, vc))
    s_fin, o_inter = lax.scan(step, s0, xs)
    return _unchunk(o_intra + jnp.moveaxis(o_inter, 0, 1)), s_fin


def _delta_chunk(q, k, v, beta, log_a, s0):
    C = DN_CHUNK
    dk = q.shape[-1]
    qc, kc, vc = (_chunks(t, C) for t in (q, k, v))
    bc = _chunks(beta[..., None], C)[..., 0]
    g = jnp.cumsum(_chunks(log_a[..., None], C)[..., 0], axis=-1)
    pos = jnp.arange(C)
    strict = pos[:, None] > pos[None, :]
    incl = pos[:, None] >= pos[None, :]
    rel = g[..., :, None] - g[..., None, :]
    kk = jnp.einsum('bnhrd,bnhsd->bnhrs', kc, kc)
    a_mat = jnp.eye(C, dtype=F32) + bc[..., None] * kk * jnp.exp(jnp.where(strict, rel, -jnp.inf))
    rhs = jnp.concatenate([(bc * jnp.exp(g))[..., None] * kc, bc[..., None] * vc], axis=-1)
    sol = lax.linalg.triangular_solve(a_mat, rhs, left_side=True, lower=True, unit_diagonal=True)
    w, u0 = sol[..., :dk], sol[..., dk:]
    qk = jnp.einsum('bnhrd,bnhsd->bnhrs', qc, kc) * jnp.exp(jnp.where(incl, rel, -jnp.inf))
    q_in = qc * jnp.exp(g)[..., None]
    k_out = kc * jnp.exp(g[..., -1:] - g)[..., None]
    g_last = g[..., -1]

    def step(S, xs):
        qi, ki, wi, ui, qki, gl = xs
        u = ui - jnp.einsum('bhrd,bhdv->bhrv', wi, S)
        o = jnp.einsum('bhrd,bhdv->bhrv', qi, S) + jnp.einsum('bhrs,bhsv->bhrv', qki, u)
        S = jnp.exp(gl)[..., None, None] * S + jnp.einsum('bhsd,bhsv->bhdv', ki, u)
        return S, o

    xs = tuple(jnp.moveaxis(t, 1, 0) for t in (q_in, k_out, w, u0, qk, g_last))
    s_fin, o_inter = lax.scan(step, s0, xs)
    return _unchunk(jnp.moveaxis(o_inter, 0, 1)), s_fin


def _s5_scan(bu, h0, lam_bar):
    a = jnp.broadcast_to(lam_bar, (1,) + bu.shape[1:])

    def combine(e1, e2):
        a1, b1 = e1
        a2, b2 = e2
        return a1 * a2, a2 * b1 + b2

    a_cum, b_cum = lax.associative_scan(combine, (a, bu), axis=1)
    h = b_cum + a_cum * h0[:, None]
    return h, h[:, -1]


def _axial_rope(t, rows, cols):
    nf = t.shape[-1] // 4
    inv = ROPE_BASE ** (-jnp.arange(nf, dtype=F32) / nf)
    ang = jnp.concatenate([rows[:, None] * inv, cols[:, None] * inv], axis=-1)[None, :, None, :]
    cos, sin = jnp.cos(ang), jnp.sin(ang)
    tf = t.astype(F32).reshape(t.shape[:-1] + (t.shape[-1] // 2, 2))
    t1, t2 = tf[..., 0], tf[..., 1]
    return jnp.stack([t1 * cos - t2 * sin, t1 * sin + t2 * cos], axis=-1).reshape(t.shape)


def _short_conv(t, w):
    return lax.conv_general_dilated(t, w.astype(t.dtype)[:, None, :], window_strides=(1,),
                                    padding=[(DN_CONV // 2, DN_CONV // 2)],
                                    dimension_numbers=('NWC', 'WIO', 'NWC'),
                                    feature_group_count=t.shape[-1])


def _gla_mixer(q, k, v, g, gz_f, gz_b, lc, gk_w, gk_b):
    B_, T, _ = q.shape
    qh = q.astype(F32).reshape(B_, T, GLA_HEADS, GLA_DK) * GLA_DK ** -0.5
    kh = k.astype(F32).reshape(B_, T, GLA_HEADS, GLA_DK)
    vh = v.astype(F32).reshape(B_, T, GLA_HEADS, GLA_DV)

    def log_decay(z, d):
        logit = z.astype(F32) @ gk_w[d].astype(F32) + gk_b[d].astype(F32)
        return (jax.nn.log_sigmoid(logit) / GLA_GATE_NORM).reshape(B_, T, GLA_HEADS, GLA_DK)

    ctx_f, lat_f = _split_seq((qh, kh, vh, log_decay(gz_f, 0)), lc)
    ctx_b, lat_b = _split_seq((qh, kh, vh, log_decay(gz_b, 1)), lc)
    s0 = jnp.zeros((B_, GLA_HEADS, GLA_DK, GLA_DV), F32)
    o = _bidirectional(_gla_chunk, _gla_chunk, ctx_f, lat_f, ctx_b, lat_b, s0)
    o = _rmsnorm(o) * jax.nn.silu(g.astype(F32).reshape(B_, T, GLA_HEADS, GLA_DV))
    return o.reshape(B_, T, MIX_W)


def _ret_mixer(q, k, v, g, lc, rows, cols, decay_logit):
    B_, T, _ = q.shape
    qh = q.astype(F32).reshape(B_, T, RET_HEADS, RET_DK) * RET_DK ** -0.5
    kh = k.astype(F32).reshape(B_, T, RET_HEADS, RET_DK)
    vh = v.astype(F32).reshape(B_, T, RET_HEADS, RET_DV)
    (qc, kc, vc), (ql, kl, vl) = _split_seq((qh, kh, vh), lc)
    ctx_args = (qc, kc, vc)
    lat_args = (_axial_rope(ql, rows, cols), _axial_rope(kl, rows, cols), vl)
    log_gamma = jax.nn.log_sigmoid(decay_logit.astype(F32))
    run_f = functools.partial(_ret_chunk, log_gamma=log_gamma[0])
    run_b = functools.partial(_ret_chunk, log_gamma=log_gamma[1])
    s0 = jnp.zeros((B_, RET_HEADS, RET_DK, RET_DV), F32)
    o = _bidirectional(run_f, run_b, ctx_args, lat_args, ctx_args, lat_args, s0)
    o = _groupnorm(o) * jax.nn.silu(g.astype(F32).reshape(B_, T, RET_HEADS, RET_DV))
    return o.reshape(B_, T, MIX_W)


def _s5_mixer(u, lc, lam_re, lam_im, log_dt, b_re, b_im, c_re, c_im, d_skip, glu_w, glu_b):
    B_, T, _ = u.shape
    uf = u.astype(F32)
    ug = uf.reshape(B_, T, S5_GROUPS, S5_GROUP_CH)
    bu = lax.complex(jnp.einsum('gpc,btgc->btgp', b_re.astype(F32), ug),
                     jnp.einsum('gpc,btgc->btgp', b_im.astype(F32), ug))
    lam = lax.complex(lam_re.astype(F32), lam_im.astype(F32))
    dt = jnp.exp(log_dt.astype(F32))[..., None]
    lam_bar = jnp.exp(lam * dt)
    b_scale = (lam_bar - 1.0) / lam
    bu_f, bu_b = b_scale[0] * bu, b_scale[1] * bu
    ctx_f, lat_f = _split_seq((bu_f,), lc)
    ctx_b, lat_b = _split_seq((bu_b,), lc)
    run_f = functools.partial(_s5_scan, lam_bar=lam_bar[0])
    run_b = functools.partial(_s5_scan, lam_bar=lam_bar[1])
    h0 = jnp.zeros((B_, S5_GROUPS, S5_STATE), jnp.complex64)
    h = _bidirectional(run_f, run_b, ctx_f, lat_f, ctx_b, lat_b, h0)
    cmat = lax.complex(c_re.astype(F32), c_im.astype(F32))
    y = jnp.real(jnp.einsum('gcp,btgp->btgc', cmat, h)).reshape(B_, T, MIX_W) + d_skip.astype(F32) * uf
    y = jax.nn.gelu(y)
    return y * jax.nn.sigmoid(y @ glu_w.astype(F32) + glu_b.astype(F32))


def _dn_mixer(q, k, v, g, a, b, lc, conv_w, a_log, dt_bias):
    B_, T, _ = q.shape
    qkv = jnp.concatenate([q, k, v], axis=-1)
    qkv = jax.nn.silu(jnp.concatenate([_short_conv(qkv[:, :lc], conv_w),
                                       _short_conv(qkv[:, lc:], conv_w)], axis=1))
    q, k, v = jnp.split(qkv, [DN_HEADS * DN_DK, 2 * DN_HEADS * DN_DK], axis=-1)
    qh = _l2norm(q.reshape(B_, T, DN_HEADS, DN_DK)) * DN_DK ** -0.5
    kh = _l2norm(k.reshape(B_, T, DN_HEADS, DN_DK))
    vh = v.astype(F32).reshape(B_, T, DN_HEADS, DN_DV)
    a = a.astype(F32).reshape(B_, T, 2, DN_HEADS)
    b = b.astype(F32).reshape(B_, T, 2, DN_HEADS)
    log_a = -jnp.exp(a_log.astype(F32)) * jax.nn.softplus(a + dt_bias.astype(F32))
    beta = jax.nn.sigmoid(b)
    ctx_f, lat_f = _split_seq((qh, kh, vh, beta[:, :, 0], log_a[:, :, 0]), lc)
    ctx_b, lat_b = _split_seq((qh, kh, vh, beta[:, :, 1], log_a[:, :, 1]), lc)
    s0 = jnp.zeros((B_, DN_HEADS, DN_DK, DN_DV), F32)
    o = _bidirectional(_delta_chunk, _delta_chunk, ctx_f, lat_f, ctx_b, lat_b, s0)
    o = _rmsnorm(o) * jax.nn.silu(g.astype(F32).reshape(B_, T, DN_HEADS, DN_DV))
    return o.reshape(B_, T, MIX_W)


def _token_mixers(h, lc, rows, cols, w_in, gla_gk_w, gla_gk_b, ret_decay_logit, s5_lambda_re, s5_lambda_im,
                  s5_log_dt, s5_b_re, s5_b_im, s5_c_re, s5_c_im, s5_d, s5_glu_w, s5_glu_b,
                  dn_conv_w, dn_a_log, dn_dt_bias):
    p = h @ w_in
    cuts = np.cumsum(IN_SIZES)[:-1].tolist()
    (gq, gk, gv, gg, gz_f, gz_b, rq, rk, rv, rg, su,
     dq, dk, dv, dg, da, db) = jnp.split(p, cuts, axis=-1)
    o_gla = _gla_mixer(gq, gk, gv, gg, gz_f, gz_b, lc, gla_gk_w, gla_gk_b)
    o_ret = _ret_mixer(rq, rk, rv, rg, lc, rows, cols, ret_decay_logit)
    o_s5 = _s5_mixer(su, lc, s5_lambda_re, s5_lambda_im, s5_log_dt, s5_b_re, s5_b_im, s5_c_re, s5_c_im,
                     s5_d, s5_glu_w, s5_glu_b)
    o_dn = _dn_mixer(dq, dk, dv, dg, da, db, lc, dn_conv_w, dn_a_log, dn_dt_bias)
    return jnp.concatenate([o_gla, o_ret, o_s5, o_dn], axis=-1).astype(h.dtype)


def _moe(h, router_w, router_b, w_gate, w_up, w_down):
    scores = jax.nn.sigmoid(h.astype(F32) @ router_w.astype(F32))
    sel = scores + router_b.astype(F32)
    grp_score = lax.top_k(sel.reshape(-1, N_GROUPS, EXPERTS_PER_GROUP), TOP_K)[0].sum(-1)
    best = jnp.argmax(grp_score, axis=-1)
    in_grp = (jnp.arange(N_EXPERTS) // EXPERTS_PER_GROUP)[None, :] == best[:, None]
    _, idx = lax.top_k(jnp.where(in_grp, sel, -jnp.inf), TOP_K)
    w = jnp.take_along_axis(scores, idx, axis=-1)
    w = w / jnp.sum(w, axis=-1, keepdims=True)
    combine = jnp.sum(jax.nn.one_hot(idx, N_EXPERTS, dtype=F32) * w[..., None], axis=1)
    out = jnp.zeros(h.shape, F32)
    for e in range(N_EXPERTS):
        act = jax.nn.silu(h @ w_gate[e]) * (h @ w_up[e])
        out = out + combine[:, e:e + 1] * (act @ w_down[e])
    return out.astype(h.dtype)


def setup_inputs(seed: int = 0) -> dict:
    key = jax.random.key(seed)
    ks = jax.random.split(key, 31)

    def nrm(k, shape, scale):
        return jax.random.normal(k, shape, F32) * scale

    gammas = 1.0 - 2.0 ** (-5.0 - jnp.arange(RET_HEADS, dtype=F32))
    ret_logit = jnp.log(gammas) - jnp.log1p(-gammas)
    s5_n = jnp.arange(S5_STATE, dtype=F32)
    dt_dn = jnp.exp(jax.random.uniform(ks[24], (DEPTH, 2, DN_HEADS), F32, math.log(1e-3), math.log(1e-1)))
    return {
        'x': nrm(ks[0], (BATCH, SEQ, D_MODEL), 1.0),
        'c': nrm(ks[1], (BATCH, D_MODEL), 1.0),
        'ctx': nrm(ks[2], (BATCH, CTX_LEN, D_MODEL), 1.0),
        'c_ctx': nrm(ks[3], (D_MODEL,), 1.0),
        'w_ada': nrm(ks[4], (DEPTH, D_MODEL, N_MOD * D_MODEL), 0.5 * D_MODEL ** -0.5),
        'b_ada': nrm(ks[5], (DEPTH, N_MOD * D_MODEL), 0.02),
        'norm_g': 1.0 + nrm(ks[6], (DEPTH, 2, D_MODEL), 0.02),
        'w_in': nrm(ks[7], (DEPTH, D_MODEL, D_IN), D_MODEL ** -0.5),
        'w_out': nrm(ks[8], (DEPTH, D_MIX, D_MODEL), D_MIX ** -0.5),
        'gla_gk_w': nrm(ks[9], (DEPTH, 2, GLA_RANK, GLA_HEADS * GLA_DK), GLA_RANK ** -0.5),
        'gla_gk_b': nrm(ks[10], (DEPTH, 2, GLA_HEADS * GLA_DK), 0.1),
        'ret_decay_logit': ret_logit + nrm(ks[11], (DEPTH, 2, RET_HEADS), 0.01),
        's5_lambda_re': -0.5 + nrm(ks[12], (DEPTH, 2, S5_GROUPS, S5_STATE), 0.01),
        's5_lambda_im': math.pi * s5_n + nrm(ks[13], (DEPTH, 2, S5_GROUPS, S5_STATE), 0.01),
        's5_log_dt': jax.random.uniform(ks[14], (DEPTH, 2, S5_GROUPS), F32, math.log(1e-3), math.log(1e-1)),
        's5_b_re': nrm(ks[15], (DEPTH, S5_GROUPS, S5_STATE, S5_GROUP_CH), (2 * S5_GROUP_CH) ** -0.5),
        's5_b_im': nrm(ks[16], (DEPTH, S5_GROUPS, S5_STATE, S5_GROUP_CH), (2 * S5_GROUP_CH) ** -0.5),
        's5_c_re': nrm(ks[17], (DEPTH, S5_GROUPS, S5_GROUP_CH, S5_STATE), (2 * S5_STATE) ** -0.5),
        's5_c_im': nrm(ks[18], (DEPTH, S5_GROUPS, S5_GROUP_CH, S5_STATE), (2 * S5_STATE) ** -0.5),
        's5_d': nrm(ks[19], (DEPTH, MIX_W), 1.0),
        's5_glu_w': nrm(ks[20], (DEPTH, MIX_W, MIX_W), MIX_W ** -0.5),
        's5_glu_b': nrm(ks[21], (DEPTH, MIX_W), 0.02),
        'dn_conv_w': nrm(ks[22], (DEPTH, DN_CONV, 3 * DN_HEADS * DN_DK), DN_CONV ** -0.5),
        'dn_a_log': jnp.log(jax.random.uniform(ks[23], (DEPTH, 2, DN_HEADS), F32, 1.0, 16.0)),
        'dn_dt_bias': dt_dn + jnp.log(-jnp.expm1(-dt_dn)),
        'router_w': nrm(ks[25], (D_MODEL, N_EXPERTS), D_MODEL ** -0.5),
        'router_b': nrm(ks[26], (N_EXPERTS,), 0.01),
        'moe_w_gate': nrm(ks[27], (DEPTH, N_EXPERTS, D_MODEL, D_FF_EXPERT), D_MODEL ** -0.5),
        'moe_w_up': nrm(ks[28], (DEPTH, N_EXPERTS, D_MODEL, D_FF_EXPERT), D_MODEL ** -0.5),
        'moe_w_down': nrm(ks[29], (DEPTH, N_EXPERTS, D_FF_EXPERT, D_MODEL), D_FF_EXPERT ** -0.5),
        'final_norm_g': 1.0 + nrm(ks[30], (D_MODEL,), 0.02),
    }


def reference(x, c, ctx, c_ctx, w_ada, b_ada, norm_g, w_in, w_out, gla_gk_w, gla_gk_b, ret_decay_logit,
              s5_lambda_re, s5_lambda_im, s5_log_dt, s5_b_re, s5_b_im, s5_c_re, s5_c_im, s5_d, s5_glu_w,
              s5_glu_b, dn_conv_w, dn_a_log, dn_dt_bias, router_w, router_b, moe_w_gate, moe_w_up,
              moe_w_down, final_norm_g):
    B_, L, D = x.shape
    lc = ctx.shape[1]
    n_rows = L // GRID_W
    pos = jnp.arange(n_rows * GRID_W)
    rows = (pos // GRID_W).astype(F32)
    cols = (pos % GRID_W).astype(F32)
    silu_c = jax.nn.silu(c)
    silu_cc = jax.nn.silu(c_ctx)
    z = ctx
    for i in range(DEPTH):
        last = i == DEPTH - 1
        mod_l = (silu_c @ w_ada[i] + b_ada[i]).reshape(B_, N_MOD, 1, D)
        mod_c = (silu_cc @ w_ada[i] + b_ada[i]).reshape(N_MOD, 1, D)
        h = jnp.concatenate([_modulate(z, norm_g[i, 0], mod_c[0], mod_c[1]),
                             _modulate(x, norm_g[i, 0], mod_l[:, 0], mod_l[:, 1])], axis=1)
        mix = _token_mixers(h, lc, rows, cols, w_in[i], gla_gk_w[i], gla_gk_b[i], ret_decay_logit[i],
                            s5_lambda_re[i], s5_lambda_im[i], s5_log_dt[i], s5_b_re[i], s5_b_im[i],
                            s5_c_re[i], s5_c_im[i], s5_d[i], s5_glu_w[i], s5_glu_b[i],
                            dn_conv_w[i], dn_a_log[i], dn_dt_bias[i])
        if last:
            mix = mix[:, lc:]
        mix = mix @ w_out[i]
        x = x + mod_l[:, 2] * mix[:, -L:]
        if last:
            h2 = _modulate(x, norm_g[i, 1], mod_l[:, 3], mod_l[:, 4])
        else:
            z = z + mod_c[2] * mix[:, :lc]
            h2 = jnp.concatenate([_modulate(z, norm_g[i, 1], mod_c[3], mod_c[4]),
                                  _modulate(x, norm_g[i, 1], mod_l[:, 3], mod_l[:, 4])], axis=1)
        f = _moe(h2.reshape(-1, D), router_w, router_b, moe_w_gate[i], moe_w_up[i],
                 moe_w_down[i]).reshape(h2.shape)
        x = x + mod_l[:, 5] * f[:, -L:]
        if not last:
            z = z + mod_c[5] * f[:, :lc]
    return _rmsnorm(x, final_norm_g)
```

```python
import math
import numpy as np
from contextlib import ExitStack
import concourse.bass as bass
import concourse.mybir as mybir
from concourse.bass_utils import run_bass_kernel_spmd

F32 = mybir.dt.float32
BF16 = mybir.dt.bfloat16
I32 = mybir.dt.int32
AF = mybir.ActivationFunctionType
ALU = mybir.AluOpType
AX = mybir.AxisListType

D = 1024
L = 2048
LC = 256
T = L + LC
NT = T // 128
DEPTH = 2
NB = 2
NE = 16
DFF = 512
EPS = 1e-6
NFM = 27 * 128
NTM = 512
TBLK = [(0, 512), (512, 512), (1024, 512), (1536, 512), (2048, 256)]

ENGS = ['tensor', 'vector', 'scalar', 'gpsimd', 'sync']
SAME_ENGINE_WAIT = True
N_DMA_SEMS = 24


_UID = [0]


def _sbt(nc, name, shape, dt):
    _UID[0] += 1
    return nc.sbuf_tensor("%s_%d" % (name, _UID[0]), shape, dt)


def _pst(nc, name, shape, dt):
    _UID[0] += 1
    return nc.psum_tensor("%s_%d" % (name, _UID[0]), shape, dt)


class Prog:
    def __init__(self, nc, es):
        self.nc = nc
        self.sem = {e: es.enter_context(nc.semaphore("s_" + e)) for e in ENGS}
        self.cnt = {e: 0 for e in ENGS}
        self.dsem = [es.enter_context(nc.semaphore("d_%d" % i)) for i in range(N_DMA_SEMS)]
        self.dval = [0] * N_DMA_SEMS
        self.drr = 0
        self.semobj = {}
        for e in ENGS:
            self.semobj[('e', e)] = self.sem[e]
        for i in range(N_DMA_SEMS):
            self.semobj[('d', i)] = self.dsem[i]
        self.nops = 0
        self.begin()

    def begin(self):
        self.ops = {e: [] for e in ENGS}
        self.known = {e: {} for e in ENGS}
        self.lastw = {}
        self.readers = {}
        self.pending_dma = {e: [] for e in ENGS}

    def op(self, eng, fn, reads=(), writes=(), dma=False):
        deps = set()
        for k in reads:
            t = self.lastw.get(k)
            if t is not None:
                deps.add(t)
        for k in writes:
            t = self.lastw.get(k)
            if t is not None:
                deps.add(t)
            for t in self.readers.get(k, ()):
                deps.add(t)
        waits = []
        if dma:
            idx = self.drr % N_DMA_SEMS
            self.drr += 1
            if self.dval[idx] > 0:
                deps.add((('d', idx), self.dval[idx]))
            self.dval[idx] += 16
            tok = (('d', idx), self.dval[idx])
            inc = (('d', idx), 16)
            self.pending_dma[eng].append(tok)
        else:
            self.cnt[eng] += 1
            tok = (('e', eng), self.cnt[eng])
            inc = (('e', eng), 1)
        kn = self.known[eng]
        best = {}
        for (s, v) in deps:
            if s == ('e', eng) and not dma:
                if eng == 'tensor' or not SAME_ENGINE_WAIT:
                    continue
            if kn.get(s, 0) >= v:
                continue
            if best.get(s, 0) < v:
                best[s] = v
        for s, v in best.items():
            kn[s] = v
            waits.append((s, v))
        self.ops[eng].append((waits, fn, inc))
        self.nops += 1
        for k in reads:
            self.readers.setdefault(k, []).append(tok)
        for k in writes:
            self.lastw[k] = tok
            self.readers[k] = []
        return tok

    def flush(self):
        nc = self.nc
        tails = {}
        for e in ENGS:
            seen = {}
            for (s, v) in self.pending_dma[e]:
                if seen.get(s, 0) < v:
                    seen[s] = v
            tails[e] = [(s, v) for s, v in seen.items() if self.known[e].get(s, 0) < v]
        ops = self.ops
        semobj = self.semobj
        with nc.Block() as block:
            for e in ENGS:
                if not ops[e] and not tails[e]:
                    continue

                def body(eng, e=e):
                    for (waits, fn, inc) in ops[e]:
                        for (s, v) in waits:
                            eng.wait_ge(semobj[s], v)
                        ins = fn(eng)
                        ins.then_inc(semobj[inc[0]], inc[1])
                    for (s, v) in tails[e]:
                        eng.wait_ge(semobj[s], v)
                getattr(block, e)(body)
        self.begin()

    def dma(self, out, in_, reads=(), writes=(), eng='sync'):
        return self.op(eng, lambda g: g.dma_start(out=out, in_=in_), reads, writes, dma=True)

    def mm(self, out, lhsT, rhs, start, stop, reads=(), writes=()):
        return self.op('tensor', lambda t: t.matmul(out, lhsT=lhsT, rhs=rhs, start=start, stop=stop), reads, writes)

    def tr(self, out, in_, ident, reads=(), writes=()):
        return self.op('tensor', lambda t: t.transpose(out, in_, ident), reads, writes)

    def act(self, out, in_, func, reads=(), writes=(), **kw):
        return self.op('scalar', lambda s: s.activation(out=out, in_=in_, func=func, **kw), reads, writes)

    def ts(self, out, in0, s1, s2, op0, op1=None, reads=(), writes=(), eng='vector', **kw):
        if op1 is None:
            return self.op(eng, lambda v: v.tensor_scalar(out=out, in0=in0, scalar1=s1, scalar2=None, op0=op0, **kw), reads, writes)
        return self.op(eng, lambda v: v.tensor_scalar(out=out, in0=in0, scalar1=s1, scalar2=s2, op0=op0, op1=op1, **kw), reads, writes)

    def tt(self, out, in0, in1, op, reads=(), writes=(), eng='vector'):
        return self.op(eng, lambda v: v.tensor_tensor(out=out, in0=in0, in1=in1, op=op), reads, writes)

    def stt(self, out, in0, scalar, in1, op0, op1, reads=(), writes=()):
        return self.op('vector', lambda v: v.scalar_tensor_tensor(out=out, in0=in0, scalar=scalar, in1=in1, op0=op0, op1=op1), reads, writes)

    def copy(self, out, in_, reads=(), writes=(), eng='vector'):
        if eng == 'scalar':
            return self.op(eng, lambda s: s.copy(out=out, in_=in_), reads, writes)
        return self.op(eng, lambda v: v.tensor_copy(out=out, in_=in_), reads, writes)

    def memset(self, out, val, writes=(), eng='vector'):
        return self.op(eng, lambda v: v.memset(out, val), (), writes)

    def recip(self, out, in_, reads=(), writes=()):
        return self.op('vector', lambda v: v.reciprocal(out=out, in_=in_), reads, writes)

    def red(self, out, in_, op, reads=(), writes=()):
        return self.op('vector', lambda v: v.tensor_reduce(out=out, in_=in_, axis=AX.X, op=op), reads, writes)

    def scan(self, out, d0, d1, init, reads=(), writes=()):
        return self.op('vector', lambda v: v.tensor_tensor_scan(out=out, data0=d0, data1=d1, initial=init, op0=ALU.mult, op1=ALU.add), reads, writes)


IN_OFF = {}
_o = 0
for _n, _s in [('gq', 128), ('gk', 128), ('gv', 256), ('gg', 256), ('gzf', 16), ('gzb', 16),
               ('rq', 256), ('rk', 256), ('rv', 256), ('rg', 256), ('su', 256),
               ('dq', 256), ('dk', 256), ('dv', 256), ('dg', 256), ('da', 8), ('db', 8)]:
    IN_OFF[_n] = _o
    _o += _s


def _rope_perm(off):
    main, sw = [], []
    for h in range(4):
        base = off + h * 64
        ev = [base + 2 * i for i in range(32)]
        od = [base + 2 * i + 1 for i in range(32)]
        main += ev + od
        sw += od + ev
    return main, sw


def _fm_cols():
    c = []
    rng = lambda n, k: list(range(IN_OFF[n], IN_OFF[n] + k))
    pad = lambda k: [-1] * k
    c += rng('gq', 128)
    c += rng('gk', 128)
    c += rng('gzf', 16) + pad(16) + rng('gzb', 16) + pad(80)
    m, s = _rope_perm(IN_OFF['rq'])
    c += m + s
    m, s = _rope_perm(IN_OFF['rk'])
    c += m + s
    c += rng('su', 256)
    c += rng('dq', 256) + rng('dk', 256) + rng('dv', 256)
    da, db = rng('da', 8), rng('db', 8)
    c += da[0:4] + pad(28) + da[4:8] + pad(92)
    c += rng('gg', 256) + rng('rg', 256) + rng('dg', 256)
    c += db[0:4] + pad(28) + db[4:8] + pad(92)
    assert len(c) == NFM
    return np.array(c)


def _tm_cols():
    c = []
    for n in ['gv', 'rv']:
        c += list(range(IN_OFF[n], IN_OFF[n] + 256))
    return np.array(c)


def _kmajor(w):
    K, N = w.shape
    return np.ascontiguousarray(w.reshape(K // 128, 128, N).transpose(1, 0, 2))


def _fmvec(v):
    return np.ascontiguousarray(v.reshape(-1, 128).T)


def host_prep(inp):
    f = np.float32
    sh = {}
    sh['ident'] = np.eye(128, dtype=f)
    w_ada = inp['w_ada']
    sh['w_ada'] = np.stack([_kmajor(w_ada[i]) for i in range(DEPTH)])
    sh['b_adaFM'] = np.stack([_fmvec(inp['b_ada'][i]) for i in range(DEPTH)], 1)
    sh['b_ada4'] = np.ascontiguousarray(np.broadcast_to(inp['b_ada'][None], (4, DEPTH, 6 * D)))
    ng = inp['norm_g']
    sh['norm_gFM'] = np.ascontiguousarray(
        np.stack([np.stack([_fmvec(ng[i, n]) for n in range(2)], 1) for i in range(DEPTH)], 1))
    sh['final_g_rep'] = np.ascontiguousarray(np.broadcast_to(inp['final_norm_g'][None], (128, D)))
    fm = _fm_cols()
    tm = _tm_cols()
    w_in = inp['w_in']
    wfm = np.zeros((DEPTH, D, NFM), f)
    wfm[:, :, fm >= 0] = w_in[:, :, fm[fm >= 0]]
    sh['w_inFM'] = np.stack([_kmajor(wfm[i]) for i in range(DEPTH)])
    sh['w_inTM'] = np.stack([_kmajor(w_in[i][:, tm]) for i in range(DEPTH)])
    sh['w_out'] = np.stack([_kmajor(inp['w_out'][i]) for i in range(DEPTH)])
    sh['router_w'] = _kmajor(inp['router_w'])
    sh['router_b_rep'] = np.ascontiguousarray(np.broadcast_to(inp['router_b'][None], (128, NE)))
    sh['moe_wg'] = np.ascontiguousarray(
        inp['moe_w_gate'].reshape(DEPTH, NE, 8, 128, DFF).transpose(0, 1, 3, 2, 4))
    sh['moe_wu'] = np.ascontiguousarray(
        inp['moe_w_up'].reshape(DEPTH, NE, 8, 128, DFF).transpose(0, 1, 3, 2, 4))
    sh['moe_wd'] = np.ascontiguousarray(
        inp['moe_w_down'].reshape(DEPTH, NE, 4, 128, D).transpose(0, 1, 3, 2, 4))
    jj = np.arange(128, dtype=f)
    diff = jj[None, :] - jj[:, None]
    sh['DIFFf'] = np.where(diff >= 0, diff, 1e6).astype(f)
    sh['DIFFb'] = np.where(diff <= 0, -diff, 1e6).astype(f)
    sh['MSKf'] = (diff >= 0).astype(f)
    sh['MSKb'] = (diff <= 0).astype(f)
    sh['POS'] = np.ascontiguousarray(np.stack([np.broadcast_to(jj + 1, (128, 128)), np.broadcast_to(128 - jj, (128, 128)),
                          np.broadcast_to(127 - jj, (128, 128)), np.broadcast_to(jj, (128, 128))], 1)).astype(f)
    blk2 = np.kron(np.eye(2, dtype=f), np.ones((64, 64), f))
    sh['BLK2'] = blk2
    sh['BLK64'] = blk2 / 64.0
    n_rows = L // 64
    pos = np.arange(n_rows * 64)
    rows = (pos // 64).astype(f)
    cols = (pos % 64).astype(f)
    nf = 16
    inv = (np.float32(10000.0) ** (-np.arange(nf, dtype=f) / nf)).astype(f)
    ang = np.concatenate([rows[:, None] * inv, cols[:, None] * inv], -1).astype(f)
    cs, sn = np.cos(ang).T.astype(f), np.sin(ang).T.astype(f)
    sh['COS'] = np.ascontiguousarray(np.concatenate([cs, cs, cs, cs], 0))
    sh['SIN'] = np.ascontiguousarray(np.concatenate([-sn, sn, -sn, sn], 0))
    sh['ret_logit_rep'] = np.ascontiguousarray(np.broadcast_to(inp['ret_decay_logit'].reshape(DEPTH, 1, 8), (DEPTH, 128, 8)))
    hm = np.zeros((128, 4), f)
    for h in range(4):
        hm[h * 32:(h + 1) * 32, h] = 1.0
    sh['HM4'] = hm
    sh['BLKG'] = np.kron(np.eye(4, dtype=f), np.ones((32, 64), f))
    gkw = np.zeros((DEPTH, 128, 128), f)
    gkw[:, 0:16, :] = inp['gla_gk_w'][:, 0]
    gkw[:, 32:48, :] = inp['gla_gk_w'][:, 1]
    sh['gkw'] = gkw
    sh['gkbFM'] = np.ascontiguousarray(inp['gla_gk_b'].transpose(2, 0, 1))
    def s5_state_layout(a):
        a = a.reshape(DEPTH, 2, 8, 2, 64)
        return np.ascontiguousarray(a.transpose(3, 4, 0, 1, 2).reshape(128, DEPTH, 16))
    sh['s5_lre'] = s5_state_layout(inp['s5_lambda_re'])
    sh['s5_lim'] = s5_state_layout(inp['s5_lambda_im'])
    sh['s5_ldt'] = s5_state_layout(np.broadcast_to(inp['s5_log_dt'][..., None], (DEPTH, 2, 16, 64)))
    WB = np.zeros((DEPTH, 128, 8, 2, 128), f)
    WC = np.zeros((DEPTH, 128, 8, 2, 128), f)
    for g in range(16):
        j, gg = g // 2, g % 2
        r0 = (g % 8) * 16
        for ri, (bsrc, csrc) in enumerate([('s5_b_re', 's5_c_re'), ('s5_b_im', 's5_c_im')]):
            WB[:, r0:r0 + 16, j, ri, gg * 64:(gg + 1) * 64] = inp[bsrc][:, g].transpose(0, 2, 1)
            WC[:, gg * 64:(gg + 1) * 64, j, ri, r0:r0 + 16] = inp[csrc][:, g].transpose(0, 2, 1)
    sh['s5_WB'] = WB
    sh['s5_WC'] = WC
    sh['s5_dFM'] = np.ascontiguousarray(inp['s5_d'].reshape(DEPTH, 2, 128).transpose(2, 0, 1))
    sh['s5_gbFM'] = np.ascontiguousarray(inp['s5_glu_b'].reshape(DEPTH, 2, 128).transpose(2, 0, 1))
    sh['s5_gw'] = np.ascontiguousarray(inp['s5_glu_w'].reshape(DEPTH, 2, 128, 256).transpose(0, 2, 1, 3))
    sh['convFM'] = np.ascontiguousarray(inp['dn_conv_w'].reshape(DEPTH, 5, 6, 128).transpose(3, 0, 2, 1))
    par = np.zeros((128, DEPTH, 2), f)
    for d_ in range(2):
        par[32 * d_:32 * d_ + 4, :, 0] = inp['dn_a_log'][:, d_, :].T
        par[32 * d_:32 * d_ + 4, :, 1] = inp['dn_dt_bias'][:, d_, :].T
    sh['dn_par'] = par
    selr = np.zeros((128, 4, 128), f)
    selp = np.zeros((128, 2, 128), f)
    for d_ in range(2):
        for h in range(4):
            selr[32 * d_ + h, h, :] = 1.0
            selp[32 * d_ + h, h // 2, (h % 2) * 64:(h % 2 + 1) * 64] = 1.0
    sh['SELR'] = selr
    mhh = np.zeros((128, 4), f)
    ohh = np.zeros((128, 2, 2, 2, 128), f)
    for d_ in range(2):
        for h in range(4):
            mhh[32 * d_ + h, h] = 1.0
            ohh[32 * d_ + h, h // 2, h % 2, :, :] = 1.0
    sh['MH'] = mhh
    sh['OH'] = ohh.reshape(128, 2, 512)
    hm2 = np.zeros((128, 2), f)
    hm2[0:64, 0] = 1.0
    hm2[64:128, 1] = 1.0
    sh['HM2'] = hm2
    sh['SELP'] = selp
    NEG = -1.0e4
    mbm = np.zeros((128, 2, 4, 128), f)
    mbm[:, 0, 0, :] = np.where(diff >= 0, 0.0, NEG); mbm[:, 0, 1, :] = np.where(diff > 0, 0.0, NEG)
    mbm[:, 1, 0, :] = np.where(diff <= 0, 0.0, NEG); mbm[:, 1, 1, :] = np.where(diff < 0, 0.0, NEG)
    mbm[:, :, 2, :] = mbm[:, :, 0, :]; mbm[:, :, 3, :] = mbm[:, :, 1, :]
    sh['MB'] = mbm
    sel = np.zeros((4, 4, 128), f)
    for j in range(4):
        sel[j, j, :] = 1.0
    sh['sel4'] = sel
    return sh


def core_inputs(inp, core):
    f = np.float32
    b0 = core * NB
    d = {}
    d['x'] = np.ascontiguousarray(inp['x'][b0:b0 + NB])
    d['ctx'] = np.ascontiguousarray(inp['ctx'][b0:b0 + NB])
    cvec = np.stack([inp['c'][b0], inp['c'][b0 + 1], inp['c_ctx'], inp['c_ctx']], 1)
    d['cT'] = np.ascontiguousarray(cvec.reshape(8, 128, 4).transpose(1, 0, 2)).astype(f)
    return d


class K:
    pass


def build(dbg=None):
    dbg = dbg or {}
    nc = bass.Bass("TRN2", target_bir_lowering=False)
    k = K()
    k.nc = nc
    k.dbg = dbg

    def din(name, shape, dt=F32):
        return nc.dram_tensor(name, list(shape), dt, kind="ExternalInput").ap()

    def dscr(name, shape, dt=F32):
        return nc.dram_tensor(name, list(shape), dt, kind="Internal").ap()

    k.x = din('x', [NB, L, D])
    k.ctx = din('ctx', [NB, LC, D])
    k.cT = din('cT', [128, 8, 4])
    k.ident = din('ident', [128, 128])
    k.w_ada = din('w_ada', [DEPTH, 128, 8, 6 * D])
    k.b_adaFM = din('b_adaFM', [128, DEPTH, 48])
    k.b_ada4 = din('b_ada4', [4, DEPTH, 6 * D])
    k.norm_gFM = din('norm_gFM', [128, DEPTH, 2, 8])
    k.final_g_rep = din('final_g_rep', [128, D])
    k.w_inFM = din('w_inFM', [DEPTH, 128, 8, NFM])
    k.w_inTM = din('w_inTM', [DEPTH, 128, 8, NTM])
    k.w_out = din('w_out', [DEPTH, 128, 8, D])
    k.router_w = din('router_w', [128, 8, NE])
    k.router_b_rep = din('router_b_rep', [128, NE])
    if not dbg.get('nomoe'):
        k.moe_wg = din('moe_wg', [DEPTH, NE, 128, 8, DFF])
        k.moe_wu = din('moe_wu', [DEPTH, NE, 128, 8, DFF])
        k.moe_wd = din('moe_wd', [DEPTH, NE, 128, 4, D])
    k.sel4 = din('sel4', [4, 4, 128])
    k.DIFFf = din('DIFFf', [128, 128]); k.DIFFb = din('DIFFb', [128, 128])
    k.MSKf = din('MSKf', [128, 128]); k.MSKb = din('MSKb', [128, 128])
    k.POS = din('POS', [128, 4, 128])
    k.BLK2 = din('BLK2', [128, 128]); k.BLK64 = din('BLK64', [128, 128])
    k.COS = din('COS', [128, L]); k.SIN = din('SIN', [128, L])
    k.ret_logit_rep = din('ret_logit_rep', [DEPTH, 128, 8])
    k.HM4 = din('HM4', [128, 4]); k.BLKG = din('BLKG', [128, 256])
    k.s5_lre = din('s5_lre', [128, DEPTH, 16]); k.s5_lim = din('s5_lim', [128, DEPTH, 16]); k.s5_ldt = din('s5_ldt', [128, DEPTH, 16])
    k.s5_WB = din('s5_WB', [DEPTH, 128, 8, 2, 128]); k.s5_WC = din('s5_WC', [DEPTH, 128, 8, 2, 128])
    k.s5_dFM = din('s5_dFM', [128, DEPTH, 2]); k.s5_gbFM = din('s5_gbFM', [128, DEPTH, 2])
    k.s5_gw = din('s5_gw', [DEPTH, 128, 2, 256])
    k.convFM = din('convFM', [128, DEPTH, 6, 5]); k.dn_par = din('dn_par', [128, DEPTH, 2])
    k.MH = din('MH', [128, 4]); k.OH = din('OH', [128, 2, 512]); k.HM2 = din('HM2', [128, 2]); k.SELR = din('SELR', [128, 4, 128]); k.SELP = din('SELP', [128, 2, 128]); k.MB = din('MB', [128, 2, 4, 128])
    k.gkw = din('gkw', [DEPTH, 128, 128]); k.gkbFM = din('gkbFM', [128, DEPTH, 2])
    if 'mix_in' in dbg:
        k.mix_in = din('mix_in', [NB, D, T])
    k.out = nc.dram_tensor('out', [NB, L, D], F32, kind="ExternalOutput").ap()

    k.pFM = dscr('pFM', [NB, NFM, T])
    k.pTM = dscr('pTM', [NB, T, NTM])
    k.mixFM = dscr('mixFM', [NB, D, T])
    k.Xs = dscr('Xs', [NB, L, D])
    k.Zs = dscr('Zs', [NB, LC, D])
    k.gateD = dscr('gateD', [DEPTH, 2, 4, D])
    k.dbg_out = {}
    for name, shape in dbg.get('outs', {}).items():
        k.dbg_out[name] = nc.dram_tensor('dbg_' + name, list(shape), F32, kind="ExternalOutput").ap()

    with ExitStack() as es:
        P = Prog(nc, es)
        k.P = P
        sb = lambda name, shape, dt=F32: es.enter_context(_sbt(nc, name, list(shape), dt))
        k.identf = sb('identf', [128, 128])
        k.identb = sb('identb', [128, 128], BF16)
        k.silu_c = sb('silu_c', [128, 8, 4])
        k.modFM = sb('modFM', [128, DEPTH, 48, 4])
        k.ngFM = sb('ngFM', [128, DEPTH, 2, 8])
        k.Amod = sb('Amod', [128, DEPTH, 2, 8, 4])
        k.lnq8 = sb('lnq8', [128, 1])

        phase_init(k)
        stages = dbg.get('stages', None)
        for l in range(DEPTH):
            last = (l == DEPTH - 1)
            if stages is None or ('mod', l) in stages:
                phase_mod(k, l)
            for b in range(NB):
                if stages is None or ('inproj', l) in stages:
                    phase_inproj(k, l, b)
                if stages is None or ('mixers', l) in stages:
                    phase_mixers(k, l, b)
                if stages is None or ('outproj', l) in stages:
                    phase_outproj_moe(k, l, b, last)
        phase_dbg(k)
    return nc


def resid_src(k, l, b, tt):
    if tt < 2:
        src = k.ctx if l == 0 else k.Zs
        return src[b, tt * 128:(tt + 1) * 128, :]
    src = k.x if l == 0 else k.Xs
    return src[b, (tt - 2) * 128:(tt - 1) * 128, :]


def resid_dst(k, b, tt):
    if tt < 2:
        return k.Zs[b, tt * 128:(tt + 1) * 128, :]
    return k.Xs[b, (tt - 2) * 128:(tt - 1) * 128, :]


def phase_init(k):
    P, nc = k.P, k.nc
    P.dma(k.identf[:], k.ident[:, :], writes=['identf'])
    P.dma(k.identb[:], k.ident[:, :], writes=['identb'], eng='gpsimd')
    P.dma(k.silu_c[:], k.cT[:, :, :], writes=['silu_c'])
    P.dma(k.ngFM[:], k.norm_gFM[:, :, :, :], writes=['ngFM'])
    P.act(k.silu_c[:], k.silu_c[:], AF.Silu, reads=['silu_c'], writes=['silu_c'])
    P.memset(k.lnq8[:], math.log(0.125), writes=['lnq8'])
    P.flush()


def phase_mod(k, l):
    P, nc = k.P, k.nc
    with ExitStack() as es:
        wbuf = [es.enter_context(_sbt(nc, 'wada%d' % i, [128, 8, 512], F32)) for i in range(2)]
        bfm = es.enter_context(_sbt(nc, 'bfm', [128, 48], F32))
        b4 = es.enter_context(_sbt(nc, 'b4', [4, 6 * D], F32))
        gtmp = es.enter_context(_sbt(nc, 'gtmp', [4, 512], F32))
        ps = [es.enter_context(_pst(nc, 'psm%d' % i, [128, 512], F32)) for i in range(2)]
        psg = es.enter_context(_pst(nc, 'psg', [4, 512], F32))
        P.dma(bfm[:], k.b_adaFM[:, l, :], writes=['bfm'])
        P.dma(b4[:], k.b_ada4[:, l, :], writes=['b4'])
        for blk in range(12):
            w = wbuf[blk % 2]
            wk = 'wada%d' % (blk % 2)
            P.dma(w[:], k.w_ada[l, :, :, blk * 512:(blk + 1) * 512], writes=[wk])
            pst = ps[blk % 2]
            pk = 'psm%d' % (blk % 2)
            for j in range(4):
                for kc in range(8):
                    P.mm(pst[:, j * 4:(j + 1) * 4], w[:, kc, j * 128:(j + 1) * 128], k.silu_c[:, kc, :],
                         kc == 0, kc == 7, reads=[wk, 'silu_c'], writes=[pk])
            for j in range(4):
                jj = blk * 4 + j
                P.ts(k.modFM[:, l, jj, :], pst[:, j * 4:(j + 1) * 4], bfm[:, jj:jj + 1], None, ALU.add,
                     reads=[pk, 'bfm'], writes=['modFM'])
            m = blk // 2
            if m in (2, 5):
                n = 0 if m == 2 else 1
                c0 = (blk % 2) * 512
                for kc in range(8):
                    P.mm(psg[:, :], k.silu_c[:, kc, :], w[:, kc, :], kc == 0, kc == 7,
                         reads=[wk, 'silu_c'], writes=['psg'])
                P.tt(gtmp[:, :], psg[:, :], b4[:, blk * 512:(blk + 1) * 512], ALU.add,
                     reads=['psg', 'b4'], writes=['gtmp'])
                P.dma(k.gateD[l, n, :, c0:c0 + 512], gtmp[:, :], reads=['gtmp'])
        for n in range(2):
            for fc in range(8):
                P.ts(k.Amod[:, l, n, fc, :], k.modFM[:, l, (1 + 3 * n) * 8 + fc, :], 1.0,
                     k.ngFM[:, l, n, fc:fc + 1], ALU.add, ALU.mult,
                     reads=['modFM', 'ngFM'], writes=['Amod'])
        P.flush()


def norm_modulate_tile(k, P, xt, xk, l, n, col, ssq, rstd, xn, pst, pkeys, hT_slices, hkeys, tag,
                       h32=None, h32key=None):
    P.op('scalar', lambda s: s.activation(out=xn[:], in_=xt, func=AF.Square, accum_out=ssq[:]),
         reads=[xk], writes=['xn' + tag, 'ssq' + tag])
    P.act(rstd[:], ssq[:], AF.Sqrt, reads=['ssq' + tag], writes=['rstd' + tag], scale=1.0 / D, bias=k.eps_col[:])
    P.recip(rstd[:], rstd[:], reads=['rstd' + tag], writes=['rstd' + tag])
    P.ts(xn[:], xt, rstd[:, 0:1], None, ALU.mult, reads=[xk, 'rstd' + tag], writes=['xn' + tag])
    for half in range(2):
        pt = pst[half]
        for j in range(4):
            fc = half * 4 + j
            P.tr(pt[:, j * 128:(j + 1) * 128], xn[:, fc * 128:(fc + 1) * 128], k.identf[:],
                 reads=['xn' + tag, 'identf'], writes=[pkeys[half]])
        for j in range(4):
            fc = half * 4 + j
            P.ts(hT_slices[fc], pt[:, j * 128:(j + 1) * 128], k.Amod[:, l, n, fc, col:col + 1],
                 k.modFM[:, l, (3 * n) * 8 + fc, col:col + 1], ALU.mult, ALU.add,
                 reads=[pkeys[half], 'Amod', 'modFM'], writes=[hkeys[fc]])
            if h32 is not None:
                P.ts(h32[:, fc, :], pt[:, j * 128:(j + 1) * 128], k.Amod[:, l, n, fc, col:col + 1],
                     k.modFM[:, l, (3 * n) * 8 + fc, col:col + 1], ALU.mult, ALU.add,
                     reads=[pkeys[half], 'Amod', 'modFM'], writes=[h32key])


def phase_inproj(k, l, b):
    P, nc = k.P, k.nc
    with ExitStack() as es:
        sbt = lambda name, shape, dt=F32: es.enter_context(_sbt(nc, name, list(shape), dt))
        hT = sbt('hT', [128, 8, T], BF16)
        wfm = sbt('wfm', [128, 8, NFM], BF16)
        wtm = sbt('wtm', [128, 8, NTM], BF16)
        xt = [sbt('xt%d' % i, [128, D]) for i in range(2)]
        xn = [sbt('xn%d' % i, [128, D]) for i in range(2)]
        ssq = [sbt('ssq%d' % i, [128, 1]) for i in range(2)]
        rstd = [sbt('rstd%d' % i, [128, 1]) for i in range(2)]
        k.eps_col = sbt('eps_col', [128, 1])
        stg = [sbt('stg%d' % i, [128, 512]) for i in range(3)]
        ps = [es.enter_context(_pst(nc, 'ps%d' % i, [128, 512], F32)) for i in range(6)]
        P.memset(k.eps_col[:], EPS, writes=['eps'])
        for kc in range(8):
            P.dma(wfm[:, kc, :], k.w_inFM[l, :, kc, :], writes=['wfm'], eng='gpsimd')
            P.dma(wtm[:, kc, :], k.w_inTM[l, :, kc, :], writes=['wtm'], eng='gpsimd')
        for tt in range(NT):
            i = tt % 2
            tag = str(i)
            P.dma(xt[i][:], resid_src(k, l, b, tt), writes=['xt' + tag])
            col = 2 if tt < 2 else b
            norm_modulate_tile(k, P, xt[i][:], 'xt' + tag, l, 0, col, ssq[i], rstd[i], xn[i],
                               [ps[2 * i], ps[2 * i + 1]], ['ps%d' % (2 * i), 'ps%d' % (2 * i + 1)],
                               [hT[:, fc, tt * 128:(tt + 1) * 128] for fc in range(8)],
                               [('hT', tt)] * 8, tag)
        cnt = 0
        for cb in range(NFM // 128):
            for (t0, tn) in TBLK:
                pt = ps[4 + cnt % 2]
                pk = 'ps%d' % (4 + cnt % 2)
                st = stg[cnt % 3]
                sk = 'stg%d' % (cnt % 3)
                cnt += 1
                hk = [('hT', t0 // 128 + j) for j in range(tn // 128)]
                for kc in range(8):
                    P.mm(pt[:, :tn], wfm[:, kc, cb * 128:(cb + 1) * 128], hT[:, kc, t0:t0 + tn], kc == 0, kc == 7,
                         reads=['wfm'] + hk, writes=[pk])
                if cnt % 2:
                    P.copy(st[:, :tn], pt[:, :tn], reads=[pk], writes=[sk])
                else:
                    P.copy(st[:, :tn], pt[:, :tn], reads=[pk], writes=[sk], eng='scalar')
                P.dma(k.pFM[b, cb * 128:(cb + 1) * 128, t0:t0 + tn], st[:, :tn], reads=[sk])
        CB = [(0, 512)]
        for tt in range(NT):
            for (c0, cn) in CB:
                pt = ps[4 + cnt % 2]
                pk = 'ps%d' % (4 + cnt % 2)
                st = stg[cnt % 3]
                sk = 'stg%d' % (cnt % 3)
                cnt += 1
                for kc in range(8):
                    P.mm(pt[:, :cn], hT[:, kc, tt * 128:(tt + 1) * 128], wtm[:, kc, c0:c0 + cn], kc == 0, kc == 7,
                         reads=['wtm', ('hT', tt)], writes=[pk])
                if cnt % 2:
                    P.copy(st[:, :cn], pt[:, :cn], reads=[pk], writes=[sk])
                else:
                    P.copy(st[:, :cn], pt[:, :cn], reads=[pk], writes=[sk], eng='scalar')
                P.dma(k.pTM[b, tt * 128:(tt + 1) * 128, c0:c0 + cn], st[:, :cn], reads=[sk])
        P.flush()


def phase_mixers(k, l, b):
    P, nc = k.P, k.nc
    if 'mix_in' in k.dbg:
        with ExitStack() as es:
            t = es.enter_context(_sbt(nc, 'mixcp', [128, T], F32))
            for fc in range(8):
                P.dma(t[:], k.mix_in[b, fc * 128:(fc + 1) * 128, :], writes=['mixcp'])
                P.dma(k.mixFM[b, fc * 128:(fc + 1) * 128, :], t[:], reads=['mixcp'])
            P.flush()
        return
    which = k.dbg.get('mixers', ['gla', 'ret', 's5', 'dn'])
    if 'ret' in which:
        phase_ret(k, l, b)
    if 'gla' in which:
        phase_gla(k, l, b)
    if 's5' in which:
        phase_s5(k, l, b)
    if 'dn' in which:
        phase_dn(k, l, b)


def chunk_order(d):
    if d == 0:
        return list(range(NT))
    return [1, 0] + list(range(NT - 1, 1, -1))


def norm_gate(k, P, nc, es, b, oacc, okeys, g_blk, mix_row0, center, tagp):
    sbt = lambda name, shape, dt=F32: es.enter_context(_sbt(nc, name + tagp, list(shape), dt))
    g = sbt('ng_g', [128, T])
    blk = sbt('ng_blk', [128, 128])
    xc = [sbt('ng_xc%d' % i, [128, 512]) for i in range(2)]
    sq = [sbt('ng_sq%d' % i, [128, 512]) for i in range(2)]
    eps = sbt('ng_eps', [128, 1])
    psA = [es.enter_context(_pst(nc, 'ng_psA%d' % i, [128, 512], F32)) for i in range(2)]
    P.memset(eps[:], EPS, writes=['ng_eps'])
    P.dma(blk[:], k.BLK64[:, :], writes=['ng_blk'])
    P.dma(g[:], k.pFM[b, g_blk * 128:(g_blk + 1) * 128, :], writes=['ng_g'])
    P.act(g[:], g[:], AF.Silu, reads=['ng_g'], writes=['ng_g'])
    for bi, (t0, tn) in enumerate(TBLK):
        i = bi % 2
        xk, sk, pk = 'ng_xc%d' % i, 'ng_sq%d' % i, 'ng_ps%d' % i
        src = oacc[:, t0:t0 + tn]
        if center:
            P.mm(psA[i][:, :tn], blk[:], src, True, True, reads=['ng_blk'] + okeys, writes=[pk])
            P.tt(xc[i][:, :tn], src, psA[i][:, :tn], ALU.subtract, reads=[pk] + okeys, writes=[xk])
        else:
            P.copy(xc[i][:, :tn], src, reads=okeys, writes=[xk])
        P.tt(sq[i][:, :tn], xc[i][:, :tn], xc[i][:, :tn], ALU.mult, reads=[xk], writes=[sk])
        P.mm(psA[i][:, :tn], blk[:], sq[i][:, :tn], True, True, reads=['ng_blk', sk], writes=[pk])
        P.act(sq[i][:, :tn], psA[i][:, :tn], AF.Sqrt, reads=[pk], writes=[sk], bias=eps[:], scale=1.0)
        P.recip(sq[i][:, :tn], sq[i][:, :tn], reads=[sk], writes=[sk])
        P.tt(xc[i][:, :tn], xc[i][:, :tn], sq[i][:, :tn], ALU.mult, reads=[xk, sk], writes=[xk])
        P.tt(xc[i][:, :tn], xc[i][:, :tn], g[:, t0:t0 + tn], ALU.mult, reads=[xk, 'ng_g'], writes=[xk])
        P.dma(k.mixFM[b, mix_row0:mix_row0 + 128, t0:t0 + tn], xc[i][:, :tn], reads=[xk])


def phase_ret(k, l, b):
    P, nc = k.P, k.nc
    with ExitStack() as es:
        sbt = lambda name, shape, dt=F32: es.enter_context(_sbt(nc, name, list(shape), dt))
        lg = sbt('r_lg', [128, 8])
        lgc = sbt('r_lgc', [128, 4])
        GC = sbt('r_GC', [128, 4])
        EQ = sbt('r_EQ', [128, 4, 128])
        EK = sbt('r_EK', [128, 4, 128])
        GAM = sbt('r_GAM', [128, 8, 128])
        pos = sbt('r_pos', [128, 4, 128])
        dif = sbt('r_dif', [128, 2, 128])
        blk2 = sbt('r_blk2', [128, 128])
        P.dma(lg[:], k.ret_logit_rep[l, :, :], writes=['r_lg'])
        P.dma(pos[:], k.POS[:, :, :], writes=['r_pos'])
        P.dma(dif[:, 0, :], k.DIFFf[:, :], writes=['r_dif'])
        P.dma(dif[:, 1, :], k.DIFFb[:, :], writes=['r_dif'])
        P.dma(blk2[:], k.BLK2[:, :], writes=['r_blk2'])
        P.act(lg[:], lg[:], AF.Exp, reads=['r_lg'], writes=['r_lg'], scale=-1.0)
        P.act(lg[:], lg[:], AF.Ln, reads=['r_lg'], writes=['r_lg'], bias=1.0, scale=1.0)
        P.ts(lg[:], lg[:], -1.0, None, ALU.mult, reads=['r_lg'], writes=['r_lg'])
        for d in range(2):
            for hp in range(2):
                j = d * 2 + hp
                P.copy(lgc[0:64, j:j + 1], lg[0:64, d * 4 + hp * 2:d * 4 + hp * 2 + 1], reads=['r_lg'], writes=['r_lgc'])
                P.copy(lgc[64:128, j:j + 1], lg[64:128, d * 4 + hp * 2 + 1:d * 4 + hp * 2 + 2], reads=['r_lg'], writes=['r_lgc'])
        for d in range(2):
            for hp in range(2):
                j = d * 2 + hp
                P.act(EQ[:, j, :], pos[:, d, :], AF.Exp, reads=['r_pos', 'r_lgc'], writes=['r_EQ'],
                      scale=lgc[:, j:j + 1])
                P.act(EK[:, j, :], pos[:, 2 + d, :], AF.Exp, reads=['r_pos', 'r_lgc'], writes=['r_EK'], scale=lgc[:, j:j + 1])
                P.act(GC[:, j:j + 1], lgc[:, j:j + 1], AF.Exp, reads=['r_lgc'], writes=['r_GC'], scale=128.0)
            for h in range(4):
                P.act(GAM[:, d * 4 + h, :], dif[:, d, :], AF.Exp, reads=['r_dif', 'r_lg'], writes=['r_GAM'],
                      scale=lg[:, d * 4 + h:d * 4 + h + 1])
        qr = sbt('r_q', [128, 2, T], BF16)
        kr = sbt('r_k', [128, 2, T], BF16)
        v = sbt('r_v', [128, NT, 256], BF16)
        vpad = sbt('r_vpad', [128, 2, 2, NT, 128], BF16)
        oacc = sbt('r_oacc', [128, 2, T])
        with ExitStack() as es2:
            sb2 = lambda name, shape, dt=F32: es2.enter_context(_sbt(nc, name, list(shape), dt))
            cos = sb2('r_cos', [128, L])
            sin = sb2('r_sin', [128, L])
            ta = sb2('r_ta', [128, T])
            tb = sb2('r_tb', [128, T])
            P.dma(cos[:], k.COS[:, :], writes=['r_cos'])
            P.dma(sin[:], k.SIN[:, :], writes=['r_sin'])
            P.dma(v[:], k.pTM[b].rearrange("(c p) n -> p c n", p=128)[:, :, 256:512], writes=['r_v'], eng='gpsimd')
            P.memset(vpad[:].rearrange("p a b c d -> p (a b c d)"), 0.0, writes=['r_vpad'], eng='gpsimd')
            for hp in range(2):
                for hh in range(2):
                    P.copy(vpad[:, hp, hh, :, hh * 64:(hh + 1) * 64], v[:, :, hp * 128 + hh * 64:hp * 128 + (hh + 1) * 64],
                           reads=['r_v', 'r_vpad'], writes=['r_vpad'], eng='gpsimd')
            for (dst, dk_, bm, bs) in [(qr, 'r_q', 3, 5), (kr, 'r_k', 7, 9)]:
                for hp in range(2):
                    P.dma(ta[:], k.pFM[b, (bm + hp) * 128:(bm + hp + 1) * 128, :], writes=['r_ta'])
                    P.dma(tb[:], k.pFM[b, (bs + hp) * 128:(bs + hp + 1) * 128, :], writes=['r_tb'])
                    if dk_ == 'r_q':
                        P.ts(ta[:], ta[:], 0.125, None, ALU.mult, reads=['r_ta'], writes=['r_ta'])
                        P.ts(tb[:], tb[:], 0.125, None, ALU.mult, reads=['r_tb'], writes=['r_tb'], eng='gpsimd')
                    P.copy(dst[:, hp, 0:LC], ta[:, 0:LC], reads=['r_ta'], writes=[dk_])
                    P.tt(ta[:, LC:], ta[:, LC:], cos[:], ALU.mult, reads=['r_ta', 'r_cos'], writes=['r_ta'])
                    P.tt(tb[:, LC:], tb[:, LC:], sin[:], ALU.mult, reads=['r_tb', 'r_sin'], writes=['r_tb'], eng='gpsimd')
                    P.tt(dst[:, hp, LC:], ta[:, LC:], tb[:, LC:], ALU.add, reads=['r_ta', 'r_tb'], writes=[dk_])
            P.flush()
        with ExitStack() as es2:
            sb2 = lambda name, shape, dt=F32: es2.enter_context(_sbt(nc, name, list(shape), dt))
            NBUF = 2
            Pm = [[sb2('r_Pm%d_%d' % (i, hh), [128, 128], BF16) for hh in range(2)] for i in range(NBUF)]
            qin = [sb2('r_qin%d' % i, [128, 128], BF16) for i in range(NBUF)]
            kout = [sb2('r_kout%d' % i, [128, 128], BF16) for i in range(NBUF)]
            koT = [sb2('r_koT%d' % i, [128, 128], BF16) for i in range(NBUF)]
            tmp = [sb2('r_tmp%d' % i, [128, 128]) for i in range(NBUF)]
            S32 = [sb2('r_S32_%d' % i, [128, 128]) for i in range(4)]
            Sb = [sb2('r_Sb_%d' % i, [128, 128], BF16) for i in range(4)]
            psS = [es2.enter_context(_pst(nc, 'r_psS%d' % i, [128, 128], F32)) for i in range(4)]
            psO = [es2.enter_context(_pst(nc, 'r_psO%d' % i, [128, 128], F32)) for i in range(2)]
            psT = [es2.enter_context(_pst(nc, 'r_psT%d' % i, [128, 128], BF16)) for i in range(1)]
            psU = [es2.enter_context(_pst(nc, 'r_psU%d' % i, [128, 128], F32)) for i in range(1)]
            step = 0
            seen = set()
            for j in range(4):
                P.memset(S32[j][:], 0.0, writes=['r_S32_%d' % j])
                P.memset(Sb[j][:], 0.0, writes=['r_Sb_%d' % j])
            for ci in range(NT):
                for hp in range(2):
                    for d in range(2):
                        j = d * 2 + hp
                        c = chunk_order(d)[ci]
                        cs = slice(c * 128, (c + 1) * 128)
                        i = step % NBUF
                        step += 1
                        si = str(i)
                        Sk, Sbk = 'r_S32_%d' % j, 'r_Sb_%d' % j
                        for hh in range(2):
                            pr = slice(hh * 64, (hh + 1) * 64)
                            pS = psS[2 * i + hh]
                            pSk = 'r_psS%d' % (2 * i + hh)
                            P.mm(pS[:, :], kr[pr, hp, cs], qr[pr, hp, cs], True, True, reads=['r_q', 'r_k'], writes=[pSk])
                            P.tt(Pm[i][hh][:], pS[:, :], GAM[:, d * 4 + hp * 2 + hh, :], ALU.mult,
                                 reads=[pSk, 'r_GAM'], writes=['r_Pm%s_%d' % (si, hh)])
                        P.tt(qin[i][:], qr[:, hp, cs], EQ[:, j, :], ALU.mult, reads=['r_q', 'r_EQ'], writes=['r_qin' + si], eng='gpsimd')
                        P.tt(kout[i][:], kr[:, hp, cs], EK[:, j, :], ALU.mult, reads=['r_k', 'r_EK'], writes=['r_kout' + si], eng='gpsimd')
                        pO = psO[i]
                        pOk = 'r_psO%d' % i
                        P.mm(pO[:, :], vpad[:, hp, 0, c, :], Pm[i][0][:], True, False, reads=['r_vpad', 'r_Pm%s_0' % si], writes=[pOk])
                        P.mm(pO[:, :], vpad[:, hp, 1, c, :], Pm[i][1][:], False, False, reads=['r_vpad', 'r_Pm%s_1' % si], writes=[pOk])
                        P.mm(pO[:, :], Sb[j][:], qin[i][:], False, True, reads=[Sbk, 'r_qin' + si], writes=[pOk])
                        ok = ('r_oacc', hp, c)
                        if (ok, 0) not in seen:
                            seen.add((ok, 0))
                            P.copy(oacc[:, hp, cs], pO[:, :], reads=[pOk], writes=[ok], eng='scalar')
                        else:
                            P.tt(oacc[:, hp, cs], oacc[:, hp, cs], pO[:, :], ALU.add, reads=[pOk, ok], writes=[ok])
                        P.tr(psT[0][:, :], kout[i][:], k.identb[:], reads=['r_kout' + si, 'identb'], writes=['r_psT'])
                        P.copy(koT[i][:], psT[0][:, :], reads=['r_psT'], writes=['r_koT' + si], eng='scalar')
                        P.mm(psU[0][:, :], koT[i][:], v[:, c, hp * 128:(hp + 1) * 128], True, True,
                             reads=['r_koT' + si, 'r_v'], writes=['r_psU'])
                        P.tt(tmp[i][:], psU[0][:, :], blk2[:], ALU.mult, reads=['r_psU', 'r_blk2'], writes=['r_tmp' + si])
                        P.stt(S32[j][:], S32[j][:], GC[:, j:j + 1], tmp[i][:], ALU.mult, ALU.add,
                              reads=[Sk, 'r_GC', 'r_tmp' + si], writes=[Sk])
                        P.copy(Sb[j][:], S32[j][:], reads=[Sk], writes=[Sbk], eng='scalar')
            P.flush()
        for hp in range(2):
            with ExitStack() as es2:
                norm_gate(k, P, nc, es2, b, oacc[:, hp, :], [('r_oacc', hp, c) for c in range(NT)], 22 + hp, 256 + hp * 128, True, 'r%d' % hp)
                P.flush()


def phase_gla(k, l, b):
    P, nc = k.P, k.nc
    with ExitStack() as es:
        sbt = lambda name, shape, dt=F32: es.enter_context(_sbt(nc, name, list(shape), dt))
        qh = sbt('g_qh', [128, 2, 4, T], BF16)
        qt = sbt('g_qt', [128, 2, T], BF16)
        kh = sbt('g_kh', [128, 2, T], BF16)
        elast = sbt('g_el', [128, 2, NT])
        v = sbt('g_v', [128, NT, 256], BF16)
        vpad = sbt('g_vpad', [128, 4, NT, 128], BF16)
        oacc = sbt('g_oacc', [128, 2, T])
        msk = sbt('g_msk', [128, 2, 128])
        blkg = sbt('g_blkg', [128, 256])
        hm = sbt('g_hm', [128, 4])
        P.dma(msk[:, 0, :], k.MSKf[:, :], writes=['g_msk'])
        P.dma(msk[:, 1, :], k.MSKb[:, :], writes=['g_msk'])
        P.dma(blkg[:], k.BLKG[:, :], writes=['g_blkg'])
        P.dma(hm[:], k.HM4[:, :], writes=['g_hm'])
        P.dma(v[:], k.pTM[b].rearrange("(c p) n -> p c n", p=128)[:, :, 0:256], writes=['g_v'], eng='gpsimd')
        P.memset(vpad[:].rearrange("p a c d -> p (a c d)"), 0.0, writes=['g_vpad'], eng='gpsimd')
        for h in range(4):
            hh = h % 2
            P.copy(vpad[:, h, :, hh * 64:(hh + 1) * 64], v[:, :, h * 64:(h + 1) * 64], reads=['g_v', 'g_vpad'], writes=['g_vpad'], eng='gpsimd')
        with ExitStack() as es2:
            sb2 = lambda name, shape, dt=F32: es2.enter_context(_sbt(nc, name, list(shape), dt))
            z = sb2('g_z', [128, T])
            gkw = sb2('g_gkw', [128, 128])
            nb = sb2('g_nb', [128, 2])
            sp = sb2('g_sp', [128, T])
            bc = sb2('g_bc', [128, T])
            ee = sb2('g_ee', [128, T])
            qf = sb2('g_qf', [128, T])
            kf = sb2('g_kf', [128, T])
            ones = sb2('g_ones', [128, 128])
            lnq = sb2('g_lnq', [128, 1])
            ps = [es2.enter_context(_pst(nc, 'g_ps%d' % i, [128, 512], F32)) for i in range(2)]
            P.dma(z[:], k.pFM[b, 2 * 128:3 * 128, :], writes=['g_z'])
            P.dma(gkw[:], k.gkw[l, :, :], writes=['g_gkw'])
            P.dma(nb[:], k.gkbFM[:, l, :], writes=['g_nb'])
            P.dma(qf[:], k.pFM[b, 0:128, :], writes=['g_qf'])
            P.dma(kf[:], k.pFM[b, 128:256, :], writes=['g_kf'])
            P.ts(nb[:], nb[:], -1.0, None, ALU.mult, reads=['g_nb'], writes=['g_nb'])
            P.memset(ones[:], 1.0, writes=['g_ones'])
            P.memset(lnq[:], math.log(32.0 ** -0.5), writes=['g_lnq'])
            for d in range(2):
                pr = slice(d * 32, d * 32 + 16)
                for bi, (t0, tn) in enumerate(TBLK):
                    i = bi % 2
                    P.mm(ps[i][:, :tn], gkw[pr, :], z[pr, t0:t0 + tn], True, True, reads=['g_gkw', 'g_z'], writes=['g_ps%d' % i])
                    P.act(sp[:, t0:t0 + tn], ps[i][:, :tn], AF.Exp, reads=['g_ps%d' % i, 'g_nb'], writes=['g_sp'],
                          scale=-1.0, bias=nb[:, d:d + 1])
                P.act(sp[:], sp[:], AF.Ln, reads=['g_sp'], writes=['g_sp'], bias=1.0, scale=1.0)
                for c in range(NT):
                    cs = slice(c * 128, (c + 1) * 128)
                    if d == 0:
                        P.scan(bc[:, cs], ones[:], sp[:, cs], 0.0, reads=['g_ones', 'g_sp'], writes=['g_bc'])
                    else:
                        P.scan(bc[:, cs][:, ::-1], ones[:], sp[:, cs][:, ::-1], 0.0, reads=['g_ones', 'g_sp'], writes=['g_bc'])
                P.act(ee[:], bc[:], AF.Exp, reads=['g_bc'], writes=['g_ee'], scale=-1.0 / 16.0, bias=lnq[:])
                P.tt(qt[:, d, :], qf[:], ee[:], ALU.mult, reads=['g_qf', 'g_ee'], writes=['g_qt'])
                for h in range(4):
                    P.ts(qh[:, d, h, :], qt[:, d, :], hm[:, h:h + 1], None, ALU.mult, reads=['g_qt', 'g_hm'], writes=['g_qh'],
                         eng='gpsimd' if h % 2 else 'vector')
                lastv = bc[:, 127::128] if d == 0 else bc[:, 0::128]
                P.act(elast[:, d, :], lastv, AF.Exp, reads=['g_bc'], writes=['g_el'], scale=-1.0 / 16.0)
                P.act(ee[:], bc[:], AF.Exp, reads=['g_bc', 'g_qt'], writes=['g_ee'], scale=1.0 / 16.0)
                P.tt(kh[:, d, :], kf[:], ee[:], ALU.mult, reads=['g_kf', 'g_ee'], writes=['g_kh'])
            P.flush()
        with ExitStack() as es2:
            sb2 = lambda name, shape, dt=F32: es2.enter_context(_sbt(nc, name, list(shape), dt))
            NBUF = 2
            Pm = [[sb2('g_Pm%d_%d' % (i, h), [128, 128], BF16) for h in range(4)] for i in range(NBUF)]
            koT = [sb2('g_koT%d' % i, [128, 128], BF16) for i in range(NBUF)]
            tmp = [sb2('g_tmp%d' % i, [128, 256]) for i in range(NBUF)]
            S32 = [sb2('g_S32_%d' % i, [128, 256]) for i in range(2)]
            Sb = [sb2('g_Sb_%d' % i, [128, 256], BF16) for i in range(2)]
            psS = [es2.enter_context(_pst(nc, 'g_psS%d' % i, [128, 128], F32)) for i in range(4)]
            psO = [es2.enter_context(_pst(nc, 'g_psO%d' % i, [128, 128], F32)) for i in range(2)]
            psT = es2.enter_context(_pst(nc, 'g_psT', [128, 128], BF16))
            psU = es2.enter_context(_pst(nc, 'g_psU', [128, 256], F32))
            for j in range(2):
                P.memset(S32[j][:], 0.0, writes=['g_S32_%d' % j])
                P.memset(Sb[j][:], 0.0, writes=['g_Sb_%d' % j])
            step = 0
            seen = set()
            for ci in range(NT):
                for d in range(2):
                    c = chunk_order(d)[ci]
                    cs = slice(c * 128, (c + 1) * 128)
                    i = step % NBUF
                    step += 1
                    si = str(i)
                    Sk, Sbk = 'g_S32_%d' % d, 'g_Sb_%d' % d
                    for h in range(4):
                        P.mm(psS[h][:, :], kh[:, d, cs], qh[:, d, h, cs], True, True, reads=['g_kh', 'g_qh'], writes=['g_psS%d' % h])
                        P.tt(Pm[i][h][:], psS[h][:, :], msk[:, d, :], ALU.mult, reads=['g_psS%d' % h, 'g_msk'],
                             writes=['g_Pm%s_%d' % (si, h)])
                    for vp in range(2):
                        pO = psO[vp]
                        pOk = 'g_psO%d' % vp
                        P.mm(pO[:, :], vpad[:, 2 * vp, c, :], Pm[i][2 * vp][:], True, False,
                             reads=['g_vpad', 'g_Pm%s_%d' % (si, 2 * vp)], writes=[pOk])
                        P.mm(pO[:, :], vpad[:, 2 * vp + 1, c, :], Pm[i][2 * vp + 1][:], False, False,
                             reads=['g_vpad', 'g_Pm%s_%d' % (si, 2 * vp + 1)], writes=[pOk])
                        P.mm(pO[:, :], Sb[d][:, vp * 128:(vp + 1) * 128], qt[:, d, cs], False, True, reads=[Sbk, 'g_qt'], writes=[pOk])
                        ok = ('g_oacc', vp, c)
                        if ok not in seen:
                            seen.add(ok)
                            P.copy(oacc[:, vp, cs], pO[:, :], reads=[pOk], writes=[ok], eng='scalar')
                        else:
                            P.tt(oacc[:, vp, cs], oacc[:, vp, cs], pO[:, :], ALU.add, reads=[pOk, ok], writes=[ok])
                    P.tr(psT[:, :], kh[:, d, cs], k.identb[:], reads=['g_kh', 'identb'], writes=['g_psT'])
                    P.copy(koT[i][:], psT[:, :], reads=['g_psT'], writes=['g_koT' + si], eng='scalar')
                    P.mm(psU[:, :], koT[i][:], v[:, c, :], True, True, reads=['g_koT' + si, 'g_v'], writes=['g_psU'])
                    P.tt(tmp[i][:], psU[:, :], blkg[:], ALU.mult, reads=['g_psU', 'g_blkg'], writes=['g_tmp' + si])
                    P.tt(S32[d][:], S32[d][:], tmp[i][:], ALU.add, reads=[Sk, 'g_tmp' + si], writes=[Sk])
                    P.ts(S32[d][:], S32[d][:], elast[:, d, c:c + 1], None, ALU.mult, reads=[Sk, 'g_el'], writes=[Sk])
                    P.copy(Sb[d][:], S32[d][:], reads=[Sk], writes=[Sbk], eng='scalar')
            P.flush()
        for vp in range(2):
            with ExitStack() as es2:
                norm_gate(k, P, nc, es2, b, oacc[:, vp, :], [('g_oacc', vp, c) for c in range(NT)], 20 + vp, vp * 128, False, 'g%d' % vp)
                P.flush()


TWO_PI = 2.0 * math.pi


def sincos(P, ang, sn, cs, ki, t1, keys):
    R = dict(reads=keys, writes=keys)
    P.ts(t1, ang, 1.0 / TWO_PI, None, ALU.mult, **R)
    P.copy(ki, t1, **R)
    P.copy(t1, ki, **R)
    P.stt(ang, t1, -TWO_PI, ang, ALU.mult, ALU.add, **R)
    P.ts(t1, ang, math.pi, None, ALU.is_gt, **R)
    P.stt(ang, t1, -TWO_PI, ang, ALU.mult, ALU.add, **R)
    P.ts(t1, ang, -math.pi, None, ALU.is_lt, **R)
    P.stt(ang, t1, TWO_PI, ang, ALU.mult, ALU.add, **R)
    P.act(sn, ang, AF.Sin, **R)
    P.ts(ang, ang, math.pi / 2, None, ALU.add, **R)
    P.ts(t1, ang, math.pi, None, ALU.is_gt, **R)
    P.stt(ang, t1, -TWO_PI, ang, ALU.mult, ALU.add, **R)
    P.act(cs, ang, AF.Sin, **R)


def phase_s5(k, l, b):
    P, nc = k.P, k.nc
    with ExitStack() as es:
        sbt = lambda name, shape, dt=F32: es.enter_context(_sbt(nc, name, list(shape), dt))
        TAB = sbt('s_tab', [128, 16, 4, 128])
        rr = sbt('s_r', [128, 16])
        cb = sbt('s_cb', [128, 2, 16])
        WB = sbt('s_WB', [128, 8, 2, 128], BF16)
        WC = sbt('s_WC', [128, 8, 2, 128], BF16)
        u32 = sbt('s_u32', [128, 2, T])
        ub = sbt('s_ub', [128, 2, T], BF16)
        yacc = sbt('s_yacc', [128, 2, T])
        pos = sbt('s_pos', [128, 2, 128])
        P.dma(pos[:], k.POS[:, 0:2, :], writes=['s_pos'])
        P.dma(WB[:], k.s5_WB[l, :, :, :, :], writes=['s_WB'], eng='gpsimd')
        P.dma(WC[:], k.s5_WC[l, :, :, :, :], writes=['s_WC'], eng='gpsimd')
        for ct in range(2):
            P.dma(u32[:, ct, :], k.pFM[b, (11 + ct) * 128:(12 + ct) * 128, :], writes=['s_u32'])
        P.copy(ub[:], u32[:], reads=['s_u32'], writes=['s_ub'], eng='gpsimd')
        with ExitStack() as es2:
            sb2 = lambda name, shape, dt=F32: es2.enter_context(_sbt(nc, name, list(shape), dt))
            lre = sb2('s_lre', [128, 16]); lim = sb2('s_lim', [128, 16]); dt_ = sb2('s_dt', [128, 16])
            th = sb2('s_th', [128, 16]); ang = sb2('s_ang', [128, 16]); sn = sb2('s_sn', [128, 16]); cs = sb2('s_cs', [128, 16])
            ki = sb2('s_ki', [128, 16], I32); t1 = sb2('s_t1', [128, 16]); t2 = sb2('s_t2', [128, 16])
            bre = sb2('s_bre', [128, 16]); bim = sb2('s_bim', [128, 16]); den = sb2('s_den', [128, 16])
            A = sb2('s_A', [128, 128]); K2 = sb2('s_K2', [128, 128], I32); T1 = sb2('s_T1', [128, 128])
            pk = ['s_par']
            R = dict(reads=pk, writes=pk)
            P.dma(lre[:], k.s5_lre[:, l, :], writes=pk)
            P.dma(lim[:], k.s5_lim[:, l, :], writes=pk)
            P.dma(dt_[:], k.s5_ldt[:, l, :], writes=pk)
            P.act(dt_[:], dt_[:], AF.Exp, **R)
            P.tt(th[:], lim[:], dt_[:], ALU.mult, **R)
            P.tt(t2[:], lre[:], dt_[:], ALU.mult, **R)
            P.act(rr[:], t2[:], AF.Exp, reads=pk, writes=pk + ['s_r'])
            P.copy(ang[:], th[:], **R)
            sincos(P, ang[:], sn[:], cs[:], ki[:], t1[:], pk)
            P.tt(t1[:], rr[:], cs[:], ALU.mult, **R)
            P.ts(t1[:], t1[:], -1.0, None, ALU.add, **R)
            P.tt(t2[:], rr[:], sn[:], ALU.mult, **R)
            P.tt(den[:], lre[:], lre[:], ALU.mult, **R)
            P.tt(bre[:], lim[:], lim[:], ALU.mult, **R)
            P.tt(den[:], den[:], bre[:], ALU.add, **R)
            P.recip(den[:], den[:], **R)
            P.tt(bre[:], t1[:], lre[:], ALU.mult, **R)
            P.tt(bim[:], t2[:], lim[:], ALU.mult, **R)
            P.tt(bre[:], bre[:], bim[:], ALU.add, **R)
            P.tt(bre[:], bre[:], den[:], ALU.mult, **R)
            P.tt(bim[:], t2[:], lre[:], ALU.mult, **R)
            P.tt(t2[:], t1[:], lim[:], ALU.mult, **R)
            P.tt(bim[:], bim[:], t2[:], ALU.subtract, **R)
            P.tt(bim[:], bim[:], den[:], ALU.mult, **R)
            for dj in range(16):
                d = dj // 8
                tk = ('s_tab', dj)
                Rt = dict(reads=pk + ['s_pos', tk, 's_A'], writes=[tk, 's_A'])
                P.ts(A[:], pos[:, d, :], th[:, dj:dj + 1], None, ALU.mult, **Rt)
                sincos(P, A[:], TAB[:, dj, 3, :], TAB[:, dj, 2, :], K2[:], T1[:], [tk, 's_A'])
                P.ts(T1[:], TAB[:, dj, 3, :], bim[:, dj:dj + 1], None, ALU.mult, **Rt)
                P.stt(TAB[:, dj, 0, :], TAB[:, dj, 2, :], bre[:, dj:dj + 1], T1[:], ALU.mult, ALU.add, **Rt)
                P.ts(T1[:], TAB[:, dj, 3, :], bre[:, dj:dj + 1], None, ALU.mult, **Rt)
                P.stt(TAB[:, dj, 1, :], TAB[:, dj, 2, :], bim[:, dj:dj + 1], T1[:], ALU.mult, ALU.subtract, **Rt)
                li = 127 if d == 0 else 0
                P.copy(cb[:, 0, dj:dj + 1], TAB[:, dj, 2, li:li + 1], reads=[tk], writes=['s_cb'])
                P.copy(cb[:, 1, dj:dj + 1], TAB[:, dj, 3, li:li + 1], reads=[tk], writes=['s_cb'])
            P.flush()
        with ExitStack() as es2:
            sb2 = lambda name, shape, dt=F32: es2.enter_context(_sbt(nc, name, list(shape), dt))
            NB2 = 2
            BU = [sb2('s_BU%d' % i, [128, 2, 128]) for i in range(NB2)]
            M1 = [sb2('s_M1%d' % i, [128, 2, 128]) for i in range(NB2)]
            M2 = [sb2('s_M2%d' % i, [128, 2, 128]) for i in range(NB2)]
            BP = [sb2('s_BP%d' % i, [128, 2, 128]) for i in range(NB2)]
            G = [sb2('s_G%d' % d, [128, 8, 2, 128]) for d in range(2)]
            H = [sb2('s_H%d' % i, [128, 2, 128], BF16) for i in range(NB2)]
            G0 = [sb2('s_G0%d' % d, [128, 2, 8]) for d in range(2)]
            GL = [sb2('s_GL%d' % d, [128, 2, 8]) for d in range(2)]
            rfull = sb2('s_rfull', [128, 16, 128])
            psB = [es2.enter_context(_pst(nc, 's_psB%d' % i, [128, 2, 128], F32)) for i in range(2)]
            psY = [es2.enter_context(_pst(nc, 's_psY%d' % i, [128, 2, 128], F32)) for i in range(2)]
            for d in range(2):
                P.memset(G0[d][:].rearrange("p a b -> p (a b)"), 0.0, writes=['s_G0%d' % d])
            for dj in range(16):
                P.ts(rfull[:, dj, :], pos[:, 0, :], 0.0, rr[:, dj:dj + 1], ALU.mult, ALU.add, reads=['s_pos', 's_r'], writes=['s_rfull'])
            step = 0
            seen = set()
            for ci in range(NT):
                for d in range(2):
                    c = chunk_order(d)[ci]
                    cs_ = slice(c * 128, (c + 1) * 128)
                    pY = psY[d]
                    pYk = 's_psY%d' % d
                    for j in range(8):
                        dj = d * 8 + j
                        ct = j // 4
                        i = step % NB2
                        step += 1
                        si = str(i)
                        tk = ('s_tab', dj)
                        pB = psB[i]
                        pBk = 's_psB' + si
                        P.mm(pB[:, 0, :], WB[:, j, 0, :], ub[:, ct, cs_], True, True, reads=['s_WB', 's_ub'], writes=[pBk])
                        P.mm(pB[:, 1, :], WB[:, j, 1, :], ub[:, ct, cs_], True, True, reads=['s_WB', 's_ub'], writes=[pBk])
                        P.copy(BU[i][:], pB[:], reads=[pBk], writes=['s_BU' + si], eng='scalar')
                        cre = TAB[:, dj, 0, :].unsqueeze(1).to_broadcast([128, 2, 128])
                        cim = TAB[:, dj, 1, :].unsqueeze(1).to_broadcast([128, 2, 128])
                        P.tt(M1[i][:], BU[i][:], cre, ALU.mult, reads=['s_BU' + si, tk], writes=['s_M1' + si])
                        P.tt(M2[i][:], BU[i][:], cim, ALU.mult, reads=['s_BU' + si, tk], writes=['s_M2' + si], eng='gpsimd')
                        P.tt(BP[i][:, 0, :], M1[i][:, 0, :], M2[i][:, 1, :], ALU.subtract, reads=['s_M1' + si, 's_M2' + si], writes=['s_BP' + si], eng='gpsimd')
                        P.tt(BP[i][:, 1, :], M1[i][:, 1, :], M2[i][:, 0, :], ALU.add, reads=['s_M1' + si, 's_M2' + si], writes=['s_BP' + si])
                        gk = ('s_G', d, j)
                        for ri in range(2):
                            o_ = G[d][:, j, ri, :]
                            d1 = BP[i][:, ri, :]
                            rf = rfull[:, dj, :]
                            if d == 1:
                                o_, d1 = o_[:, ::-1], d1[:, ::-1]
                            P.scan(o_, rf, d1, G0[d][:, ri, j:j + 1], reads=['s_rfull', 's_BP' + si, 's_G0%d' % d], writes=[gk])
                        cosb = TAB[:, dj, 2, :].unsqueeze(1).to_broadcast([128, 2, 128])
                        sinb = TAB[:, dj, 3, :].unsqueeze(1).to_broadcast([128, 2, 128])
                        P.tt(M1[i][:], G[d][:, j, :, :], cosb, ALU.mult, reads=[gk, tk], writes=['s_M1' + si])
                        P.tt(M2[i][:], G[d][:, j, :, :], sinb, ALU.mult, reads=[gk, tk], writes=['s_M2' + si], eng='gpsimd')
                        P.tt(H[i][:, 0, :], M1[i][:, 0, :], M2[i][:, 1, :], ALU.subtract, reads=['s_M1' + si, 's_M2' + si], writes=['s_H' + si], eng='gpsimd')
                        P.stt(H[i][:, 1, :], M1[i][:, 1, :], -1.0, M2[i][:, 0, :], ALU.mult, ALU.subtract,
                              reads=['s_M1' + si, 's_M2' + si], writes=['s_H' + si])
                        jj = j % 4
                        P.mm(pY[:, ct, :], WC[:, j, 0, :], H[i][:, 0, :], jj == 0, False, reads=['s_WC', 's_H' + si], writes=[pYk])
                        P.mm(pY[:, ct, :], WC[:, j, 1, :], H[i][:, 1, :], False, jj == 3, reads=['s_WC', 's_H' + si], writes=[pYk])
                    ok = ('s_yacc', c)
                    if ok not in seen:
                        seen.add(ok)
                        P.copy(yacc[:, :, cs_], pY[:], reads=[pYk], writes=[ok], eng='scalar')
                    else:
                        P.tt(yacc[:, :, cs_], yacc[:, :, cs_], pY[:], ALU.add, reads=[pYk, ok], writes=[ok])
                    li = 127 if d == 0 else 0
                    gks = [('s_G', d, j) for j in range(8)]
                    g0k = 's_G0%d' % d
                    glk = 's_GL%d' % d
                    P.copy(GL[d][:], G[d][:, :, :, li].rearrange("p j r -> p r j"), reads=gks, writes=[glk])
                    cbc = cb[:, 0, d * 8:(d + 1) * 8]
                    cbs = cb[:, 1, d * 8:(d + 1) * 8]
                    Rg = dict(reads=[glk, 's_cb', g0k, 's_hv%d' % d], writes=[g0k, 's_hv%d' % d])
                    P.tt(G0[d][:, 0, :], GL[d][:, 1, :], cbs, ALU.mult, **Rg)
                    P.tt(G0[d][:, 1, :], GL[d][:, 0, :], cbs, ALU.mult, **Rg)
                    P.tt(GL[d][:, 0, :], GL[d][:, 0, :], cbc, ALU.mult, reads=[glk, 's_cb', g0k], writes=[glk])
                    P.tt(GL[d][:, 1, :], GL[d][:, 1, :], cbc, ALU.mult, reads=[glk, 's_cb', g0k], writes=[glk])
                    P.tt(G0[d][:, 0, :], GL[d][:, 0, :], G0[d][:, 0, :], ALU.subtract, reads=[glk, g0k], writes=[g0k])
                    P.tt(G0[d][:, 1, :], GL[d][:, 1, :], G0[d][:, 1, :], ALU.add, reads=[glk, g0k], writes=[g0k])
            P.flush()
        with ExitStack() as es2:
            sb2 = lambda name, shape, dt=F32: es2.enter_context(_sbt(nc, name, list(shape), dt))
            dsk = sb2('s_dsk', [128, 2])
            gb = sb2('s_gb', [128, 2])
            gw = sb2('s_gw', [128, 2, 256], BF16)
            yb = sb2('s_yb', [128, 2, T], BF16)
            t1 = sb2('s_e1', [128, T])
            zt = [sb2('s_zt%d' % i, [128, 512]) for i in range(2)]
            ps = [es2.enter_context(_pst(nc, 's_psz%d' % i, [128, 512], F32)) for i in range(2)]
            P.dma(dsk[:], k.s5_dFM[:, l, :], writes=['s_dsk'])
            P.dma(gb[:], k.s5_gbFM[:, l, :], writes=['s_gb'])
            P.dma(gw[:], k.s5_gw[l, :, :, :], writes=['s_gw'], eng='gpsimd')
            yk = [('s_yacc', c) for c in range(NT)]
            for ct in range(2):
                y = yacc[:, ct, :]
                P.stt(y, u32[:, ct, :], dsk[:, ct:ct + 1], y, ALU.mult, ALU.add, reads=yk + ['s_u32', 's_dsk'], writes=yk)
                P.tt(t1[:], y, y, ALU.mult, reads=yk, writes=['s_e1'])
                P.ts(t1[:], t1[:], 0.044715, 1.0, ALU.mult, ALU.add, reads=['s_e1'], writes=['s_e1'])
                P.tt(t1[:], t1[:], y, ALU.mult, reads=yk + ['s_e1'], writes=['s_e1'])
                P.act(t1[:], t1[:], AF.Sigmoid, reads=['s_e1'], writes=['s_e1'], scale=2.0 * math.sqrt(2.0 / math.pi))
                P.tt(y, y, t1[:], ALU.mult, reads=yk + ['s_e1'], writes=yk)
                P.copy(yb[:, ct, :], y, reads=yk, writes=['s_yb'], eng='gpsimd')
            cnt = 0
            for nt in range(2):
                for (t0, tn) in TBLK:
                    i = cnt % 2
                    cnt += 1
                    P.mm(ps[i][:, :tn], gw[:, 0, nt * 128:(nt + 1) * 128], yb[:, 0, t0:t0 + tn], True, False, reads=['s_gw', 's_yb'], writes=['s_psz%d' % i])
                    P.mm(ps[i][:, :tn], gw[:, 1, nt * 128:(nt + 1) * 128], yb[:, 1, t0:t0 + tn], False, True, reads=['s_gw', 's_yb'], writes=['s_psz%d' % i])
                    P.act(zt[i][:, :tn], ps[i][:, :tn], AF.Sigmoid, reads=['s_psz%d' % i, 's_gb'], writes=['s_zt%d' % i], bias=gb[:, nt:nt + 1], scale=1.0)
                    P.tt(zt[i][:, :tn], zt[i][:, :tn], yacc[:, nt, t0:t0 + tn], ALU.mult, reads=['s_zt%d' % i] + yk, writes=['s_zt%d' % i])
                    P.dma(k.mixFM[b, 512 + nt * 128:512 + (nt + 1) * 128, t0:t0 + tn], zt[i][:, :tn], reads=['s_zt%d' % i])
            P.flush()


def phase_dn(k, l, b):
    P, nc = k.P, k.nc
    with ExitStack() as es:
        sbt = lambda name, shape, dt=F32: es.enter_context(_sbt(nc, name, list(shape), dt))
        qb = sbt('d_qb', [128, 2, T], BF16)
        kb = sbt('d_kb', [128, 2, T], BF16)
        kn = sbt('d_kn', [128, 2, T])
        vTM = sbt('d_vTM', [128, NT, 256])
        bTM = sbt('d_bTM', [128, NT, 64])
        gall = sbt('d_gall', [128, NT, 3, 128])
        ngc = sbt('d_ngc', [128, T])
        mh = sbt('d_mh', [128, 4])
        oh = sbt('d_oh', [128, 2, 512])
        ones4 = sbt('d_ones4', [128, 128])
        P.dma(mh[:], k.MH[:, :], writes=['d_mh'])
        P.dma(oh[:], k.OH[:, :, :], writes=['d_oh'])
        P.memset(ones4[:], 1.0, writes=['d_ones4'])
        oacc = sbt('d_oacc', [128, 2, T])
        selr = sbt('d_selr', [128, 4, 128])
        selp = sbt('d_selp', [128, 2, 128])
        mb = sbt('d_mb', [128, 2, 4, 128])
        blk2 = sbt('d_blk2', [128, 128])
        P.dma(selr[:], k.SELR[:, :, :], writes=['d_selr'])
        P.dma(selp[:], k.SELP[:, :, :], writes=['d_selp'])
        P.dma(mb[:], k.MB[:, :, :, :], writes=['d_mb'])
        P.dma(blk2[:], k.BLK2[:, :], writes=['d_blk2'])
        hm2 = sbt('d_hm2', [128, 2])
        P.dma(hm2[:], k.HM2[:, :], writes=['d_hm2'])
        with ExitStack() as es2:
            sb2 = lambda name, shape, dt=F32: es2.enter_context(_sbt(nc, name, list(shape), dt))
            x = [sb2('d_x%d' % i, [128, T]) for i in range(2)]
            acc = [sb2('d_acc%d' % i, [128, T]) for i in range(2)]
            sq = sb2('d_sq', [128, T])
            cw = sb2('d_cw', [128, 6, 5])
            eps = sb2('d_eps', [128, 1])
            ps = [es2.enter_context(_pst(nc, 'd_psA%d' % i, [128, 512], F32)) for i in range(2)]
            pst = [es2.enter_context(_pst(nc, 'd_psT%d' % i, [128, 128], F32)) for i in range(2)]
            P.dma(cw[:], k.convFM[:, l, :, :], writes=['d_cw'])
            P.memset(eps[:], EPS, writes=['d_eps'])
            cnt = 0
            for ti in range(6):
                i = ti % 2
                xk, ak = 'd_x%d' % i, 'd_acc%d' % i
                P.dma(x[i][:], k.pFM[b, (13 + ti) * 128:(14 + ti) * 128, :], writes=[xk])
                P.ts(acc[i][:], x[i][:], cw[:, ti, 2:3], None, ALU.mult, reads=[xk, 'd_cw'], writes=[ak])
                for (s0, s1) in [(0, LC), (LC, T)]:
                    for j in (0, 1, 3, 4):
                        sft = j - 2
                        lo, hi = max(s0, s0 - sft), min(s1, s1 - sft)
                        P.stt(acc[i][:, lo:hi], x[i][:, lo + sft:hi + sft], cw[:, ti, j:j + 1], acc[i][:, lo:hi], ALU.mult, ALU.add,
                              reads=[xk, 'd_cw', ak], writes=[ak])
                P.act(acc[i][:], acc[i][:], AF.Silu, reads=[ak], writes=[ak])
                if ti < 4:
                    hp = ti % 2
                    P.tt(sq[:], acc[i][:], acc[i][:], ALU.mult, reads=[ak], writes=['d_sq'], eng='gpsimd')
                    for (t0, tn) in TBLK:
                        pp = ps[cnt % 2]
                        ppk = 'd_psA%d' % (cnt % 2)
                        cnt += 1
                        P.mm(pp[:, :tn], blk2[:], sq[:, t0:t0 + tn], True, True, reads=['d_blk2', 'd_sq'], writes=[ppk])
                        P.act(x[i][:, t0:t0 + tn], pp[:, :tn], AF.Sqrt, reads=[ppk, 'd_eps', xk], writes=[xk], bias=eps[:], scale=1.0)
                    P.recip(x[i][:], x[i][:], reads=[xk], writes=[xk])
                    if ti < 2:
                        P.stt(qb[:, hp, :], acc[i][:], 0.125, x[i][:], ALU.mult, ALU.mult, reads=[ak, xk], writes=['d_qb'])
                    else:
                        P.tt(kn[:, hp, :], acc[i][:], x[i][:], ALU.mult, reads=[ak, xk], writes=['d_kn'])
                        P.copy(kb[:, hp, :], kn[:, hp, :], reads=['d_kn'], writes=['d_kb'], eng='gpsimd')
                else:
                    vt = ti - 4
                    for c in range(NT):
                        pt = pst[c % 2]
                        ptk = 'd_psT%d' % (c % 2)
                        P.tr(pt[:, :], acc[i][:, c * 128:(c + 1) * 128], k.identf[:], reads=[ak, 'identf'], writes=[ptk])
                        P.copy(vTM[:, c, vt * 128:(vt + 1) * 128], pt[:, :], reads=[ptk], writes=['d_vTM'], eng='scalar' if c % 2 else 'vector')
            P.flush()
        if k.dbg.get('dn_stop') == 'A':
            return
        with ExitStack() as es2:
            sb2 = lambda name, shape, dt=F32: es2.enter_context(_sbt(nc, name, list(shape), dt))
            ga = sb2('d_ga', [128, T])
            lb = sb2('d_lb', [128, T])
            bt = sb2('d_bt', [128, T])
            par = sb2('d_par', [128, 2])
            nA = sb2('d_nA', [128, 1])
            ones = sb2('d_ones', [128, 128])
            pst = [es2.enter_context(_pst(nc, 'd_psB%d' % i, [128, 128], F32)) for i in range(2)]
            P.dma(ga[:], k.pFM[b, 19 * 128:20 * 128, :], writes=['d_ga'])
            P.dma(lb[:], k.pFM[b, 26 * 128:27 * 128, :], writes=['d_lb'])
            P.dma(par[:], k.dn_par[:, l, :], writes=['d_par'])
            P.memset(ones[:], 1.0, writes=['d_ones'])
            P.memset(gall[:].rearrange("p a b c -> p (a b c)"), 0.0, writes=['d_gall'], eng='gpsimd')
            P.act(nA[:], par[:, 0:1], AF.Exp, reads=['d_par'], writes=['d_nA'])
            P.ts(nA[:], nA[:], -1.0, None, ALU.mult, reads=['d_nA'], writes=['d_nA'])
            P.act(ga[:], ga[:], AF.Exp, reads=['d_ga', 'd_par'], writes=['d_ga'], bias=par[:, 1:2], scale=1.0)
            P.act(ga[:], ga[:], AF.Ln, reads=['d_ga'], writes=['d_ga'], bias=1.0, scale=1.0)
            P.ts(ga[:], ga[:], nA[:, 0:1], None, ALU.mult, reads=['d_ga', 'd_nA'], writes=['d_ga'])
            P.act(lb[:], lb[:], AF.Exp, reads=['d_lb'], writes=['d_lb'], scale=-1.0)
            P.act(lb[:], lb[:], AF.Ln, reads=['d_lb'], writes=['d_lb'], bias=1.0, scale=1.0)
            P.ts(lb[:], lb[:], -1.0, None, ALU.mult, reads=['d_lb'], writes=['d_lb'])
            P.act(bt[:], lb[:], AF.Exp, reads=['d_lb'], writes=['d_bt'])
            for c in range(NT):
                cs = slice(c * 128, (c + 1) * 128)
                P.scan(gall[0:32, c, 0, :], ones[0:32, :], ga[0:32, cs], 0.0, reads=['d_ones', 'd_ga'], writes=['d_gall'])
                P.scan(gall[32:64, c, 0, :][:, ::-1], ones[32:64, :], ga[32:64, cs][:, ::-1], 0.0, reads=['d_ones', 'd_ga'], writes=['d_gall'])
            for c in range(NT):
                cs = slice(c * 128, (c + 1) * 128)
                P.tt(gall[:, c, 1, :], gall[:, c, 0, :], lb[:, cs], ALU.add, reads=['d_gall', 'd_lb'], writes=['d_gall'])
                P.ts(ngc[:, cs], gall[:, c, 0, :], -1.0, None, ALU.mult, reads=['d_gall'], writes=['d_ngc'], eng='gpsimd')
                P.ts(gall[0:32, c, 2, :], gall[0:32, c, 0, :], -1.0, gall[0:32, c, 0, 127:128], ALU.mult, ALU.add, reads=['d_gall'], writes=['d_gall'])
                P.ts(gall[32:64, c, 2, :], gall[32:64, c, 0, :], -1.0, gall[32:64, c, 0, 0:1], ALU.mult, ALU.add, reads=['d_gall'], writes=['d_gall'])
                pt = pst[c % 2]
                ptk = 'd_psB%d' % (c % 2)
                P.tr(pt[:, :], bt[:, cs], k.identf[:], reads=['d_bt', 'identf'], writes=[ptk])
                P.copy(bTM[:, c, :], pt[:, 0:64], reads=[ptk], writes=['d_bTM'], eng='scalar')
            P.flush()
        if k.dbg.get('dn_stop') == 'B':
            return
        with ExitStack() as es2:
            sb2 = lambda name, shape, dt=F32: es2.enter_context(_sbt(nc, name, list(shape), dt))
            CH = []
            for ch in range(4):
                B_ = {}
                t = 'd%d_' % ch
                B_['E'] = sb2(t + 'E', [128, 3, 128])
                B_['Qin'] = sb2(t + 'Qin', [128, 128], BF16)
                B_['KE2'] = sb2(t + 'KE2', [128, 128])
                B_['KE3'] = sb2(t + 'KE3', [128, 128], BF16)
                B_['RWpad'] = sb2(t + 'RWpad', [128, 2, 128])
                B_['RUpad'] = sb2(t + 'RUpad', [128, 2, 128])
                B_['Upad'] = sb2(t + 'Upad', [128, 2, 128], BF16)
                B_['koT'] = sb2(t + 'koT', [128, 128], BF16)
                B_['Gm'] = sb2(t + 'Gm', [128, 4, 128])
                B_['AT'] = sb2(t + 'AT', [128, 2, 128])
                B_['QKm'] = sb2(t + 'QKm', [128, 2, 128], BF16)
                B_['X'] = sb2(t + 'X', [128, 2, 2, 128])
                B_['Y'] = sb2(t + 'Y', [128, 2, 2, 128])
                B_['TT'] = sb2(t + 'TT', [128, 2, 128])
                B_['WTb'] = sb2(t + 'WTb', [128, 128])
                B_['Ub'] = sb2(t + 'Ub', [128, 128], BF16)
                B_['tmp'] = sb2(t + 'tmp', [128, 128])
                B_['S32'] = sb2(t + 'S32', [128, 128])
                B_['Sb'] = sb2(t + 'Sb', [128, 128], BF16)
                B_['Sn'] = sb2(t + 'Sn', [128, 128])
                B_['knm'] = sb2(t + 'knm', [128, 2, 128])
                B_['Rsel'] = sb2(t + 'Rsel', [128, 2, 2, 128])
                for nm in ['RWpad', 'RUpad', 'Upad']:
                    P.memset(B_[nm][:].rearrange("p a c -> p (a c)"), 0.0, writes=[t + nm], eng='gpsimd')
                for nm in ['S32', 'Sb', 'Sn']:
                    P.memset(B_[nm][:], 0.0, writes=[t + nm])
                CH.append(B_)
            bk = [es2.enter_context(_pst(nc, 'd_bank%d' % i, [128, 512], F32)) for i in range(7)]
            bA, bB, bC, bD, bE, bF, bG = bk
            psTb = es2.enter_context(_pst(nc, 'd_psTb', [128, 128], BF16))
            kk0, W0 = bA[:, 0:128], bA[:, 128:256]
            kk1, W1 = bB[:, 0:128], bB[:, 128:256]
            N0, T32, W2 = bC[:, 0:128], bC[:, 128:256], bC[:, 256:384]
            psS = bE[:, 256:384]
            N1, qk0, qk1 = bD[:, 0:128], bD[:, 128:256], bD[:, 256:384]
            N2 = bE[:, 0:128]
            psE = bF[:, 0:384]
            psD = bG[:, :]
            psKK = [kk0, kk1]
            psQK = [qk0, qk1]
            seen = set()
            for ci in range(NT):
                for d in range(2):
                    c = chunk_order(d)[ci]
                    cs = slice(c * 128, (c + 1) * 128)
                    rows = slice(32 * d, 32 * d + 4)
                    lloc = 127 if d == 0 else 0
                    for hp in range(2):
                        ch = d * 2 + hp
                        B_ = CH[ch]
                        t = 'd%d_' % ch
                        kk_ = lambda nm: t + nm
                        P.mm(psE, selp[rows, hp, :], gall[rows, c, :, :].rearrange("p a b -> p (a b)"), True, True,
                             reads=['d_selp', 'd_gall'], writes=['d_psE'])
                        P.act(B_['E'][:].rearrange("p a b -> p (a b)"), psE, AF.Exp, reads=['d_psE'], writes=[kk_('E')])
                        P.tt(B_['Qin'][:], qb[:, hp, cs], B_['E'][:, 0, :], ALU.mult, reads=['d_qb', kk_('E')], writes=[kk_('Qin')])
                        P.tt(B_['KE2'][:], kn[:, hp, cs], B_['E'][:, 1, :], ALU.mult, reads=['d_kn', kk_('E')], writes=[kk_('KE2')], eng='gpsimd')
                        P.tt(B_['KE3'][:], kn[:, hp, cs], B_['E'][:, 2, :], ALU.mult, reads=['d_kn', kk_('E')], writes=[kk_('KE3')], eng='gpsimd')
                        if k.dbg.get('dn_lvl', 99) < 1:
                            continue
                        P.tr(T32, B_['KE2'][:], k.identf[:], reads=[kk_('KE2'), 'identf'], writes=['d_psT32'])
                        for hh in range(2):
                            P.copy(B_['RWpad'][:, hh, hh * 64:(hh + 1) * 64], T32[:, hh * 64:(hh + 1) * 64], reads=['d_psT32'],
                                   writes=[kk_('RWpad')], eng='scalar' if hh else 'vector')
                        P.tr(psTb[:, :], B_['KE3'][:], k.identb[:], reads=[kk_('KE3'), 'identb'], writes=['d_psTb'])
                        P.copy(B_['koT'][:], psTb[:, :], reads=['d_psTb'], writes=[kk_('koT')], eng='scalar')
                        if k.dbg.get('dn_lvl', 99) < 2:
                            continue
                        for hh in range(2):
                            h = 2 * hp + hh
                            P.ts(B_['Rsel'][rows, hh, :, :], gall[rows, c, 0:2, :], mh[rows, h:h + 1], None, ALU.mult,
                                 reads=['d_gall', 'd_mh'], writes=[kk_('Rsel')])
                        P.mm(psD, ones4[rows, :], B_['Rsel'][rows, :, :, :].rearrange("p a b c -> p (a b c)"), True, False,
                             reads=['d_ones4', kk_('Rsel')], writes=['d_psD'])
                        P.mm(psD, ngc[rows, cs], oh[rows, hp, :], False, True, reads=['d_ngc', 'd_oh'], writes=['d_psD'])
                        P.tt(B_['Gm'][:].rearrange("p a b -> p (a b)"), psD, mb[:, d, :, :].rearrange("p a b -> p (a b)"), ALU.add,
                             reads=['d_psD', 'd_mb'], writes=[kk_('Gm')])
                        P.act(B_['Gm'][:], B_['Gm'][:], AF.Exp, reads=[kk_('Gm')], writes=[kk_('Gm')])
                        if k.dbg.get('dn_lvl', 99) < 3:
                            continue
                        for hh in range(2):
                            pr = slice(hh * 64, (hh + 1) * 64)
                            P.ts(B_['knm'][:, hh, :], kn[:, hp, cs], hm2[:, hh:hh + 1], None, ALU.mult, reads=['d_kn', 'd_hm2'], writes=[kk_('knm')],
                                 eng='gpsimd')
                            P.mm(psKK[hh], B_['knm'][:, hh, :], kn[:, hp, cs], True, True, reads=['d_kn', kk_('knm')], writes=['d_psKK%d' % hh])
                            P.mm(psQK[hh], kb[pr, hp, cs], qb[pr, hp, cs], True, True, reads=['d_kb', 'd_qb'], writes=['d_psQK%d' % hh])
                        for hh in range(2):
                            P.tt(B_['AT'][:, hh, :], psKK[hh], B_['Gm'][:, 2 * hh + 1, :], ALU.mult, reads=['d_psKK%d' % hh, kk_('Gm')], writes=[kk_('AT')])
                            P.tt(B_['QKm'][:, hh, :], psQK[hh], B_['Gm'][:, 2 * hh, :], ALU.mult, reads=['d_psQK%d' % hh, kk_('Gm')], writes=[kk_('QKm')])
                        if k.dbg.get('dn_lvl', 99) < 4:
                            continue
                        for hh in range(2):
                            X0 = B_['AT'][:, hh, :]
                            P.tr(N0, X0, k.identf[:], reads=[kk_('AT'), 'identf'], writes=['d_psN0'])
                            P.copy(B_['Y'][:, hh, 0, :], N0, reads=['d_psN0'], writes=[kk_('Y')], eng='scalar')
                            P.tt(B_['TT'][:, hh, :], k.identf[:], X0, ALU.subtract, reads=['identf', kk_('AT')], writes=[kk_('TT')])
                            Xc, Yc = X0, B_['Y'][:, hh, 0, :]
                            for lv in range(1, 7):
                                Yn = B_['Y'][:, hh, lv % 2, :]
                                if lv < 6:
                                    Xn = B_['X'][:, hh, lv % 2, :]
                                    P.mm(N0, Yc, Xc, True, True, reads=[kk_('X'), kk_('Y'), kk_('AT')], writes=['d_psN0'])
                                P.mm(N1, Xc, Yc, True, True, reads=[kk_('X'), kk_('Y'), kk_('AT')], writes=['d_psN1'])
                                if lv < 6:
                                    P.copy(Xn, N0, reads=['d_psN0'], writes=[kk_('X')], eng='scalar')
                                P.copy(Yn, N1, reads=['d_psN1'], writes=[kk_('Y')])
                                P.mm(N2, Yn, B_['TT'][:, hh, :], True, True, reads=[kk_('Y'), kk_('TT')], writes=['d_psN2'])
                                P.tt(B_['TT'][:, hh, :], B_['TT'][:, hh, :], N2, ALU.add, reads=['d_psN2', kk_('TT')], writes=[kk_('TT')])
                                if lv < 6:
                                    Xc = Xn
                                Yc = Yn
                        if k.dbg.get('dn_lvl', 99) < 5:
                            continue
                        for hh in range(2):
                            P.mm(W0, B_['RWpad'][:, hh, :], B_['TT'][:, hh, :], hh == 0, hh == 1, reads=[kk_('RWpad'), kk_('TT')], writes=['d_psW0'])
                        if k.dbg.get('dn_sub', 9) < 1:
                            continue
                        P.copy(B_['WTb'][:], W0, reads=['d_psW0'], writes=[kk_('WTb')], eng='scalar')
                        for hh in range(2):
                            h = 2 * hp + hh
                            P.ts(B_['RUpad'][:, hh, hh * 64:(hh + 1) * 64], vTM[:, c, hp * 128 + hh * 64:hp * 128 + (hh + 1) * 64],
                                 bTM[:, c, 32 * d + h:32 * d + h + 1], None, ALU.mult, reads=['d_vTM', 'd_bTM'], writes=[kk_('RUpad')], eng='gpsimd')
                        if k.dbg.get('dn_sub', 9) < 2:
                            continue
                        P.mm(W1, B_['TT'][:, 0, :], B_['RUpad'][:, 0, :], True, False, reads=[kk_('TT'), kk_('RUpad')], writes=['d_psW1'])
                        P.mm(W1, B_['TT'][:, 1, :], B_['RUpad'][:, 1, :], False, False, reads=[kk_('TT'), kk_('RUpad')], writes=['d_psW1'])
                        P.mm(W1, B_['WTb'][:], B_['Sn'][:], False, True, reads=[kk_('WTb'), kk_('Sn')], writes=['d_psW1'])
                        if k.dbg.get('dn_sub', 9) < 3:
                            continue
                        P.ts(B_['Ub'][:], W1, 1.0, None, ALU.mult, reads=['d_psW1'], writes=[kk_('Ub')])
                        for hh in range(2):
                            P.ts(B_['Upad'][:, hh, hh * 64:(hh + 1) * 64], W1[:, hh * 64:(hh + 1) * 64], 1.0, None, ALU.mult, reads=['d_psW1'], writes=[kk_('Upad')])
                        if k.dbg.get('dn_lvl', 99) < 6:
                            continue
                        P.mm(W2, B_['Upad'][:, 0, :], B_['QKm'][:, 0, :], True, False, reads=[kk_('Upad'), kk_('QKm')], writes=['d_psW2'])
                        P.mm(W2, B_['Upad'][:, 1, :], B_['QKm'][:, 1, :], False, False, reads=[kk_('Upad'), kk_('QKm')], writes=['d_psW2'])
                        P.mm(W2, B_['Sb'][:], B_['Qin'][:], False, True, reads=[kk_('Sb'), kk_('Qin')], writes=['d_psW2'])
                        ok = ('d_oacc', hp, c)
                        if ok not in seen:
                            seen.add(ok)
                            P.copy(oacc[:, hp, cs], W2, reads=['d_psW2'], writes=[ok], eng='scalar')
                        else:
                            P.tt(oacc[:, hp, cs], oacc[:, hp, cs], W2, ALU.add, reads=['d_psW2', ok], writes=[ok])
                        if k.dbg.get('dn_lvl', 99) < 7:
                            continue
                        P.mm(psS, B_['koT'][:], B_['Ub'][:], True, True, reads=[kk_('koT'), kk_('Ub')], writes=['d_psS'])
                        if k.dbg.get('dn_sub8', 9) < 1:
                            continue
                        P.tt(B_['tmp'][:], psS, blk2[:], ALU.mult, reads=['d_psS', 'd_blk2'], writes=[kk_('tmp')])
                        if k.dbg.get('dn_sub8', 9) < 2:
                            continue
                        P.stt(B_['S32'][:], B_['S32'][:], B_['E'][:, 0, lloc:lloc + 1], B_['tmp'][:], ALU.mult, ALU.add,
                              reads=[kk_('S32'), kk_('E'), kk_('tmp')], writes=[kk_('S32')])
                        if k.dbg.get('dn_sub8', 9) < 3:
                            continue
                        P.copy(B_['Sb'][:], B_['S32'][:], reads=[kk_('S32')], writes=[kk_('Sb')], eng='scalar')
                        if k.dbg.get('dn_sub8', 9) < 4:
                            continue
                        P.ts(B_['Sn'][:], B_['S32'][:], -1.0, None, ALU.mult, reads=[kk_('S32')], writes=[kk_('Sn')])
            P.flush()
        for hp in range(2):
            with ExitStack() as es2:
                norm_gate(k, P, nc, es2, b, oacc[:, hp, :], [('d_oacc', hp, c) for c in range(NT)], 24 + hp, 768 + hp * 128, False, 'd%d' % hp)
                P.flush()


def phase_outproj_moe(k, l, b, last):
    P, nc = k.P, k.nc
    t_first = 2 if last else 0
    with ExitStack() as es:
        sbt = lambda name, shape, dt=F32: es.enter_context(_sbt(nc, name, list(shape), dt))
        h2T = sbt('h2T', [128, 8, T], BF16)
        comb = sbt('comb', [128, NT, NE])
        k.eps_col = sbt('eps_col', [128, 1])
        rw = sbt('rw', [128, 8, NE])
        rb = sbt('rb', [128, NE])
        fg = sbt('fg', [128, D])
        P.memset(k.eps_col[:], EPS, writes=['eps'])
        P.dma(rw[:], k.router_w[:, :, :], writes=['rw'])
        P.dma(rb[:], k.router_b_rep[:, :], writes=['rb'])
        P.dma(fg[:], k.final_g_rep[:, :], writes=['fg'])
        ps = [es.enter_context(_pst(nc, 'ps%d' % i, [128, 512], F32)) for i in range(8)]
        with ExitStack() as es2:
            sb2 = lambda name, shape, dt=F32: es2.enter_context(_sbt(nc, name, list(shape), dt))
            mixT = sb2('mixT', [128, 8, T], BF16)
            wo = sb2('wo', [128, 8, D], BF16)
            grep_ = sb2('grep', [128, 2, D])
            xt = [sb2('xt%d' % i, [128, D]) for i in range(2)]
            xn = [sb2('xn%d' % i, [128, D]) for i in range(2)]
            h32 = [sb2('h32_%d' % i, [128, 8, 128]) for i in range(2)]
            ssq = [sb2('ssq%d' % i, [128, 1]) for i in range(2)]
            rstd = [sb2('rstd%d' % i, [128, 1]) for i in range(2)]
            sc = [sb2('rsc%d' % i, [128, 8, NE]) for i in range(2)]
            for kc in range(8):
                P.dma(wo[:, kc, :], k.w_out[l, :, kc, :], writes=['wo'], eng='gpsimd')
                P.dma(mixT[:, kc, :], k.mixFM[b, kc * 128:(kc + 1) * 128, :], writes=['mixT'], eng='gpsimd')
            for gi, col in enumerate([b, 2]):
                P.dma(grep_[:, gi, :], k.gateD[l, 0, col, :].partition_broadcast(128), writes=['grep'])
            for tt in range(t_first, NT):
                i = tt % 2
                tag = str(i)
                gi = 1 if tt < 2 else 0
                col = 2 if tt < 2 else b
                P.dma(xt[i][:], resid_src(k, l, b, tt), writes=['xt' + tag])
                for half in range(2):
                    pt = ps[4 + half]
                    pk = 'ps%d' % (4 + half)
                    for kc in range(8):
                        P.mm(pt[:, :], mixT[:, kc, tt * 128:(tt + 1) * 128], wo[:, kc, half * 512:(half + 1) * 512],
                             kc == 0, kc == 7, reads=['mixT', 'wo'], writes=[pk])
                    P.tt(xn[i][:, half * 512:(half + 1) * 512], pt[:, :], grep_[:, gi, half * 512:(half + 1) * 512], ALU.mult,
                         reads=[pk, 'grep'], writes=['xn' + tag])
                P.tt(xt[i][:], xt[i][:], xn[i][:], ALU.add, reads=['xt' + tag, 'xn' + tag], writes=['xt' + tag])
                P.dma(resid_dst(k, b, tt), xt[i][:], reads=['xt' + tag])
                if 'x_mid' in k.dbg_out and l == k.dbg.get('l', 0) and b == 0:
                    P.dma(k.dbg_out['x_mid'][tt * 128:(tt + 1) * 128, :], xt[i][:], reads=['xt' + tag])
                norm_modulate_tile(k, P, xt[i][:], 'xt' + tag, l, 1, col, ssq[i], rstd[i], xn[i],
                                   [ps[2 * i], ps[2 * i + 1]], ['ps%d' % (2 * i), 'ps%d' % (2 * i + 1)],
                                   [h2T[:, fc, tt * 128:(tt + 1) * 128] for fc in range(8)],
                                   [('h2T', tt)] * 8, tag, h32=None if k.dbg.get('norouter') else h32[i], h32key='h32_' + tag)
                pr = ps[6 + i]
                prk = 'ps%d' % (6 + i)
                for kc in range(0 if k.dbg.get('norouter') else 8):
                    P.mm(pr[:, 0:NE], h32[i][:, kc, :], rw[:, kc, :], kc == 0, kc == 7,
                         reads=['h32_' + tag, 'rw'], writes=[prk])
                if not k.dbg.get('norouter'):
                    router_tile(k, P, pr[:, 0:NE], prk, rb, sc[i], 'rsc' + tag, comb[:, tt, :], ('comb', tt))
            P.flush()
        if k.dbg.get('stopA'):
            return
        with ExitStack() as es2:
            sb2 = lambda name, shape, dt=F32: es2.enter_context(_sbt(nc, name, list(shape), dt))
            facc = sb2('facc', [128, NT, D])
            for tt in range(NT):
                P.memset(facc[:, tt, :], 0.0, writes=[('facc', tt, 0), ('facc', tt, 1)], eng='gpsimd')
            blks = [(t0, tn) for (t0, tn) in TBLK]
            if last:
                blks = [(256, 512), (768, 512), (1280, 512), (1792, 512)]
            with ExitStack() as es3:
                sb3 = lambda name, shape, dt=F32: es3.enter_context(_sbt(nc, name, list(shape), dt))
                wg = [sb3('wg%d' % i, [128, 8, DFF], BF16) for i in range(2)]
                wu = [sb3('wu%d' % i, [128, 8, DFF], BF16) for i in range(2)]
                wd = [sb3('wd%d' % i, [128, 4, D], BF16) for i in range(2)]
                actT = [sb3('actT%d' % i, [128, 4, 512], BF16) for i in range(2)]
                sg = [sb3('sg%d' % i, [128, 512]) for i in range(2)]
                cnt = 0
                for e in range(NE):
                    i = e % 2
                    si = str(i)
                    for kc in range(8):
                        P.dma(wg[i][:, kc, :], k.moe_wg[l, e, :, kc, :], writes=['wg' + si], eng='gpsimd')
                        P.dma(wu[i][:, kc, :], k.moe_wu[l, e, :, kc, :], writes=['wu' + si], eng='gpsimd')
                    for fc in range(4):
                        P.dma(wd[i][:, fc, :], k.moe_wd[l, e, :, fc, :], writes=['wd' + si], eng='gpsimd')
                    for (t0, tn) in blks:
                        a = actT[cnt % 2]
                        ak = 'actT%d' % (cnt % 2)
                        cnt += 1
                        hk = [('h2T', t0 // 128 + j) for j in range(tn // 128)]
                        for fc in range(4):
                            pg = ps[fc % 2]
                            pu = ps[2 + fc % 2]
                            pgk, puk = 'ps%d' % (fc % 2), 'ps%d' % (2 + fc % 2)
                            s_ = sg[fc % 2]
                            sk = 'sg%d' % (fc % 2)
                            for kc in range(8):
                                P.mm(pg[:, :tn], wg[i][:, kc, fc * 128:(fc + 1) * 128], h2T[:, kc, t0:t0 + tn], kc == 0, kc == 7,
                                     reads=['wg' + si] + hk, writes=[pgk])
                            for kc in range(8):
                                P.mm(pu[:, :tn], wu[i][:, kc, fc * 128:(fc + 1) * 128], h2T[:, kc, t0:t0 + tn], kc == 0, kc == 7,
                                     reads=['wu' + si] + hk, writes=[puk])
                            P.act(s_[:, :tn], pg[:, :tn], AF.Silu, reads=[pgk], writes=[sk])
                            P.tt(a[:, fc, :tn], s_[:, :tn], pu[:, :tn], ALU.mult, reads=[sk, puk], writes=[ak])
                        for j in range(tn // 128):
                            tt = t0 // 128 + j
                            for half in range(2):
                                pd = ps[4 + (2 * j + half) % 4]
                                pdk = 'ps%d' % (4 + (2 * j + half) % 4)
                                for fc in range(4):
                                    P.mm(pd[:, :], a[:, fc, j * 128:(j + 1) * 128], wd[i][:, fc, half * 512:(half + 1) * 512],
                                         fc == 0, fc == 3, reads=[ak, 'wd' + si], writes=[pdk])
                                fs = facc[:, tt, half * 512:(half + 1) * 512]
                                P.stt(fs, pd[:, :], comb[:, tt, e:e + 1], fs, ALU.mult, ALU.add,
                                      reads=[pdk, ('comb', tt), ('facc', tt, half)], writes=[('facc', tt, half)])
                P.flush()
            grep2 = sb2('grep2', [128, 2, D])
            xm = [sb2('xm%d' % i, [128, D]) for i in range(2)]
            ssq = sb2('ssqf', [128, 1])
            rstd = sb2('rstdf', [128, 1])
            junk = sb2('junkf', [128, D])
            for gi, col in enumerate([b, 2]):
                P.dma(grep2[:, gi, :], k.gateD[l, 1, col, :].partition_broadcast(128), writes=['grep2'])
            for tt in range(t_first, NT):
                gi = 1 if tt < 2 else 0
                i = tt % 2
                xk = 'xm%d' % i
                fk = [('facc', tt, 0), ('facc', tt, 1)]
                P.dma(xm[i][:], resid_dst(k, b, tt), writes=[xk])
                P.tt(facc[:, tt, :], facc[:, tt, :], grep2[:, gi, :], ALU.mult, reads=fk + ['grep2'], writes=fk)
                P.tt(xm[i][:], xm[i][:], facc[:, tt, :], ALU.add, reads=fk + [xk], writes=[xk])
                if 'x_end' in k.dbg_out and l == k.dbg.get('l', 0) and b == 0:
                    P.dma(k.dbg_out['x_end'][tt * 128:(tt + 1) * 128, :], xm[i][:], reads=[xk])
                if 'f_out' in k.dbg_out and l == k.dbg.get('l', 0) and b == 0:
                    P.dma(k.dbg_out['f_out'][tt * 128:(tt + 1) * 128, :], facc[:, tt, :], reads=fk)
                if not last:
                    P.dma(resid_dst(k, b, tt), xm[i][:], reads=[xk])
                else:
                    P.op('scalar', lambda s, i=i: s.activation(out=junk[:], in_=xm[i][:], func=AF.Square, accum_out=ssq[:]),
                         reads=[xk], writes=['junkf', 'ssqf'])
                    P.act(rstd[:], ssq[:], AF.Sqrt, reads=['ssqf'], writes=['rstdf'], scale=1.0 / D, bias=k.eps_col[:])
                    P.recip(rstd[:], rstd[:], reads=['rstdf'], writes=['rstdf'])
                    P.stt(xm[i][:], xm[i][:], rstd[:, 0:1], fg[:], ALU.mult, ALU.mult,
                          reads=[xk, 'rstdf', 'fg'], writes=[xk])
                    P.dma(k.out[b, (tt - 2) * 128:(tt - 1) * 128, :], xm[i][:], reads=[xk])
            P.flush()


def router_tile(k, P, logits, lk, rb, sc, sck, comb_out, ck):
    BIG = 1.0e4
    s = sc[:, 0, :]
    sel = sc[:, 1, :]
    t1 = sc[:, 2, :]
    t2 = sc[:, 3, :]
    m1 = sc[:, 4, 0:4]
    m2 = sc[:, 4, 4:8]
    gs = sc[:, 4, 8:12]
    gm = sc[:, 4, 12:13]
    ing = sc[:, 5, 0:4]
    e1 = sc[:, 6, :]
    mx = sc[:, 5, 4:5]
    mx2 = sc[:, 5, 5:6]
    ws = sc[:, 5, 6:7]
    R = dict(reads=[sck], writes=[sck])
    P.act(s, logits, AF.Sigmoid, reads=[lk], writes=[sck])
    P.tt(sel, s, rb[:, :], ALU.add, reads=[sck, 'rb'], writes=[sck])
    sel3 = sc[:, 1, :].rearrange("p (g e) -> p g e", g=4)
    t13 = sc[:, 2, :].rearrange("p (g e) -> p g e", g=4)
    P.red(m1, sel3, ALU.max, **R)
    P.tt(t13, sel3, m1.unsqueeze(2).to_broadcast([128, 4, 4]), ALU.is_equal, **R)
    P.stt(t1, t1, -BIG, sel, ALU.mult, ALU.add, **R)
    P.red(m2, t13, ALU.max, **R)
    P.tt(gs, m1, m2, ALU.add, **R)
    P.red(gm, gs, ALU.max, **R)
    P.ts(ing, gs, gm, None, ALU.is_equal, **R)
    P.ts(ing, ing, -1.0, BIG, ALU.add, ALU.mult, **R)
    P.tt(t13, sel3, ing.unsqueeze(2).to_broadcast([128, 4, 4]), ALU.add, **R)
    P.red(mx, t1, ALU.max, **R)
    P.ts(e1, t1, mx, None, ALU.is_equal, **R)
    P.stt(t2, e1, -BIG, t1, ALU.mult, ALU.add, **R)
    P.red(mx2, t2, ALU.max, **R)
    P.ts(t2, t2, mx2, None, ALU.is_equal, **R)
    P.tt(e1, e1, t2, ALU.add, **R)
    P.tt(e1, e1, s, ALU.mult, **R)
    P.red(ws, e1, ALU.add, **R)
    P.recip(ws, ws, **R)
    P.ts(comb_out, e1, ws, None, ALU.mult, reads=[sck], writes=[ck])


def phase_dbg(k):
    P, nc = k.P, k.nc
    outs = k.dbg_out
    if not outs:
        return
    with ExitStack() as es:
        t = es.enter_context(_sbt(nc, 'dbgt', [128, T], F32))
        if 'pFM' in outs:
            for cb in range(NFM // 128):
                P.dma(t[:], k.pFM[0, cb * 128:(cb + 1) * 128, :], writes=['dbgt'])
                P.dma(outs['pFM'][cb * 128:(cb + 1) * 128, :], t[:], reads=['dbgt'])
        if 'pTM' in outs:
            for tt in range(NT):
                P.dma(t[:, :NTM], k.pTM[0, tt * 128:(tt + 1) * 128, :], writes=['dbgt'])
                P.dma(outs['pTM'][tt * 128:(tt + 1) * 128, :], t[:, :NTM], reads=['dbgt'])
        if 'mixFM' in outs:
            for cb in range(8):
                P.dma(t[:], k.mixFM[0, cb * 128:(cb + 1) * 128, :], writes=['dbgt'])
                P.dma(outs['mixFM'][cb * 128:(cb + 1) * 128, :], t[:], reads=['dbgt'])
        if 'modFM' in outs:
            P.dma(outs['modFM'][:, :], k.modFM[:].rearrange("p a b c -> p (a b c)"), reads=['modFM'])
        P.flush()


_CACHE = {}


def kernel(**inputs):
    inp = {kk: np.asarray(v) for kk, v in inputs.items()}
    sh = host_prep(inp)
    if 'nc' not in _CACHE:
        _CACHE['nc'] = build()
    nc = _CACHE['nc']
    in_maps = []
    for c in range(8):
        m = dict(sh)
        m.update(core_inputs(inp, c))
        in_maps.append(m)
    res = run_bass_kernel_spmd(nc, in_maps, core_ids=list(range(8)))
    out = np.concatenate([r['out'] for r in res.results], axis=0)
    return out.astype(np.float32)
```

```python
import math
import numpy as np
from contextlib import ExitStack
import concourse.bass as bass
import concourse.mybir as mybir
from concourse.bass_utils import run_bass_kernel_spmd

F32 = mybir.dt.float32
BF16 = mybir.dt.bfloat16
I32 = mybir.dt.int32
AF = mybir.ActivationFunctionType
ALU = mybir.AluOpType
AX = mybir.AxisListType

D = 1024
L = 2048
LC = 256
T = L + LC
NT = T // 128
DEPTH = 2
NB = 2
NE = 16
DFF = 512
EPS = 1e-6
NFM = 27 * 128
NTM = 512
TBLK = [(0, 512), (512, 512), (1024, 512), (1536, 512), (2048, 256)]

ENGS = ['tensor', 'vector', 'scalar', 'gpsimd', 'sync']
SAME_ENGINE_WAIT = True
N_DMA_SEMS = 24


_UID = [0]


def _sbt(nc, name, shape, dt):
    _UID[0] += 1
    return nc.sbuf_tensor("%s_%d" % (name, _UID[0]), shape, dt)


def _pst(nc, name, shape, dt):
    _UID[0] += 1
    return nc.psum_tensor("%s_%d" % (name, _UID[0]), shape, dt)


class Prog:
    def __init__(self, nc, es):
        self.nc = nc
        self.sem = {e: es.enter_context(nc.semaphore("s_" + e)) for e in ENGS}
        self.cnt = {e: 0 for e in ENGS}
        self.dsem = [es.enter_context(nc.semaphore("d_%d" % i)) for i in range(N_DMA_SEMS)]
        self.dval = [0] * N_DMA_SEMS
        self.drr = 0
        self.semobj = {}
        for e in ENGS:
            self.semobj[('e', e)] = self.sem[e]
        for i in range(N_DMA_SEMS):
            self.semobj[('d', i)] = self.dsem[i]
        self.nops = 0
        self.begin()

    def begin(self):
        self.ops = {e: [] for e in ENGS}
        self.known = {e: {} for e in ENGS}
        self.lastw = {}
        self.readers = {}
        self.pending_dma = {e: [] for e in ENGS}

    def op(self, eng, fn, reads=(), writes=(), dma=False):
        deps = set()
        for k in reads:
            t = self.lastw.get(k)
            if t is not None:
                deps.add(t)
        for k in writes:
            t = self.lastw.get(k)
            if t is not None:
                deps.add(t)
            for t in self.readers.get(k, ()):
                deps.add(t)
        waits = []
        if dma:
            idx = self.drr % N_DMA_SEMS
            self.drr += 1
            if self.dval[idx] > 0:
                deps.add((('d', idx), self.dval[idx]))
            self.dval[idx] += 16
            tok = (('d', idx), self.dval[idx])
            inc = (('d', idx), 16)
            self.pending_dma[eng].append(tok)
        else:
            self.cnt[eng] += 1
            tok = (('e', eng), self.cnt[eng])
            inc = (('e', eng), 1)
        kn = self.known[eng]
        best = {}
        for (s, v) in deps:
            if s == ('e', eng) and not dma:
                if eng == 'tensor' or not SAME_ENGINE_WAIT:
                    continue
            if kn.get(s, 0) >= v:
                continue
            if best.get(s, 0) < v:
                best[s] = v
        for s, v in best.items():
            kn[s] = v
            waits.append((s, v))
        self.ops[eng].append((waits, fn, inc))
        self.nops += 1
        for k in reads:
            self.readers.setdefault(k, []).append(tok)
        for k in writes:
            self.lastw[k] = tok
            self.readers[k] = []
        return tok

    def flush(self):
        nc = self.nc
        tails = {}
        for e in ENGS:
            seen = {}
            for (s, v) in self.pending_dma[e]:
                if seen.get(s, 0) < v:
                    seen[s] = v
            tails[e] = [(s, v) for s, v in seen.items() if self.known[e].get(s, 0) < v]
        ops = self.ops
        semobj = self.semobj
        with nc.Block() as block:
            for e in ENGS:
                if not ops[e] and not tails[e]:
                    continue

                def body(eng, e=e):
                    for (waits, fn, inc) in ops[e]:
                        for (s, v) in waits:
                            eng.wait_ge(semobj[s], v)
                        ins = fn(eng)
                        ins.then_inc(semobj[inc[0]], inc[1])
                    for (s, v) in tails[e]:
                        eng.wait_ge(semobj[s], v)
                getattr(block, e)(body)
        self.begin()

    def dma(self, out, in_, reads=(), writes=(), eng='sync'):
        return self.op(eng, lambda g: g.dma_start(out=out, in_=in_), reads, writes, dma=True)

    def mm(self, out, lhsT, rhs, start, stop, reads=(), writes=()):
        return self.op('tensor', lambda t: t.matmul(out, lhsT=lhsT, rhs=rhs, start=start, stop=stop), reads, writes)

    def tr(self, out, in_, ident, reads=(), writes=()):
        return self.op('tensor', lambda t: t.transpose(out, in_, ident), reads, writes)

    def act(self, out, in_, func, reads=(), writes=(), **kw):
        return self.op('scalar', lambda s: s.activation(out=out, in_=in_, func=func, **kw), reads, writes)

    def ts(self, out, in0, s1, s2, op0, op1=None, reads=(), writes=(), eng='vector', **kw):
        if op1 is None:
            return self.op(eng, lambda v: v.tensor_scalar(out=out, in0=in0, scalar1=s1, scalar2=None, op0=op0, **kw), reads, writes)
        return self.op(eng, lambda v: v.tensor_scalar(out=out, in0=in0, scalar1=s1, scalar2=s2, op0=op0, op1=op1, **kw), reads, writes)

    def tt(self, out, in0, in1, op, reads=(), writes=(), eng='vector'):
        return self.op(eng, lambda v: v.tensor_tensor(out=out, in0=in0, in1=in1, op=op), reads, writes)

    def stt(self, out, in0, scalar, in1, op0, op1, reads=(), writes=()):
        return self.op('vector', lambda v: v.scalar_tensor_tensor(out=out, in0=in0, scalar=scalar, in1=in1, op0=op0, op1=op1), reads, writes)

    def copy(self, out, in_, reads=(), writes=(), eng='vector'):
        if eng == 'scalar':
            return self.op(eng, lambda s: s.copy(out=out, in_=in_), reads, writes)
        return self.op(eng, lambda v: v.tensor_copy(out=out, in_=in_), reads, writes)

    def memset(self, out, val, writes=(), eng='vector'):
        return self.op(eng, lambda v: v.memset(out, val), (), writes)

    def recip(self, out, in_, reads=(), writes=()):
        return self.op('vector', lambda v: v.reciprocal(out=out, in_=in_), reads, writes)

    def red(self, out, in_, op, reads=(), writes=()):
        return self.op('vector', lambda v: v.tensor_reduce(out=out, in_=in_, axis=AX.X, op=op), reads, writes)

    def scan(self, out, d0, d1, init, reads=(), writes=()):
        return self.op('vector', lambda v: v.tensor_tensor_scan(out=out, data0=d0, data1=d1, initial=init, op0=ALU.mult, op1=ALU.add), reads, writes)


IN_OFF = {}
_o = 0
for _n, _s in [('gq', 128), ('gk', 128), ('gv', 256), ('gg', 256), ('gzf', 16), ('gzb', 16),
               ('rq', 256), ('rk', 256), ('rv', 256), ('rg', 256), ('su', 256),
               ('dq', 256), ('dk', 256), ('dv', 256), ('dg', 256), ('da', 8), ('db', 8)]:
    IN_OFF[_n] = _o
    _o += _s


def _rope_perm(off):
    main, sw = [], []
    for h in range(4):
        base = off + h * 64
        ev = [base + 2 * i for i in range(32)]
        od = [base + 2 * i + 1 for i in range(32)]
        main += ev + od
        sw += od + ev
    return main, sw


def _fm_cols():
    c = []
    rng = lambda n, k: list(range(IN_OFF[n], IN_OFF[n] + k))
    pad = lambda k: [-1] * k
    c += rng('gq', 128)
    c += rng('gk', 128)
    c += rng('gzf', 16) + pad(16) + rng('gzb', 16) + pad(80)
    m, s = _rope_perm(IN_OFF['rq'])
    c += m + s
    m, s = _rope_perm(IN_OFF['rk'])
    c += m + s
    c += rng('su', 256)
    c += rng('dq', 256) + rng('dk', 256) + rng('dv', 256)
    da, db = rng('da', 8), rng('db', 8)
    c += da[0:4] + pad(28) + da[4:8] + pad(92)
    c += rng('gg', 256) + rng('rg', 256) + rng('dg', 256)
    c += db[0:4] + pad(28) + db[4:8] + pad(92)
    assert len(c) == NFM
    return np.array(c)


def _tm_cols():
    c = []
    for n in ['gv', 'rv']:
        c += list(range(IN_OFF[n], IN_OFF[n] + 256))
    return np.array(c)


def _kmajor(w):
    K, N = w.shape
    return np.ascontiguousarray(w.reshape(K // 128, 128, N).transpose(1, 0, 2))


def _fmvec(v):
    return np.ascontiguousarray(v.reshape(-1, 128).T)


def host_prep(inp):
    f = np.float32
    sh = {}
    sh['ident'] = np.eye(128, dtype=f)
    w_ada = inp['w_ada']
    sh['w_ada'] = np.stack([_kmajor(w_ada[i]) for i in range(DEPTH)])
    sh['b_adaFM'] = np.stack([_fmvec(inp['b_ada'][i]) for i in range(DEPTH)], 1)
    sh['b_ada4'] = np.ascontiguousarray(np.broadcast_to(inp['b_ada'][None], (4, DEPTH, 6 * D)))
    ng = inp['norm_g']
    sh['norm_gFM'] = np.ascontiguousarray(
        np.stack([np.stack([_fmvec(ng[i, n]) for n in range(2)], 1) for i in range(DEPTH)], 1))
    sh['final_g_rep'] = np.ascontiguousarray(np.broadcast_to(inp['final_norm_g'][None], (128, D)))
    fm = _fm_cols()
    tm = _tm_cols()
    w_in = inp['w_in']
    wfm = np.zeros((DEPTH, D, NFM), f)
    wfm[:, :, fm >= 0] = w_in[:, :, fm[fm >= 0]]
    sh['w_inFM'] = np.stack([_kmajor(wfm[i]) for i in range(DEPTH)])
    sh['w_inTM'] = np.stack([_kmajor(w_in[i][:, tm]) for i in range(DEPTH)])
    sh['w_out'] = np.stack([_kmajor(inp['w_out'][i]) for i in range(DEPTH)])
    sh['router_w'] = _kmajor(inp['router_w'])
    sh['router_b_rep'] = np.ascontiguousarray(np.broadcast_to(inp['router_b'][None], (128, NE)))
    sh['moe_wg'] = np.ascontiguousarray(
        inp['moe_w_gate'].reshape(DEPTH, NE, 8, 128, DFF).transpose(0, 1, 3, 2, 4))
    sh['moe_wu'] = np.ascontiguousarray(
        inp['moe_w_up'].reshape(DEPTH, NE, 8, 128, DFF).transpose(0, 1, 3, 2, 4))
    sh['moe_wd'] = np.ascontiguousarray(
        inp['moe_w_down'].reshape(DEPTH, NE, 4, 128, D).transpose(0, 1, 3, 2, 4))
    jj = np.arange(128, dtype=f)
    diff = jj[None, :] - jj[:, None]
    sh['DIFFf'] = np.where(diff >= 0, diff, 1e6).astype(f)
    sh['DIFFb'] = np.where(diff <= 0, -diff, 1e6).astype(f)
    sh['MSKf'] = (diff >= 0).astype(f)
    sh['MSKb'] = (diff <= 0).astype(f)
    sh['POS'] = np.ascontiguousarray(np.stack([np.broadcast_to(jj + 1, (128, 128)), np.broadcast_to(128 - jj, (128, 128)),
                          np.broadcast_to(127 - jj, (128, 128)), np.broadcast_to(jj, (128, 128))], 1)).astype(f)
    blk2 = np.kron(np.eye(2, dtype=f), np.ones((64, 64), f))
    sh['BLK2'] = blk2
    sh['BLK64'] = blk2 / 64.0
    n_rows = L // 64
    pos = np.arange(n_rows * 64)
    rows = (pos // 64).astype(f)
    cols = (pos % 64).astype(f)
    nf = 16
    inv = (np.float32(10000.0) ** (-np.arange(nf, dtype=f) / nf)).astype(f)
    ang = np.concatenate([rows[:, None] * inv, cols[:, None] * inv], -1).astype(f)
    cs, sn = np.cos(ang).T.astype(f), np.sin(ang).T.astype(f)
    sh['COS'] = np.ascontiguousarray(np.concatenate([cs, cs, cs, cs], 0))
    sh['SIN'] = np.ascontiguousarray(np.concatenate([-sn, sn, -sn, sn], 0))
    sh['ret_logit_rep'] = np.ascontiguousarray(np.broadcast_to(inp['ret_decay_logit'].reshape(DEPTH, 1, 8), (DEPTH, 128, 8)))
    hm = np.zeros((128, 4), f)
    for h in range(4):
        hm[h * 32:(h + 1) * 32, h] = 1.0
    sh['HM4'] = hm
    sh['BLKG'] = np.kron(np.eye(4, dtype=f), np.ones((32, 64), f))
    gkw = np.zeros((DEPTH, 128, 128), f)
    gkw[:, 0:16, :] = inp['gla_gk_w'][:, 0]
    gkw[:, 32:48, :] = inp['gla_gk_w'][:, 1]
    sh['gkw'] = gkw
    sh['gkbFM'] = np.ascontiguousarray(inp['gla_gk_b'].transpose(2, 0, 1))
    def s5_state_layout(a):
        a = a.reshape(DEPTH, 2, 8, 2, 64)
        return np.ascontiguousarray(a.transpose(3, 4, 0, 1, 2).reshape(128, DEPTH, 16))
    sh['s5_lre'] = s5_state_layout(inp['s5_lambda_re'])
    sh['s5_lim'] = s5_state_layout(inp['s5_lambda_im'])
    sh['s5_ldt'] = s5_state_layout(np.broadcast_to(inp['s5_log_dt'][..., None], (DEPTH, 2, 16, 64)))
    WB = np.zeros((DEPTH, 128, 8, 2, 128), f)
    WC = np.zeros((DEPTH, 128, 8, 2, 128), f)
    for g in range(16):
        j, gg = g // 2, g % 2
        r0 = (g % 8) * 16
        for ri, (bsrc, csrc) in enumerate([('s5_b_re', 's5_c_re'), ('s5_b_im', 's5_c_im')]):
            WB[:, r0:r0 + 16, j, ri, gg * 64:(gg + 1) * 64] = inp[bsrc][:, g].transpose(0, 2, 1)
            WC[:, gg * 64:(gg + 1) * 64, j, ri, r0:r0 + 16] = inp[csrc][:, g].transpose(0, 2, 1)
    sh['s5_WB'] = WB
    sh['s5_WC'] = WC
    sh['s5_dFM'] = np.ascontiguousarray(inp['s5_d'].reshape(DEPTH, 2, 128).transpose(2, 0, 1))
    sh['s5_gbFM'] = np.ascontiguousarray(inp['s5_glu_b'].reshape(DEPTH, 2, 128).transpose(2, 0, 1))
    sh['s5_gw'] = np.ascontiguousarray(inp['s5_glu_w'].reshape(DEPTH, 2, 128, 256).transpose(0, 2, 1, 3))
    sh['convFM'] = np.ascontiguousarray(inp['dn_conv_w'].reshape(DEPTH, 5, 6, 128).transpose(3, 0, 2, 1))
    par = np.zeros((128, DEPTH, 2), f)
    for d_ in range(2):
        par[32 * d_:32 * d_ + 4, :, 0] = inp['dn_a_log'][:, d_, :].T
        par[32 * d_:32 * d_ + 4, :, 1] = inp['dn_dt_bias'][:, d_, :].T
    sh['dn_par'] = par
    selr = np.zeros((128, 4, 128), f)
    selp = np.zeros((128, 2, 128), f)
    for d_ in range(2):
        for h in range(4):
            selr[32 * d_ + h, h, :] = 1.0
            selp[32 * d_ + h, h // 2, (h % 2) * 64:(h % 2 + 1) * 64] = 1.0
    sh['SELR'] = selr
    mhh = np.zeros((128, 4), f)
    ohh = np.zeros((128, 2, 2, 2, 128), f)
    for d_ in range(2):
        for h in range(4):
            mhh[32 * d_ + h, h] = 1.0
            ohh[32 * d_ + h, h // 2, h % 2, :, :] = 1.0
    sh['MH'] = mhh
    sh['OH'] = ohh.reshape(128, 2, 512)
    hm2 = np.zeros((128, 2), f)
    hm2[0:64, 0] = 1.0
    hm2[64:128, 1] = 1.0
    sh['HM2'] = hm2
    sh['SELP'] = selp
    NEG = -1.0e4
    mbm = np.zeros((128, 2, 4, 128), f)
    mbm[:, 0, 0, :] = np.where(diff >= 0, 0.0, NEG); mbm[:, 0, 1, :] = np.where(diff > 0, 0.0, NEG)
    mbm[:, 1, 0, :] = np.where(diff <= 0, 0.0, NEG); mbm[:, 1, 1, :] = np.where(diff < 0, 0.0, NEG)
    mbm[:, :, 2, :] = mbm[:, :, 0, :]; mbm[:, :, 3, :] = mbm[:, :, 1, :]
    sh['MB'] = mbm
    sel = np.zeros((4, 4, 128), f)
    for j in range(4):
        sel[j, j, :] = 1.0
    sh['sel4'] = sel
    return sh


def core_inputs(inp, core):
    f = np.float32
    b0 = core * NB
    d = {}
    d['x'] = np.ascontiguousarray(inp['x'][b0:b0 + NB])
    d['ctx'] = np.ascontiguousarray(inp['ctx'][b0:b0 + NB])
    cvec = np.stack([inp['c'][b0], inp['c'][b0 + 1], inp['c_ctx'], inp['c_ctx']], 1)
    d['cT'] = np.ascontiguousarray(cvec.reshape(8, 128, 4).transpose(1, 0, 2)).astype(f)
    return d


class K:
    pass


def build(dbg=None):
    dbg = dbg or {}
    nc = bass.Bass("TRN2", target_bir_lowering=False)
    k = K()
    k.nc = nc
    k.dbg = dbg

    def din(name, shape, dt=F32):
        return nc.dram_tensor(name, list(shape), dt, kind="ExternalInput").ap()

    def dscr(name, shape, dt=F32):
        return nc.dram_tensor(name, list(shape), dt, kind="Internal").ap()

    k.x = din('x', [NB, L, D])
    k.ctx = din('ctx', [NB, LC, D])
    k.cT = din('cT', [128, 8, 4])
    k.ident = din('ident', [128, 128])
    k.w_ada = din('w_ada', [DEPTH, 128, 8, 6 * D])
    k.b_adaFM = din('b_adaFM', [128, DEPTH, 48])
    k.b_ada4 = din('b_ada4', [4, DEPTH, 6 * D])
    k.norm_gFM = din('norm_gFM', [128, DEPTH, 2, 8])
    k.final_g_rep = din('final_g_rep', [128, D])
    k.w_inFM = din('w_inFM', [DEPTH, 128, 8, NFM])
    k.w_inTM = din('w_inTM', [DEPTH, 128, 8, NTM])
    k.w_out = din('w_out', [DEPTH, 128, 8, D])
    k.router_w = din('router_w', [128, 8, NE])
    k.router_b_rep = din('router_b_rep', [128, NE])
    if not dbg.get('nomoe'):
        k.moe_wg = din('moe_wg', [DEPTH, NE, 128, 8, DFF])
        k.moe_wu = din('moe_wu', [DEPTH, NE, 128, 8, DFF])
        k.moe_wd = din('moe_wd', [DEPTH, NE, 128, 4, D])
    k.sel4 = din('sel4', [4, 4, 128])
    k.DIFFf = din('DIFFf', [128, 128]); k.DIFFb = din('DIFFb', [128, 128])
    k.MSKf = din('MSKf', [128, 128]); k.MSKb = din('MSKb', [128, 128])
    k.POS = din('POS', [128, 4, 128])
    k.BLK2 = din('BLK2', [128, 128]); k.BLK64 = din('BLK64', [128, 128])
    k.COS = din('COS', [128, L]); k.SIN = din('SIN', [128, L])
    k.ret_logit_rep = din('ret_logit_rep', [DEPTH, 128, 8])
    k.HM4 = din('HM4', [128, 4]); k.BLKG = din('BLKG', [128, 256])
    k.s5_lre = din('s5_lre', [128, DEPTH, 16]); k.s5_lim = din('s5_lim', [128, DEPTH, 16]); k.s5_ldt = din('s5_ldt', [128, DEPTH, 16])
    k.s5_WB = din('s5_WB', [DEPTH, 128, 8, 2, 128]); k.s5_WC = din('s5_WC', [DEPTH, 128, 8, 2, 128])
    k.s5_dFM = din('s5_dFM', [128, DEPTH, 2]); k.s5_gbFM = din('s5_gbFM', [128, DEPTH, 2])
    k.s5_gw = din('s5_gw', [DEPTH, 128, 2, 256])
    k.convFM = din('convFM', [128, DEPTH, 6, 5]); k.dn_par = din('dn_par', [128, DEPTH, 2])
    k.MH = din('MH', [128, 4]); k.OH = din('OH', [128, 2, 512]); k.HM2 = din('HM2', [128, 2]); k.SELR = din('SELR', [128, 4, 128]); k.SELP = din('SELP', [128, 2, 128]); k.MB = din('MB', [128, 2, 4, 128])
    k.gkw = din('gkw', [DEPTH, 128, 128]); k.gkbFM = din('gkbFM', [128, DEPTH, 2])
    if 'mix_in' in dbg:
        k.mix_in = din('mix_in', [NB, D, T])
    k.out = nc.dram_tensor('out', [NB, L, D], F32, kind="ExternalOutput").ap()

    k.pFM = dscr('pFM', [NB, NFM, T])
    k.pTM = dscr('pTM', [NB, T, NTM])
    k.mixFM = dscr('mixFM', [NB, D, T])
    k.Xs = dscr('Xs', [NB, L, D])
    k.Zs = dscr('Zs', [NB, LC, D])
    k.gateD = dscr('gateD', [DEPTH, 2, 4, D])
    k.dbg_out = {}
    for name, shape in dbg.get('outs', {}).items():
        k.dbg_out[name] = nc.dram_tensor('dbg_' + name, list(shape), F32, kind="ExternalOutput").ap()

    with ExitStack() as es:
        P = Prog(nc, es)
        k.P = P
        sb = lambda name, shape, dt=F32: es.enter_context(_sbt(nc, name, list(shape), dt))
        k.identf = sb('identf', [128, 128])
        k.identb = sb('identb', [128, 128], BF16)
        k.silu_c = sb('silu_c', [128, 8, 4])
        k.modFM = sb('modFM', [128, DEPTH, 48, 4])
        k.ngFM = sb('ngFM', [128, DEPTH, 2, 8])
        k.Amod = sb('Amod', [128, DEPTH, 2, 8, 4])
        k.lnq8 = sb('lnq8', [128, 1])

        phase_init(k)
        stages = dbg.get('stages', None)
        for l in range(DEPTH):
            last = (l == DEPTH - 1)
            if stages is None or ('mod', l) in stages:
                phase_mod(k, l)
            for b in range(NB):
                if stages is None or ('inproj', l) in stages:
                    phase_inproj(k, l, b)
                if stages is None or ('mixers', l) in stages:
                    phase_mixers(k, l, b)
                if stages is None or ('outproj', l) in stages:
                    phase_outproj_moe(k, l, b, last)
        phase_dbg(k)
    return nc


def resid_src(k, l, b, tt):
    if tt < 2:
        src = k.ctx if l == 0 else k.Zs
        return src[b, tt * 128:(tt + 1) * 128, :]
    src = k.x if l == 0 else k.Xs
    return src[b, (tt - 2) * 128:(tt - 1) * 128, :]


def resid_dst(k, b, tt):
    if tt < 2:
        return k.Zs[b, tt * 128:(tt + 1) * 128, :]
    return k.Xs[b, (tt - 2) * 128:(tt - 1) * 128, :]


def phase_init(k):
    P, nc = k.P, k.nc
    P.dma(k.identf[:], k.ident[:, :], writes=['identf'])
    P.dma(k.identb[:], k.ident[:, :], writes=['identb'], eng='gpsimd')
    P.dma(k.silu_c[:], k.cT[:, :, :], writes=['silu_c'])
    P.dma(k.ngFM[:], k.norm_gFM[:, :, :, :], writes=['ngFM'])
    P.act(k.silu_c[:], k.silu_c[:], AF.Silu, reads=['silu_c'], writes=['silu_c'])
    P.memset(k.lnq8[:], math.log(0.125), writes=['lnq8'])
    P.flush()


def phase_mod(k, l):
    P, nc = k.P, k.nc
    with ExitStack() as es:
        wbuf = [es.enter_context(_sbt(nc, 'wada%d' % i, [128, 8, 512], F32)) for i in range(2)]
        bfm = es.enter_context(_sbt(nc, 'bfm', [128, 48], F32))
        b4 = es.enter_context(_sbt(nc, 'b4', [4, 6 * D], F32))
        gtmp = es.enter_context(_sbt(nc, 'gtmp', [4, 512], F32))
        ps = [es.enter_context(_pst(nc, 'psm%d' % i, [128, 512], F32)) for i in range(2)]
        psg = es.enter_context(_pst(nc, 'psg', [4, 512], F32))
        P.dma(bfm[:], k.b_adaFM[:, l, :], writes=['bfm'])
        P.dma(b4[:], k.b_ada4[:, l, :], writes=['b4'])
        for blk in range(12):
            w = wbuf[blk % 2]
            wk = 'wada%d' % (blk % 2)
            P.dma(w[:], k.w_ada[l, :, :, blk * 512:(blk + 1) * 512], writes=[wk])
            pst = ps[blk % 2]
            pk = 'psm%d' % (blk % 2)
            for j in range(4):
                for kc in range(8):
                    P.mm(pst[:, j * 4:(j + 1) * 4], w[:, kc, j * 128:(j + 1) * 128], k.silu_c[:, kc, :],
                         kc == 0, kc == 7, reads=[wk, 'silu_c'], writes=[pk])
            for j in range(4):
                jj = blk * 4 + j
                P.ts(k.modFM[:, l, jj, :], pst[:, j * 4:(j + 1) * 4], bfm[:, jj:jj + 1], None, ALU.add,
                     reads=[pk, 'bfm'], writes=['modFM'])
            m = blk // 2
            if m in (2, 5):
                n = 0 if m == 2 else 1
                c0 = (blk % 2) * 512
                for kc in range(8):
                    P.mm(psg[:, :], k.silu_c[:, kc, :], w[:, kc, :], kc == 0, kc == 7,
                         reads=[wk, 'silu_c'], writes=['psg'])
                P.tt(gtmp[:, :], psg[:, :], b4[:, blk * 512:(blk + 1) * 512], ALU.add,
                     reads=['psg', 'b4'], writes=['gtmp'])
                P.dma(k.gateD[l, n, :, c0:c0 + 512], gtmp[:, :], reads=['gtmp'])
        for n in range(2):
            for fc in range(8):
                P.ts(k.Amod[:, l, n, fc, :], k.modFM[:, l, (1 + 3 * n) * 8 + fc, :], 1.0,
                     k.ngFM[:, l, n, fc:fc + 1], ALU.add, ALU.mult,
                     reads=['modFM', 'ngFM'], writes=['Amod'])
        P.flush()


def norm_modulate_tile(k, P, xt, xk, l, n, col, ssq, rstd, xn, pst, pkeys, hT_slices, hkeys, tag,
                       h32=None, h32key=None):
    P.op('scalar', lambda s: s.activation(out=xn[:], in_=xt, func=AF.Square, accum_out=ssq[:]),
         reads=[xk], writes=['xn' + tag, 'ssq' + tag])
    P.act(rstd[:], ssq[:], AF.Sqrt, reads=['ssq' + tag], writes=['rstd' + tag], scale=1.0 / D, bias=k.eps_col[:])
    P.recip(rstd[:], rstd[:], reads=['rstd' + tag], writes=['rstd' + tag])
    P.ts(xn[:], xt, rstd[:, 0:1], None, ALU.mult, reads=[xk, 'rstd' + tag], writes=['xn' + tag])
    for half in range(2):
        pt = pst[half]
        for j in range(4):
            fc = half * 4 + j
            P.tr(pt[:, j * 128:(j + 1) * 128], xn[:, fc * 128:(fc + 1) * 128], k.identf[:],
                 reads=['xn' + tag, 'identf'], writes=[pkeys[half]])
        for j in range(4):
            fc = half * 4 + j
            P.ts(hT_slices[fc], pt[:, j * 128:(j + 1) * 128], k.Amod[:, l, n, fc, col:col + 1],
                 k.modFM[:, l, (3 * n) * 8 + fc, col:col + 1], ALU.mult, ALU.add,
                 reads=[pkeys[half], 'Amod', 'modFM'], writes=[hkeys[fc]])
            if h32 is not None:
                P.ts(h32[:, fc, :], pt[:, j * 128:(j + 1) * 128], k.Amod[:, l, n, fc, col:col + 1],
                     k.modFM[:, l, (3 * n) * 8 + fc, col:col + 1], ALU.mult, ALU.add,
                     reads=[pkeys[half], 'Amod', 'modFM'], writes=[h32key])


def phase_inproj(k, l, b):
    P, nc = k.P, k.nc
    with ExitStack() as es:
        sbt = lambda name, shape, dt=F32: es.enter_context(_sbt(nc, name, list(shape), dt))
        hT = sbt('hT', [128, 8, T], BF16)
        wfm = sbt('wfm', [128, 8, NFM], BF16)
        wtm = sbt('wtm', [128, 8, NTM], BF16)
        xt = [sbt('xt%d' % i, [128, D]) for i in range(2)]
        xn = [sbt('xn%d' % i, [128, D]) for i in range(2)]
        ssq = [sbt('ssq%d' % i, [128, 1]) for i in range(2)]
        rstd = [sbt('rstd%d' % i, [128, 1]) for i in range(2)]
        k.eps_col = sbt('eps_col', [128, 1])
        stg = [sbt('stg%d' % i, [128, 512]) for i in range(3)]
        ps = [es.enter_context(_pst(nc, 'ps%d' % i, [128, 512], F32)) for i in range(6)]
        P.memset(k.eps_col[:], EPS, writes=['eps'])
        for kc in range(8):
            P.dma(wfm[:, kc, :], k.w_inFM[l, :, kc, :], writes=['wfm'], eng='gpsimd')
            P.dma(wtm[:, kc, :], k.w_inTM[l, :, kc, :], writes=['wtm'], eng='gpsimd')
        for tt in range(NT):
            i = tt % 2
            tag = str(i)
            P.dma(xt[i][:], resid_src(k, l, b, tt), writes=['xt' + tag])
            col = 2 if tt < 2 else b
            norm_modulate_tile(k, P, xt[i][:], 'xt' + tag, l, 0, col, ssq[i], rstd[i], xn[i],
                               [ps[2 * i], ps[2 * i + 1]], ['ps%d' % (2 * i), 'ps%d' % (2 * i + 1)],
                               [hT[:, fc, tt * 128:(tt + 1) * 128] for fc in range(8)],
                               [('hT', tt)] * 8, tag)
        cnt = 0
        for cb in range(NFM // 128):
            for (t0, tn) in TBLK:
                pt = ps[4 + cnt % 2]
                pk = 'ps%d' % (4 + cnt % 2)
                st = stg[cnt % 3]
                sk = 'stg%d' % (cnt % 3)
                cnt += 1
                hk = [('hT', t0 // 128 + j) for j in range(tn // 128)]
                for kc in range(8):
                    P.mm(pt[:, :tn], wfm[:, kc, cb * 128:(cb + 1) * 128], hT[:, kc, t0:t0 + tn], kc == 0, kc == 7,
                         reads=['wfm'] + hk, writes=[pk])
                if cnt % 2:
                    P.copy(st[:, :tn], pt[:, :tn], reads=[pk], writes=[sk])
                else:
                    P.copy(st[:, :tn], pt[:, :tn], reads=[pk], writes=[sk], eng='scalar')
                P.dma(k.pFM[b, cb * 128:(cb + 1) * 128, t0:t0 + tn], st[:, :tn], reads=[sk])
        CB = [(0, 512)]
        for tt in range(NT):
            for (c0, cn) in CB:
                pt = ps[4 + cnt % 2]
                pk = 'ps%d' % (4 + cnt % 2)
                st = stg[cnt % 3]
                sk = 'stg%d' % (cnt % 3)
                cnt += 1
                for kc in range(8):
                    P.mm(pt[:, :cn], hT[:, kc, tt * 128:(tt + 1) * 128], wtm[:, kc, c0:c0 + cn], kc == 0, kc == 7,
                         reads=['wtm', ('hT', tt)], writes=[pk])
                if cnt % 2:
                    P.copy(st[:, :cn], pt[:, :cn], reads=[pk], writes=[sk])
                else:
                    P.copy(st[:, :cn], pt[:, :cn], reads=[pk], writes=[sk], eng='scalar')
                P.dma(k.pTM[b, tt * 128:(tt + 1) * 128, c0:c0 + cn], st[:, :cn], reads=[sk])
        P.flush()


def phase_mixers(k, l, b):
    P, nc = k.P, k.nc
    if 'mix_in' in k.dbg:
        with ExitStack() as es:
            t = es.enter_context(_sbt(nc, 'mixcp', [128, T], F32))
            for fc in range(8):
                P.dma(t[:], k.mix_in[b, fc * 128:(fc + 1) * 128, :], writes=['mixcp'])
                P.dma(k.mixFM[b, fc * 128:(fc + 1) * 128, :], t[:], reads=['mixcp'])
            P.flush()
        return
    which = k.dbg.get('mixers', ['gla', 'ret', 's5', 'dn'])
    if 'ret' in which:
        phase_ret(k, l, b)
    if 'gla' in which:
        phase_gla(k, l, b)
    if 's5' in which:
        phase_s5(k, l, b)
    if 'dn' in which:
        phase_dn(k, l, b)


def chunk_order(d):
    if d == 0:
        return list(range(NT))
    return [1, 0] + list(range(NT - 1, 1, -1))


def norm_gate(k, P, nc, es, b, oacc, okeys, g_blk, mix_row0, center, tagp):
    sbt = lambda name, shape, dt=F32: es.enter_context(_sbt(nc, name + tagp, list(shape), dt))
    g = sbt('ng_g', [128, T])
    blk = sbt('ng_blk', [128, 128])
    xc = [sbt('ng_xc%d' % i, [128, 512]) for i in range(2)]
    sq = [sbt('ng_sq%d' % i, [128, 512]) for i in range(2)]
    eps = sbt('ng_eps', [128, 1])
    psA = [es.enter_context(_pst(nc, 'ng_psA%d' % i, [128, 512], F32)) for i in range(2)]
    P.memset(eps[:], EPS, writes=['ng_eps'])
    P.dma(blk[:], k.BLK64[:, :], writes=['ng_blk'])
    P.dma(g[:], k.pFM[b, g_blk * 128:(g_blk + 1) * 128, :], writes=['ng_g'])
    P.act(g[:], g[:], AF.Silu, reads=['ng_g'], writes=['ng_g'])
    for bi, (t0, tn) in enumerate(TBLK):
        i = bi % 2
        xk, sk, pk = 'ng_xc%d' % i, 'ng_sq%d' % i, 'ng_ps%d' % i
        src = oacc[:, t0:t0 + tn]
        if center:
            P.mm(psA[i][:, :tn], blk[:], src, True, True, reads=['ng_blk'] + okeys, writes=[pk])
            P.tt(xc[i][:, :tn], src, psA[i][:, :tn], ALU.subtract, reads=[pk] + okeys, writes=[xk])
        else:
            P.copy(xc[i][:, :tn], src, reads=okeys, writes=[xk])
        P.tt(sq[i][:, :tn], xc[i][:, :tn], xc[i][:, :tn], ALU.mult, reads=[xk], writes=[sk])
        P.mm(psA[i][:, :tn], blk[:], sq[i][:, :tn], True, True, reads=['ng_blk', sk], writes=[pk])
        P.act(sq[i][:, :tn], psA[i][:, :tn], AF.Sqrt, reads=[pk], writes=[sk], bias=eps[:], scale=1.0)
        P.recip(sq[i][:, :tn], sq[i][:, :tn], reads=[sk], writes=[sk])
        P.tt(xc[i][:, :tn], xc[i][:, :tn], sq[i][:, :tn], ALU.mult, reads=[xk, sk], writes=[xk])
        P.tt(xc[i][:, :tn], xc[i][:, :tn], g[:, t0:t0 + tn], ALU.mult, reads=[xk, 'ng_g'], writes=[xk])
        P.dma(k.mixFM[b, mix_row0:mix_row0 + 128, t0:t0 + tn], xc[i][:, :tn], reads=[xk])


def phase_ret(k, l, b):
    P, nc = k.P, k.nc
    with ExitStack() as es:
        sbt = lambda name, shape, dt=F32: es.enter_context(_sbt(nc, name, list(shape), dt))
        lg = sbt('r_lg', [128, 8])
        lgc = sbt('r_lgc', [128, 4])
        GC = sbt('r_GC', [128, 4])
        EQ = sbt('r_EQ', [128, 4, 128])
        EK = sbt('r_EK', [128, 4, 128])
        GAM = sbt('r_GAM', [128, 8, 128])
        pos = sbt('r_pos', [128, 4, 128])
        dif = sbt('r_dif', [128, 2, 128])
        blk2 = sbt('r_blk2', [128, 128])
        P.dma(lg[:], k.ret_logit_rep[l, :, :], writes=['r_lg'])
        P.dma(pos[:], k.POS[:, :, :], writes=['r_pos'])
        P.dma(dif[:, 0, :], k.DIFFf[:, :], writes=['r_dif'])
        P.dma(dif[:, 1, :], k.DIFFb[:, :], writes=['r_dif'])
        P.dma(blk2[:], k.BLK2[:, :], writes=['r_blk2'])
        P.act(lg[:], lg[:], AF.Exp, reads=['r_lg'], writes=['r_lg'], scale=-1.0)
        P.act(lg[:], lg[:], AF.Ln, reads=['r_lg'], writes=['r_lg'], bias=1.0, scale=1.0)
        P.ts(lg[:], lg[:], -1.0, None, ALU.mult, reads=['r_lg'], writes=['r_lg'])
        for d in range(2):
            for hp in range(2):
                j = d * 2 + hp
                P.copy(lgc[0:64, j:j + 1], lg[0:64, d * 4 + hp * 2:d * 4 + hp * 2 + 1], reads=['r_lg'], writes=['r_lgc'])
                P.copy(lgc[64:128, j:j + 1], lg[64:128, d * 4 + hp * 2 + 1:d * 4 + hp * 2 + 2], reads=['r_lg'], writes=['r_lgc'])
        for d in range(2):
            for hp in range(2):
                j = d * 2 + hp
                P.act(EQ[:, j, :], pos[:, d, :], AF.Exp, reads=['r_pos', 'r_lgc'], writes=['r_EQ'],
                      scale=lgc[:, j:j + 1])
                P.act(EK[:, j, :], pos[:, 2 + d, :], AF.Exp, reads=['r_pos', 'r_lgc'], writes=['r_EK'], scale=lgc[:, j:j + 1])
                P.act(GC[:, j:j + 1], lgc[:, j:j + 1], AF.Exp, reads=['r_lgc'], writes=['r_GC'], scale=128.0)
            for h in range(4):
                P.act(GAM[:, d * 4 + h, :], dif[:, d, :], AF.Exp, reads=['r_dif', 'r_lg'], writes=['r_GAM'],
                      scale=lg[:, d * 4 + h:d * 4 + h + 1])
        qr = sbt('r_q', [128, 2, T], BF16)
        kr = sbt('r_k', [128, 2, T], BF16)
        v = sbt('r_v', [128, NT, 256], BF16)
        vpad = sbt('r_vpad', [128, 2, 2, NT, 128], BF16)
        oacc = sbt('r_oacc', [128, 2, T])
        with ExitStack() as es2:
            sb2 = lambda name, shape, dt=F32: es2.enter_context(_sbt(nc, name, list(shape), dt))
            cos = sb2('r_cos', [128, L])
            sin = sb2('r_sin', [128, L])
            ta = sb2('r_ta', [128, T])
            tb = sb2('r_tb', [128, T])
            P.dma(cos[:], k.COS[:, :], writes=['r_cos'])
            P.dma(sin[:], k.SIN[:, :], writes=['r_sin'])
            P.dma(v[:], k.pTM[b].rearrange("(c p) n -> p c n", p=128)[:, :, 256:512], writes=['r_v'], eng='gpsimd')
            P.memset(vpad[:].rearrange("p a b c d -> p (a b c d)"), 0.0, writes=['r_vpad'], eng='gpsimd')
            for hp in range(2):
                for hh in range(2):
                    P.copy(vpad[:, hp, hh, :, hh * 64:(hh + 1) * 64], v[:, :, hp * 128 + hh * 64:hp * 128 + (hh + 1) * 64],
                           reads=['r_v', 'r_vpad'], writes=['r_vpad'], eng='gpsimd')
            for (dst, dk_, bm, bs) in [(qr, 'r_q', 3, 5), (kr, 'r_k', 7, 9)]:
                for hp in range(2):
                    P.dma(ta[:], k.pFM[b, (bm + hp) * 128:(bm + hp + 1) * 128, :], writes=['r_ta'])
                    P.dma(tb[:], k.pFM[b, (bs + hp) * 128:(bs + hp + 1) * 128, :], writes=['r_tb'])
                    if dk_ == 'r_q':
                        P.ts(ta[:], ta[:], 0.125, None, ALU.mult, reads=['r_ta'], writes=['r_ta'])
                        P.ts(tb[:], tb[:], 0.125, None, ALU.mult, reads=['r_tb'], writes=['r_tb'], eng='gpsimd')
                    P.copy(dst[:, hp, 0:LC], ta[:, 0:LC], reads=['r_ta'], writes=[dk_])
                    P.tt(ta[:, LC:], ta[:, LC:], cos[:], ALU.mult, reads=['r_ta', 'r_cos'], writes=['r_ta'])
                    P.tt(tb[:, LC:], tb[:, LC:], sin[:], ALU.mult, reads=['r_tb', 'r_sin'], writes=['r_tb'], eng='gpsimd')
                    P.tt(dst[:, hp, LC:], ta[:, LC:], tb[:, LC:], ALU.add, reads=['r_ta', 'r_tb'], writes=[dk_])
            P.flush()
        with ExitStack() as es2:
            sb2 = lambda name, shape, dt=F32: es2.enter_context(_sbt(nc, name, list(shape), dt))
            NBUF = 2
            Pm = [[sb2('r_Pm%d_%d' % (i, hh), [128, 128], BF16) for hh in range(2)] for i in range(NBUF)]
            qin = [sb2('r_qin%d' % i, [128, 128], BF16) for i in range(NBUF)]
            kout = [sb2('r_kout%d' % i, [128, 128], BF16) for i in range(NBUF)]
            koT = [sb2('r_koT%d' % i, [128, 128], BF16) for i in range(NBUF)]
            tmp = [sb2('r_tmp%d' % i, [128, 128]) for i in range(NBUF)]
            S32 = [sb2('r_S32_%d' % i, [128, 128]) for i in range(4)]
            Sb = [sb2('r_Sb_%d' % i, [128, 128], BF16) for i in range(4)]
            psS = [es2.enter_context(_pst(nc, 'r_psS%d' % i, [128, 128], F32)) for i in range(4)]
            psO = [es2.enter_context(_pst(nc, 'r_psO%d' % i, [128, 128], F32)) for i in range(2)]
            psT = [es2.enter_context(_pst(nc, 'r_psT%d' % i, [128, 128], BF16)) for i in range(1)]
            psU = [es2.enter_context(_pst(nc, 'r_psU%d' % i, [128, 128], F32)) for i in range(1)]
            step = 0
            seen = set()
            for j in range(4):
                P.memset(S32[j][:], 0.0, writes=['r_S32_%d' % j])
                P.memset(Sb[j][:], 0.0, writes=['r_Sb_%d' % j])
            for ci in range(NT):
                for hp in range(2):
                    for d in range(2):
                        j = d * 2 + hp
                        c = chunk_order(d)[ci]
                        cs = slice(c * 128, (c + 1) * 128)
                        i = step % NBUF
                        step += 1
                        si = str(i)
                        Sk, Sbk = 'r_S32_%d' % j, 'r_Sb_%d' % j
                        for hh in range(2):
                            pr = slice(hh * 64, (hh + 1) * 64)
                            pS = psS[2 * i + hh]
                            pSk = 'r_psS%d' % (2 * i + hh)
                            P.mm(pS[:, :], kr[pr, hp, cs], qr[pr, hp, cs], True, True, reads=['r_q', 'r_k'], writes=[pSk])
                            P.tt(Pm[i][hh][:], pS[:, :], GAM[:, d * 4 + hp * 2 + hh, :], ALU.mult,
                                 reads=[pSk, 'r_GAM'], writes=['r_Pm%s_%d' % (si, hh)])
                        P.tt(qin[i][:], qr[:, hp, cs], EQ[:, j, :], ALU.mult, reads=['r_q', 'r_EQ'], writes=['r_qin' + si])
                        P.tt(kout[i][:], kr[:, hp, cs], EK[:, j, :], ALU.mult, reads=['r_k', 'r_EK'], writes=['r_kout' + si])
                        pO = psO[i]
                        pOk = 'r_psO%d' % i
                        P.mm(pO[:, :], vpad[:, hp, 0, c, :], Pm[i][0][:], True, False, reads=['r_vpad', 'r_Pm%s_0' % si], writes=[pOk])
                        P.mm(pO[:, :], vpad[:, hp, 1, c, :], Pm[i][1][:], False, False, reads=['r_vpad', 'r_Pm%s_1' % si], writes=[pOk])
                        P.mm(pO[:, :], Sb[j][:], qin[i][:], False, True, reads=[Sbk, 'r_qin' + si], writes=[pOk])
                        ok = ('r_oacc', hp, c)
                        if (ok, 0) not in seen:
                            seen.add((ok, 0))
                            P.copy(oacc[:, hp, cs], pO[:, :], reads=[pOk], writes=[ok], eng='scalar')
                        else:
                            P.tt(oacc[:, hp, cs], oacc[:, hp, cs], pO[:, :], ALU.add, reads=[pOk, ok], writes=[ok])
                        P.tr(psT[0][:, :], kout[i][:], k.identb[:], reads=['r_kout' + si, 'identb'], writes=['r_psT'])
                        P.copy(koT[i][:], psT[0][:, :], reads=['r_psT'], writes=['r_koT' + si], eng='scalar')
                        P.mm(psU[0][:, :], koT[i][:], v[:, c, hp * 128:(hp + 1) * 128], True, True,
                             reads=['r_koT' + si, 'r_v'], writes=['r_psU'])
                        P.tt(tmp[i][:], psU[0][:, :], blk2[:], ALU.mult, reads=['r_psU', 'r_blk2'], writes=['r_tmp' + si])
                        P.stt(S32[j][:], S32[j][:], GC[:, j:j + 1], tmp[i][:], ALU.mult, ALU.add,
                              reads=[Sk, 'r_GC', 'r_tmp' + si], writes=[Sk])
                        P.copy(Sb[j][:], S32[j][:], reads=[Sk], writes=[Sbk], eng='scalar')
            P.flush()
        for hp in range(2):
            with ExitStack() as es2:
                norm_gate(k, P, nc, es2, b, oacc[:, hp, :], [('r_oacc', hp, c) for c in range(NT)], 22 + hp, 256 + hp * 128, True, 'r%d' % hp)
                P.flush()


def phase_gla(k, l, b):
    P, nc = k.P, k.nc
    with ExitStack() as es:
        sbt = lambda name, shape, dt=F32: es.enter_context(_sbt(nc, name, list(shape), dt))
        qh = sbt('g_qh', [128, 2, 4, T], BF16)
        qt = sbt('g_qt', [128, 2, T], BF16)
        kh = sbt('g_kh', [128, 2, T], BF16)
        elast = sbt('g_el', [128, 2, NT])
        v = sbt('g_v', [128, NT, 256], BF16)
        vpad = sbt('g_vpad', [128, 4, NT, 128], BF16)
        oacc = sbt('g_oacc', [128, 2, T])
        msk = sbt('g_msk', [128, 2, 128])
        blkg = sbt('g_blkg', [128, 256])
        hm = sbt('g_hm', [128, 4])
        P.dma(msk[:, 0, :], k.MSKf[:, :], writes=['g_msk'])
        P.dma(msk[:, 1, :], k.MSKb[:, :], writes=['g_msk'])
        P.dma(blkg[:], k.BLKG[:, :], writes=['g_blkg'])
        P.dma(hm[:], k.HM4[:, :], writes=['g_hm'])
        P.dma(v[:], k.pTM[b].rearrange("(c p) n -> p c n", p=128)[:, :, 0:256], writes=['g_v'], eng='gpsimd')
        P.memset(vpad[:].rearrange("p a c d -> p (a c d)"), 0.0, writes=['g_vpad'], eng='gpsimd')
        for h in range(4):
            hh = h % 2
            P.copy(vpad[:, h, :, hh * 64:(hh + 1) * 64], v[:, :, h * 64:(h + 1) * 64], reads=['g_v', 'g_vpad'], writes=['g_vpad'], eng='gpsimd')
        with ExitStack() as es2:
            sb2 = lambda name, shape, dt=F32: es2.enter_context(_sbt(nc, name, list(shape), dt))
            z = sb2('g_z', [128, T])
            gkw = sb2('g_gkw', [128, 128])
            nb = sb2('g_nb', [128, 2])
            sp = sb2('g_sp', [128, T])
            bc = sb2('g_bc', [128, T])
            ee = sb2('g_ee', [128, T])
            qf = sb2('g_qf', [128, T])
            kf = sb2('g_kf', [128, T])
            ones = sb2('g_ones', [128, 128])
            lnq = sb2('g_lnq', [128, 1])
            ps = [es2.enter_context(_pst(nc, 'g_ps%d' % i, [128, 512], F32)) for i in range(2)]
            P.dma(z[:], k.pFM[b, 2 * 128:3 * 128, :], writes=['g_z'])
            P.dma(gkw[:], k.gkw[l, :, :], writes=['g_gkw'])
            P.dma(nb[:], k.gkbFM[:, l, :], writes=['g_nb'])
            P.dma(qf[:], k.pFM[b, 0:128, :], writes=['g_qf'])
            P.dma(kf[:], k.pFM[b, 128:256, :], writes=['g_kf'])
            P.ts(nb[:], nb[:], -1.0, None, ALU.mult, reads=['g_nb'], writes=['g_nb'])
            P.memset(ones[:], 1.0, writes=['g_ones'])
            P.memset(lnq[:], math.log(32.0 ** -0.5), writes=['g_lnq'])
            for d in range(2):
                pr = slice(d * 32, d * 32 + 16)
                for bi, (t0, tn) in enumerate(TBLK):
                    i = bi % 2
                    P.mm(ps[i][:, :tn], gkw[pr, :], z[pr, t0:t0 + tn], True, True, reads=['g_gkw', 'g_z'], writes=['g_ps%d' % i])
                    P.act(sp[:, t0:t0 + tn], ps[i][:, :tn], AF.Exp, reads=['g_ps%d' % i, 'g_nb'], writes=['g_sp'],
                          scale=-1.0, bias=nb[:, d:d + 1])
                P.act(sp[:], sp[:], AF.Ln, reads=['g_sp'], writes=['g_sp'], bias=1.0, scale=1.0)
                for c in range(NT):
                    cs = slice(c * 128, (c + 1) * 128)
                    if d == 0:
                        P.scan(bc[:, cs], ones[:], sp[:, cs], 0.0, reads=['g_ones', 'g_sp'], writes=['g_bc'])
                    else:
                        P.scan(bc[:, cs][:, ::-1], ones[:], sp[:, cs][:, ::-1], 0.0, reads=['g_ones', 'g_sp'], writes=['g_bc'])
                P.act(ee[:], bc[:], AF.Exp, reads=['g_bc'], writes=['g_ee'], scale=-1.0 / 16.0, bias=lnq[:])
                P.tt(qt[:, d, :], qf[:], ee[:], ALU.mult, reads=['g_qf', 'g_ee'], writes=['g_qt'])
                for h in range(4):
                    P.ts(qh[:, d, h, :], qt[:, d, :], hm[:, h:h + 1], None, ALU.mult, reads=['g_qt', 'g_hm'], writes=['g_qh'],
                         eng='gpsimd' if h % 2 else 'vector')
                lastv = bc[:, 127::128] if d == 0 else bc[:, 0::128]
                P.act(elast[:, d, :], lastv, AF.Exp, reads=['g_bc'], writes=['g_el'], scale=-1.0 / 16.0)
                P.act(ee[:], bc[:], AF.Exp, reads=['g_bc', 'g_qt'], writes=['g_ee'], scale=1.0 / 16.0)
                P.tt(kh[:, d, :], kf[:], ee[:], ALU.mult, reads=['g_kf', 'g_ee'], writes=['g_kh'])
            P.flush()
        with ExitStack() as es2:
            sb2 = lambda name, shape, dt=F32: es2.enter_context(_sbt(nc, name, list(shape), dt))
            NBUF = 2
            Pm = [[sb2('g_Pm%d_%d' % (i, h), [128, 128], BF16) for h in range(4)] for i in range(NBUF)]
            koT = [sb2('g_koT%d' % i, [128, 128], BF16) for i in range(NBUF)]
            tmp = [sb2('g_tmp%d' % i, [128, 256]) for i in range(NBUF)]
            S32 = [sb2('g_S32_%d' % i, [128, 256]) for i in range(2)]
            Sb = [sb2('g_Sb_%d' % i, [128, 256], BF16) for i in range(2)]
            psS = [es2.enter_context(_pst(nc, 'g_psS%d' % i, [128, 128], F32)) for i in range(4)]
            psO = [es2.enter_context(_pst(nc, 'g_psO%d' % i, [128, 128], F32)) for i in range(2)]
            psT = es2.enter_context(_pst(nc, 'g_psT', [128, 128], BF16))
            psU = es2.enter_context(_pst(nc, 'g_psU', [128, 256], F32))
            for j in range(2):
                P.memset(S32[j][:], 0.0, writes=['g_S32_%d' % j])
                P.memset(Sb[j][:], 0.0, writes=['g_Sb_%d' % j])
            step = 0
            seen = set()
            for ci in range(NT):
                for d in range(2):
                    c = chunk_order(d)[ci]
                    cs = slice(c * 128, (c + 1) * 128)
                    i = step % NBUF
                    step += 1
                    si = str(i)
                    Sk, Sbk = 'g_S32_%d' % d, 'g_Sb_%d' % d
                    for h in range(4):
                        P.mm(psS[h][:, :], kh[:, d, cs], qh[:, d, h, cs], True, True, reads=['g_kh', 'g_qh'], writes=['g_psS%d' % h])
                        P.tt(Pm[i][h][:], psS[h][:, :], msk[:, d, :], ALU.mult, reads=['g_psS%d' % h, 'g_msk'],
                             writes=['g_Pm%s_%d' % (si, h)])
                    for vp in range(2):
                        pO = psO[vp]
                        pOk = 'g_psO%d' % vp
                        P.mm(pO[:, :], vpad[:, 2 * vp, c, :], Pm[i][2 * vp][:], True, False,
                             reads=['g_vpad', 'g_Pm%s_%d' % (si, 2 * vp)], writes=[pOk])
                        P.mm(pO[:, :], vpad[:, 2 * vp + 1, c, :], Pm[i][2 * vp + 1][:], False, False,
                             reads=['g_vpad', 'g_Pm%s_%d' % (si, 2 * vp + 1)], writes=[pOk])
                        P.mm(pO[:, :], Sb[d][:, vp * 128:(vp + 1) * 128], qt[:, d, cs], False, True, reads=[Sbk, 'g_qt'], writes=[pOk])
                        ok = ('g_oacc', vp, c)
                        if ok not in seen:
                            seen.add(ok)
                            P.copy(oacc[:, vp, cs], pO[:, :], reads=[pOk], writes=[ok], eng='scalar')
                        else:
                            P.tt(oacc[:, vp, cs], oacc[:, vp, cs], pO[:, :], ALU.add, reads=[pOk, ok], writes=[ok])
                    P.tr(psT[:, :], kh[:, d, cs], k.identb[:], reads=['g_kh', 'identb'], writes=['g_psT'])
                    P.copy(koT[i][:], psT[:, :], reads=['g_psT'], writes=['g_koT' + si], eng='scalar')
                    P.mm(psU[:, :], koT[i][:], v[:, c, :], True, True, reads=['g_koT' + si, 'g_v'], writes=['g_psU'])
                    P.tt(tmp[i][:], psU[:, :], blkg[:], ALU.mult, reads=['g_psU', 'g_blkg'], writes=['g_tmp' + si])
                    P.tt(S32[d][:], S32[d][:], tmp[i][:], ALU.add, reads=[Sk, 'g_tmp' + si], writes=[Sk])
                    P.ts(S32[d][:], S32[d][:], elast[:, d, c:c + 1], None, ALU.mult, reads=[Sk, 'g_el'], writes=[Sk])
                    P.copy(Sb[d][:], S32[d][:], reads=[Sk], writes=[Sbk], eng='scalar')
            P.flush()
        for vp in range(2):
            with ExitStack() as es2:
                norm_gate(k, P, nc, es2, b, oacc[:, vp, :], [('g_oacc', vp, c) for c in range(NT)], 20 + vp, vp * 128, False, 'g%d' % vp)
                P.flush()


TWO_PI = 2.0 * math.pi


def sincos(P, ang, sn, cs, ki, t1, keys):
    R = dict(reads=keys, writes=keys)
    P.ts(t1, ang, 1.0 / TWO_PI, None, ALU.mult, **R)
    P.copy(ki, t1, **R)
    P.copy(t1, ki, **R)
    P.stt(ang, t1, -TWO_PI, ang, ALU.mult, ALU.add, **R)
    P.ts(t1, ang, math.pi, None, ALU.is_gt, **R)
    P.stt(ang, t1, -TWO_PI, ang, ALU.mult, ALU.add, **R)
    P.ts(t1, ang, -math.pi, None, ALU.is_lt, **R)
    P.stt(ang, t1, TWO_PI, ang, ALU.mult, ALU.add, **R)
    P.act(sn, ang, AF.Sin, **R)
    P.ts(ang, ang, math.pi / 2, None, ALU.add, **R)
    P.ts(t1, ang, math.pi, None, ALU.is_gt, **R)
    P.stt(ang, t1, -TWO_PI, ang, ALU.mult, ALU.add, **R)
    P.act(cs, ang, AF.Sin, **R)


def phase_s5(k, l, b):
    P, nc = k.P, k.nc
    with ExitStack() as es:
        sbt = lambda name, shape, dt=F32: es.enter_context(_sbt(nc, name, list(shape), dt))
        TAB = sbt('s_tab', [128, 16, 4, 128])
        rr = sbt('s_r', [128, 16])
        cb = sbt('s_cb', [128, 2, 16])
        WB = sbt('s_WB', [128, 8, 2, 128], BF16)
        WC = sbt('s_WC', [128, 8, 2, 128], BF16)
        u32 = sbt('s_u32', [128, 2, T])
        ub = sbt('s_ub', [128, 2, T], BF16)
        yacc = sbt('s_yacc', [128, 2, T])
        pos = sbt('s_pos', [128, 2, 128])
        P.dma(pos[:], k.POS[:, 0:2, :], writes=['s_pos'])
        P.dma(WB[:], k.s5_WB[l, :, :, :, :], writes=['s_WB'], eng='gpsimd')
        P.dma(WC[:], k.s5_WC[l, :, :, :, :], writes=['s_WC'], eng='gpsimd')
        for ct in range(2):
            P.dma(u32[:, ct, :], k.pFM[b, (11 + ct) * 128:(12 + ct) * 128, :], writes=['s_u32'])
        P.copy(ub[:], u32[:], reads=['s_u32'], writes=['s_ub'], eng='gpsimd')
        with ExitStack() as es2:
            sb2 = lambda name, shape, dt=F32: es2.enter_context(_sbt(nc, name, list(shape), dt))
            lre = sb2('s_lre', [128, 16]); lim = sb2('s_lim', [128, 16]); dt_ = sb2('s_dt', [128, 16])
            th = sb2('s_th', [128, 16]); ang = sb2('s_ang', [128, 16]); sn = sb2('s_sn', [128, 16]); cs = sb2('s_cs', [128, 16])
            ki = sb2('s_ki', [128, 16], I32); t1 = sb2('s_t1', [128, 16]); t2 = sb2('s_t2', [128, 16])
            bre = sb2('s_bre', [128, 16]); bim = sb2('s_bim', [128, 16]); den = sb2('s_den', [128, 16])
            A = sb2('s_A', [128, 128]); K2 = sb2('s_K2', [128, 128], I32); T1 = sb2('s_T1', [128, 128])
            pk = ['s_par']
            R = dict(reads=pk, writes=pk)
            P.dma(lre[:], k.s5_lre[:, l, :], writes=pk)
            P.dma(lim[:], k.s5_lim[:, l, :], writes=pk)
            P.dma(dt_[:], k.s5_ldt[:, l, :], writes=pk)
            P.act(dt_[:], dt_[:], AF.Exp, **R)
            P.tt(th[:], lim[:], dt_[:], ALU.mult, **R)
            P.tt(t2[:], lre[:], dt_[:], ALU.mult, **R)
            P.act(rr[:], t2[:], AF.Exp, reads=pk, writes=pk + ['s_r'])
            P.copy(ang[:], th[:], **R)
            sincos(P, ang[:], sn[:], cs[:], ki[:], t1[:], pk)
            P.tt(t1[:], rr[:], cs[:], ALU.mult, **R)
            P.ts(t1[:], t1[:], -1.0, None, ALU.add, **R)
            P.tt(t2[:], rr[:], sn[:], ALU.mult, **R)
            P.tt(den[:], lre[:], lre[:], ALU.mult, **R)
            P.tt(bre[:], lim[:], lim[:], ALU.mult, **R)
            P.tt(den[:], den[:], bre[:], ALU.add, **R)
            P.recip(den[:], den[:], **R)
            P.tt(bre[:], t1[:], lre[:], ALU.mult, **R)
            P.tt(bim[:], t2[:], lim[:], ALU.mult, **R)
            P.tt(bre[:], bre[:], bim[:], ALU.add, **R)
            P.tt(bre[:], bre[:], den[:], ALU.mult, **R)
            P.tt(bim[:], t2[:], lre[:], ALU.mult, **R)
            P.tt(t2[:], t1[:], lim[:], ALU.mult, **R)
            P.tt(bim[:], bim[:], t2[:], ALU.subtract, **R)
            P.tt(bim[:], bim[:], den[:], ALU.mult, **R)
            for dj in range(16):
                d = dj // 8
                tk = ('s_tab', dj)
                Rt = dict(reads=pk + ['s_pos', tk, 's_A'], writes=[tk, 's_A'])
                P.ts(A[:], pos[:, d, :], th[:, dj:dj + 1], None, ALU.mult, **Rt)
                sincos(P, A[:], TAB[:, dj, 3, :], TAB[:, dj, 2, :], K2[:], T1[:], [tk, 's_A'])
                P.ts(T1[:], TAB[:, dj, 3, :], bim[:, dj:dj + 1], None, ALU.mult, **Rt)
                P.stt(TAB[:, dj, 0, :], TAB[:, dj, 2, :], bre[:, dj:dj + 1], T1[:], ALU.mult, ALU.add, **Rt)
                P.ts(T1[:], TAB[:, dj, 3, :], bre[:, dj:dj + 1], None, ALU.mult, **Rt)
                P.stt(TAB[:, dj, 1, :], TAB[:, dj, 2, :], bim[:, dj:dj + 1], T1[:], ALU.mult, ALU.subtract, **Rt)
                li = 127 if d == 0 else 0
                P.copy(cb[:, 0, dj:dj + 1], TAB[:, dj, 2, li:li + 1], reads=[tk], writes=['s_cb'])
                P.copy(cb[:, 1, dj:dj + 1], TAB[:, dj, 3, li:li + 1], reads=[tk], writes=['s_cb'])
            P.flush()
        with ExitStack() as es2:
            sb2 = lambda name, shape, dt=F32: es2.enter_context(_sbt(nc, name, list(shape), dt))
            BUs = [sb2('s_BU%d' % d, [128, 8, 2, 128]) for d in range(2)]
            M1s = [sb2('s_M1%d' % d, [128, 8, 2, 128]) for d in range(2)]
            M2s = [sb2('s_M2%d' % d, [128, 8, 2, 128]) for d in range(2)]
            G = [sb2('s_G%d' % d, [128, 8, 2, 128]) for d in range(2)]
            H = [sb2('s_H%d' % d, [128, 8, 2, 128], BF16) for d in range(2)]
            G0 = [sb2('s_G0%d' % d, [128, 2, 8]) for d in range(2)]
            GL = [sb2('s_GL%d' % d, [128, 2, 8]) for d in range(2)]
            rfull = sb2('s_rfull', [128, 16, 128])
            psB = [es2.enter_context(_pst(nc, 's_psB%d' % i, [128, 2, 2, 128], F32)) for i in range(4)]
            psY = [es2.enter_context(_pst(nc, 's_psY%d' % i, [128, 2, 128], F32)) for i in range(2)]
            for d in range(2):
                P.memset(G0[d][:].rearrange("p a b -> p (a b)"), 0.0, writes=['s_G0%d' % d])
            for dj in range(16):
                P.ts(rfull[:, dj, :], pos[:, 0, :], 0.0, rr[:, dj:dj + 1], ALU.mult, ALU.add, reads=['s_pos', 's_r'], writes=['s_rfull'])
            seen = set()
            tabk = [('s_tab', dj) for dj in range(16)]
            for ci in range(NT):
                for d in range(2):
                    c = chunk_order(d)[ci]
                    cs_ = slice(c * 128, (c + 1) * 128)
                    pY = psY[d]
                    pYk = 's_psY%d' % d
                    tk = tabk[d * 8:(d + 1) * 8]
                    gk = 's_G%d' % d
                    hk = 's_H%d' % d
                    bc4 = lambda q_: TAB[:, d * 8:(d + 1) * 8, q_, :].unsqueeze(2).to_broadcast([128, 8, 2, 128])
                    BU, M1, M2 = BUs[d], M1s[d], M2s[d]
                    kBU, kM1, kM2 = 's_BU%d' % d, 's_M1%d' % d, 's_M2%d' % d
                    for j in range(8):
                        ct = j // 4
                        pB = psB[j // 2]
                        pBk = 's_psB%d' % (j // 2)
                        P.mm(pB[:, j % 2, 0, :], WB[:, j, 0, :], ub[:, ct, cs_], True, True, reads=['s_WB', 's_ub'], writes=[pBk])
                        P.mm(pB[:, j % 2, 1, :], WB[:, j, 1, :], ub[:, ct, cs_], True, True, reads=['s_WB', 's_ub'], writes=[pBk])
                    for q_ in range(4):
                        P.copy(BU[:, 2 * q_:2 * q_ + 2, :, :], psB[q_][:], reads=['s_psB%d' % q_], writes=[kBU], eng='scalar')
                    P.tt(M1[:], BU[:], bc4(0), ALU.mult, reads=[kBU] + tk, writes=[kM1])
                    P.tt(M2[:], BU[:], bc4(1), ALU.mult, reads=[kBU] + tk, writes=[kM2])
                    P.tt(M1[:, :, 0, :], M1[:, :, 0, :], M2[:, :, 1, :], ALU.subtract, reads=[kM1, kM2], writes=[kM1])
                    P.tt(M1[:, :, 1, :], M1[:, :, 1, :], M2[:, :, 0, :], ALU.add, reads=[kM1, kM2], writes=[kM1])
                    for j in range(8):
                        dj = d * 8 + j
                        for ri in range(2):
                            o_ = G[d][:, j, ri, :]
                            d1 = M1[:, j, ri, :]
                            if d == 1:
                                o_, d1 = o_[:, ::-1], d1[:, ::-1]
                            P.scan(o_, rfull[:, dj, :], d1, G0[d][:, ri, j:j + 1], reads=['s_rfull', kM1, 's_G0%d' % d], writes=[gk])
                    P.tt(M1[:], G[d][:], bc4(2), ALU.mult, reads=[gk] + tk, writes=[kM1])
                    P.tt(M2[:], G[d][:], bc4(3), ALU.mult, reads=[gk] + tk, writes=[kM2])
                    P.tt(H[d][:, :, 0, :], M1[:, :, 0, :], M2[:, :, 1, :], ALU.subtract, reads=[kM1, kM2], writes=[hk])
                    P.stt(H[d][:, :, 1, :], M1[:, :, 1, :], -1.0, M2[:, :, 0, :], ALU.mult, ALU.subtract, reads=[kM1, kM2], writes=[hk])
                    for j in range(8):
                        ct = j // 4
                        jj = j % 4
                        P.mm(pY[:, ct, :], WC[:, j, 0, :], H[d][:, j, 0, :], jj == 0, False, reads=['s_WC', hk], writes=[pYk])
                        P.mm(pY[:, ct, :], WC[:, j, 1, :], H[d][:, j, 1, :], False, jj == 3, reads=['s_WC', hk], writes=[pYk])
                    ok = ('s_yacc', c)
                    if ok not in seen:
                        seen.add(ok)
                        P.copy(yacc[:, :, cs_], pY[:], reads=[pYk], writes=[ok], eng='scalar')
                    else:
                        P.tt(yacc[:, :, cs_], yacc[:, :, cs_], pY[:], ALU.add, reads=[pYk, ok], writes=[ok])
                    li = 127 if d == 0 else 0
                    g0k = 's_G0%d' % d
                    glk = 's_GL%d' % d
                    P.copy(GL[d][:], G[d][:, :, :, li].rearrange("p j r -> p r j"), reads=[gk], writes=[glk])
                    cbc = cb[:, 0, d * 8:(d + 1) * 8]
                    cbs = cb[:, 1, d * 8:(d + 1) * 8]
                    Rg = dict(reads=[glk, 's_cb', g0k], writes=[g0k])
                    P.tt(G0[d][:, 0, :], GL[d][:, 1, :], cbs, ALU.mult, **Rg)
                    P.tt(G0[d][:, 1, :], GL[d][:, 0, :], cbs, ALU.mult, **Rg)
                    P.tt(GL[d][:, 0, :], GL[d][:, 0, :], cbc, ALU.mult, reads=[glk, 's_cb', g0k], writes=[glk])
                    P.tt(GL[d][:, 1, :], GL[d][:, 1, :], cbc, ALU.mult, reads=[glk, 's_cb', g0k], writes=[glk])
                    P.tt(G0[d][:, 0, :], GL[d][:, 0, :], G0[d][:, 0, :], ALU.subtract, reads=[glk, g0k], writes=[g0k])
                    P.tt(G0[d][:, 1, :], GL[d][:, 1, :], G0[d][:, 1, :], ALU.add, reads=[glk, g0k], writes=[g0k])
            P.flush()
        with ExitStack() as es2:
            sb2 = lambda name, shape, dt=F32: es2.enter_context(_sbt(nc, name, list(shape), dt))
            dsk = sb2('s_dsk', [128, 2])
            gb = sb2('s_gb', [128, 2])
            gw = sb2('s_gw', [128, 2, 256], BF16)
            yb = sb2('s_yb', [128, 2, T], BF16)
            t1 = sb2('s_e1', [128, T])
            zt = [sb2('s_zt%d' % i, [128, 512]) for i in range(2)]
            ps = [es2.enter_context(_pst(nc, 's_psz%d' % i, [128, 512], F32)) for i in range(2)]
            P.dma(dsk[:], k.s5_dFM[:, l, :], writes=['s_dsk'])
            P.dma(gb[:], k.s5_gbFM[:, l, :], writes=['s_gb'])
            P.dma(gw[:], k.s5_gw[l, :, :, :], writes=['s_gw'], eng='gpsimd')
            yk = [('s_yacc', c) for c in range(NT)]
            for ct in range(2):
                y = yacc[:, ct, :]
                P.stt(y, u32[:, ct, :], dsk[:, ct:ct + 1], y, ALU.mult, ALU.add, reads=yk + ['s_u32', 's_dsk'], writes=yk)
                P.tt(t1[:], y, y, ALU.mult, reads=yk, writes=['s_e1'])
                P.ts(t1[:], t1[:], 0.044715, 1.0, ALU.mult, ALU.add, reads=['s_e1'], writes=['s_e1'])
                P.tt(t1[:], t1[:], y, ALU.mult, reads=yk + ['s_e1'], writes=['s_e1'])
                P.act(t1[:], t1[:], AF.Sigmoid, reads=['s_e1'], writes=['s_e1'], scale=2.0 * math.sqrt(2.0 / math.pi))
                P.tt(y, y, t1[:], ALU.mult, reads=yk + ['s_e1'], writes=yk)
                P.copy(yb[:, ct, :], y, reads=yk, writes=['s_yb'], eng='gpsimd')
            cnt = 0
            for nt in range(2):
                for (t0, tn) in TBLK:
                    i = cnt % 2
                    cnt += 1
                    P.mm(ps[i][:, :tn], gw[:, 0, nt * 128:(nt + 1) * 128], yb[:, 0, t0:t0 + tn], True, False, reads=['s_gw', 's_yb'], writes=['s_psz%d' % i])
                    P.mm(ps[i][:, :tn], gw[:, 1, nt * 128:(nt + 1) * 128], yb[:, 1, t0:t0 + tn], False, True, reads=['s_gw', 's_yb'], writes=['s_psz%d' % i])
                    P.act(zt[i][:, :tn], ps[i][:, :tn], AF.Sigmoid, reads=['s_psz%d' % i, 's_gb'], writes=['s_zt%d' % i], bias=gb[:, nt:nt + 1], scale=1.0)
                    P.tt(zt[i][:, :tn], zt[i][:, :tn], yacc[:, nt, t0:t0 + tn], ALU.mult, reads=['s_zt%d' % i] + yk, writes=['s_zt%d' % i])
                    P.dma(k.mixFM[b, 512 + nt * 128:512 + (nt + 1) * 128, t0:t0 + tn], zt[i][:, :tn], reads=['s_zt%d' % i])
            P.flush()


def phase_dn(k, l, b):
    P, nc = k.P, k.nc
    with ExitStack() as es:
        sbt = lambda name, shape, dt=F32: es.enter_context(_sbt(nc, name, list(shape), dt))
        qb = sbt('d_qb', [128, 2, T], BF16)
        kb = sbt('d_kb', [128, 2, T], BF16)
        kn = sbt('d_kn', [128, 2, T])
        vTM = sbt('d_vTM', [128, NT, 256])
        bTM = sbt('d_bTM', [128, NT, 64])
        gall = sbt('d_gall', [128, NT, 3, 128])
        ngc = sbt('d_ngc', [128, T])
        mh = sbt('d_mh', [128, 4])
        oh = sbt('d_oh', [128, 2, 512])
        ones4 = sbt('d_ones4', [128, 128])
        P.dma(mh[:], k.MH[:, :], writes=['d_mh'])
        P.dma(oh[:], k.OH[:, :, :], writes=['d_oh'])
        P.memset(ones4[:], 1.0, writes=['d_ones4'])
        oacc = sbt('d_oacc', [128, 2, T])
        selr = sbt('d_selr', [128, 4, 128])
        selp = sbt('d_selp', [128, 2, 128])
        mb = sbt('d_mb', [128, 2, 4, 128])
        blk2 = sbt('d_blk2', [128, 128])
        P.dma(selr[:], k.SELR[:, :, :], writes=['d_selr'])
        P.dma(selp[:], k.SELP[:, :, :], writes=['d_selp'])
        P.dma(mb[:], k.MB[:, :, :, :], writes=['d_mb'])
        P.dma(blk2[:], k.BLK2[:, :], writes=['d_blk2'])
        hm2 = sbt('d_hm2', [128, 2])
        P.dma(hm2[:], k.HM2[:, :], writes=['d_hm2'])
        with ExitStack() as es2:
            sb2 = lambda name, shape, dt=F32: es2.enter_context(_sbt(nc, name, list(shape), dt))
            x = [sb2('d_x%d' % i, [128, T]) for i in range(2)]
            acc = [sb2('d_acc%d' % i, [128, T]) for i in range(2)]
            sq = sb2('d_sq', [128, T])
            cw = sb2('d_cw', [128, 6, 5])
            eps = sb2('d_eps', [128, 1])
            ps = [es2.enter_context(_pst(nc, 'd_psA%d' % i, [128, 512], F32)) for i in range(2)]
            pst = [es2.enter_context(_pst(nc, 'd_psT%d' % i, [128, 128], F32)) for i in range(2)]
            P.dma(cw[:], k.convFM[:, l, :, :], writes=['d_cw'])
            P.memset(eps[:], EPS, writes=['d_eps'])
            cnt = 0
            for ti in range(6):
                i = ti % 2
                xk, ak = 'd_x%d' % i, 'd_acc%d' % i
                P.dma(x[i][:], k.pFM[b, (13 + ti) * 128:(14 + ti) * 128, :], writes=[xk])
                P.ts(acc[i][:], x[i][:], cw[:, ti, 2:3], None, ALU.mult, reads=[xk, 'd_cw'], writes=[ak])
                for (s0, s1) in [(0, LC), (LC, T)]:
                    for j in (0, 1, 3, 4):
                        sft = j - 2
                        lo, hi = max(s0, s0 - sft), min(s1, s1 - sft)
                        P.stt(acc[i][:, lo:hi], x[i][:, lo + sft:hi + sft], cw[:, ti, j:j + 1], acc[i][:, lo:hi], ALU.mult, ALU.add,
                              reads=[xk, 'd_cw', ak], writes=[ak])
                P.act(acc[i][:], acc[i][:], AF.Silu, reads=[ak], writes=[ak])
                if ti < 4:
                    hp = ti % 2
                    P.tt(sq[:], acc[i][:], acc[i][:], ALU.mult, reads=[ak], writes=['d_sq'], eng='gpsimd')
                    for (t0, tn) in TBLK:
                        pp = ps[cnt % 2]
                        ppk = 'd_psA%d' % (cnt % 2)
                        cnt += 1
                        P.mm(pp[:, :tn], blk2[:], sq[:, t0:t0 + tn], True, True, reads=['d_blk2', 'd_sq'], writes=[ppk])
                        P.act(x[i][:, t0:t0 + tn], pp[:, :tn], AF.Sqrt, reads=[ppk, 'd_eps', xk], writes=[xk], bias=eps[:], scale=1.0)
                    P.recip(x[i][:], x[i][:], reads=[xk], writes=[xk])
                    if ti < 2:
                        P.stt(qb[:, hp, :], acc[i][:], 0.125, x[i][:], ALU.mult, ALU.mult, reads=[ak, xk], writes=['d_qb'])
                    else:
                        P.tt(kn[:, hp, :], acc[i][:], x[i][:], ALU.mult, reads=[ak, xk], writes=['d_kn'])
                        P.copy(kb[:, hp, :], kn[:, hp, :], reads=['d_kn'], writes=['d_kb'], eng='gpsimd')
                else:
                    vt = ti - 4
                    for c in range(NT):
                        pt = pst[c % 2]
                        ptk = 'd_psT%d' % (c % 2)
                        P.tr(pt[:, :], acc[i][:, c * 128:(c + 1) * 128], k.identf[:], reads=[ak, 'identf'], writes=[ptk])
                        P.copy(vTM[:, c, vt * 128:(vt + 1) * 128], pt[:, :], reads=[ptk], writes=['d_vTM'], eng='scalar' if c % 2 else 'vector')
            P.flush()
        if k.dbg.get('dn_stop') == 'A':
            return
        with ExitStack() as es2:
            sb2 = lambda name, shape, dt=F32: es2.enter_context(_sbt(nc, name, list(shape), dt))
            ga = sb2('d_ga', [128, T])
            lb = sb2('d_lb', [128, T])
            bt = sb2('d_bt', [128, T])
            par = sb2('d_par', [128, 2])
            nA = sb2('d_nA', [128, 1])
            ones = sb2('d_ones', [128, 128])
            pst = [es2.enter_context(_pst(nc, 'd_psB%d' % i, [128, 128], F32)) for i in range(2)]
            P.dma(ga[:], k.pFM[b, 19 * 128:20 * 128, :], writes=['d_ga'])
            P.dma(lb[:], k.pFM[b, 26 * 128:27 * 128, :], writes=['d_lb'])
            P.dma(par[:], k.dn_par[:, l, :], writes=['d_par'])
            P.memset(ones[:], 1.0, writes=['d_ones'])
            P.memset(gall[:].rearrange("p a b c -> p (a b c)"), 0.0, writes=['d_gall'], eng='gpsimd')
            P.act(nA[:], par[:, 0:1], AF.Exp, reads=['d_par'], writes=['d_nA'])
            P.ts(nA[:], nA[:], -1.0, None, ALU.mult, reads=['d_nA'], writes=['d_nA'])
            P.act(ga[:], ga[:], AF.Exp, reads=['d_ga', 'd_par'], writes=['d_ga'], bias=par[:, 1:2], scale=1.0)
            P.act(ga[:], ga[:], AF.Ln, reads=['d_ga'], writes=['d_ga'], bias=1.0, scale=1.0)
            P.ts(ga[:], ga[:], nA[:, 0:1], None, ALU.mult, reads=['d_ga', 'd_nA'], writes=['d_ga'])
            P.act(lb[:], lb[:], AF.Exp, reads=['d_lb'], writes=['d_lb'], scale=-1.0)
            P.act(lb[:], lb[:], AF.Ln, reads=['d_lb'], writes=['d_lb'], bias=1.0, scale=1.0)
            P.ts(lb[:], lb[:], -1.0, None, ALU.mult, reads=['d_lb'], writes=['d_lb'])
            P.act(bt[:], lb[:], AF.Exp, reads=['d_lb'], writes=['d_bt'])
            for c in range(NT):
                cs = slice(c * 128, (c + 1) * 128)
                P.scan(gall[0:32, c, 0, :], ones[0:32, :], ga[0:32, cs], 0.0, reads=['d_ones', 'd_ga'], writes=['d_gall'])
                P.scan(gall[32:64, c, 0, :][:, ::-1], ones[32:64, :], ga[32:64, cs][:, ::-1], 0.0, reads=['d_ones', 'd_ga'], writes=['d_gall'])
            for c in range(NT):
                cs = slice(c * 128, (c + 1) * 128)
                P.tt(gall[:, c, 1, :], gall[:, c, 0, :], lb[:, cs], ALU.add, reads=['d_gall', 'd_lb'], writes=['d_gall'])
                P.ts(ngc[:, cs], gall[:, c, 0, :], -1.0, None, ALU.mult, reads=['d_gall'], writes=['d_ngc'], eng='gpsimd')
                P.ts(gall[0:32, c, 2, :], gall[0:32, c, 0, :], -1.0, gall[0:32, c, 0, 127:128], ALU.mult, ALU.add, reads=['d_gall'], writes=['d_gall'])
                P.ts(gall[32:64, c, 2, :], gall[32:64, c, 0, :], -1.0, gall[32:64, c, 0, 0:1], ALU.mult, ALU.add, reads=['d_gall'], writes=['d_gall'])
                pt = pst[c % 2]
                ptk = 'd_psB%d' % (c % 2)
                P.tr(pt[:, :], bt[:, cs], k.identf[:], reads=['d_bt', 'identf'], writes=[ptk])
                P.copy(bTM[:, c, :], pt[:, 0:64], reads=[ptk], writes=['d_bTM'], eng='scalar')
            P.flush()
        if k.dbg.get('dn_stop') == 'B':
            return
        with ExitStack() as es2:
            sb2 = lambda name, shape, dt=F32: es2.enter_context(_sbt(nc, name, list(shape), dt))
            CH = []
            for ch in range(4):
                B_ = {}
                t = 'd%d_' % ch
                B_['E'] = sb2(t + 'E', [128, 3, 128])
                B_['Qin'] = sb2(t + 'Qin', [128, 128], BF16)
                B_['KE2'] = sb2(t + 'KE2', [128, 128])
                B_['KE3'] = sb2(t + 'KE3', [128, 128], BF16)
                B_['RWpad'] = sb2(t + 'RWpad', [128, 2, 128])
                B_['RUpad'] = sb2(t + 'RUpad', [128, 2, 128])
                B_['Upad'] = sb2(t + 'Upad', [128, 2, 128], BF16)
                B_['koT'] = sb2(t + 'koT', [128, 128], BF16)
                B_['Gm'] = sb2(t + 'Gm', [128, 4, 128])
                B_['AT'] = sb2(t + 'AT', [128, 2, 128])
                B_['QKm'] = sb2(t + 'QKm', [128, 2, 128], BF16)
                B_['X'] = sb2(t + 'X', [128, 2, 2, 128])
                B_['Y'] = sb2(t + 'Y', [128, 2, 2, 128])
                B_['TT'] = sb2(t + 'TT', [128, 2, 128])
                B_['WTb'] = sb2(t + 'WTb', [128, 128])
                B_['Ub'] = sb2(t + 'Ub', [128, 128], BF16)
                B_['tmp'] = sb2(t + 'tmp', [128, 128])
                B_['S32'] = sb2(t + 'S32', [128, 128])
                B_['Sb'] = sb2(t + 'Sb', [128, 128], BF16)
                B_['Sn'] = sb2(t + 'Sn', [128, 128])
                B_['knm'] = sb2(t + 'knm', [128, 2, 128])
                B_['Rsel'] = sb2(t + 'Rsel', [128, 2, 2, 128])
                for nm in ['RWpad', 'RUpad', 'Upad']:
                    P.memset(B_[nm][:].rearrange("p a c -> p (a c)"), 0.0, writes=[t + nm], eng='gpsimd')
                for nm in ['S32', 'Sb', 'Sn']:
                    P.memset(B_[nm][:], 0.0, writes=[t + nm])
                CH.append(B_)
            bk = [es2.enter_context(_pst(nc, 'd_bank%d' % i, [128, 512], F32)) for i in range(7)]
            bA, bB, bC, bD, bE, bF, bG = bk
            psTb = es2.enter_context(_pst(nc, 'd_psTb', [128, 128], BF16))
            kk0, W0 = bA[:, 0:128], bA[:, 128:256]
            kk1, W1 = bB[:, 0:128], bB[:, 128:256]
            N0, T32, W2 = bC[:, 0:128], bC[:, 128:256], bC[:, 256:384]
            psS = bE[:, 256:384]
            N1, qk0, qk1 = bD[:, 0:128], bD[:, 128:256], bD[:, 256:384]
            N2 = bE[:, 0:128]
            N0b, N1b, N2b = bA[:, 256:384], bB[:, 256:384], bF[:, 384:512]
            psE = bF[:, 0:384]
            psD = bG[:, :]
            psKK = [kk0, kk1]
            psQK = [qk0, qk1]
            seen = set()
            for ci in range(NT):
                for d in range(2):
                    c = chunk_order(d)[ci]
                    cs = slice(c * 128, (c + 1) * 128)
                    rows = slice(32 * d, 32 * d + 4)
                    lloc = 127 if d == 0 else 0
                    for hp in range(2):
                        ch = d * 2 + hp
                        B_ = CH[ch]
                        t = 'd%d_' % ch
                        kk_ = lambda nm: t + nm
                        P.mm(psE, selp[rows, hp, :], gall[rows, c, :, :].rearrange("p a b -> p (a b)"), True, True,
                             reads=['d_selp', 'd_gall'], writes=['d_psE'])
                        P.act(B_['E'][:].rearrange("p a b -> p (a b)"), psE, AF.Exp, reads=['d_psE'], writes=[kk_('E')])
                        P.tt(B_['Qin'][:], qb[:, hp, cs], B_['E'][:, 0, :], ALU.mult, reads=['d_qb', kk_('E')], writes=[kk_('Qin')])
                        P.tt(B_['KE2'][:], kn[:, hp, cs], B_['E'][:, 1, :], ALU.mult, reads=['d_kn', kk_('E')], writes=[kk_('KE2')])
                        P.tt(B_['KE3'][:], kn[:, hp, cs], B_['E'][:, 2, :], ALU.mult, reads=['d_kn', kk_('E')], writes=[kk_('KE3')])
                        if k.dbg.get('dn_lvl', 99) < 1:
                            continue
                        P.tr(T32, B_['KE2'][:], k.identf[:], reads=[kk_('KE2'), 'identf'], writes=['d_psT32'])
                        for hh in range(2):
                            P.copy(B_['RWpad'][:, hh, hh * 64:(hh + 1) * 64], T32[:, hh * 64:(hh + 1) * 64], reads=['d_psT32'],
                                   writes=[kk_('RWpad')], eng='scalar' if hh else 'vector')
                        P.tr(psTb[:, :], B_['KE3'][:], k.identb[:], reads=[kk_('KE3'), 'identb'], writes=['d_psTb'])
                        P.copy(B_['koT'][:], psTb[:, :], reads=['d_psTb'], writes=[kk_('koT')], eng='scalar')
                        if k.dbg.get('dn_lvl', 99) < 2:
                            continue
                        for hh in range(2):
                            h = 2 * hp + hh
                            P.ts(B_['Rsel'][rows, hh, :, :], gall[rows, c, 0:2, :], mh[rows, h:h + 1], None, ALU.mult,
                                 reads=['d_gall', 'd_mh'], writes=[kk_('Rsel')])
                        P.mm(psD, ones4[rows, :], B_['Rsel'][rows, :, :, :].rearrange("p a b c -> p (a b c)"), True, False,
                             reads=['d_ones4', kk_('Rsel')], writes=['d_psD'])
                        P.mm(psD, ngc[rows, cs], oh[rows, hp, :], False, True, reads=['d_ngc', 'd_oh'], writes=['d_psD'])
                        P.tt(B_['Gm'][:].rearrange("p a b -> p (a b)"), psD, mb[:, d, :, :].rearrange("p a b -> p (a b)"), ALU.add,
                             reads=['d_psD', 'd_mb'], writes=[kk_('Gm')])
                        P.act(B_['Gm'][:], B_['Gm'][:], AF.Exp, reads=[kk_('Gm')], writes=[kk_('Gm')])
                        if k.dbg.get('dn_lvl', 99) < 3:
                            continue
                        for hh in range(2):
                            pr = slice(hh * 64, (hh + 1) * 64)
                            P.ts(B_['knm'][:, hh, :], kn[:, hp, cs], hm2[:, hh:hh + 1], None, ALU.mult, reads=['d_kn', 'd_hm2'], writes=[kk_('knm')],
                                 eng='gpsimd')
                            P.mm(psKK[hh], B_['knm'][:, hh, :], kn[:, hp, cs], True, True, reads=['d_kn', kk_('knm')], writes=['d_psKK%d' % hh])
                            P.mm(psQK[hh], kb[pr, hp, cs], qb[pr, hp, cs], True, True, reads=['d_kb', 'd_qb'], writes=['d_psQK%d' % hh])
                        for hh in range(2):
                            P.tt(B_['AT'][:, hh, :], psKK[hh], B_['Gm'][:, 2 * hh + 1, :], ALU.mult, reads=['d_psKK%d' % hh, kk_('Gm')], writes=[kk_('AT')])
                            P.tt(B_['QKm'][:, hh, :], psQK[hh], B_['Gm'][:, 2 * hh, :], ALU.mult, reads=['d_psQK%d' % hh, kk_('Gm')], writes=[kk_('QKm')])
                        if k.dbg.get('dn_lvl', 99) < 4:
                            continue
                        NB_ = [(N0, N1, N2), (N0b, N1b, N2b)]
                        Xc, Yc = [None, None], [None, None]
                        for hh in range(2):
                            n0, n1, n2 = NB_[hh]
                            X0 = B_['AT'][:, hh, :]
                            P.tr(n0, X0, k.identf[:], reads=[kk_('AT'), 'identf'], writes=['d_psN0_%d' % hh])
                            P.copy(B_['Y'][:, hh, 0, :], n0, reads=['d_psN0_%d' % hh], writes=[kk_('Y%d' % hh)], eng='scalar')
                            P.tt(B_['TT'][:, hh, :], k.identf[:], X0, ALU.subtract, reads=['identf', kk_('AT')], writes=[kk_('TT%d' % hh)])
                            Xc[hh], Yc[hh] = X0, B_['Y'][:, hh, 0, :]
                        for lv in range(1, 7):
                            for hh in range(2):
                                n0, n1, n2 = NB_[hh]
                                ks = [kk_('X%d' % hh), kk_('Y%d' % hh), kk_('AT')]
                                if lv < 6:
                                    P.mm(n0, Yc[hh], Xc[hh], True, True, reads=ks, writes=['d_psN0_%d' % hh])
                                P.mm(n1, Xc[hh], Yc[hh], True, True, reads=ks, writes=['d_psN1_%d' % hh])
                            for hh in range(2):
                                n0, n1, n2 = NB_[hh]
                                Yn = B_['Y'][:, hh, lv % 2, :]
                                if lv < 6:
                                    Xn = B_['X'][:, hh, lv % 2, :]
                                    P.copy(Xn, n0, reads=['d_psN0_%d' % hh], writes=[kk_('X%d' % hh)], eng='scalar')
                                    Xc[hh] = Xn
                                P.copy(Yn, n1, reads=['d_psN1_%d' % hh], writes=[kk_('Y%d' % hh)])
                                Yc[hh] = Yn
                            for hh in range(2):
                                n0, n1, n2 = NB_[hh]
                                P.mm(n2, Yc[hh], B_['TT'][:, hh, :], True, True, reads=[kk_('Y%d' % hh), kk_('TT%d' % hh)], writes=['d_psN2_%d' % hh])
                            for hh in range(2):
                                n0, n1, n2 = NB_[hh]
                                P.tt(B_['TT'][:, hh, :], B_['TT'][:, hh, :], n2, ALU.add, reads=['d_psN2_%d' % hh, kk_('TT%d' % hh)],
                                     writes=[kk_('TT%d' % hh)], eng='vector')
                        if k.dbg.get('dn_lvl', 99) < 5:
                            continue
                        for hh in range(2):
                            P.mm(W0, B_['RWpad'][:, hh, :], B_['TT'][:, hh, :], hh == 0, hh == 1, reads=[kk_('RWpad'), kk_('TT0'), kk_('TT1')], writes=['d_psW0'])
                        if k.dbg.get('dn_sub', 9) < 1:
                            continue
                        P.copy(B_['WTb'][:], W0, reads=['d_psW0'], writes=[kk_('WTb')], eng='scalar')
                        for hh in range(2):
                            h = 2 * hp + hh
                            P.ts(B_['RUpad'][:, hh, hh * 64:(hh + 1) * 64], vTM[:, c, hp * 128 + hh * 64:hp * 128 + (hh + 1) * 64],
                                 bTM[:, c, 32 * d + h:32 * d + h + 1], None, ALU.mult, reads=['d_vTM', 'd_bTM'], writes=[kk_('RUpad')], eng='gpsimd')
                        if k.dbg.get('dn_sub', 9) < 2:
                            continue
                        P.mm(W1, B_['TT'][:, 0, :], B_['RUpad'][:, 0, :], True, False, reads=[kk_('TT0'), kk_('RUpad')], writes=['d_psW1'])
                        P.mm(W1, B_['TT'][:, 1, :], B_['RUpad'][:, 1, :], False, False, reads=[kk_('TT1'), kk_('RUpad')], writes=['d_psW1'])
                        P.mm(W1, B_['WTb'][:], B_['Sn'][:], False, True, reads=[kk_('WTb'), kk_('Sn')], writes=['d_psW1'])
                        if k.dbg.get('dn_sub', 9) < 3:
                            continue
                        P.ts(B_['Ub'][:], W1, 1.0, None, ALU.mult, reads=['d_psW1'], writes=[kk_('Ub')])
                        for hh in range(2):
                            P.ts(B_['Upad'][:, hh, hh * 64:(hh + 1) * 64], W1[:, hh * 64:(hh + 1) * 64], 1.0, None, ALU.mult, reads=['d_psW1'], writes=[kk_('Upad')])
                        if k.dbg.get('dn_lvl', 99) < 6:
                            continue
                        P.mm(W2, B_['Upad'][:, 0, :], B_['QKm'][:, 0, :], True, False, reads=[kk_('Upad'), kk_('QKm')], writes=['d_psW2'])
                        P.mm(W2, B_['Upad'][:, 1, :], B_['QKm'][:, 1, :], False, False, reads=[kk_('Upad'), kk_('QKm')], writes=['d_psW2'])
                        P.mm(W2, B_['Sb'][:], B_['Qin'][:], False, True, reads=[kk_('Sb'), kk_('Qin')], writes=['d_psW2'])
                        ok = ('d_oacc', hp, c)
                        if ok not in seen:
                            seen.add(ok)
                            P.copy(oacc[:, hp, cs], W2, reads=['d_psW2'], writes=[ok], eng='scalar')
                        else:
                            P.tt(oacc[:, hp, cs], oacc[:, hp, cs], W2, ALU.add, reads=['d_psW2', ok], writes=[ok])
                        if k.dbg.get('dn_lvl', 99) < 7:
                            continue
                        P.mm(psS, B_['koT'][:], B_['Ub'][:], True, True, reads=[kk_('koT'), kk_('Ub')], writes=['d_psS'])
                        if k.dbg.get('dn_sub8', 9) < 1:
                            continue
                        P.tt(B_['tmp'][:], psS, blk2[:], ALU.mult, reads=['d_psS', 'd_blk2'], writes=[kk_('tmp')])
                        if k.dbg.get('dn_sub8', 9) < 2:
                            continue
                        P.stt(B_['S32'][:], B_['S32'][:], B_['E'][:, 0, lloc:lloc + 1], B_['tmp'][:], ALU.mult, ALU.add,
                              reads=[kk_('S32'), kk_('E'), kk_('tmp')], writes=[kk_('S32')])
                        if k.dbg.get('dn_sub8', 9) < 3:
                            continue
                        P.copy(B_['Sb'][:], B_['S32'][:], reads=[kk_('S32')], writes=[kk_('Sb')], eng='scalar')
                        if k.dbg.get('dn_sub8', 9) < 4:
                            continue
                        P.ts(B_['Sn'][:], B_['S32'][:], -1.0, None, ALU.mult, reads=[kk_('S32')], writes=[kk_('Sn')])
            P.flush()
        for hp in range(2):
            with ExitStack() as es2:
                norm_gate(k, P, nc, es2, b, oacc[:, hp, :], [('d_oacc', hp, c) for c in range(NT)], 24 + hp, 768 + hp * 128, False, 'd%d' % hp)
                P.flush()


def phase_outproj_moe(k, l, b, last):
    P, nc = k.P, k.nc
    t_first = 2 if last else 0
    with ExitStack() as es:
        sbt = lambda name, shape, dt=F32: es.enter_context(_sbt(nc, name, list(shape), dt))
        h2T = sbt('h2T', [128, 8, T], BF16)
        comb = sbt('comb', [128, NT, NE])
        k.eps_col = sbt('eps_col', [128, 1])
        rw = sbt('rw', [128, 8, NE])
        rb = sbt('rb', [128, NE])
        fg = sbt('fg', [128, D])
        P.memset(k.eps_col[:], EPS, writes=['eps'])
        P.dma(rw[:], k.router_w[:, :, :], writes=['rw'])
        P.dma(rb[:], k.router_b_rep[:, :], writes=['rb'])
        P.dma(fg[:], k.final_g_rep[:, :], writes=['fg'])
        ps = [es.enter_context(_pst(nc, 'ps%d' % i, [128, 512], F32)) for i in range(8)]
        with ExitStack() as es2:
            sb2 = lambda name, shape, dt=F32: es2.enter_context(_sbt(nc, name, list(shape), dt))
            mixT = sb2('mixT', [128, 8, T], BF16)
            wo = sb2('wo', [128, 8, D], BF16)
            grep_ = sb2('grep', [128, 2, D])
            xt = [sb2('xt%d' % i, [128, D]) for i in range(2)]
            xn = [sb2('xn%d' % i, [128, D]) for i in range(2)]
            h32 = [sb2('h32_%d' % i, [128, 8, 128]) for i in range(2)]
            ssq = [sb2('ssq%d' % i, [128, 1]) for i in range(2)]
            rstd = [sb2('rstd%d' % i, [128, 1]) for i in range(2)]
            sc = [sb2('rsc%d' % i, [128, 8, NE]) for i in range(2)]
            for kc in range(8):
                P.dma(wo[:, kc, :], k.w_out[l, :, kc, :], writes=['wo'], eng='gpsimd')
                P.dma(mixT[:, kc, :], k.mixFM[b, kc * 128:(kc + 1) * 128, :], writes=['mixT'], eng='gpsimd')
            for gi, col in enumerate([b, 2]):
                P.dma(grep_[:, gi, :], k.gateD[l, 0, col, :].partition_broadcast(128), writes=['grep'])
            for tt in range(t_first, NT):
                i = tt % 2
                tag = str(i)
                gi = 1 if tt < 2 else 0
                col = 2 if tt < 2 else b
                P.dma(xt[i][:], resid_src(k, l, b, tt), writes=['xt' + tag])
                for half in range(2):
                    pt = ps[4 + half]
                    pk = 'ps%d' % (4 + half)
                    for kc in range(8):
                        P.mm(pt[:, :], mixT[:, kc, tt * 128:(tt + 1) * 128], wo[:, kc, half * 512:(half + 1) * 512],
                             kc == 0, kc == 7, reads=['mixT', 'wo'], writes=[pk])
                    P.tt(xn[i][:, half * 512:(half + 1) * 512], pt[:, :], grep_[:, gi, half * 512:(half + 1) * 512], ALU.mult,
                         reads=[pk, 'grep'], writes=['xn' + tag])
                P.tt(xt[i][:], xt[i][:], xn[i][:], ALU.add, reads=['xt' + tag, 'xn' + tag], writes=['xt' + tag])
                P.dma(resid_dst(k, b, tt), xt[i][:], reads=['xt' + tag])
                if 'x_mid' in k.dbg_out and l == k.dbg.get('l', 0) and b == 0:
                    P.dma(k.dbg_out['x_mid'][tt * 128:(tt + 1) * 128, :], xt[i][:], reads=['xt' + tag])
                norm_modulate_tile(k, P, xt[i][:], 'xt' + tag, l, 1, col, ssq[i], rstd[i], xn[i],
                                   [ps[2 * i], ps[2 * i + 1]], ['ps%d' % (2 * i), 'ps%d' % (2 * i + 1)],
                                   [h2T[:, fc, tt * 128:(tt + 1) * 128] for fc in range(8)],
                                   [('h2T', tt)] * 8, tag, h32=None if k.dbg.get('norouter') else h32[i], h32key='h32_' + tag)
                pr = ps[6 + i]
                prk = 'ps%d' % (6 + i)
                for kc in range(0 if k.dbg.get('norouter') else 8):
                    P.mm(pr[:, 0:NE], h32[i][:, kc, :], rw[:, kc, :], kc == 0, kc == 7,
                         reads=['h32_' + tag, 'rw'], writes=[prk])
                if not k.dbg.get('norouter'):
                    router_tile(k, P, pr[:, 0:NE], prk, rb, sc[i], 'rsc' + tag, comb[:, tt, :], ('comb', tt))
            P.flush()
        if k.dbg.get('stopA'):
            return
        with ExitStack() as es2:
            sb2 = lambda name, shape, dt=F32: es2.enter_context(_sbt(nc, name, list(shape), dt))
            facc = sb2('facc', [128, NT, D])
            for tt in range(NT):
                P.memset(facc[:, tt, :], 0.0, writes=[('facc', tt, 0), ('facc', tt, 1)], eng='gpsimd')
            blks = [(t0, tn) for (t0, tn) in TBLK]
            if last:
                blks = [(256, 512), (768, 512), (1280, 512), (1792, 512)]
            with ExitStack() as es3:
                sb3 = lambda name, shape, dt=F32: es3.enter_context(_sbt(nc, name, list(shape), dt))
                wg = [sb3('wg%d' % i, [128, 8, DFF], BF16) for i in range(2)]
                wu = [sb3('wu%d' % i, [128, 8, DFF], BF16) for i in range(2)]
                wd = [sb3('wd%d' % i, [128, 4, D], BF16) for i in range(2)]
                actT = [sb3('actT%d' % i, [128, 4, 512], BF16) for i in range(2)]
                sg = [sb3('sg%d' % i, [128, 512]) for i in range(2)]
                cnt = 0
                for e in range(NE):
                    i = e % 2
                    si = str(i)
                    for kc in range(8):
                        P.dma(wg[i][:, kc, :], k.moe_wg[l, e, :, kc, :], writes=['wg' + si], eng='gpsimd')
                        P.dma(wu[i][:, kc, :], k.moe_wu[l, e, :, kc, :], writes=['wu' + si], eng='gpsimd')
                    for fc in range(4):
                        P.dma(wd[i][:, fc, :], k.moe_wd[l, e, :, fc, :], writes=['wd' + si], eng='gpsimd')
                    for (t0, tn) in blks:
                        a = actT[cnt % 2]
                        ak = 'actT%d' % (cnt % 2)
                        cnt += 1
                        hk = [('h2T', t0 // 128 + j) for j in range(tn // 128)]
                        for fc in range(4):
                            pg = ps[fc % 2]
                            pu = ps[2 + fc % 2]
                            pgk, puk = 'ps%d' % (fc % 2), 'ps%d' % (2 + fc % 2)
                            s_ = sg[fc % 2]
                            sk = 'sg%d' % (fc % 2)
                            for kc in range(8):
                                P.mm(pg[:, :tn], wg[i][:, kc, fc * 128:(fc + 1) * 128], h2T[:, kc, t0:t0 + tn], kc == 0, kc == 7,
                                     reads=['wg' + si] + hk, writes=[pgk])
                            for kc in range(8):
                                P.mm(pu[:, :tn], wu[i][:, kc, fc * 128:(fc + 1) * 128], h2T[:, kc, t0:t0 + tn], kc == 0, kc == 7,
                                     reads=['wu' + si] + hk, writes=[puk])
                            P.act(s_[:, :tn], pg[:, :tn], AF.Silu, reads=[pgk], writes=[sk])
                            P.tt(a[:, fc, :tn], s_[:, :tn], pu[:, :tn], ALU.mult, reads=[sk, puk], writes=[ak])
                        for j in range(tn // 128):
                            tt = t0 // 128 + j
                            for half in range(2):
                                pd = ps[4 + (2 * j + half) % 4]
                                pdk = 'ps%d' % (4 + (2 * j + half) % 4)
                                for fc in range(4):
                                    P.mm(pd[:, :], a[:, fc, j * 128:(j + 1) * 128], wd[i][:, fc, half * 512:(half + 1) * 512],
                                         fc == 0, fc == 3, reads=[ak, 'wd' + si], writes=[pdk])
                                fs = facc[:, tt, half * 512:(half + 1) * 512]
                                P.stt(fs, pd[:, :], comb[:, tt, e:e + 1], fs, ALU.mult, ALU.add,
                                      reads=[pdk, ('comb', tt), ('facc', tt, half)], writes=[('facc', tt, half)])
                P.flush()
            grep2 = sb2('grep2', [128, 2, D])
            xm = [sb2('xm%d' % i, [128, D]) for i in range(2)]
            ssq = sb2('ssqf', [128, 1])
            rstd = sb2('rstdf', [128, 1])
            junk = sb2('junkf', [128, D])
            for gi, col in enumerate([b, 2]):
                P.dma(grep2[:, gi, :], k.gateD[l, 1, col, :].partition_broadcast(128), writes=['grep2'])
            for tt in range(t_first, NT):
                gi = 1 if tt < 2 else 0
                i = tt % 2
                xk = 'xm%d' % i
                fk = [('facc', tt, 0), ('facc', tt, 1)]
                P.dma(xm[i][:], resid_dst(k, b, tt), writes=[xk])
                P.tt(facc[:, tt, :], facc[:, tt, :], grep2[:, gi, :], ALU.mult, reads=fk + ['grep2'], writes=fk)
                P.tt(xm[i][:], xm[i][:], facc[:, tt, :], ALU.add, reads=fk + [xk], writes=[xk])
                if 'x_end' in k.dbg_out and l == k.dbg.get('l', 0) and b == 0:
                    P.dma(k.dbg_out['x_end'][tt * 128:(tt + 1) * 128, :], xm[i][:], reads=[xk])
                if 'f_out' in k.dbg_out and l == k.dbg.get('l', 0) and b == 0:
                    P.dma(k.dbg_out['f_out'][tt * 128:(tt + 1) * 128, :], facc[:, tt, :], reads=fk)
                if not last:
                    P.dma(resid_dst(k, b, tt), xm[i][:], reads=[xk])
                else:
                    P.op('scalar', lambda s, i=i: s.activation(out=junk[:], in_=xm[i][:], func=AF.Square, accum_out=ssq[:]),
                         reads=[xk], writes=['junkf', 'ssqf'])
                    P.act(rstd[:], ssq[:], AF.Sqrt, reads=['ssqf'], writes=['rstdf'], scale=1.0 / D, bias=k.eps_col[:])
                    P.recip(rstd[:], rstd[:], reads=['rstdf'], writes=['rstdf'])
                    P.stt(xm[i][:], xm[i][:], rstd[:, 0:1], fg[:], ALU.mult, ALU.mult,
                          reads=[xk, 'rstdf', 'fg'], writes=[xk])
                    P.dma(k.out[b, (tt - 2) * 128:(tt - 1) * 128, :], xm[i][:], reads=[xk])
            P.flush()


def router_tile(k, P, logits, lk, rb, sc, sck, comb_out, ck):
    BIG = 1.0e4
    s = sc[:, 0, :]
    sel = sc[:, 1, :]
    t1 = sc[:, 2, :]
    t2 = sc[:, 3, :]
    m1 = sc[:, 4, 0:4]
    m2 = sc[:, 4, 4:8]
    gs = sc[:, 4, 8:12]
    gm = sc[:, 4, 12:13]
    ing = sc[:, 5, 0:4]
    e1 = sc[:, 6, :]
    mx = sc[:, 5, 4:5]
    mx2 = sc[:, 5, 5:6]
    ws = sc[:, 5, 6:7]
    R = dict(reads=[sck], writes=[sck])
    P.act(s, logits, AF.Sigmoid, reads=[lk], writes=[sck])
    P.tt(sel, s, rb[:, :], ALU.add, reads=[sck, 'rb'], writes=[sck])
    sel3 = sc[:, 1, :].rearrange("p (g e) -> p g e", g=4)
    t13 = sc[:, 2, :].rearrange("p (g e) -> p g e", g=4)
    P.red(m1, sel3, ALU.max, **R)
    P.tt(t13, sel3, m1.unsqueeze(2).to_broadcast([128, 4, 4]), ALU.is_equal, **R)
    P.stt(t1, t1, -BIG, sel, ALU.mult, ALU.add, **R)
    P.red(m2, t13, ALU.max, **R)
    P.tt(gs, m1, m2, ALU.add, **R)
    P.red(gm, gs, ALU.max, **R)
    P.ts(ing, gs, gm, None, ALU.is_equal, **R)
    P.ts(ing, ing, -1.0, BIG, ALU.add, ALU.mult, **R)
    P.tt(t13, sel3, ing.unsqueeze(2).to_broadcast([128, 4, 4]), ALU.add, **R)
    P.red(mx, t1, ALU.max, **R)
    P.ts(e1, t1, mx, None, ALU.is_equal, **R)
    P.stt(t2, e1, -BIG, t1, ALU.mult, ALU.add, **R)
    P.red(mx2, t2, ALU.max, **R)
    P.ts(t2, t2, mx2, None, ALU.is_equal, **R)
    P.tt(e1, e1, t2, ALU.add, **R)
    P.tt(e1, e1, s, ALU.mult, **R)
    P.red(ws, e1, ALU.add, **R)
    P.recip(ws, ws, **R)
    P.ts(comb_out, e1, ws, None, ALU.mult, reads=[sck], writes=[ck])


def phase_dbg(k):
    P, nc = k.P, k.nc
    outs = k.dbg_out
    if not outs:
        return
    with ExitStack() as es:
        t = es.enter_context(_sbt(nc, 'dbgt', [128, T], F32))
        if 'pFM' in outs:
            for cb in range(NFM // 128):
                P.dma(t[:], k.pFM[0, cb * 128:(cb + 1) * 128, :], writes=['dbgt'])
                P.dma(outs['pFM'][cb * 128:(cb + 1) * 128, :], t[:], reads=['dbgt'])
        if 'pTM' in outs:
            for tt in range(NT):
                P.dma(t[:, :NTM], k.pTM[0, tt * 128:(tt + 1) * 128, :], writes=['dbgt'])
                P.dma(outs['pTM'][tt * 128:(tt + 1) * 128, :], t[:, :NTM], reads=['dbgt'])
        if 'mixFM' in outs:
            for cb in range(8):
                P.dma(t[:], k.mixFM[0, cb * 128:(cb + 1) * 128, :], writes=['dbgt'])
                P.dma(outs['mixFM'][cb * 128:(cb + 1) * 128, :], t[:], reads=['dbgt'])
        if 'modFM' in outs:
            P.dma(outs['modFM'][:, :], k.modFM[:].rearrange("p a b c -> p (a b c)"), reads=['modFM'])
        P.flush()


_CACHE = {}


def kernel(**inputs):
    inp = {kk: np.asarray(v) for kk, v in inputs.items()}
    sh = host_prep(inp)
    if 'nc' not in _CACHE:
        _CACHE['nc'] = build()
    nc = _CACHE['nc']
    in_maps = []
    for c in range(8):
        m = dict(sh)
        m.update(core_inputs(inp, c))
        in_maps.append(m)
    res = run_bass_kernel_spmd(nc, in_maps, core_ids=list(range(8)))
    out = np.concatenate([r['out'] for r in res.results], axis=0)
    return out.astype(np.float32)
```

```python
import math
import numpy as np
from contextlib import ExitStack
import concourse.bass as bass
import concourse.mybir as mybir
from concourse.bass_utils import run_bass_kernel_spmd

F32 = mybir.dt.float32
BF16 = mybir.dt.bfloat16
I32 = mybir.dt.int32
AF = mybir.ActivationFunctionType
ALU = mybir.AluOpType
AX = mybir.AxisListType

D = 1024
L = 2048
LC = 256
T = L + LC
NT = T // 128
DEPTH = 2
NB = 2
NE = 16
DFF = 512
EPS = 1e-6
NFM = 27 * 128
NTM = 512
TBLK = [(0, 512), (512, 512), (1024, 512), (1536, 512), (2048, 256)]

ENGS = ['tensor', 'vector', 'scalar', 'gpsimd', 'sync']
SAME_ENGINE_WAIT = True
N_DMA_SEMS = 24


_UID = [0]


def _sbt(nc, name, shape, dt):
    _UID[0] += 1
    return nc.sbuf_tensor("%s_%d" % (name, _UID[0]), shape, dt)


def _pst(nc, name, shape, dt):
    _UID[0] += 1
    return nc.psum_tensor("%s_%d" % (name, _UID[0]), shape, dt)


class Prog:
    def __init__(self, nc, es):
        self.nc = nc
        self.sem = {e: es.enter_context(nc.semaphore("s_" + e)) for e in ENGS}
        self.cnt = {e: 0 for e in ENGS}
        self.dsem = [es.enter_context(nc.semaphore("d_%d" % i)) for i in range(N_DMA_SEMS)]
        self.dval = [0] * N_DMA_SEMS
        self.drr = 0
        self.semobj = {}
        for e in ENGS:
            self.semobj[('e', e)] = self.sem[e]
        for i in range(N_DMA_SEMS):
            self.semobj[('d', i)] = self.dsem[i]
        self.nops = 0
        self.begin()

    def begin(self):
        self.ops = {e: [] for e in ENGS}
        self.known = {e: {} for e in ENGS}
        self.lastw = {}
        self.readers = {}
        self.pending_dma = {e: [] for e in ENGS}

    def op(self, eng, fn, reads=(), writes=(), dma=False):
        deps = set()
        for k in reads:
            t = self.lastw.get(k)
            if t is not None:
                deps.add(t)
        for k in writes:
            t = self.lastw.get(k)
            if t is not None:
                deps.add(t)
            for t in self.readers.get(k, ()):
                deps.add(t)
        waits = []
        if dma:
            idx = self.drr % N_DMA_SEMS
            self.drr += 1
            if self.dval[idx] > 0:
                deps.add((('d', idx), self.dval[idx]))
            self.dval[idx] += 16
            tok = (('d', idx), self.dval[idx])
            inc = (('d', idx), 16)
            self.pending_dma[eng].append(tok)
        else:
            self.cnt[eng] += 1
            tok = (('e', eng), self.cnt[eng])
            inc = (('e', eng), 1)
        kn = self.known[eng]
        best = {}
        for (s, v) in deps:
            if s == ('e', eng) and not dma:
                if eng == 'tensor' or not SAME_ENGINE_WAIT:
                    continue
            if kn.get(s, 0) >= v:
                continue
            if best.get(s, 0) < v:
                best[s] = v
        for s, v in best.items():
            kn[s] = v
            waits.append((s, v))
        self.ops[eng].append((waits, fn, inc))
        self.nops += 1
        for k in reads:
            self.readers.setdefault(k, []).append(tok)
        for k in writes:
            self.lastw[k] = tok
            self.readers[k] = []
        return tok

    def flush(self):
        nc = self.nc
        tails = {}
        for e in ENGS:
            seen = {}
            for (s, v) in self.pending_dma[e]:
                if seen.get(s, 0) < v:
                    seen[s] = v
            tails[e] = [(s, v) for s, v in seen.items() if self.known[e].get(s, 0) < v]
        ops = self.ops
        semobj = self.semobj
        with nc.Block() as block:
            for e in ENGS:
                if not ops[e] and not tails[e]:
                    continue

                def body(eng, e=e):
                    for (waits, fn, inc) in ops[e]:
                        for (s, v) in waits:
                            eng.wait_ge(semobj[s], v)
                        ins = fn(eng)
                        ins.then_inc(semobj[inc[0]], inc[1])
                    for (s, v) in tails[e]:
                        eng.wait_ge(semobj[s], v)
                getattr(block, e)(body)
        self.begin()

    def dma(self, out, in_, reads=(), writes=(), eng='sync'):
        return self.op(eng, lambda g: g.dma_start(out=out, in_=in_), reads, writes, dma=True)

    def mm(self, out, lhsT, rhs, start, stop, reads=(), writes=()):
        return self.op('tensor', lambda t: t.matmul(out, lhsT=lhsT, rhs=rhs, start=start, stop=stop), reads, writes)

    def tr(self, out, in_, ident, reads=(), writes=()):
        return self.op('tensor', lambda t: t.transpose(out, in_, ident), reads, writes)

    def act(self, out, in_, func, reads=(), writes=(), **kw):
        return self.op('scalar', lambda s: s.activation(out=out, in_=in_, func=func, **kw), reads, writes)

    def ts(self, out, in0, s1, s2, op0, op1=None, reads=(), writes=(), eng='vector', **kw):
        if op1 is None:
            return self.op(eng, lambda v: v.tensor_scalar(out=out, in0=in0, scalar1=s1, scalar2=None, op0=op0, **kw), reads, writes)
        return self.op(eng, lambda v: v.tensor_scalar(out=out, in0=in0, scalar1=s1, scalar2=s2, op0=op0, op1=op1, **kw), reads, writes)

    def tt(self, out, in0, in1, op, reads=(), writes=(), eng='vector'):
        return self.op(eng, lambda v: v.tensor_tensor(out=out, in0=in0, in1=in1, op=op), reads, writes)

    def stt(self, out, in0, scalar, in1, op0, op1, reads=(), writes=()):
        return self.op('vector', lambda v: v.scalar_tensor_tensor(out=out, in0=in0, scalar=scalar, in1=in1, op0=op0, op1=op1), reads, writes)

    def copy(self, out, in_, reads=(), writes=(), eng='vector'):
        if eng == 'scalar':
            return self.op(eng, lambda s: s.copy(out=out, in_=in_), reads, writes)
        return self.op(eng, lambda v: v.tensor_copy(out=out, in_=in_), reads, writes)

    def memset(self, out, val, writes=(), eng='vector'):
        return self.op(eng, lambda v: v.memset(out, val), (), writes)

    def recip(self, out, in_, reads=(), writes=()):
        return self.op('vector', lambda v: v.reciprocal(out=out, in_=in_), reads, writes)

    def red(self, out, in_, op, reads=(), writes=()):
        return self.op('vector', lambda v: v.tensor_reduce(out=out, in_=in_, axis=AX.X, op=op), reads, writes)

    def scan(self, out, d0, d1, init, reads=(), writes=()):
        return self.op('vector', lambda v: v.tensor_tensor_scan(out=out, data0=d0, data1=d1, initial=init, op0=ALU.mult, op1=ALU.add), reads, writes)


IN_OFF = {}
_o = 0
for _n, _s in [('gq', 128), ('gk', 128), ('gv', 256), ('gg', 256), ('gzf', 16), ('gzb', 16),
               ('rq', 256), ('rk', 256), ('rv', 256), ('rg', 256), ('su', 256),
               ('dq', 256), ('dk', 256), ('dv', 256), ('dg', 256), ('da', 8), ('db', 8)]:
    IN_OFF[_n] = _o
    _o += _s


def _rope_perm(off):
    main, sw = [], []
    for h in range(4):
        base = off + h * 64
        ev = [base + 2 * i for i in range(32)]
        od = [base + 2 * i + 1 for i in range(32)]
        main += ev + od
        sw += od + ev
    return main, sw


def _fm_cols():
    c = []
    rng = lambda n, k: list(range(IN_OFF[n], IN_OFF[n] + k))
    pad = lambda k: [-1] * k
    c += rng('gq', 128)
    c += rng('gk', 128)
    c += rng('gzf', 16) + pad(16) + rng('gzb', 16) + pad(80)
    m, s = _rope_perm(IN_OFF['rq'])
    c += m + s
    m, s = _rope_perm(IN_OFF['rk'])
    c += m + s
    c += rng('su', 256)
    c += rng('dq', 256) + rng('dk', 256) + rng('dv', 256)
    da, db = rng('da', 8), rng('db', 8)
    c += da[0:4] + pad(28) + da[4:8] + pad(92)
    c += rng('gg', 256) + rng('rg', 256) + rng('dg', 256)
    c += db[0:4] + pad(28) + db[4:8] + pad(92)
    assert len(c) == NFM
    return np.array(c)


def _tm_cols():
    c = []
    for n in ['gv', 'rv']:
        c += list(range(IN_OFF[n], IN_OFF[n] + 256))
    return np.array(c)


def _kmajor(w):
    K, N = w.shape
    return np.ascontiguousarray(w.reshape(K // 128, 128, N).transpose(1, 0, 2))


def _fmvec(v):
    return np.ascontiguousarray(v.reshape(-1, 128).T)


def host_prep(inp):
    f = np.float32
    sh = {}
    sh['ident'] = np.eye(128, dtype=f)
    w_ada = inp['w_ada']
    sh['w_ada'] = np.stack([_kmajor(w_ada[i]) for i in range(DEPTH)])
    sh['b_adaFM'] = np.stack([_fmvec(inp['b_ada'][i]) for i in range(DEPTH)], 1)
    sh['b_ada4'] = np.ascontiguousarray(np.broadcast_to(inp['b_ada'][None], (4, DEPTH, 6 * D)))
    ng = inp['norm_g']
    sh['norm_gFM'] = np.ascontiguousarray(
        np.stack([np.stack([_fmvec(ng[i, n]) for n in range(2)], 1) for i in range(DEPTH)], 1))
    sh['final_g_rep'] = np.ascontiguousarray(np.broadcast_to(inp['final_norm_g'][None], (128, D)))
    fm = _fm_cols()
    tm = _tm_cols()
    w_in = inp['w_in']
    wfm = np.zeros((DEPTH, D, NFM), f)
    wfm[:, :, fm >= 0] = w_in[:, :, fm[fm >= 0]]
    sh['w_inFM'] = np.stack([_kmajor(wfm[i]) for i in range(DEPTH)])
    sh['w_inTM'] = np.stack([_kmajor(w_in[i][:, tm]) for i in range(DEPTH)])
    sh['w_out'] = np.stack([_kmajor(inp['w_out'][i]) for i in range(DEPTH)])
    sh['router_w'] = _kmajor(inp['router_w'])
    sh['router_b_rep'] = np.ascontiguousarray(np.broadcast_to(inp['router_b'][None], (128, NE)))
    sh['moe_wg'] = np.ascontiguousarray(
        inp['moe_w_gate'].reshape(DEPTH, NE, 8, 128, DFF).transpose(0, 1, 3, 2, 4))
    sh['moe_wu'] = np.ascontiguousarray(
        inp['moe_w_up'].reshape(DEPTH, NE, 8, 128, DFF).transpose(0, 1, 3, 2, 4))
    sh['moe_wd'] = np.ascontiguousarray(
        inp['moe_w_down'].reshape(DEPTH, NE, 4, 128, D).transpose(0, 1, 3, 2, 4))
    jj = np.arange(128, dtype=f)
    diff = jj[None, :] - jj[:, None]
    sh['DIFFf'] = np.where(diff >= 0, diff, 1e6).astype(f)
    sh['DIFFb'] = np.where(diff <= 0, -diff, 1e6).astype(f)
    sh['MSKf'] = (diff >= 0).astype(f)
    sh['MSKb'] = (diff <= 0).astype(f)
    sh['POS'] = np.ascontiguousarray(np.stack([np.broadcast_to(jj + 1, (128, 128)), np.broadcast_to(128 - jj, (128, 128)),
                          np.broadcast_to(127 - jj, (128, 128)), np.broadcast_to(jj, (128, 128))], 1)).astype(f)
    blk2 = np.kron(np.eye(2, dtype=f), np.ones((64, 64), f))
    sh['BLK2'] = blk2
    sh['BLK64'] = blk2 / 64.0
    n_rows = L // 64
    pos = np.arange(n_rows * 64)
    rows = (pos // 64).astype(f)
    cols = (pos % 64).astype(f)
    nf = 16
    inv = (np.float32(10000.0) ** (-np.arange(nf, dtype=f) / nf)).astype(f)
    ang = np.concatenate([rows[:, None] * inv, cols[:, None] * inv], -1).astype(f)
    cs, sn = np.cos(ang).T.astype(f), np.sin(ang).T.astype(f)
    sh['COS'] = np.ascontiguousarray(np.concatenate([cs, cs, cs, cs], 0))
    sh['SIN'] = np.ascontiguousarray(np.concatenate([-sn, sn, -sn, sn], 0))
    sh['ret_logit_rep'] = np.ascontiguousarray(np.broadcast_to(inp['ret_decay_logit'].reshape(DEPTH, 1, 8), (DEPTH, 128, 8)))
    hm = np.zeros((128, 4), f)
    for h in range(4):
        hm[h * 32:(h + 1) * 32, h] = 1.0
    sh['HM4'] = hm
    sh['BLKG'] = np.kron(np.eye(4, dtype=f), np.ones((32, 64), f))
    gkw = np.zeros((DEPTH, 128, 128), f)
    gkw[:, 0:16, :] = inp['gla_gk_w'][:, 0]
    gkw[:, 32:48, :] = inp['gla_gk_w'][:, 1]
    sh['gkw'] = gkw
    sh['gkbFM'] = np.ascontiguousarray(inp['gla_gk_b'].transpose(2, 0, 1))
    def s5_state_layout(a):
        a = a.reshape(DEPTH, 2, 8, 2, 64)
        return np.ascontiguousarray(a.transpose(3, 4, 0, 1, 2).reshape(128, DEPTH, 16))
    sh['s5_lre'] = s5_state_layout(inp['s5_lambda_re'])
    sh['s5_lim'] = s5_state_layout(inp['s5_lambda_im'])
    sh['s5_ldt'] = s5_state_layout(np.broadcast_to(inp['s5_log_dt'][..., None], (DEPTH, 2, 16, 64)))
    WB = np.zeros((DEPTH, 128, 8, 2, 128), f)
    WC = np.zeros((DEPTH, 128, 8, 2, 128), f)
    for g in range(16):
        j, gg = g // 2, g % 2
        r0 = (g % 8) * 16
        for ri, (bsrc, csrc) in enumerate([('s5_b_re', 's5_c_re'), ('s5_b_im', 's5_c_im')]):
            WB[:, r0:r0 + 16, j, ri, gg * 64:(gg + 1) * 64] = inp[bsrc][:, g].transpose(0, 2, 1)
            WC[:, gg * 64:(gg + 1) * 64, j, ri, r0:r0 + 16] = inp[csrc][:, g].transpose(0, 2, 1)
    sh['s5_WB'] = WB
    sh['s5_WC'] = WC
    sh['s5_dFM'] = np.ascontiguousarray(inp['s5_d'].reshape(DEPTH, 2, 128).transpose(2, 0, 1))
    sh['s5_gbFM'] = np.ascontiguousarray(inp['s5_glu_b'].reshape(DEPTH, 2, 128).transpose(2, 0, 1))
    sh['s5_gw'] = np.ascontiguousarray(inp['s5_glu_w'].reshape(DEPTH, 2, 128, 256).transpose(0, 2, 1, 3))
    sh['convFM'] = np.ascontiguousarray(inp['dn_conv_w'].reshape(DEPTH, 5, 6, 128).transpose(3, 0, 2, 1))
    par = np.zeros((128, DEPTH, 2), f)
    for d_ in range(2):
        par[32 * d_:32 * d_ + 4, :, 0] = inp['dn_a_log'][:, d_, :].T
        par[32 * d_:32 * d_ + 4, :, 1] = inp['dn_dt_bias'][:, d_, :].T
    sh['dn_par'] = par
    selr = np.zeros((128, 4, 128), f)
    selp = np.zeros((128, 2, 128), f)
    for d_ in range(2):
        for h in range(4):
            selr[32 * d_ + h, h, :] = 1.0
            selp[32 * d_ + h, h // 2, (h % 2) * 64:(h % 2 + 1) * 64] = 1.0
    sh['SELR'] = selr
    mhh = np.zeros((128, 4), f)
    ohh = np.zeros((128, 2, 2, 2, 128), f)
    for d_ in range(2):
        for h in range(4):
            mhh[32 * d_ + h, h] = 1.0
            ohh[32 * d_ + h, h // 2, h % 2, :, :] = 1.0
    sh['MH'] = mhh
    sh['OH'] = ohh.reshape(128, 2, 512)
    hm2 = np.zeros((128, 2), f)
    hm2[0:64, 0] = 1.0
    hm2[64:128, 1] = 1.0
    sh['HM2'] = hm2
    sh['SELP'] = selp
    NEG = -1.0e4
    mbm = np.zeros((128, 2, 4, 128), f)
    mbm[:, 0, 0, :] = np.where(diff >= 0, 0.0, NEG); mbm[:, 0, 1, :] = np.where(diff > 0, 0.0, NEG)
    mbm[:, 1, 0, :] = np.where(diff <= 0, 0.0, NEG); mbm[:, 1, 1, :] = np.where(diff < 0, 0.0, NEG)
    mbm[:, :, 2, :] = mbm[:, :, 0, :]; mbm[:, :, 3, :] = mbm[:, :, 1, :]
    sh['MB'] = mbm
    sel = np.zeros((4, 4, 128), f)
    for j in range(4):
        sel[j, j, :] = 1.0
    sh['sel4'] = sel
    return sh


def core_inputs(inp, core):
    f = np.float32
    b0 = core * NB
    d = {}
    d['x'] = np.ascontiguousarray(inp['x'][b0:b0 + NB])
    d['ctx'] = np.ascontiguousarray(inp['ctx'][b0:b0 + NB])
    cvec = np.stack([inp['c'][b0], inp['c'][b0 + 1], inp['c_ctx'], inp['c_ctx']], 1)
    d['cT'] = np.ascontiguousarray(cvec.reshape(8, 128, 4).transpose(1, 0, 2)).astype(f)
    return d


class K:
    pass


def build(dbg=None):
    dbg = dbg or {}
    nc = bass.Bass("TRN2", target_bir_lowering=False)
    k = K()
    k.nc = nc
    k.dbg = dbg

    def din(name, shape, dt=F32):
        return nc.dram_tensor(name, list(shape), dt, kind="ExternalInput").ap()

    def dscr(name, shape, dt=F32):
        return nc.dram_tensor(name, list(shape), dt, kind="Internal").ap()

    k.x = din('x', [NB, L, D])
    k.ctx = din('ctx', [NB, LC, D])
    k.cT = din('cT', [128, 8, 4])
    k.ident = din('ident', [128, 128])
    k.w_ada = din('w_ada', [DEPTH, 128, 8, 6 * D])
    k.b_adaFM = din('b_adaFM', [128, DEPTH, 48])
    k.b_ada4 = din('b_ada4', [4, DEPTH, 6 * D])
    k.norm_gFM = din('norm_gFM', [128, DEPTH, 2, 8])
    k.final_g_rep = din('final_g_rep', [128, D])
    k.w_inFM = din('w_inFM', [DEPTH, 128, 8, NFM])
    k.w_inTM = din('w_inTM', [DEPTH, 128, 8, NTM])
    k.w_out = din('w_out', [DEPTH, 128, 8, D])
    k.router_w = din('router_w', [128, 8, NE])
    k.router_b_rep = din('router_b_rep', [128, NE])
    if not dbg.get('nomoe'):
        k.moe_wg = din('moe_wg', [DEPTH, NE, 128, 8, DFF])
        k.moe_wu = din('moe_wu', [DEPTH, NE, 128, 8, DFF])
        k.moe_wd = din('moe_wd', [DEPTH, NE, 128, 4, D])
    k.sel4 = din('sel4', [4, 4, 128])
    k.DIFFf = din('DIFFf', [128, 128]); k.DIFFb = din('DIFFb', [128, 128])
    k.MSKf = din('MSKf', [128, 128]); k.MSKb = din('MSKb', [128, 128])
    k.POS = din('POS', [128, 4, 128])
    k.BLK2 = din('BLK2', [128, 128]); k.BLK64 = din('BLK64', [128, 128])
    k.COS = din('COS', [128, L]); k.SIN = din('SIN', [128, L])
    k.ret_logit_rep = din('ret_logit_rep', [DEPTH, 128, 8])
    k.HM4 = din('HM4', [128, 4]); k.BLKG = din('BLKG', [128, 256])
    k.s5_lre = din('s5_lre', [128, DEPTH, 16]); k.s5_lim = din('s5_lim', [128, DEPTH, 16]); k.s5_ldt = din('s5_ldt', [128, DEPTH, 16])
    k.s5_WB = din('s5_WB', [DEPTH, 128, 8, 2, 128]); k.s5_WC = din('s5_WC', [DEPTH, 128, 8, 2, 128])
    k.s5_dFM = din('s5_dFM', [128, DEPTH, 2]); k.s5_gbFM = din('s5_gbFM', [128, DEPTH, 2])
    k.s5_gw = din('s5_gw', [DEPTH, 128, 2, 256])
    k.convFM = din('convFM', [128, DEPTH, 6, 5]); k.dn_par = din('dn_par', [128, DEPTH, 2])
    k.MH = din('MH', [128, 4]); k.OH = din('OH', [128, 2, 512]); k.HM2 = din('HM2', [128, 2]); k.SELR = din('SELR', [128, 4, 128]); k.SELP = din('SELP', [128, 2, 128]); k.MB = din('MB', [128, 2, 4, 128])
    k.gkw = din('gkw', [DEPTH, 128, 128]); k.gkbFM = din('gkbFM', [128, DEPTH, 2])
    if 'mix_in' in dbg:
        k.mix_in = din('mix_in', [NB, D, T])
    k.out = nc.dram_tensor('out', [NB, L, D], F32, kind="ExternalOutput").ap()

    k.pFM = dscr('pFM', [NB, NFM, T])
    k.pTM = dscr('pTM', [NB, T, NTM])
    k.mixFM = dscr('mixFM', [NB, D, T])
    k.Xs = dscr('Xs', [NB, L, D])
    k.Zs = dscr('Zs', [NB, LC, D])
    k.gateD = dscr('gateD', [DEPTH, 2, 4, D])
    k.dbg_out = {}
    for name, shape in dbg.get('outs', {}).items():
        k.dbg_out[name] = nc.dram_tensor('dbg_' + name, list(shape), F32, kind="ExternalOutput").ap()

    with ExitStack() as es:
        P = Prog(nc, es)
        k.P = P
        sb = lambda name, shape, dt=F32: es.enter_context(_sbt(nc, name, list(shape), dt))
        k.identf = sb('identf', [128, 128])
        k.identb = sb('identb', [128, 128], BF16)
        k.silu_c = sb('silu_c', [128, 8, 4])
        k.modFM = sb('modFM', [128, DEPTH, 48, 4])
        k.ngFM = sb('ngFM', [128, DEPTH, 2, 8])
        k.Amod = sb('Amod', [128, DEPTH, 2, 8, 4])
        k.lnq8 = sb('lnq8', [128, 1])

        phase_init(k)
        stages = dbg.get('stages', None)
        for l in range(DEPTH):
            last = (l == DEPTH - 1)
            if stages is None or ('mod', l) in stages:
                phase_mod(k, l)
            for b in range(NB):
                if stages is None or ('inproj', l) in stages:
                    phase_inproj(k, l, b)
                if stages is None or ('mixers', l) in stages:
                    phase_mixers(k, l, b)
                if stages is None or ('outproj', l) in stages:
                    phase_outproj_moe(k, l, b, last)
        phase_dbg(k)
    return nc


def resid_src(k, l, b, tt):
    if tt < 2:
        src = k.ctx if l == 0 else k.Zs
        return src[b, tt * 128:(tt + 1) * 128, :]
    src = k.x if l == 0 else k.Xs
    return src[b, (tt - 2) * 128:(tt - 1) * 128, :]


def resid_dst(k, b, tt):
    if tt < 2:
        return k.Zs[b, tt * 128:(tt + 1) * 128, :]
    return k.Xs[b, (tt - 2) * 128:(tt - 1) * 128, :]


def phase_init(k):
    P, nc = k.P, k.nc
    P.dma(k.identf[:], k.ident[:, :], writes=['identf'])
    P.dma(k.identb[:], k.ident[:, :], writes=['identb'], eng='gpsimd')
    P.dma(k.silu_c[:], k.cT[:, :, :], writes=['silu_c'])
    P.dma(k.ngFM[:], k.norm_gFM[:, :, :, :], writes=['ngFM'])
    P.act(k.silu_c[:], k.silu_c[:], AF.Silu, reads=['silu_c'], writes=['silu_c'])
    P.memset(k.lnq8[:], math.log(0.125), writes=['lnq8'])
    P.flush()


def phase_mod(k, l):
    P, nc = k.P, k.nc
    with ExitStack() as es:
        wbuf = [es.enter_context(_sbt(nc, 'wada%d' % i, [128, 8, 512], F32)) for i in range(2)]
        bfm = es.enter_context(_sbt(nc, 'bfm', [128, 48], F32))
        b4 = es.enter_context(_sbt(nc, 'b4', [4, 6 * D], F32))
        gtmp = es.enter_context(_sbt(nc, 'gtmp', [4, 512], F32))
        ps = [es.enter_context(_pst(nc, 'psm%d' % i, [128, 512], F32)) for i in range(2)]
        psg = es.enter_context(_pst(nc, 'psg', [4, 512], F32))
        P.dma(bfm[:], k.b_adaFM[:, l, :], writes=['bfm'])
        P.dma(b4[:], k.b_ada4[:, l, :], writes=['b4'])
        for blk in range(12):
            w = wbuf[blk % 2]
            wk = 'wada%d' % (blk % 2)
            P.dma(w[:], k.w_ada[l, :, :, blk * 512:(blk + 1) * 512], writes=[wk])
            pst = ps[blk % 2]
            pk = 'psm%d' % (blk % 2)
            for j in range(4):
                for kc in range(8):
                    P.mm(pst[:, j * 4:(j + 1) * 4], w[:, kc, j * 128:(j + 1) * 128], k.silu_c[:, kc, :],
                         kc == 0, kc == 7, reads=[wk, 'silu_c'], writes=[pk])
            for j in range(4):
                jj = blk * 4 + j
                P.ts(k.modFM[:, l, jj, :], pst[:, j * 4:(j + 1) * 4], bfm[:, jj:jj + 1], None, ALU.add,
                     reads=[pk, 'bfm'], writes=['modFM'])
            m = blk // 2
            if m in (2, 5):
                n = 0 if m == 2 else 1
                c0 = (blk % 2) * 512
                for kc in range(8):
                    P.mm(psg[:, :], k.silu_c[:, kc, :], w[:, kc, :], kc == 0, kc == 7,
                         reads=[wk, 'silu_c'], writes=['psg'])
                P.tt(gtmp[:, :], psg[:, :], b4[:, blk * 512:(blk + 1) * 512], ALU.add,
                     reads=['psg', 'b4'], writes=['gtmp'])
                P.dma(k.gateD[l, n, :, c0:c0 + 512], gtmp[:, :], reads=['gtmp'])
        for n in range(2):
            for fc in range(8):
                P.ts(k.Amod[:, l, n, fc, :], k.modFM[:, l, (1 + 3 * n) * 8 + fc, :], 1.0,
                     k.ngFM[:, l, n, fc:fc + 1], ALU.add, ALU.mult,
                     reads=['modFM', 'ngFM'], writes=['Amod'])
        P.flush()


def norm_modulate_tile(k, P, xt, xk, l, n, col, ssq, rstd, xn, pst, pkeys, hT_slices, hkeys, tag,
                       h32=None, h32key=None):
    P.op('scalar', lambda s: s.activation(out=xn[:], in_=xt, func=AF.Square, accum_out=ssq[:]),
         reads=[xk], writes=['xn' + tag, 'ssq' + tag])
    P.act(rstd[:], ssq[:], AF.Sqrt, reads=['ssq' + tag], writes=['rstd' + tag], scale=1.0 / D, bias=k.eps_col[:])
    P.recip(rstd[:], rstd[:], reads=['rstd' + tag], writes=['rstd' + tag])
    P.ts(xn[:], xt, rstd[:, 0:1], None, ALU.mult, reads=[xk, 'rstd' + tag], writes=['xn' + tag])
    for half in range(2):
        pt = pst[half]
        for j in range(4):
            fc = half * 4 + j
            P.tr(pt[:, j * 128:(j + 1) * 128], xn[:, fc * 128:(fc + 1) * 128], k.identf[:],
                 reads=['xn' + tag, 'identf'], writes=[pkeys[half]])
        for j in range(4):
            fc = half * 4 + j
            P.ts(hT_slices[fc], pt[:, j * 128:(j + 1) * 128], k.Amod[:, l, n, fc, col:col + 1],
                 k.modFM[:, l, (3 * n) * 8 + fc, col:col + 1], ALU.mult, ALU.add,
                 reads=[pkeys[half], 'Amod', 'modFM'], writes=[hkeys[fc]])
            if h32 is not None:
                P.ts(h32[:, fc, :], pt[:, j * 128:(j + 1) * 128], k.Amod[:, l, n, fc, col:col + 1],
                     k.modFM[:, l, (3 * n) * 8 + fc, col:col + 1], ALU.mult, ALU.add,
                     reads=[pkeys[half], 'Amod', 'modFM'], writes=[h32key])


def phase_inproj(k, l, b):
    P, nc = k.P, k.nc
    with ExitStack() as es:
        sbt = lambda name, shape, dt=F32: es.enter_context(_sbt(nc, name, list(shape), dt))
        hT = sbt('hT', [128, 8, T], BF16)
        wfm = sbt('wfm', [128, 8, NFM], BF16)
        wtm = sbt('wtm', [128, 8, NTM], BF16)
        xt = [sbt('xt%d' % i, [128, D]) for i in range(2)]
        xn = [sbt('xn%d' % i, [128, D]) for i in range(2)]
        ssq = [sbt('ssq%d' % i, [128, 1]) for i in range(2)]
        rstd = [sbt('rstd%d' % i, [128, 1]) for i in range(2)]
        k.eps_col = sbt('eps_col', [128, 1])
        stg = [sbt('stg%d' % i, [128, 512]) for i in range(3)]
        ps = [es.enter_context(_pst(nc, 'ps%d' % i, [128, 512], F32)) for i in range(6)]
        P.memset(k.eps_col[:], EPS, writes=['eps'])
        for kc in range(8):
            P.dma(wfm[:, kc, :], k.w_inFM[l, :, kc, :], writes=['wfm'], eng='gpsimd')
            P.dma(wtm[:, kc, :], k.w_inTM[l, :, kc, :], writes=['wtm'], eng='gpsimd')
        for tt in range(NT):
            i = tt % 2
            tag = str(i)
            P.dma(xt[i][:], resid_src(k, l, b, tt), writes=['xt' + tag])
            col = 2 if tt < 2 else b
            norm_modulate_tile(k, P, xt[i][:], 'xt' + tag, l, 0, col, ssq[i], rstd[i], xn[i],
                               [ps[2 * i], ps[2 * i + 1]], ['ps%d' % (2 * i), 'ps%d' % (2 * i + 1)],
                               [hT[:, fc, tt * 128:(tt + 1) * 128] for fc in range(8)],
                               [('hT', tt)] * 8, tag)
        cnt = 0
        for cb in range(NFM // 128):
            for (t0, tn) in TBLK:
                pt = ps[4 + cnt % 2]
                pk = 'ps%d' % (4 + cnt % 2)
                st = stg[cnt % 3]
                sk = 'stg%d' % (cnt % 3)
                cnt += 1
                hk = [('hT', t0 // 128 + j) for j in range(tn // 128)]
                for kc in range(8):
                    P.mm(pt[:, :tn], wfm[:, kc, cb * 128:(cb + 1) * 128], hT[:, kc, t0:t0 + tn], kc == 0, kc == 7,
                         reads=['wfm'] + hk, writes=[pk])
                if cnt % 2:
                    P.copy(st[:, :tn], pt[:, :tn], reads=[pk], writes=[sk])
                else:
                    P.copy(st[:, :tn], pt[:, :tn], reads=[pk], writes=[sk], eng='scalar')
                P.dma(k.pFM[b, cb * 128:(cb + 1) * 128, t0:t0 + tn], st[:, :tn], reads=[sk])
        CB = [(0, 512)]
        for tt in range(NT):
            for (c0, cn) in CB:
                pt = ps[4 + cnt % 2]
                pk = 'ps%d' % (4 + cnt % 2)
                st = stg[cnt % 3]
                sk = 'stg%d' % (cnt % 3)
                cnt += 1
                for kc in range(8):
                    P.mm(pt[:, :cn], hT[:, kc, tt * 128:(tt + 1) * 128], wtm[:, kc, c0:c0 + cn], kc == 0, kc == 7,
                         reads=['wtm', ('hT', tt)], writes=[pk])
                if cnt % 2:
                    P.copy(st[:, :cn], pt[:, :cn], reads=[pk], writes=[sk])
                else:
                    P.copy(st[:, :cn], pt[:, :cn], reads=[pk], writes=[sk], eng='scalar')
                P.dma(k.pTM[b, tt * 128:(tt + 1) * 128, c0:c0 + cn], st[:, :cn], reads=[sk])
        P.flush()


def phase_mixers(k, l, b):
    P, nc = k.P, k.nc
    if 'mix_in' in k.dbg:
        with ExitStack() as es:
            t = es.enter_context(_sbt(nc, 'mixcp', [128, T], F32))
            for fc in range(8):
                P.dma(t[:], k.mix_in[b, fc * 128:(fc + 1) * 128, :], writes=['mixcp'])
                P.dma(k.mixFM[b, fc * 128:(fc + 1) * 128, :], t[:], reads=['mixcp'])
            P.flush()
        return
    which = k.dbg.get('mixers', ['gla', 'ret', 's5', 'dn'])
    if 'ret' in which:
        phase_ret(k, l, b)
    if 'gla' in which:
        phase_gla(k, l, b)
    if 's5' in which:
        phase_s5(k, l, b)
    if 'dn' in which:
        phase_dn(k, l, b)


def chunk_order(d):
    if d == 0:
        return list(range(NT))
    return [1, 0] + list(range(NT - 1, 1, -1))


def norm_gate(k, P, nc, es, b, oacc, okeys, g_blk, mix_row0, center, tagp):
    sbt = lambda name, shape, dt=F32: es.enter_context(_sbt(nc, name + tagp, list(shape), dt))
    g = sbt('ng_g', [128, T])
    blk = sbt('ng_blk', [128, 128])
    xc = [sbt('ng_xc%d' % i, [128, 512]) for i in range(2)]
    sq = [sbt('ng_sq%d' % i, [128, 512]) for i in range(2)]
    eps = sbt('ng_eps', [128, 1])
    psA = [es.enter_context(_pst(nc, 'ng_psA%d' % i, [128, 512], F32)) for i in range(2)]
    P.memset(eps[:], EPS, writes=['ng_eps'])
    P.dma(blk[:], k.BLK64[:, :], writes=['ng_blk'])
    P.dma(g[:], k.pFM[b, g_blk * 128:(g_blk + 1) * 128, :], writes=['ng_g'])
    P.act(g[:], g[:], AF.Silu, reads=['ng_g'], writes=['ng_g'])
    for bi, (t0, tn) in enumerate(TBLK):
        i = bi % 2
        xk, sk, pk = 'ng_xc%d' % i, 'ng_sq%d' % i, 'ng_ps%d' % i
        src = oacc[:, t0:t0 + tn]
        if center:
            P.mm(psA[i][:, :tn], blk[:], src, True, True, reads=['ng_blk'] + okeys, writes=[pk])
            P.tt(xc[i][:, :tn], src, psA[i][:, :tn], ALU.subtract, reads=[pk] + okeys, writes=[xk])
        else:
            P.copy(xc[i][:, :tn], src, reads=okeys, writes=[xk])
        P.tt(sq[i][:, :tn], xc[i][:, :tn], xc[i][:, :tn], ALU.mult, reads=[xk], writes=[sk])
        P.mm(psA[i][:, :tn], blk[:], sq[i][:, :tn], True, True, reads=['ng_blk', sk], writes=[pk])
        P.act(sq[i][:, :tn], psA[i][:, :tn], AF.Sqrt, reads=[pk], writes=[sk], bias=eps[:], scale=1.0)
        P.recip(sq[i][:, :tn], sq[i][:, :tn], reads=[sk], writes=[sk])
        P.tt(xc[i][:, :tn], xc[i][:, :tn], sq[i][:, :tn], ALU.mult, reads=[xk, sk], writes=[xk])
        P.tt(xc[i][:, :tn], xc[i][:, :tn], g[:, t0:t0 + tn], ALU.mult, reads=[xk, 'ng_g'], writes=[xk])
        P.dma(k.mixFM[b, mix_row0:mix_row0 + 128, t0:t0 + tn], xc[i][:, :tn], reads=[xk])


def phase_ret(k, l, b):
    P, nc = k.P, k.nc
    with ExitStack() as es:
        sbt = lambda name, shape, dt=F32: es.enter_context(_sbt(nc, name, list(shape), dt))
        lg = sbt('r_lg', [128, 8])
        lgc = sbt('r_lgc', [128, 4])
        GC = sbt('r_GC', [128, 4])
        EQ = sbt('r_EQ', [128, 4, 128])
        EK = sbt('r_EK', [128, 4, 128])
        GAM = sbt('r_GAM', [128, 8, 128])
        pos = sbt('r_pos', [128, 4, 128])
        dif = sbt('r_dif', [128, 2, 128])
        blk2 = sbt('r_blk2', [128, 128])
        P.dma(lg[:], k.ret_logit_rep[l, :, :], writes=['r_lg'])
        P.dma(pos[:], k.POS[:, :, :], writes=['r_pos'])
        P.dma(dif[:, 0, :], k.DIFFf[:, :], writes=['r_dif'])
        P.dma(dif[:, 1, :], k.DIFFb[:, :], writes=['r_dif'])
        P.dma(blk2[:], k.BLK2[:, :], writes=['r_blk2'])
        P.act(lg[:], lg[:], AF.Exp, reads=['r_lg'], writes=['r_lg'], scale=-1.0)
        P.act(lg[:], lg[:], AF.Ln, reads=['r_lg'], writes=['r_lg'], bias=1.0, scale=1.0)
        P.ts(lg[:], lg[:], -1.0, None, ALU.mult, reads=['r_lg'], writes=['r_lg'])
        for d in range(2):
            for hp in range(2):
                j = d * 2 + hp
                P.copy(lgc[0:64, j:j + 1], lg[0:64, d * 4 + hp * 2:d * 4 + hp * 2 + 1], reads=['r_lg'], writes=['r_lgc'])
                P.copy(lgc[64:128, j:j + 1], lg[64:128, d * 4 + hp * 2 + 1:d * 4 + hp * 2 + 2], reads=['r_lg'], writes=['r_lgc'])
        for d in range(2):
            for hp in range(2):
                j = d * 2 + hp
                P.act(EQ[:, j, :], pos[:, d, :], AF.Exp, reads=['r_pos', 'r_lgc'], writes=['r_EQ'],
                      scale=lgc[:, j:j + 1])
                P.act(EK[:, j, :], pos[:, 2 + d, :], AF.Exp, reads=['r_pos', 'r_lgc'], writes=['r_EK'], scale=lgc[:, j:j + 1])
                P.act(GC[:, j:j + 1], lgc[:, j:j + 1], AF.Exp, reads=['r_lgc'], writes=['r_GC'], scale=128.0)
            for h in range(4):
                P.act(GAM[:, d * 4 + h, :], dif[:, d, :], AF.Exp, reads=['r_dif', 'r_lg'], writes=['r_GAM'],
                      scale=lg[:, d * 4 + h:d * 4 + h + 1])
        qr = sbt('r_q', [128, 2, T], BF16)
        kr = sbt('r_k', [128, 2, T], BF16)
        v = sbt('r_v', [128, NT, 256], BF16)
        vpad = sbt('r_vpad', [128, 2, 2, NT, 128], BF16)
        oacc = sbt('r_oacc', [128, 2, T])
        with ExitStack() as es2:
            sb2 = lambda name, shape, dt=F32: es2.enter_context(_sbt(nc, name, list(shape), dt))
            cos = sb2('r_cos', [128, L])
            sin = sb2('r_sin', [128, L])
            ta = sb2('r_ta', [128, T])
            tb = sb2('r_tb', [128, T])
            P.dma(cos[:], k.COS[:, :], writes=['r_cos'])
            P.dma(sin[:], k.SIN[:, :], writes=['r_sin'])
            P.dma(v[:], k.pTM[b].rearrange("(c p) n -> p c n", p=128)[:, :, 256:512], writes=['r_v'], eng='gpsimd')
            P.memset(vpad[:].rearrange("p a b c d -> p (a b c d)"), 0.0, writes=['r_vpad'], eng='gpsimd')
            for hp in range(2):
                for hh in range(2):
                    P.copy(vpad[:, hp, hh, :, hh * 64:(hh + 1) * 64], v[:, :, hp * 128 + hh * 64:hp * 128 + (hh + 1) * 64],
                           reads=['r_v', 'r_vpad'], writes=['r_vpad'], eng='gpsimd')
            for (dst, dk_, bm, bs) in [(qr, 'r_q', 3, 5), (kr, 'r_k', 7, 9)]:
                for hp in range(2):
                    P.dma(ta[:], k.pFM[b, (bm + hp) * 128:(bm + hp + 1) * 128, :], writes=['r_ta'])
                    P.dma(tb[:], k.pFM[b, (bs + hp) * 128:(bs + hp + 1) * 128, :], writes=['r_tb'])
                    if dk_ == 'r_q':
                        P.ts(ta[:], ta[:], 0.125, None, ALU.mult, reads=['r_ta'], writes=['r_ta'])
                        P.ts(tb[:], tb[:], 0.125, None, ALU.mult, reads=['r_tb'], writes=['r_tb'], eng='gpsimd')
                    P.copy(dst[:, hp, 0:LC], ta[:, 0:LC], reads=['r_ta'], writes=[dk_])
                    P.tt(ta[:, LC:], ta[:, LC:], cos[:], ALU.mult, reads=['r_ta', 'r_cos'], writes=['r_ta'])
                    P.tt(tb[:, LC:], tb[:, LC:], sin[:], ALU.mult, reads=['r_tb', 'r_sin'], writes=['r_tb'], eng='gpsimd')
                    P.tt(dst[:, hp, LC:], ta[:, LC:], tb[:, LC:], ALU.add, reads=['r_ta', 'r_tb'], writes=[dk_])
            P.flush()
        with ExitStack() as es2:
            sb2 = lambda name, shape, dt=F32: es2.enter_context(_sbt(nc, name, list(shape), dt))
            NBUF = 2
            Pm = [[sb2('r_Pm%d_%d' % (i, hh), [128, 128], BF16) for hh in range(2)] for i in range(NBUF)]
            qin = [sb2('r_qin%d' % i, [128, 128], BF16) for i in range(NBUF)]
            kout = [sb2('r_kout%d' % i, [128, 128], BF16) for i in range(NBUF)]
            koT = [sb2('r_koT%d' % i, [128, 128], BF16) for i in range(NBUF)]
            tmp = [sb2('r_tmp%d' % i, [128, 128]) for i in range(NBUF)]
            S32 = [sb2('r_S32_%d' % i, [128, 128]) for i in range(4)]
            Sb = [sb2('r_Sb_%d' % i, [128, 128], BF16) for i in range(4)]
            psS = [es2.enter_context(_pst(nc, 'r_psS%d' % i, [128, 128], F32)) for i in range(4)]
            psO = [es2.enter_context(_pst(nc, 'r_psO%d' % i, [128, 128], F32)) for i in range(2)]
            psT = [es2.enter_context(_pst(nc, 'r_psT%d' % i, [128, 128], BF16)) for i in range(1)]
            psU = [es2.enter_context(_pst(nc, 'r_psU%d' % i, [128, 128], F32)) for i in range(1)]
            step = 0
            seen = set()
            for j in range(4):
                P.memset(S32[j][:], 0.0, writes=['r_S32_%d' % j])
                P.memset(Sb[j][:], 0.0, writes=['r_Sb_%d' % j])
            for ci in range(NT):
                for hp in range(2):
                    for d in range(2):
                        j = d * 2 + hp
                        c = chunk_order(d)[ci]
                        cs = slice(c * 128, (c + 1) * 128)
                        i = step % NBUF
                        step += 1
                        si = str(i)
                        Sk, Sbk = 'r_S32_%d' % j, 'r_Sb_%d' % j
                        for hh in range(2):
                            pr = slice(hh * 64, (hh + 1) * 64)
                            pS = psS[2 * i + hh]
                            pSk = 'r_psS%d' % (2 * i + hh)
                            P.mm(pS[:, :], kr[pr, hp, cs], qr[pr, hp, cs], True, True, reads=['r_q', 'r_k'], writes=[pSk])
                            P.tt(Pm[i][hh][:], pS[:, :], GAM[:, d * 4 + hp * 2 + hh, :], ALU.mult,
                                 reads=[pSk, 'r_GAM'], writes=['r_Pm%s_%d' % (si, hh)])
                        P.tt(qin[i][:], qr[:, hp, cs], EQ[:, j, :], ALU.mult, reads=['r_q', 'r_EQ'], writes=['r_qin' + si])
                        P.tt(kout[i][:], kr[:, hp, cs], EK[:, j, :], ALU.mult, reads=['r_k', 'r_EK'], writes=['r_kout' + si])
                        pO = psO[i]
                        pOk = 'r_psO%d' % i
                        P.mm(pO[:, :], vpad[:, hp, 0, c, :], Pm[i][0][:], True, False, reads=['r_vpad', 'r_Pm%s_0' % si], writes=[pOk])
                        P.mm(pO[:, :], vpad[:, hp, 1, c, :], Pm[i][1][:], False, False, reads=['r_vpad', 'r_Pm%s_1' % si], writes=[pOk])
                        P.mm(pO[:, :], Sb[j][:], qin[i][:], False, True, reads=[Sbk, 'r_qin' + si], writes=[pOk])
                        ok = ('r_oacc', hp, c)
                        if (ok, 0) not in seen:
                            seen.add((ok, 0))
                            P.copy(oacc[:, hp, cs], pO[:, :], reads=[pOk], writes=[ok], eng='scalar')
                        else:
                            P.tt(oacc[:, hp, cs], oacc[:, hp, cs], pO[:, :], ALU.add, reads=[pOk, ok], writes=[ok])
                        P.tr(psT[0][:, :], kout[i][:], k.identb[:], reads=['r_kout' + si, 'identb'], writes=['r_psT'])
                        P.copy(koT[i][:], psT[0][:, :], reads=['r_psT'], writes=['r_koT' + si], eng='scalar')
                        P.mm(psU[0][:, :], koT[i][:], v[:, c, hp * 128:(hp + 1) * 128], True, True,
                             reads=['r_koT' + si, 'r_v'], writes=['r_psU'])
                        P.tt(tmp[i][:], psU[0][:, :], blk2[:], ALU.mult, reads=['r_psU', 'r_blk2'], writes=['r_tmp' + si])
                        P.stt(S32[j][:], S32[j][:], GC[:, j:j + 1], tmp[i][:], ALU.mult, ALU.add,
                              reads=[Sk, 'r_GC', 'r_tmp' + si], writes=[Sk])
                        P.copy(Sb[j][:], S32[j][:], reads=[Sk], writes=[Sbk], eng='scalar')
            P.flush()
        for hp in range(2):
            with ExitStack() as es2:
                norm_gate(k, P, nc, es2, b, oacc[:, hp, :], [('r_oacc', hp, c) for c in range(NT)], 22 + hp, 256 + hp * 128, True, 'r%d' % hp)
                P.flush()


def phase_gla(k, l, b):
    P, nc = k.P, k.nc
    with ExitStack() as es:
        sbt = lambda name, shape, dt=F32: es.enter_context(_sbt(nc, name, list(shape), dt))
        qh = sbt('g_qh', [128, 2, 4, T], BF16)
        qt = sbt('g_qt', [128, 2, T], BF16)
        kh = sbt('g_kh', [128, 2, T], BF16)
        elast = sbt('g_el', [128, 2, NT])
        v = sbt('g_v', [128, NT, 256], BF16)
        vpad = sbt('g_vpad', [128, 4, NT, 128], BF16)
        oacc = sbt('g_oacc', [128, 2, T])
        msk = sbt('g_msk', [128, 2, 128])
        blkg = sbt('g_blkg', [128, 256])
        hm = sbt('g_hm', [128, 4])
        P.dma(msk[:, 0, :], k.MSKf[:, :], writes=['g_msk'])
        P.dma(msk[:, 1, :], k.MSKb[:, :], writes=['g_msk'])
        P.dma(blkg[:], k.BLKG[:, :], writes=['g_blkg'])
        P.dma(hm[:], k.HM4[:, :], writes=['g_hm'])
        P.dma(v[:], k.pTM[b].rearrange("(c p) n -> p c n", p=128)[:, :, 0:256], writes=['g_v'], eng='gpsimd')
        P.memset(vpad[:].rearrange("p a c d -> p (a c d)"), 0.0, writes=['g_vpad'], eng='gpsimd')
        for h in range(4):
            hh = h % 2
            P.copy(vpad[:, h, :, hh * 64:(hh + 1) * 64], v[:, :, h * 64:(h + 1) * 64], reads=['g_v', 'g_vpad'], writes=['g_vpad'], eng='gpsimd')
        with ExitStack() as es2:
            sb2 = lambda name, shape, dt=F32: es2.enter_context(_sbt(nc, name, list(shape), dt))
            z = sb2('g_z', [128, T])
            gkw = sb2('g_gkw', [128, 128])
            nb = sb2('g_nb', [128, 2])
            sp = sb2('g_sp', [128, T])
            bc = sb2('g_bc', [128, T])
            ee = sb2('g_ee', [128, T])
            qf = sb2('g_qf', [128, T])
            kf = sb2('g_kf', [128, T])
            ones = sb2('g_ones', [128, 128])
            lnq = sb2('g_lnq', [128, 1])
            ps = [es2.enter_context(_pst(nc, 'g_ps%d' % i, [128, 512], F32)) for i in range(2)]
            P.dma(z[:], k.pFM[b, 2 * 128:3 * 128, :], writes=['g_z'])
            P.dma(gkw[:], k.gkw[l, :, :], writes=['g_gkw'])
            P.dma(nb[:], k.gkbFM[:, l, :], writes=['g_nb'])
            P.dma(qf[:], k.pFM[b, 0:128, :], writes=['g_qf'])
            P.dma(kf[:], k.pFM[b, 128:256, :], writes=['g_kf'])
            P.ts(nb[:], nb[:], -1.0, None, ALU.mult, reads=['g_nb'], writes=['g_nb'])
            P.memset(ones[:], 1.0, writes=['g_ones'])
            P.memset(lnq[:], math.log(32.0 ** -0.5), writes=['g_lnq'])
            for d in range(2):
                pr = slice(d * 32, d * 32 + 16)
                for bi, (t0, tn) in enumerate(TBLK):
                    i = bi % 2
                    P.mm(ps[i][:, :tn], gkw[pr, :], z[pr, t0:t0 + tn], True, True, reads=['g_gkw', 'g_z'], writes=['g_ps%d' % i])
                    P.act(sp[:, t0:t0 + tn], ps[i][:, :tn], AF.Exp, reads=['g_ps%d' % i, 'g_nb'], writes=['g_sp'],
                          scale=-1.0, bias=nb[:, d:d + 1])
                P.act(sp[:], sp[:], AF.Ln, reads=['g_sp'], writes=['g_sp'], bias=1.0, scale=1.0)
                for c in range(NT):
                    cs = slice(c * 128, (c + 1) * 128)
                    if d == 0:
                        P.scan(bc[:, cs], ones[:], sp[:, cs], 0.0, reads=['g_ones', 'g_sp'], writes=['g_bc'])
                    else:
                        P.scan(bc[:, cs][:, ::-1], ones[:], sp[:, cs][:, ::-1], 0.0, reads=['g_ones', 'g_sp'], writes=['g_bc'])
                P.act(ee[:], bc[:], AF.Exp, reads=['g_bc'], writes=['g_ee'], scale=-1.0 / 16.0, bias=lnq[:])
                P.tt(qt[:, d, :], qf[:], ee[:], ALU.mult, reads=['g_qf', 'g_ee'], writes=['g_qt'])
                for h in range(4):
                    P.ts(qh[:, d, h, :], qt[:, d, :], hm[:, h:h + 1], None, ALU.mult, reads=['g_qt', 'g_hm'], writes=['g_qh'],
                         eng='gpsimd' if h % 2 else 'vector')
                lastv = bc[:, 127::128] if d == 0 else bc[:, 0::128]
                P.act(elast[:, d, :], lastv, AF.Exp, reads=['g_bc'], writes=['g_el'], scale=-1.0 / 16.0)
                P.act(ee[:], bc[:], AF.Exp, reads=['g_bc', 'g_qt'], writes=['g_ee'], scale=1.0 / 16.0)
                P.tt(kh[:, d, :], kf[:], ee[:], ALU.mult, reads=['g_kf', 'g_ee'], writes=['g_kh'])
            P.flush()
        with ExitStack() as es2:
            sb2 = lambda name, shape, dt=F32: es2.enter_context(_sbt(nc, name, list(shape), dt))
            NBUF = 2
            Pm = [[sb2('g_Pm%d_%d' % (i, h), [128, 128], BF16) for h in range(4)] for i in range(NBUF)]
            koT = [sb2('g_koT%d' % i, [128, 128], BF16) for i in range(NBUF)]
            tmp = [sb2('g_tmp%d' % i, [128, 256]) for i in range(NBUF)]
            S32 = [sb2('g_S32_%d' % i, [128, 256]) for i in range(2)]
            Sb = [sb2('g_Sb_%d' % i, [128, 256], BF16) for i in range(2)]
            psS = [es2.enter_context(_pst(nc, 'g_psS%d' % i, [128, 128], F32)) for i in range(4)]
            psO = [es2.enter_context(_pst(nc, 'g_psO%d' % i, [128, 128], F32)) for i in range(2)]
            psT = es2.enter_context(_pst(nc, 'g_psT', [128, 128], BF16))
            psU = es2.enter_context(_pst(nc, 'g_psU', [128, 256], F32))
            for j in range(2):
                P.memset(S32[j][:], 0.0, writes=['g_S32_%d' % j])
                P.memset(Sb[j][:], 0.0, writes=['g_Sb_%d' % j])
            step = 0
            seen = set()
            for ci in range(NT):
                for d in range(2):
                    c = chunk_order(d)[ci]
                    cs = slice(c * 128, (c + 1) * 128)
                    i = step % NBUF
                    step += 1
                    si = str(i)
                    Sk, Sbk = 'g_S32_%d' % d, 'g_Sb_%d' % d
                    for h in range(4):
                        P.mm(psS[h][:, :], kh[:, d, cs], qh[:, d, h, cs], True, True, reads=['g_kh', 'g_qh'], writes=['g_psS%d' % h])
                        P.tt(Pm[i][h][:], psS[h][:, :], msk[:, d, :], ALU.mult, reads=['g_psS%d' % h, 'g_msk'],
                             writes=['g_Pm%s_%d' % (si, h)])
                    for vp in range(2):
                        pO = psO[vp]
                        pOk = 'g_psO%d' % vp
                        P.mm(pO[:, :], vpad[:, 2 * vp, c, :], Pm[i][2 * vp][:], True, False,
                             reads=['g_vpad', 'g_Pm%s_%d' % (si, 2 * vp)], writes=[pOk])
                        P.mm(pO[:, :], vpad[:, 2 * vp + 1, c, :], Pm[i][2 * vp + 1][:], False, False,
                             reads=['g_vpad', 'g_Pm%s_%d' % (si, 2 * vp + 1)], writes=[pOk])
                        P.mm(pO[:, :], Sb[d][:, vp * 128:(vp + 1) * 128], qt[:, d, cs], False, True, reads=[Sbk, 'g_qt'], writes=[pOk])
                        ok = ('g_oacc', vp, c)
                        if ok not in seen:
                            seen.add(ok)
                            P.copy(oacc[:, vp, cs], pO[:, :], reads=[pOk], writes=[ok], eng='scalar')
                        else:
                            P.tt(oacc[:, vp, cs], oacc[:, vp, cs], pO[:, :], ALU.add, reads=[pOk, ok], writes=[ok])
                    P.tr(psT[:, :], kh[:, d, cs], k.identb[:], reads=['g_kh', 'identb'], writes=['g_psT'])
                    P.copy(koT[i][:], psT[:, :], reads=['g_psT'], writes=['g_koT' + si], eng='scalar')
                    P.mm(psU[:, :], koT[i][:], v[:, c, :], True, True, reads=['g_koT' + si, 'g_v'], writes=['g_psU'])
                    P.tt(tmp[i][:], psU[:, :], blkg[:], ALU.mult, reads=['g_psU', 'g_blkg'], writes=['g_tmp' + si])
                    P.tt(S32[d][:], S32[d][:], tmp[i][:], ALU.add, reads=[Sk, 'g_tmp' + si], writes=[Sk])
                    P.ts(S32[d][:], S32[d][:], elast[:, d, c:c + 1], None, ALU.mult, reads=[Sk, 'g_el'], writes=[Sk])
                    P.copy(Sb[d][:], S32[d][:], reads=[Sk], writes=[Sbk], eng='scalar')
            P.flush()
        for vp in range(2):
            with ExitStack() as es2:
                norm_gate(k, P, nc, es2, b, oacc[:, vp, :], [('g_oacc', vp, c) for c in range(NT)], 20 + vp, vp * 128, False, 'g%d' % vp)
                P.flush()


TWO_PI = 2.0 * math.pi


def sincos(P, ang, sn, cs, ki, t1, keys):
    R = dict(reads=keys, writes=keys)
    P.ts(t1, ang, 1.0 / TWO_PI, None, ALU.mult, **R)
    P.copy(ki, t1, **R)
    P.copy(t1, ki, **R)
    P.stt(ang, t1, -TWO_PI, ang, ALU.mult, ALU.add, **R)
    P.ts(t1, ang, math.pi, None, ALU.is_gt, **R)
    P.stt(ang, t1, -TWO_PI, ang, ALU.mult, ALU.add, **R)
    P.ts(t1, ang, -math.pi, None, ALU.is_lt, **R)
    P.stt(ang, t1, TWO_PI, ang, ALU.mult, ALU.add, **R)
    P.act(sn, ang, AF.Sin, **R)
    P.ts(ang, ang, math.pi / 2, None, ALU.add, **R)
    P.ts(t1, ang, math.pi, None, ALU.is_gt, **R)
    P.stt(ang, t1, -TWO_PI, ang, ALU.mult, ALU.add, **R)
    P.act(cs, ang, AF.Sin, **R)


def phase_s5(k, l, b):
    P, nc = k.P, k.nc
    with ExitStack() as es:
        sbt = lambda name, shape, dt=F32: es.enter_context(_sbt(nc, name, list(shape), dt))
        TAB = sbt('s_tab', [128, 16, 4, 128])
        rr = sbt('s_r', [128, 16])
        cb = sbt('s_cb', [128, 2, 16])
        WB = sbt('s_WB', [128, 8, 2, 128], BF16)
        WC = sbt('s_WC', [128, 8, 2, 128], BF16)
        u32 = sbt('s_u32', [128, 2, T])
        ub = sbt('s_ub', [128, 2, T], BF16)
        yacc = sbt('s_yacc', [128, 2, T])
        pos = sbt('s_pos', [128, 2, 128])
        P.dma(pos[:], k.POS[:, 0:2, :], writes=['s_pos'])
        P.dma(WB[:], k.s5_WB[l, :, :, :, :], writes=['s_WB'], eng='gpsimd')
        P.dma(WC[:], k.s5_WC[l, :, :, :, :], writes=['s_WC'], eng='gpsimd')
        for ct in range(2):
            P.dma(u32[:, ct, :], k.pFM[b, (11 + ct) * 128:(12 + ct) * 128, :], writes=['s_u32'])
        P.copy(ub[:], u32[:], reads=['s_u32'], writes=['s_ub'], eng='gpsimd')
        with ExitStack() as es2:
            sb2 = lambda name, shape, dt=F32: es2.enter_context(_sbt(nc, name, list(shape), dt))
            lre = sb2('s_lre', [128, 16]); lim = sb2('s_lim', [128, 16]); dt_ = sb2('s_dt', [128, 16])
            th = sb2('s_th', [128, 16]); ang = sb2('s_ang', [128, 16]); sn = sb2('s_sn', [128, 16]); cs = sb2('s_cs', [128, 16])
            ki = sb2('s_ki', [128, 16], I32); t1 = sb2('s_t1', [128, 16]); t2 = sb2('s_t2', [128, 16])
            bre = sb2('s_bre', [128, 16]); bim = sb2('s_bim', [128, 16]); den = sb2('s_den', [128, 16])
            A = sb2('s_A', [128, 16, 128]); K2 = sb2('s_K2', [128, 16, 128], I32); T1 = sb2('s_T1', [128, 16, 128])
            pk = ['s_par']
            R = dict(reads=pk, writes=pk)
            P.dma(lre[:], k.s5_lre[:, l, :], writes=pk)
            P.dma(lim[:], k.s5_lim[:, l, :], writes=pk)
            P.dma(dt_[:], k.s5_ldt[:, l, :], writes=pk)
            P.act(dt_[:], dt_[:], AF.Exp, **R)
            P.tt(th[:], lim[:], dt_[:], ALU.mult, **R)
            P.tt(t2[:], lre[:], dt_[:], ALU.mult, **R)
            P.act(rr[:], t2[:], AF.Exp, reads=pk, writes=pk + ['s_r'])
            P.copy(ang[:], th[:], **R)
            sincos(P, ang[:], sn[:], cs[:], ki[:], t1[:], pk)
            P.tt(t1[:], rr[:], cs[:], ALU.mult, **R)
            P.ts(t1[:], t1[:], -1.0, None, ALU.add, **R)
            P.tt(t2[:], rr[:], sn[:], ALU.mult, **R)
            P.tt(den[:], lre[:], lre[:], ALU.mult, **R)
            P.tt(bre[:], lim[:], lim[:], ALU.mult, **R)
            P.tt(den[:], den[:], bre[:], ALU.add, **R)
            P.recip(den[:], den[:], **R)
            P.tt(bre[:], t1[:], lre[:], ALU.mult, **R)
            P.tt(bim[:], t2[:], lim[:], ALU.mult, **R)
            P.tt(bre[:], bre[:], bim[:], ALU.add, **R)
            P.tt(bre[:], bre[:], den[:], ALU.mult, **R)
            P.tt(bim[:], t2[:], lre[:], ALU.mult, **R)
            P.tt(t2[:], t1[:], lim[:], ALU.mult, **R)
            P.tt(bim[:], bim[:], t2[:], ALU.subtract, **R)
            P.tt(bim[:], bim[:], den[:], ALU.mult, **R)
            tks = [('s_tab', dj) for dj in range(16)]
            for dj in range(16):
                d = dj // 8
                P.ts(A[:, dj, :], pos[:, d, :], th[:, dj:dj + 1], None, ALU.mult, reads=pk + ['s_pos'], writes=['s_A'])
            sincos(P, A[:], TAB[:, :, 3, :], TAB[:, :, 2, :], K2[:], T1[:], tks + ['s_A'])
            for dj in range(16):
                d = dj // 8
                tk = ('s_tab', dj)
                Rt = dict(reads=pk + [tk, 's_T1'], writes=[tk, 's_T1'])
                P.ts(T1[:, dj, :], TAB[:, dj, 3, :], bim[:, dj:dj + 1], None, ALU.mult, **Rt)
                P.stt(TAB[:, dj, 0, :], TAB[:, dj, 2, :], bre[:, dj:dj + 1], T1[:, dj, :], ALU.mult, ALU.add, **Rt)
                P.ts(T1[:, dj, :], TAB[:, dj, 3, :], bre[:, dj:dj + 1], None, ALU.mult, **Rt)
                P.stt(TAB[:, dj, 1, :], TAB[:, dj, 2, :], bim[:, dj:dj + 1], T1[:, dj, :], ALU.mult, ALU.subtract, **Rt)
                li = 127 if d == 0 else 0
                P.copy(cb[:, 0, dj:dj + 1], TAB[:, dj, 2, li:li + 1], reads=[tk], writes=['s_cb'])
                P.copy(cb[:, 1, dj:dj + 1], TAB[:, dj, 3, li:li + 1], reads=[tk], writes=['s_cb'])
            P.flush()
        with ExitStack() as es2:
            sb2 = lambda name, shape, dt=F32: es2.enter_context(_sbt(nc, name, list(shape), dt))
            BUs = [sb2('s_BU%d' % d, [128, 8, 2, 128]) for d in range(2)]
            M1s = [sb2('s_M1%d' % d, [128, 8, 2, 128]) for d in range(2)]
            M2s = [sb2('s_M2%d' % d, [128, 8, 2, 128]) for d in range(2)]
            G = [sb2('s_G%d' % d, [128, 8, 2, 128]) for d in range(2)]
            H = [sb2('s_H%d' % d, [128, 8, 2, 128], BF16) for d in range(2)]
            G0 = [sb2('s_G0%d' % d, [128, 2, 8]) for d in range(2)]
            GL = [sb2('s_GL%d' % d, [128, 2, 8]) for d in range(2)]
            rfull = sb2('s_rfull', [128, 16, 128])
            psB = [es2.enter_context(_pst(nc, 's_psB%d' % i, [128, 2, 2, 128], F32)) for i in range(4)]
            psY = [es2.enter_context(_pst(nc, 's_psY%d' % i, [128, 2, 128], F32)) for i in range(2)]
            for d in range(2):
                P.memset(G0[d][:].rearrange("p a b -> p (a b)"), 0.0, writes=['s_G0%d' % d])
            for dj in range(16):
                P.ts(rfull[:, dj, :], pos[:, 0, :], 0.0, rr[:, dj:dj + 1], ALU.mult, ALU.add, reads=['s_pos', 's_r'], writes=['s_rfull'])
            seen = set()
            tabk = [('s_tab', dj) for dj in range(16)]
            for ci in range(NT):
                for d in range(2):
                    c = chunk_order(d)[ci]
                    cs_ = slice(c * 128, (c + 1) * 128)
                    pY = psY[d]
                    pYk = 's_psY%d' % d
                    tk = tabk[d * 8:(d + 1) * 8]
                    gk = 's_G%d' % d
                    hk = 's_H%d' % d
                    bc4 = lambda q_: TAB[:, d * 8:(d + 1) * 8, q_, :].unsqueeze(2).to_broadcast([128, 8, 2, 128])
                    BU, M1, M2 = BUs[d], M1s[d], M2s[d]
                    kBU, kM1, kM2 = 's_BU%d' % d, 's_M1%d' % d, 's_M2%d' % d
                    for j in range(8):
                        ct = j // 4
                        pB = psB[j // 2]
                        pBk = 's_psB%d' % (j // 2)
                        P.mm(pB[:, j % 2, 0, :], WB[:, j, 0, :], ub[:, ct, cs_], True, True, reads=['s_WB', 's_ub'], writes=[pBk])
                        P.mm(pB[:, j % 2, 1, :], WB[:, j, 1, :], ub[:, ct, cs_], True, True, reads=['s_WB', 's_ub'], writes=[pBk])
                    for q_ in range(4):
                        P.copy(BU[:, 2 * q_:2 * q_ + 2, :, :], psB[q_][:], reads=['s_psB%d' % q_], writes=[kBU], eng='scalar')
                    P.tt(M1[:], BU[:], bc4(0), ALU.mult, reads=[kBU] + tk, writes=[kM1])
                    P.tt(M2[:], BU[:], bc4(1), ALU.mult, reads=[kBU] + tk, writes=[kM2])
                    P.tt(M1[:, :, 0, :], M1[:, :, 0, :], M2[:, :, 1, :], ALU.subtract, reads=[kM1, kM2], writes=[kM1])
                    P.tt(M1[:, :, 1, :], M1[:, :, 1, :], M2[:, :, 0, :], ALU.add, reads=[kM1, kM2], writes=[kM1])
                    for j in range(8):
                        dj = d * 8 + j
                        for ri in range(2):
                            o_ = G[d][:, j, ri, :]
                            d1 = M1[:, j, ri, :]
                            if d == 1:
                                o_, d1 = o_[:, ::-1], d1[:, ::-1]
                            P.scan(o_, rfull[:, dj, :], d1, G0[d][:, ri, j:j + 1], reads=['s_rfull', kM1, 's_G0%d' % d], writes=[gk])
                    P.tt(M1[:], G[d][:], bc4(2), ALU.mult, reads=[gk] + tk, writes=[kM1])
                    P.tt(M2[:], G[d][:], bc4(3), ALU.mult, reads=[gk] + tk, writes=[kM2])
                    P.tt(H[d][:, :, 0, :], M1[:, :, 0, :], M2[:, :, 1, :], ALU.subtract, reads=[kM1, kM2], writes=[hk])
                    P.stt(H[d][:, :, 1, :], M1[:, :, 1, :], -1.0, M2[:, :, 0, :], ALU.mult, ALU.subtract, reads=[kM1, kM2], writes=[hk])
                    for j in range(8):
                        ct = j // 4
                        jj = j % 4
                        P.mm(pY[:, ct, :], WC[:, j, 0, :], H[d][:, j, 0, :], jj == 0, False, reads=['s_WC', hk], writes=[pYk])
                        P.mm(pY[:, ct, :], WC[:, j, 1, :], H[d][:, j, 1, :], False, jj == 3, reads=['s_WC', hk], writes=[pYk])
                    ok = ('s_yacc', c)
                    if ok not in seen:
                        seen.add(ok)
                        P.copy(yacc[:, :, cs_], pY[:], reads=[pYk], writes=[ok], eng='scalar')
                    else:
                        P.tt(yacc[:, :, cs_], yacc[:, :, cs_], pY[:], ALU.add, reads=[pYk, ok], writes=[ok])
                    li = 127 if d == 0 else 0
                    g0k = 's_G0%d' % d
                    glk = 's_GL%d' % d
                    P.copy(GL[d][:], G[d][:, :, :, li].rearrange("p j r -> p r j"), reads=[gk], writes=[glk])
                    cbc = cb[:, 0, d * 8:(d + 1) * 8]
                    cbs = cb[:, 1, d * 8:(d + 1) * 8]
                    Rg = dict(reads=[glk, 's_cb', g0k], writes=[g0k])
                    P.tt(G0[d][:, 0, :], GL[d][:, 1, :], cbs, ALU.mult, **Rg)
                    P.tt(G0[d][:, 1, :], GL[d][:, 0, :], cbs, ALU.mult, **Rg)
                    P.tt(GL[d][:, 0, :], GL[d][:, 0, :], cbc, ALU.mult, reads=[glk, 's_cb', g0k], writes=[glk])
                    P.tt(GL[d][:, 1, :], GL[d][:, 1, :], cbc, ALU.mult, reads=[glk, 's_cb', g0k], writes=[glk])
                    P.tt(G0[d][:, 0, :], GL[d][:, 0, :], G0[d][:, 0, :], ALU.subtract, reads=[glk, g0k], writes=[g0k])
                    P.tt(G0[d][:, 1, :], GL[d][:, 1, :], G0[d][:, 1, :], ALU.add, reads=[glk, g0k], writes=[g0k])
            P.flush()
        with ExitStack() as es2:
            sb2 = lambda name, shape, dt=F32: es2.enter_context(_sbt(nc, name, list(shape), dt))
            dsk = sb2('s_dsk', [128, 2])
            gb = sb2('s_gb', [128, 2])
            gw = sb2('s_gw', [128, 2, 256], BF16)
            yb = sb2('s_yb', [128, 2, T], BF16)
            t1 = sb2('s_e1', [128, T])
            zt = [sb2('s_zt%d' % i, [128, 512]) for i in range(2)]
            ps = [es2.enter_context(_pst(nc, 's_psz%d' % i, [128, 512], F32)) for i in range(2)]
            P.dma(dsk[:], k.s5_dFM[:, l, :], writes=['s_dsk'])
            P.dma(gb[:], k.s5_gbFM[:, l, :], writes=['s_gb'])
            P.dma(gw[:], k.s5_gw[l, :, :, :], writes=['s_gw'], eng='gpsimd')
            yk = [('s_yacc', c) for c in range(NT)]
            for ct in range(2):
                y = yacc[:, ct, :]
                P.stt(y, u32[:, ct, :], dsk[:, ct:ct + 1], y, ALU.mult, ALU.add, reads=yk + ['s_u32', 's_dsk'], writes=yk)
                P.tt(t1[:], y, y, ALU.mult, reads=yk, writes=['s_e1'])
                P.ts(t1[:], t1[:], 0.044715, 1.0, ALU.mult, ALU.add, reads=['s_e1'], writes=['s_e1'])
                P.tt(t1[:], t1[:], y, ALU.mult, reads=yk + ['s_e1'], writes=['s_e1'])
                P.act(t1[:], t1[:], AF.Sigmoid, reads=['s_e1'], writes=['s_e1'], scale=2.0 * math.sqrt(2.0 / math.pi))
                P.tt(y, y, t1[:], ALU.mult, reads=yk + ['s_e1'], writes=yk)
                P.copy(yb[:, ct, :], y, reads=yk, writes=['s_yb'], eng='gpsimd')
            cnt = 0
            for nt in range(2):
                for (t0, tn) in TBLK:
                    i = cnt % 2
                    cnt += 1
                    P.mm(ps[i][:, :tn], gw[:, 0, nt * 128:(nt + 1) * 128], yb[:, 0, t0:t0 + tn], True, False, reads=['s_gw', 's_yb'], writes=['s_psz%d' % i])
                    P.mm(ps[i][:, :tn], gw[:, 1, nt * 128:(nt + 1) * 128], yb[:, 1, t0:t0 + tn], False, True, reads=['s_gw', 's_yb'], writes=['s_psz%d' % i])
                    P.act(zt[i][:, :tn], ps[i][:, :tn], AF.Sigmoid, reads=['s_psz%d' % i, 's_gb'], writes=['s_zt%d' % i], bias=gb[:, nt:nt + 1], scale=1.0)
                    P.tt(zt[i][:, :tn], zt[i][:, :tn], yacc[:, nt, t0:t0 + tn], ALU.mult, reads=['s_zt%d' % i] + yk, writes=['s_zt%d' % i])
                    P.dma(k.mixFM[b, 512 + nt * 128:512 + (nt + 1) * 128, t0:t0 + tn], zt[i][:, :tn], reads=['s_zt%d' % i])
            P.flush()


def phase_dn(k, l, b):
    P, nc = k.P, k.nc
    with ExitStack() as es:
        sbt = lambda name, shape, dt=F32: es.enter_context(_sbt(nc, name, list(shape), dt))
        qb = sbt('d_qb', [128, 2, T], BF16)
        kb = sbt('d_kb', [128, 2, T], BF16)
        kn = sbt('d_kn', [128, 2, T])
        vTM = sbt('d_vTM', [128, NT, 256])
        bTM = sbt('d_bTM', [128, NT, 64])
        gall = sbt('d_gall', [128, NT, 3, 128])
        ngc = sbt('d_ngc', [128, T])
        mh = sbt('d_mh', [128, 4])
        oh = sbt('d_oh', [128, 2, 512])
        ones4 = sbt('d_ones4', [128, 128])
        P.dma(mh[:], k.MH[:, :], writes=['d_mh'])
        P.dma(oh[:], k.OH[:, :, :], writes=['d_oh'])
        P.memset(ones4[:], 1.0, writes=['d_ones4'])
        oacc = sbt('d_oacc', [128, 2, T])
        selr = sbt('d_selr', [128, 4, 128])
        selp = sbt('d_selp', [128, 2, 128])
        mb = sbt('d_mb', [128, 2, 4, 128])
        blk2 = sbt('d_blk2', [128, 128])
        P.dma(selr[:], k.SELR[:, :, :], writes=['d_selr'])
        P.dma(selp[:], k.SELP[:, :, :], writes=['d_selp'])
        P.dma(mb[:], k.MB[:, :, :, :], writes=['d_mb'])
        P.dma(blk2[:], k.BLK2[:, :], writes=['d_blk2'])
        hm2 = sbt('d_hm2', [128, 2])
        P.dma(hm2[:], k.HM2[:, :], writes=['d_hm2'])
        with ExitStack() as es2:
            sb2 = lambda name, shape, dt=F32: es2.enter_context(_sbt(nc, name, list(shape), dt))
            x = [sb2('d_x%d' % i, [128, T]) for i in range(2)]
            acc = [sb2('d_acc%d' % i, [128, T]) for i in range(2)]
            sq = sb2('d_sq', [128, T])
            cw = sb2('d_cw', [128, 6, 5])
            eps = sb2('d_eps', [128, 1])
            ps = [es2.enter_context(_pst(nc, 'd_psA%d' % i, [128, 512], F32)) for i in range(2)]
            pst = [es2.enter_context(_pst(nc, 'd_psT%d' % i, [128, 128], F32)) for i in range(2)]
            P.dma(cw[:], k.convFM[:, l, :, :], writes=['d_cw'])
            P.memset(eps[:], EPS, writes=['d_eps'])
            cnt = 0
            for ti in range(6):
                i = ti % 2
                xk, ak = 'd_x%d' % i, 'd_acc%d' % i
                P.dma(x[i][:], k.pFM[b, (13 + ti) * 128:(14 + ti) * 128, :], writes=[xk])
                P.ts(acc[i][:], x[i][:], cw[:, ti, 2:3], None, ALU.mult, reads=[xk, 'd_cw'], writes=[ak])
                for (s0, s1) in [(0, LC), (LC, T)]:
                    for j in (0, 1, 3, 4):
                        sft = j - 2
                        lo, hi = max(s0, s0 - sft), min(s1, s1 - sft)
                        P.stt(acc[i][:, lo:hi], x[i][:, lo + sft:hi + sft], cw[:, ti, j:j + 1], acc[i][:, lo:hi], ALU.mult, ALU.add,
                              reads=[xk, 'd_cw', ak], writes=[ak])
                P.act(acc[i][:], acc[i][:], AF.Silu, reads=[ak], writes=[ak])
                if ti < 4:
                    hp = ti % 2
                    P.tt(sq[:], acc[i][:], acc[i][:], ALU.mult, reads=[ak], writes=['d_sq'], eng='gpsimd')
                    for (t0, tn) in TBLK:
                        pp = ps[cnt % 2]
                        ppk = 'd_psA%d' % (cnt % 2)
                        cnt += 1
                        P.mm(pp[:, :tn], blk2[:], sq[:, t0:t0 + tn], True, True, reads=['d_blk2', 'd_sq'], writes=[ppk])
                        P.act(x[i][:, t0:t0 + tn], pp[:, :tn], AF.Sqrt, reads=[ppk, 'd_eps', xk], writes=[xk], bias=eps[:], scale=1.0)
                    P.recip(x[i][:], x[i][:], reads=[xk], writes=[xk])
                    if ti < 2:
                        P.stt(qb[:, hp, :], acc[i][:], 0.125, x[i][:], ALU.mult, ALU.mult, reads=[ak, xk], writes=['d_qb'])
                    else:
                        P.tt(kn[:, hp, :], acc[i][:], x[i][:], ALU.mult, reads=[ak, xk], writes=['d_kn'])
                        P.copy(kb[:, hp, :], kn[:, hp, :], reads=['d_kn'], writes=['d_kb'], eng='gpsimd')
                else:
                    vt = ti - 4
                    for c in range(NT):
                        pt = pst[c % 2]
                        ptk = 'd_psT%d' % (c % 2)
                        P.tr(pt[:, :], acc[i][:, c * 128:(c + 1) * 128], k.identf[:], reads=[ak, 'identf'], writes=[ptk])
                        P.copy(vTM[:, c, vt * 128:(vt + 1) * 128], pt[:, :], reads=[ptk], writes=['d_vTM'], eng='scalar' if c % 2 else 'vector')
            P.flush()
        if k.dbg.get('dn_stop') == 'A':
            return
        with ExitStack() as es2:
            sb2 = lambda name, shape, dt=F32: es2.enter_context(_sbt(nc, name, list(shape), dt))
            ga = sb2('d_ga', [128, T])
            lb = sb2('d_lb', [128, T])
            bt = sb2('d_bt', [128, T])
            par = sb2('d_par', [128, 2])
            nA = sb2('d_nA', [128, 1])
            ones = sb2('d_ones', [128, 128])
            pst = [es2.enter_context(_pst(nc, 'd_psB%d' % i, [128, 128], F32)) for i in range(2)]
            P.dma(ga[:], k.pFM[b, 19 * 128:20 * 128, :], writes=['d_ga'])
            P.dma(lb[:], k.pFM[b, 26 * 128:27 * 128, :], writes=['d_lb'])
            P.dma(par[:], k.dn_par[:, l, :], writes=['d_par'])
            P.memset(ones[:], 1.0, writes=['d_ones'])
            P.memset(gall[:].rearrange("p a b c -> p (a b c)"), 0.0, writes=['d_gall'], eng='gpsimd')
            P.act(nA[:], par[:, 0:1], AF.Exp, reads=['d_par'], writes=['d_nA'])
            P.ts(nA[:], nA[:], -1.0, None, ALU.mult, reads=['d_nA'], writes=['d_nA'])
            P.act(ga[:], ga[:], AF.Exp, reads=['d_ga', 'd_par'], writes=['d_ga'], bias=par[:, 1:2], scale=1.0)
            P.act(ga[:], ga[:], AF.Ln, reads=['d_ga'], writes=['d_ga'], bias=1.0, scale=1.0)
            P.ts(ga[:], ga[:], nA[:, 0:1], None, ALU.mult, reads=['d_ga', 'd_nA'], writes=['d_ga'])
            P.act(lb[:], lb[:], AF.Exp, reads=['d_lb'], writes=['d_lb'], scale=-1.0)
            P.act(lb[:], lb[:], AF.Ln, reads=['d_lb'], writes=['d_lb'], bias=1.0, scale=1.0)
            P.ts(lb[:], lb[:], -1.0, None, ALU.mult, reads=['d_lb'], writes=['d_lb'])
            P.act(bt[:], lb[:], AF.Exp, reads=['d_lb'], writes=['d_bt'])
            for c in range(NT):
                cs = slice(c * 128, (c + 1) * 128)
                P.scan(gall[0:32, c, 0, :], ones[0:32, :], ga[0:32, cs], 0.0, reads=['d_ones', 'd_ga'], writes=['d_gall'])
                P.scan(gall[32:64, c, 0, :][:, ::-1], ones[32:64, :], ga[32:64, cs][:, ::-1], 0.0, reads=['d_ones', 'd_ga'], writes=['d_gall'])
            for c in range(NT):
                cs = slice(c * 128, (c + 1) * 128)
                P.tt(gall[:, c, 1, :], gall[:, c, 0, :], lb[:, cs], ALU.add, reads=['d_gall', 'd_lb'], writes=['d_gall'])
                P.ts(ngc[:, cs], gall[:, c, 0, :], -1.0, None, ALU.mult, reads=['d_gall'], writes=['d_ngc'], eng='gpsimd')
                P.ts(gall[0:32, c, 2, :], gall[0:32, c, 0, :], -1.0, gall[0:32, c, 0, 127:128], ALU.mult, ALU.add, reads=['d_gall'], writes=['d_gall'])
                P.ts(gall[32:64, c, 2, :], gall[32:64, c, 0, :], -1.0, gall[32:64, c, 0, 0:1], ALU.mult, ALU.add, reads=['d_gall'], writes=['d_gall'])
                pt = pst[c % 2]
                ptk = 'd_psB%d' % (c % 2)
                P.tr(pt[:, :], bt[:, cs], k.identf[:], reads=['d_bt', 'identf'], writes=[ptk])
                P.copy(bTM[:, c, :], pt[:, 0:64], reads=[ptk], writes=['d_bTM'], eng='scalar')
            P.flush()
        if k.dbg.get('dn_stop') == 'B':
            return
        with ExitStack() as es2:
            sb2 = lambda name, shape, dt=F32: es2.enter_context(_sbt(nc, name, list(shape), dt))
            CH = []
            for ch in range(4):
                B_ = {}
                t = 'd%d_' % ch
                B_['E'] = sb2(t + 'E', [128, 3, 128])
                B_['Qin'] = sb2(t + 'Qin', [128, 128], BF16)
                B_['KE2'] = sb2(t + 'KE2', [128, 128])
                B_['KE3'] = sb2(t + 'KE3', [128, 128], BF16)
                B_['RWpad'] = sb2(t + 'RWpad', [128, 2, 128])
                B_['RUpad'] = sb2(t + 'RUpad', [128, 2, 128])
                B_['Upad'] = sb2(t + 'Upad', [128, 2, 128], BF16)
                B_['koT'] = sb2(t + 'koT', [128, 128], BF16)
                B_['Gm'] = sb2(t + 'Gm', [128, 4, 128])
                B_['AT'] = sb2(t + 'AT', [128, 2, 128])
                B_['QKm'] = sb2(t + 'QKm', [128, 2, 128], BF16)
                B_['X'] = sb2(t + 'X', [128, 2, 2, 128])
                B_['Y'] = sb2(t + 'Y', [128, 2, 2, 128])
                B_['TT'] = sb2(t + 'TT', [128, 2, 128])
                B_['WTb'] = sb2(t + 'WTb', [128, 128])
                B_['Ub'] = sb2(t + 'Ub', [128, 128], BF16)
                B_['tmp'] = sb2(t + 'tmp', [128, 128])
                B_['S32'] = sb2(t + 'S32', [128, 128])
                B_['Sb'] = sb2(t + 'Sb', [128, 128], BF16)
                B_['Sn'] = sb2(t + 'Sn', [128, 128])
                B_['knm'] = sb2(t + 'knm', [128, 2, 128])
                B_['Rsel'] = sb2(t + 'Rsel', [128, 2, 2, 128])
                for nm in ['RWpad', 'RUpad', 'Upad']:
                    P.memset(B_[nm][:].rearrange("p a c -> p (a c)"), 0.0, writes=[t + nm], eng='gpsimd')
                for nm in ['S32', 'Sb', 'Sn']:
                    P.memset(B_[nm][:], 0.0, writes=[t + nm])
                CH.append(B_)
            bk = [es2.enter_context(_pst(nc, 'd_bank%d' % i, [128, 512], F32)) for i in range(7)]
            bA, bB, bC, bD, bE, bF, bG = bk
            psTb = es2.enter_context(_pst(nc, 'd_psTb', [128, 128], BF16))
            kk0, W0 = bA[:, 0:128], bA[:, 128:256]
            kk1, W1 = bB[:, 0:128], bB[:, 128:256]
            N0, T32, W2 = bC[:, 0:128], bC[:, 128:256], bC[:, 256:384]
            psS = bE[:, 256:384]
            N1, qk0, qk1 = bD[:, 0:128], bD[:, 128:256], bD[:, 256:384]
            N2 = bE[:, 0:128]
            N0b, N1b, N2b = bA[:, 256:384], bB[:, 256:384], bF[:, 384:512]
            psE = bF[:, 0:384]
            psD = bG[:, :]
            psKK = [kk0, kk1]
            psQK = [qk0, qk1]
            seen = set()
            for ci in range(NT):
                for d in range(2):
                    c = chunk_order(d)[ci]
                    cs = slice(c * 128, (c + 1) * 128)
                    rows = slice(32 * d, 32 * d + 4)
                    lloc = 127 if d == 0 else 0
                    for hp in range(2):
                        ch = d * 2 + hp
                        B_ = CH[ch]
                        t = 'd%d_' % ch
                        kk_ = lambda nm: t + nm
                        P.mm(psE, selp[rows, hp, :], gall[rows, c, :, :].rearrange("p a b -> p (a b)"), True, True,
                             reads=['d_selp', 'd_gall'], writes=['d_psE'])
                        P.act(B_['E'][:].rearrange("p a b -> p (a b)"), psE, AF.Exp, reads=['d_psE'], writes=[kk_('E')])
                        P.tt(B_['Qin'][:], qb[:, hp, cs], B_['E'][:, 0, :], ALU.mult, reads=['d_qb', kk_('E')], writes=[kk_('Qin')])
                        P.tt(B_['KE2'][:], kn[:, hp, cs], B_['E'][:, 1, :], ALU.mult, reads=['d_kn', kk_('E')], writes=[kk_('KE2')])
                        P.tt(B_['KE3'][:], kn[:, hp, cs], B_['E'][:, 2, :], ALU.mult, reads=['d_kn', kk_('E')], writes=[kk_('KE3')])
                        if k.dbg.get('dn_lvl', 99) < 1:
                            continue
                        P.tr(T32, B_['KE2'][:], k.identf[:], reads=[kk_('KE2'), 'identf'], writes=['d_psT32'])
                        for hh in range(2):
                            P.copy(B_['RWpad'][:, hh, hh * 64:(hh + 1) * 64], T32[:, hh * 64:(hh + 1) * 64], reads=['d_psT32'],
                                   writes=[kk_('RWpad')], eng='scalar' if hh else 'vector')
                        P.tr(psTb[:, :], B_['KE3'][:], k.identb[:], reads=[kk_('KE3'), 'identb'], writes=['d_psTb'])
                        P.copy(B_['koT'][:], psTb[:, :], reads=['d_psTb'], writes=[kk_('koT')], eng='scalar')
                        if k.dbg.get('dn_lvl', 99) < 2:
                            continue
                        for hh in range(2):
                            h = 2 * hp + hh
                            P.ts(B_['Rsel'][rows, hh, :, :], gall[rows, c, 0:2, :], mh[rows, h:h + 1], None, ALU.mult,
                                 reads=['d_gall', 'd_mh'], writes=[kk_('Rsel')])
                        P.mm(psD, ones4[rows, :], B_['Rsel'][rows, :, :, :].rearrange("p a b c -> p (a b c)"), True, False,
                             reads=['d_ones4', kk_('Rsel')], writes=['d_psD'])
                        P.mm(psD, ngc[rows, cs], oh[rows, hp, :], False, True, reads=['d_ngc', 'd_oh'], writes=['d_psD'])
                        P.tt(B_['Gm'][:].rearrange("p a b -> p (a b)"), psD, mb[:, d, :, :].rearrange("p a b -> p (a b)"), ALU.add,
                             reads=['d_psD', 'd_mb'], writes=[kk_('Gm')])
                        P.act(B_['Gm'][:], B_['Gm'][:], AF.Exp, reads=[kk_('Gm')], writes=[kk_('Gm')])
                        if k.dbg.get('dn_lvl', 99) < 3:
                            continue
                        for hh in range(2):
                            pr = slice(hh * 64, (hh + 1) * 64)
                            P.ts(B_['knm'][:, hh, :], kn[:, hp, cs], hm2[:, hh:hh + 1], None, ALU.mult, reads=['d_kn', 'd_hm2'], writes=[kk_('knm')],
                                 eng='gpsimd')
                            P.mm(psKK[hh], B_['knm'][:, hh, :], kn[:, hp, cs], True, True, reads=['d_kn', kk_('knm')], writes=['d_psKK%d' % hh])
                            P.mm(psQK[hh], kb[pr, hp, cs], qb[pr, hp, cs], True, True, reads=['d_kb', 'd_qb'], writes=['d_psQK%d' % hh])
                        for hh in range(2):
                            P.tt(B_['AT'][:, hh, :], psKK[hh], B_['Gm'][:, 2 * hh + 1, :], ALU.mult, reads=['d_psKK%d' % hh, kk_('Gm')], writes=[kk_('AT')])
                            P.tt(B_['QKm'][:, hh, :], psQK[hh], B_['Gm'][:, 2 * hh, :], ALU.mult, reads=['d_psQK%d' % hh, kk_('Gm')], writes=[kk_('QKm')])
                        if k.dbg.get('dn_lvl', 99) < 4:
                            continue
                        NB_ = [(N0, N1, N2), (N0b, N1b, N2b)]
                        Xc, Yc = [None, None], [None, None]
                        for hh in range(2):
                            n0, n1, n2 = NB_[hh]
                            X0 = B_['AT'][:, hh, :]
                            P.tr(n0, X0, k.identf[:], reads=[kk_('AT'), 'identf'], writes=['d_psN0_%d' % hh])
                            P.copy(B_['Y'][:, hh, 0, :], n0, reads=['d_psN0_%d' % hh], writes=[kk_('Y%d' % hh)], eng='scalar')
                            P.tt(B_['TT'][:, hh, :], k.identf[:], X0, ALU.subtract, reads=['identf', kk_('AT')], writes=[kk_('TT%d' % hh)])
                            Xc[hh], Yc[hh] = X0, B_['Y'][:, hh, 0, :]
                        for lv in range(1, 7):
                            for hh in range(2):
                                n0, n1, n2 = NB_[hh]
                                ks = [kk_('X%d' % hh), kk_('Y%d' % hh), kk_('AT')]
                                if lv < 6:
                                    P.mm(n0, Yc[hh], Xc[hh], True, True, reads=ks, writes=['d_psN0_%d' % hh])
                                P.mm(n1, Xc[hh], Yc[hh], True, True, reads=ks, writes=['d_psN1_%d' % hh])
                            for hh in range(2):
                                n0, n1, n2 = NB_[hh]
                                Yn = B_['Y'][:, hh, lv % 2, :]
                                if lv < 6:
                                    Xn = B_['X'][:, hh, lv % 2, :]
                                    P.copy(Xn, n0, reads=['d_psN0_%d' % hh], writes=[kk_('X%d' % hh)], eng='scalar')
                                    Xc[hh] = Xn
                                P.copy(Yn, n1, reads=['d_psN1_%d' % hh], writes=[kk_('Y%d' % hh)])
                                Yc[hh] = Yn
                            for hh in range(2):
                                n0, n1, n2 = NB_[hh]
                                P.mm(n2, Yc[hh], B_['TT'][:, hh, :], True, True, reads=[kk_('Y%d' % hh), kk_('TT%d' % hh)], writes=['d_psN2_%d' % hh])
                            for hh in range(2):
                                n0, n1, n2 = NB_[hh]
                                P.tt(B_['TT'][:, hh, :], B_['TT'][:, hh, :], n2, ALU.add, reads=['d_psN2_%d' % hh, kk_('TT%d' % hh)],
                                     writes=[kk_('TT%d' % hh)], eng='vector')
                        if k.dbg.get('dn_lvl', 99) < 5:
                            continue
                        for hh in range(2):
                            P.mm(W0, B_['RWpad'][:, hh, :], B_['TT'][:, hh, :], hh == 0, hh == 1, reads=[kk_('RWpad'), kk_('TT0'), kk_('TT1')], writes=['d_psW0'])
                        if k.dbg.get('dn_sub', 9) < 1:
                            continue
                        P.copy(B_['WTb'][:], W0, reads=['d_psW0'], writes=[kk_('WTb')], eng='scalar')
                        for hh in range(2):
                            h = 2 * hp + hh
                            P.ts(B_['RUpad'][:, hh, hh * 64:(hh + 1) * 64], vTM[:, c, hp * 128 + hh * 64:hp * 128 + (hh + 1) * 64],
                                 bTM[:, c, 32 * d + h:32 * d + h + 1], None, ALU.mult, reads=['d_vTM', 'd_bTM'], writes=[kk_('RUpad')], eng='gpsimd')
                        if k.dbg.get('dn_sub', 9) < 2:
                            continue
                        P.mm(W1, B_['TT'][:, 0, :], B_['RUpad'][:, 0, :], True, False, reads=[kk_('TT0'), kk_('RUpad')], writes=['d_psW1'])
                        P.mm(W1, B_['TT'][:, 1, :], B_['RUpad'][:, 1, :], False, False, reads=[kk_('TT1'), kk_('RUpad')], writes=['d_psW1'])
                        P.mm(W1, B_['WTb'][:], B_['Sn'][:], False, True, reads=[kk_('WTb'), kk_('Sn')], writes=['d_psW1'])
                        if k.dbg.get('dn_sub', 9) < 3:
                            continue
                        P.ts(B_['Ub'][:], W1, 1.0, None, ALU.mult, reads=['d_psW1'], writes=[kk_('Ub')])
                        for hh in range(2):
                            P.ts(B_['Upad'][:, hh, hh * 64:(hh + 1) * 64], W1[:, hh * 64:(hh + 1) * 64], 1.0, None, ALU.mult, reads=['d_psW1'], writes=[kk_('Upad')])
                        if k.dbg.get('dn_lvl', 99) < 6:
                            continue
                        P.mm(W2, B_['Upad'][:, 0, :], B_['QKm'][:, 0, :], True, False, reads=[kk_('Upad'), kk_('QKm')], writes=['d_psW2'])
                        P.mm(W2, B_['Upad'][:, 1, :], B_['QKm'][:, 1, :], False, False, reads=[kk_('Upad'), kk_('QKm')], writes=['d_psW2'])
                        P.mm(W2, B_['Sb'][:], B_['Qin'][:], False, True, reads=[kk_('Sb'), kk_('Qin')], writes=['d_psW2'])
                        ok = ('d_oacc', hp, c)
                        if ok not in seen:
                            seen.add(ok)
                            P.copy(oacc[:, hp, cs], W2, reads=['d_psW2'], writes=[ok], eng='scalar')
                        else:
                            P.tt(oacc[:, hp, cs], oacc[:, hp, cs], W2, ALU.add, reads=['d_psW2', ok], writes=[ok])
                        if k.dbg.get('dn_lvl', 99) < 7:
                            continue
                        P.mm(psS, B_['koT'][:], B_['Ub'][:], True, True, reads=[kk_('koT'), kk_('Ub')], writes=['d_psS'])
                        if k.dbg.get('dn_sub8', 9) < 1:
                            continue
                        P.tt(B_['tmp'][:], psS, blk2[:], ALU.mult, reads=['d_psS', 'd_blk2'], writes=[kk_('tmp')])
                        if k.dbg.get('dn_sub8', 9) < 2:
                            continue
                        P.stt(B_['S32'][:], B_['S32'][:], B_['E'][:, 0, lloc:lloc + 1], B_['tmp'][:], ALU.mult, ALU.add,
                              reads=[kk_('S32'), kk_('E'), kk_('tmp')], writes=[kk_('S32')])
                        if k.dbg.get('dn_sub8', 9) < 3:
                            continue
                        P.copy(B_['Sb'][:], B_['S32'][:], reads=[kk_('S32')], writes=[kk_('Sb')], eng='scalar')
                        if k.dbg.get('dn_sub8', 9) < 4:
                            continue
                        P.ts(B_['Sn'][:], B_['S32'][:], -1.0, None, ALU.mult, reads=[kk_('S32')], writes=[kk_('Sn')])
            P.flush()
        for hp in range(2):
            with ExitStack() as es2:
                norm_gate(k, P, nc, es2, b, oacc[:, hp, :], [('d_oacc', hp, c) for c in range(NT)], 24 + hp, 768 + hp * 128, False, 'd%d' % hp)
                P.flush()


def phase_outproj_moe(k, l, b, last):
    P, nc = k.P, k.nc
    t_first = 2 if last else 0
    with ExitStack() as es:
        sbt = lambda name, shape, dt=F32: es.enter_context(_sbt(nc, name, list(shape), dt))
        h2T = sbt('h2T', [128, 8, T], BF16)
        comb = sbt('comb', [128, NT, NE])
        k.eps_col = sbt('eps_col', [128, 1])
        rw = sbt('rw', [128, 8, NE])
        rb = sbt('rb', [128, NE])
        fg = sbt('fg', [128, D])
        P.memset(k.eps_col[:], EPS, writes=['eps'])
        P.dma(rw[:], k.router_w[:, :, :], writes=['rw'])
        P.dma(rb[:], k.router_b_rep[:, :], writes=['rb'])
        P.dma(fg[:], k.final_g_rep[:, :], writes=['fg'])
        ps = [es.enter_context(_pst(nc, 'ps%d' % i, [128, 512], F32)) for i in range(8)]
        with ExitStack() as es2:
            sb2 = lambda name, shape, dt=F32: es2.enter_context(_sbt(nc, name, list(shape), dt))
            mixT = sb2('mixT', [128, 8, T], BF16)
            wo = sb2('wo', [128, 8, D], BF16)
            grep_ = sb2('grep', [128, 2, D])
            xt = [sb2('xt%d' % i, [128, D]) for i in range(2)]
            xn = [sb2('xn%d' % i, [128, D]) for i in range(2)]
            h32 = [sb2('h32_%d' % i, [128, 8, 128]) for i in range(2)]
            ssq = [sb2('ssq%d' % i, [128, 1]) for i in range(2)]
            rstd = [sb2('rstd%d' % i, [128, 1]) for i in range(2)]
            sc = [sb2('rsc%d' % i, [128, 8, NE]) for i in range(2)]
            for kc in range(8):
                P.dma(wo[:, kc, :], k.w_out[l, :, kc, :], writes=['wo'], eng='gpsimd')
                P.dma(mixT[:, kc, :], k.mixFM[b, kc * 128:(kc + 1) * 128, :], writes=['mixT'], eng='gpsimd')
            for gi, col in enumerate([b, 2]):
                P.dma(grep_[:, gi, :], k.gateD[l, 0, col, :].partition_broadcast(128), writes=['grep'])
            for tt in range(t_first, NT):
                i = tt % 2
                tag = str(i)
                gi = 1 if tt < 2 else 0
                col = 2 if tt < 2 else b
                P.dma(xt[i][:], resid_src(k, l, b, tt), writes=['xt' + tag])
                for half in range(2):
                    pt = ps[4 + half]
                    pk = 'ps%d' % (4 + half)
                    for kc in range(8):
                        P.mm(pt[:, :], mixT[:, kc, tt * 128:(tt + 1) * 128], wo[:, kc, half * 512:(half + 1) * 512],
                             kc == 0, kc == 7, reads=['mixT', 'wo'], writes=[pk])
                    P.tt(xn[i][:, half * 512:(half + 1) * 512], pt[:, :], grep_[:, gi, half * 512:(half + 1) * 512], ALU.mult,
                         reads=[pk, 'grep'], writes=['xn' + tag])
                P.tt(xt[i][:], xt[i][:], xn[i][:], ALU.add, reads=['xt' + tag, 'xn' + tag], writes=['xt' + tag])
                P.dma(resid_dst(k, b, tt), xt[i][:], reads=['xt' + tag])
                if 'x_mid' in k.dbg_out and l == k.dbg.get('l', 0) and b == 0:
                    P.dma(k.dbg_out['x_mid'][tt * 128:(tt + 1) * 128, :], xt[i][:], reads=['xt' + tag])
                norm_modulate_tile(k, P, xt[i][:], 'xt' + tag, l, 1, col, ssq[i], rstd[i], xn[i],
                                   [ps[2 * i], ps[2 * i + 1]], ['ps%d' % (2 * i), 'ps%d' % (2 * i + 1)],
                                   [h2T[:, fc, tt * 128:(tt + 1) * 128] for fc in range(8)],
                                   [('h2T', tt)] * 8, tag, h32=None if k.dbg.get('norouter') else h32[i], h32key='h32_' + tag)
                pr = ps[6 + i]
                prk = 'ps%d' % (6 + i)
                for kc in range(0 if k.dbg.get('norouter') else 8):
                    P.mm(pr[:, 0:NE], h32[i][:, kc, :], rw[:, kc, :], kc == 0, kc == 7,
                         reads=['h32_' + tag, 'rw'], writes=[prk])
                if not k.dbg.get('norouter'):
                    router_tile(k, P, pr[:, 0:NE], prk, rb, sc[i], 'rsc' + tag, comb[:, tt, :], ('comb', tt))
            P.flush()
        if k.dbg.get('stopA'):
            return
        with ExitStack() as es2:
            sb2 = lambda name, shape, dt=F32: es2.enter_context(_sbt(nc, name, list(shape), dt))
            facc = sb2('facc', [128, NT, D])
            for tt in range(NT):
                P.memset(facc[:, tt, :], 0.0, writes=[('facc', tt, 0), ('facc', tt, 1)], eng='gpsimd')
            blks = [(t0, tn) for (t0, tn) in TBLK]
            if last:
                blks = [(256, 512), (768, 512), (1280, 512), (1792, 512)]
            with ExitStack() as es3:
                sb3 = lambda name, shape, dt=F32: es3.enter_context(_sbt(nc, name, list(shape), dt))
                wg = [sb3('wg%d' % i, [128, 8, DFF], BF16) for i in range(2)]
                wu = [sb3('wu%d' % i, [128, 8, DFF], BF16) for i in range(2)]
                wd = [sb3('wd%d' % i, [128, 4, D], BF16) for i in range(2)]
                actT = [sb3('actT%d' % i, [128, 4, 512], BF16) for i in range(2)]
                sg = [sb3('sg%d' % i, [128, 512]) for i in range(2)]
                cnt = 0
                for e in range(NE):
                    i = e % 2
                    si = str(i)
                    for kc in range(8):
                        P.dma(wg[i][:, kc, :], k.moe_wg[l, e, :, kc, :], writes=['wg' + si], eng='gpsimd')
                        P.dma(wu[i][:, kc, :], k.moe_wu[l, e, :, kc, :], writes=['wu' + si], eng='gpsimd')
                    for fc in range(4):
                        P.dma(wd[i][:, fc, :], k.moe_wd[l, e, :, fc, :], writes=['wd' + si], eng='gpsimd')
                    for (t0, tn) in blks:
                        a = actT[cnt % 2]
                        ak = 'actT%d' % (cnt % 2)
                        cnt += 1
                        hk = [('h2T', t0 // 128 + j) for j in range(tn // 128)]
                        for fc in range(4):
                            pg = ps[fc % 2]
                            pu = ps[2 + fc % 2]
                            pgk, puk = 'ps%d' % (fc % 2), 'ps%d' % (2 + fc % 2)
                            s_ = sg[fc % 2]
                            sk = 'sg%d' % (fc % 2)
                            for kc in range(8):
                                P.mm(pg[:, :tn], wg[i][:, kc, fc * 128:(fc + 1) * 128], h2T[:, kc, t0:t0 + tn], kc == 0, kc == 7,
                                     reads=['wg' + si] + hk, writes=[pgk])
                            for kc in range(8):
                                P.mm(pu[:, :tn], wu[i][:, kc, fc * 128:(fc + 1) * 128], h2T[:, kc, t0:t0 + tn], kc == 0, kc == 7,
                                     reads=['wu' + si] + hk, writes=[puk])
                            P.act(s_[:, :tn], pg[:, :tn], AF.Silu, reads=[pgk], writes=[sk])
                            P.tt(a[:, fc, :tn], s_[:, :tn], pu[:, :tn], ALU.mult, reads=[sk, puk], writes=[ak])
                        for j in range(tn // 128):
                            tt = t0 // 128 + j
                            for half in range(2):
                                pd = ps[4 + (2 * j + half) % 4]
                                pdk = 'ps%d' % (4 + (2 * j + half) % 4)
                                for fc in range(4):
                                    P.mm(pd[:, :], a[:, fc, j * 128:(j + 1) * 128], wd[i][:, fc, half * 512:(half + 1) * 512],
                                         fc == 0, fc == 3, reads=[ak, 'wd' + si], writes=[pdk])
                                fs = facc[:, tt, half * 512:(half + 1) * 512]
                                P.stt(fs, pd[:, :], comb[:, tt, e:e + 1], fs, ALU.mult, ALU.add,
                                      reads=[pdk, ('comb', tt), ('facc', tt, half)], writes=[('facc', tt, half)])
                P.flush()
            grep2 = sb2('grep2', [128, 2, D])
            xm = [sb2('xm%d' % i, [128, D]) for i in range(2)]
            ssq = sb2('ssqf', [128, 1])
            rstd = sb2('rstdf', [128, 1])
            junk = sb2('junkf', [128, D])
            for gi, col in enumerate([b, 2]):
                P.dma(grep2[:, gi, :], k.gateD[l, 1, col, :].partition_broadcast(128), writes=['grep2'])
            for tt in range(t_first, NT):
                gi = 1 if tt < 2 else 0
                i = tt % 2
                xk = 'xm%d' % i
                fk = [('facc', tt, 0), ('facc', tt, 1)]
                P.dma(xm[i][:], resid_dst(k, b, tt), writes=[xk])
                P.tt(facc[:, tt, :], facc[:, tt, :], grep2[:, gi, :], ALU.mult, reads=fk + ['grep2'], writes=fk)
                P.tt(xm[i][:], xm[i][:], facc[:, tt, :], ALU.add, reads=fk + [xk], writes=[xk])
                if 'x_end' in k.dbg_out and l == k.dbg.get('l', 0) and b == 0:
                    P.dma(k.dbg_out['x_end'][tt * 128:(tt + 1) * 128, :], xm[i][:], reads=[xk])
                if 'f_out' in k.dbg_out and l == k.dbg.get('l', 0) and b == 0:
                    P.dma(k.dbg_out['f_out'][tt * 128:(tt + 1) * 128, :], facc[:, tt, :], reads=fk)
                if not last:
                    P.dma(resid_dst(k, b, tt), xm[i][:], reads=[xk])
                else:
                    P.op('scalar', lambda s, i=i: s.activation(out=junk[:], in_=xm[i][:], func=AF.Square, accum_out=ssq[:]),
                         reads=[xk], writes=['junkf', 'ssqf'])
                    P.act(rstd[:], ssq[:], AF.Sqrt, reads=['ssqf'], writes=['rstdf'], scale=1.0 / D, bias=k.eps_col[:])
                    P.recip(rstd[:], rstd[:], reads=['rstdf'], writes=['rstdf'])
                    P.stt(xm[i][:], xm[i][:], rstd[:, 0:1], fg[:], ALU.mult, ALU.mult,
                          reads=[xk, 'rstdf', 'fg'], writes=[xk])
                    P.dma(k.out[b, (tt - 2) * 128:(tt - 1) * 128, :], xm[i][:], reads=[xk])
            P.flush()


def router_tile(k, P, logits, lk, rb, sc, sck, comb_out, ck):
    BIG = 1.0e4
    s = sc[:, 0, :]
    sel = sc[:, 1, :]
    t1 = sc[:, 2, :]
    t2 = sc[:, 3, :]
    m1 = sc[:, 4, 0:4]
    m2 = sc[:, 4, 4:8]
    gs = sc[:, 4, 8:12]
    gm = sc[:, 4, 12:13]
    ing = sc[:, 5, 0:4]
    e1 = sc[:, 6, :]
    mx = sc[:, 5, 4:5]
    mx2 = sc[:, 5, 5:6]
    ws = sc[:, 5, 6:7]
    R = dict(reads=[sck], writes=[sck])
    P.act(s, logits, AF.Sigmoid, reads=[lk], writes=[sck])
    P.tt(sel, s, rb[:, :], ALU.add, reads=[sck, 'rb'], writes=[sck])
    sel3 = sc[:, 1, :].rearrange("p (g e) -> p g e", g=4)
    t13 = sc[:, 2, :].rearrange("p (g e) -> p g e", g=4)
    P.red(m1, sel3, ALU.max, **R)
    P.tt(t13, sel3, m1.unsqueeze(2).to_broadcast([128, 4, 4]), ALU.is_equal, **R)
    P.stt(t1, t1, -BIG, sel, ALU.mult, ALU.add, **R)
    P.red(m2, t13, ALU.max, **R)
    P.tt(gs, m1, m2, ALU.add, **R)
    P.red(gm, gs, ALU.max, **R)
    P.ts(ing, gs, gm, None, ALU.is_equal, **R)
    P.ts(ing, ing, -1.0, BIG, ALU.add, ALU.mult, **R)
    P.tt(t13, sel3, ing.unsqueeze(2).to_broadcast([128, 4, 4]), ALU.add, **R)
    P.red(mx, t1, ALU.max, **R)
    P.ts(e1, t1, mx, None, ALU.is_equal, **R)
    P.stt(t2, e1, -BIG, t1, ALU.mult, ALU.add, **R)
    P.red(mx2, t2, ALU.max, **R)
    P.ts(t2, t2, mx2, None, ALU.is_equal, **R)
    P.tt(e1, e1, t2, ALU.add, **R)
    P.tt(e1, e1, s, ALU.mult, **R)
    P.red(ws, e1, ALU.add, **R)
    P.recip(ws, ws, **R)
    P.ts(comb_out, e1, ws, None, ALU.mult, reads=[sck], writes=[ck])


def phase_dbg(k):
    P, nc = k.P, k.nc
    outs = k.dbg_out
    if not outs:
        return
    with ExitStack() as es:
        t = es.enter_context(_sbt(nc, 'dbgt', [128, T], F32))
        if 'pFM' in outs:
            for cb in range(NFM // 128):
                P.dma(t[:], k.pFM[0, cb * 128:(cb + 1) * 128, :], writes=['dbgt'])
                P.dma(outs['pFM'][cb * 128:(cb + 1) * 128, :], t[:], reads=['dbgt'])
        if 'pTM' in outs:
            for tt in range(NT):
                P.dma(t[:, :NTM], k.pTM[0, tt * 128:(tt + 1) * 128, :], writes=['dbgt'])
                P.dma(outs['pTM'][tt * 128:(tt + 1) * 128, :], t[:, :NTM], reads=['dbgt'])
        if 'mixFM' in outs:
            for cb in range(8):
                P.dma(t[:], k.mixFM[0, cb * 128:(cb + 1) * 128, :], writes=['dbgt'])
                P.dma(outs['mixFM'][cb * 128:(cb + 1) * 128, :], t[:], reads=['dbgt'])
        if 'modFM' in outs:
            P.dma(outs['modFM'][:, :], k.modFM[:].rearrange("p a b c -> p (a b c)"), reads=['modFM'])
        P.flush()


_CACHE = {}


def kernel(**inputs):
    inp = {kk: np.asarray(v) for kk, v in inputs.items()}
    sh = host_prep(inp)
    if 'nc' not in _CACHE:
        _CACHE['nc'] = build()
    nc = _CACHE['nc']
    in_maps = []
    for c in range(8):
        m = dict(sh)
        m.update(core_inputs(inp, c))
        in_maps.append(m)
    res = run_bass_kernel_spmd(nc, in_maps, core_ids=list(range(8)))
    out = np.concatenate([r['out'] for r in res.results], axis=0)
    return out.astype(np.float32)
```

```python
import math
import numpy as np
from contextlib import ExitStack
import concourse.bass as bass
import concourse.mybir as mybir
from concourse.bass_utils import run_bass_kernel_spmd

F32 = mybir.dt.float32
BF16 = mybir.dt.bfloat16
I32 = mybir.dt.int32
AF = mybir.ActivationFunctionType
ALU = mybir.AluOpType
AX = mybir.AxisListType

D = 1024
L = 2048
LC = 256
T = L + LC
NT = T // 128
DEPTH = 2
NB = 2
NE = 16
DFF = 512
EPS = 1e-6
NFM = 27 * 128
NTM = 512
TBLK = [(0, 512), (512, 512), (1024, 512), (1536, 512), (2048, 256)]

ENGS = ['tensor', 'vector', 'scalar', 'gpsimd', 'sync']
SAME_ENGINE_WAIT = True
N_DMA_SEMS = 24


_UID = [0]


def _sbt(nc, name, shape, dt):
    _UID[0] += 1
    return nc.sbuf_tensor("%s_%d" % (name, _UID[0]), shape, dt)


def _pst(nc, name, shape, dt):
    _UID[0] += 1
    return nc.psum_tensor("%s_%d" % (name, _UID[0]), shape, dt)


class Prog:
    def __init__(self, nc, es):
        self.nc = nc
        self.sem = {e: es.enter_context(nc.semaphore("s_" + e)) for e in ENGS}
        self.cnt = {e: 0 for e in ENGS}
        self.dsem = [es.enter_context(nc.semaphore("d_%d" % i)) for i in range(N_DMA_SEMS)]
        self.dval = [0] * N_DMA_SEMS
        self.drr = 0
        self.semobj = {}
        for e in ENGS:
            self.semobj[('e', e)] = self.sem[e]
        for i in range(N_DMA_SEMS):
            self.semobj[('d', i)] = self.dsem[i]
        self.nops = 0
        self.begin()

    def begin(self):
        self.ops = {e: [] for e in ENGS}
        self.known = {e: {} for e in ENGS}
        self.lastw = {}
        self.readers = {}
        self.pending_dma = {e: [] for e in ENGS}

    def op(self, eng, fn, reads=(), writes=(), dma=False):
        deps = set()
        for k in reads:
            t = self.lastw.get(k)
            if t is not None:
                deps.add(t)
        for k in writes:
            t = self.lastw.get(k)
            if t is not None:
                deps.add(t)
            for t in self.readers.get(k, ()):
                deps.add(t)
        waits = []
        if dma:
            idx = self.drr % N_DMA_SEMS
            self.drr += 1
            if self.dval[idx] > 0:
                deps.add((('d', idx), self.dval[idx]))
            self.dval[idx] += 16
            tok = (('d', idx), self.dval[idx])
            inc = (('d', idx), 16)
            self.pending_dma[eng].append(tok)
        else:
            self.cnt[eng] += 1
            tok = (('e', eng), self.cnt[eng])
            inc = (('e', eng), 1)
        kn = self.known[eng]
        best = {}
        for (s, v) in deps:
            if s == ('e', eng) and not dma:
                if eng == 'tensor' or not SAME_ENGINE_WAIT:
                    continue
            if kn.get(s, 0) >= v:
                continue
            if best.get(s, 0) < v:
                best[s] = v
        for s, v in best.items():
            kn[s] = v
            waits.append((s, v))
        self.ops[eng].append((waits, fn, inc))
        self.nops += 1
        for k in reads:
            self.readers.setdefault(k, []).append(tok)
        for k in writes:
            self.lastw[k] = tok
            self.readers[k] = []
        return tok

    def flush(self):
        nc = self.nc
        tails = {}
        for e in ENGS:
            seen = {}
            for (s, v) in self.pending_dma[e]:
                if seen.get(s, 0) < v:
                    seen[s] = v
            tails[e] = [(s, v) for s, v in seen.items() if self.known[e].get(s, 0) < v]
        ops = self.ops
        semobj = self.semobj
        with nc.Block() as block:
            for e in ENGS:
                if not ops[e] and not tails[e]:
                    continue

                def body(eng, e=e):
                    for (waits, fn, inc) in ops[e]:
                        for (s, v) in waits:
                            eng.wait_ge(semobj[s], v)
                        ins = fn(eng)
                        ins.then_inc(semobj[inc[0]], inc[1])
                    for (s, v) in tails[e]:
                        eng.wait_ge(semobj[s], v)
                getattr(block, e)(body)
        self.begin()

    def dma(self, out, in_, reads=(), writes=(), eng='sync'):
        return self.op(eng, lambda g: g.dma_start(out=out, in_=in_), reads, writes, dma=True)

    def mm(self, out, lhsT, rhs, start, stop, reads=(), writes=()):
        return self.op('tensor', lambda t: t.matmul(out, lhsT=lhsT, rhs=rhs, start=start, stop=stop), reads, writes)

    def tr(self, out, in_, ident, reads=(), writes=()):
        return self.op('tensor', lambda t: t.transpose(out, in_, ident), reads, writes)

    def act(self, out, in_, func, reads=(), writes=(), **kw):
        return self.op('scalar', lambda s: s.activation(out=out, in_=in_, func=func, **kw), reads, writes)

    def ts(self, out, in0, s1, s2, op0, op1=None, reads=(), writes=(), eng='vector', **kw):
        if op1 is None:
            return self.op(eng, lambda v: v.tensor_scalar(out=out, in0=in0, scalar1=s1, scalar2=None, op0=op0, **kw), reads, writes)
        return self.op(eng, lambda v: v.tensor_scalar(out=out, in0=in0, scalar1=s1, scalar2=s2, op0=op0, op1=op1, **kw), reads, writes)

    def tt(self, out, in0, in1, op, reads=(), writes=(), eng='vector'):
        return self.op(eng, lambda v: v.tensor_tensor(out=out, in0=in0, in1=in1, op=op), reads, writes)

    def stt(self, out, in0, scalar, in1, op0, op1, reads=(), writes=()):
        return self.op('vector', lambda v: v.scalar_tensor_tensor(out=out, in0=in0, scalar=scalar, in1=in1, op0=op0, op1=op1), reads, writes)

    def copy(self, out, in_, reads=(), writes=(), eng='vector'):
        if eng == 'scalar':
            return self.op(eng, lambda s: s.copy(out=out, in_=in_), reads, writes)
        return self.op(eng, lambda v: v.tensor_copy(out=out, in_=in_), reads, writes)

    def memset(self, out, val, writes=(), eng='vector'):
        return self.op(eng, lambda v: v.memset(out, val), (), writes)

    def recip(self, out, in_, reads=(), writes=()):
        return self.op('vector', lambda v: v.reciprocal(out=out, in_=in_), reads, writes)

    def red(self, out, in_, op, reads=(), writes=()):
        return self.op('vector', lambda v: v.tensor_reduce(out=out, in_=in_, axis=AX.X, op=op), reads, writes)

    def scan(self, out, d0, d1, init, reads=(), writes=()):
        return self.op('vector', lambda v: v.tensor_tensor_scan(out=out, data0=d0, data1=d1, initial=init, op0=ALU.mult, op1=ALU.add), reads, writes)


IN_OFF = {}
_o = 0
for _n, _s in [('gq', 128), ('gk', 128), ('gv', 256), ('gg', 256), ('gzf', 16), ('gzb', 16),
               ('rq', 256), ('rk', 256), ('rv', 256), ('rg', 256), ('su', 256),
               ('dq', 256), ('dk', 256), ('dv', 256), ('dg', 256), ('da', 8), ('db', 8)]:
    IN_OFF[_n] = _o
    _o += _s


def _rope_perm(off):
    main, sw = [], []
    for h in range(4):
        base = off + h * 64
        ev = [base + 2 * i for i in range(32)]
        od = [base + 2 * i + 1 for i in range(32)]
        main += ev + od
        sw += od + ev
    return main, sw


def _fm_cols():
    c = []
    rng = lambda n, k: list(range(IN_OFF[n], IN_OFF[n] + k))
    pad = lambda k: [-1] * k
    c += rng('gq', 128)
    c += rng('gk', 128)
    c += rng('gzf', 16) + pad(16) + rng('gzb', 16) + pad(80)
    m, s = _rope_perm(IN_OFF['rq'])
    c += m + s
    m, s = _rope_perm(IN_OFF['rk'])
    c += m + s
    c += rng('su', 256)
    c += rng('dq', 256) + rng('dk', 256) + rng('dv', 256)
    da, db = rng('da', 8), rng('db', 8)
    c += da[0:4] + pad(28) + da[4:8] + pad(92)
    c += rng('gg', 256) + rng('rg', 256) + rng('dg', 256)
    c += db[0:4] + pad(28) + db[4:8] + pad(92)
    assert len(c) == NFM
    return np.array(c)


def _tm_cols():
    c = []
    for n in ['gv', 'rv']:
        c += list(range(IN_OFF[n], IN_OFF[n] + 256))
    return np.array(c)


def _kmajor(w):
    K, N = w.shape
    return np.ascontiguousarray(w.reshape(K // 128, 128, N).transpose(1, 0, 2))


def _fmvec(v):
    return np.ascontiguousarray(v.reshape(-1, 128).T)


def host_prep(inp):
    f = np.float32
    sh = {}
    sh['ident'] = np.eye(128, dtype=f)
    w_ada = inp['w_ada']
    sh['w_ada'] = np.stack([_kmajor(w_ada[i]) for i in range(DEPTH)])
    sh['b_adaFM'] = np.stack([_fmvec(inp['b_ada'][i]) for i in range(DEPTH)], 1)
    sh['b_ada4'] = np.ascontiguousarray(np.broadcast_to(inp['b_ada'][None], (4, DEPTH, 6 * D)))
    ng = inp['norm_g']
    sh['norm_gFM'] = np.ascontiguousarray(
        np.stack([np.stack([_fmvec(ng[i, n]) for n in range(2)], 1) for i in range(DEPTH)], 1))
    sh['final_g_rep'] = np.ascontiguousarray(np.broadcast_to(inp['final_norm_g'][None], (128, D)))
    fm = _fm_cols()
    tm = _tm_cols()
    w_in = inp['w_in']
    wfm = np.zeros((DEPTH, D, NFM), f)
    wfm[:, :, fm >= 0] = w_in[:, :, fm[fm >= 0]]
    sh['w_inFM'] = np.stack([_kmajor(wfm[i]) for i in range(DEPTH)])
    sh['w_inTM'] = np.stack([_kmajor(w_in[i][:, tm]) for i in range(DEPTH)])
    sh['w_out'] = np.stack([_kmajor(inp['w_out'][i]) for i in range(DEPTH)])
    sh['router_w'] = _kmajor(inp['router_w'])
    sh['router_b_rep'] = np.ascontiguousarray(np.broadcast_to(inp['router_b'][None], (128, NE)))
    sh['moe_wg'] = np.ascontiguousarray(
        inp['moe_w_gate'].reshape(DEPTH, NE, 8, 128, DFF).transpose(0, 1, 3, 2, 4))
    sh['moe_wu'] = np.ascontiguousarray(
        inp['moe_w_up'].reshape(DEPTH, NE, 8, 128, DFF).transpose(0, 1, 3, 2, 4))
    sh['moe_wd'] = np.ascontiguousarray(
        inp['moe_w_down'].reshape(DEPTH, NE, 4, 128, D).transpose(0, 1, 3, 2, 4))
    jj = np.arange(128, dtype=f)
    diff = jj[None, :] - jj[:, None]
    sh['DIFFf'] = np.where(diff >= 0, diff, 1e6).astype(f)
    sh['DIFFb'] = np.where(diff <= 0, -diff, 1e6).astype(f)
    sh['MSKf'] = (diff >= 0).astype(f)
    sh['MSKb'] = (diff <= 0).astype(f)
    sh['POS'] = np.ascontiguousarray(np.stack([np.broadcast_to(jj + 1, (128, 128)), np.broadcast_to(128 - jj, (128, 128)),
                          np.broadcast_to(127 - jj, (128, 128)), np.broadcast_to(jj, (128, 128))], 1)).astype(f)
    blk2 = np.kron(np.eye(2, dtype=f), np.ones((64, 64), f))
    sh['BLK2'] = blk2
    sh['BLK64'] = blk2 / 64.0
    n_rows = L // 64
    pos = np.arange(n_rows * 64)
    rows = (pos // 64).astype(f)
    cols = (pos % 64).astype(f)
    nf = 16
    inv = (np.float32(10000.0) ** (-np.arange(nf, dtype=f) / nf)).astype(f)
    ang = np.concatenate([rows[:, None] * inv, cols[:, None] * inv], -1).astype(f)
    cs, sn = np.cos(ang).T.astype(f), np.sin(ang).T.astype(f)
    sh['COS'] = np.ascontiguousarray(np.concatenate([cs, cs, cs, cs], 0))
    sh['SIN'] = np.ascontiguousarray(np.concatenate([-sn, sn, -sn, sn], 0))
    sh['ret_logit_rep'] = np.ascontiguousarray(np.broadcast_to(inp['ret_decay_logit'].reshape(DEPTH, 1, 8), (DEPTH, 128, 8)))
    hm = np.zeros((128, 4), f)
    for h in range(4):
        hm[h * 32:(h + 1) * 32, h] = 1.0
    sh['HM4'] = hm
    sh['BLKG'] = np.kron(np.eye(4, dtype=f), np.ones((32, 64), f))
    gkw = np.zeros((DEPTH, 128, 128), f)
    gkw[:, 0:16, :] = inp['gla_gk_w'][:, 0]
    gkw[:, 32:48, :] = inp['gla_gk_w'][:, 1]
    sh['gkw'] = gkw
    sh['gkbFM'] = np.ascontiguousarray(inp['gla_gk_b'].transpose(2, 0, 1))
    def s5_state_layout(a):
        a = a.reshape(DEPTH, 2, 8, 2, 64)
        return np.ascontiguousarray(a.transpose(3, 4, 0, 1, 2).reshape(128, DEPTH, 16))
    sh['s5_lre'] = s5_state_layout(inp['s5_lambda_re'])
    sh['s5_lim'] = s5_state_layout(inp['s5_lambda_im'])
    sh['s5_ldt'] = s5_state_layout(np.broadcast_to(inp['s5_log_dt'][..., None], (DEPTH, 2, 16, 64)))
    WB = np.zeros((DEPTH, 128, 8, 2, 128), f)
    WC = np.zeros((DEPTH, 128, 8, 2, 128), f)
    for g in range(16):
        j, gg = g // 2, g % 2
        r0 = (g % 8) * 16
        for ri, (bsrc, csrc) in enumerate([('s5_b_re', 's5_c_re'), ('s5_b_im', 's5_c_im')]):
            WB[:, r0:r0 + 16, j, ri, gg * 64:(gg + 1) * 64] = inp[bsrc][:, g].transpose(0, 2, 1)
            WC[:, gg * 64:(gg + 1) * 64, j, ri, r0:r0 + 16] = inp[csrc][:, g].transpose(0, 2, 1)
    sh['s5_WB'] = WB
    sh['s5_WC'] = WC
    sh['s5_dFM'] = np.ascontiguousarray(inp['s5_d'].reshape(DEPTH, 2, 128).transpose(2, 0, 1))
    sh['s5_gbFM'] = np.ascontiguousarray(inp['s5_glu_b'].reshape(DEPTH, 2, 128).transpose(2, 0, 1))
    sh['s5_gw'] = np.ascontiguousarray(inp['s5_glu_w'].reshape(DEPTH, 2, 128, 256).transpose(0, 2, 1, 3))
    sh['convFM'] = np.ascontiguousarray(inp['dn_conv_w'].reshape(DEPTH, 5, 6, 128).transpose(3, 0, 2, 1))
    par = np.zeros((128, DEPTH, 2), f)
    for d_ in range(2):
        par[32 * d_:32 * d_ + 4, :, 0] = inp['dn_a_log'][:, d_, :].T
        par[32 * d_:32 * d_ + 4, :, 1] = inp['dn_dt_bias'][:, d_, :].T
    sh['dn_par'] = par
    selr = np.zeros((128, 4, 128), f)
    selp = np.zeros((128, 2, 128), f)
    for d_ in range(2):
        for h in range(4):
            selr[32 * d_ + h, h, :] = 1.0
            selp[32 * d_ + h, h // 2, (h % 2) * 64:(h % 2 + 1) * 64] = 1.0
    sh['SELR'] = selr
    mhh = np.zeros((128, 4), f)
    ohh = np.zeros((128, 2, 2, 2, 128), f)
    for d_ in range(2):
        for h in range(4):
            mhh[32 * d_ + h, h] = 1.0
            ohh[32 * d_ + h, h // 2, h % 2, :, :] = 1.0
    sh['MH'] = mhh
    sh['OH'] = ohh.reshape(128, 2, 512)
    hm2 = np.zeros((128, 2), f)
    hm2[0:64, 0] = 1.0
    hm2[64:128, 1] = 1.0
    sh['HM2'] = hm2
    sh['SELP'] = selp
    NEG = -1.0e4
    mbm = np.zeros((128, 2, 4, 128), f)
    mbm[:, 0, 0, :] = np.where(diff >= 0, 0.0, NEG); mbm[:, 0, 1, :] = np.where(diff > 0, 0.0, NEG)
    mbm[:, 1, 0, :] = np.where(diff <= 0, 0.0, NEG); mbm[:, 1, 1, :] = np.where(diff < 0, 0.0, NEG)
    mbm[:, :, 2, :] = mbm[:, :, 0, :]; mbm[:, :, 3, :] = mbm[:, :, 1, :]
    sh['MB'] = mbm
    sel = np.zeros((4, 4, 128), f)
    for j in range(4):
        sel[j, j, :] = 1.0
    sh['sel4'] = sel
    return sh


def core_inputs(inp, core):
    f = np.float32
    b0 = core * NB
    d = {}
    d['x'] = np.ascontiguousarray(inp['x'][b0:b0 + NB])
    d['ctx'] = np.ascontiguousarray(inp['ctx'][b0:b0 + NB])
    cvec = np.stack([inp['c'][b0], inp['c'][b0 + 1], inp['c_ctx'], inp['c_ctx']], 1)
    d['cT'] = np.ascontiguousarray(cvec.reshape(8, 128, 4).transpose(1, 0, 2)).astype(f)
    return d


class K:
    pass


def build(dbg=None):
    dbg = dbg or {}
    nc = bass.Bass("TRN2", target_bir_lowering=False)
    k = K()
    k.nc = nc
    k.dbg = dbg

    def din(name, shape, dt=F32):
        return nc.dram_tensor(name, list(shape), dt, kind="ExternalInput").ap()

    def dscr(name, shape, dt=F32):
        return nc.dram_tensor(name, list(shape), dt, kind="Internal").ap()

    k.x = din('x', [NB, L, D])
    k.ctx = din('ctx', [NB, LC, D])
    k.cT = din('cT', [128, 8, 4])
    k.ident = din('ident', [128, 128])
    k.w_ada = din('w_ada', [DEPTH, 128, 8, 6 * D])
    k.b_adaFM = din('b_adaFM', [128, DEPTH, 48])
    k.b_ada4 = din('b_ada4', [4, DEPTH, 6 * D])
    k.norm_gFM = din('norm_gFM', [128, DEPTH, 2, 8])
    k.final_g_rep = din('final_g_rep', [128, D])
    k.w_inFM = din('w_inFM', [DEPTH, 128, 8, NFM])
    k.w_inTM = din('w_inTM', [DEPTH, 128, 8, NTM])
    k.w_out = din('w_out', [DEPTH, 128, 8, D])
    k.router_w = din('router_w', [128, 8, NE])
    k.router_b_rep = din('router_b_rep', [128, NE])
    if not dbg.get('nomoe'):
        k.moe_wg = din('moe_wg', [DEPTH, NE, 128, 8, DFF])
        k.moe_wu = din('moe_wu', [DEPTH, NE, 128, 8, DFF])
        k.moe_wd = din('moe_wd', [DEPTH, NE, 128, 4, D])
    k.sel4 = din('sel4', [4, 4, 128])
    k.DIFFf = din('DIFFf', [128, 128]); k.DIFFb = din('DIFFb', [128, 128])
    k.MSKf = din('MSKf', [128, 128]); k.MSKb = din('MSKb', [128, 128])
    k.POS = din('POS', [128, 4, 128])
    k.BLK2 = din('BLK2', [128, 128]); k.BLK64 = din('BLK64', [128, 128])
    k.COS = din('COS', [128, L]); k.SIN = din('SIN', [128, L])
    k.ret_logit_rep = din('ret_logit_rep', [DEPTH, 128, 8])
    k.HM4 = din('HM4', [128, 4]); k.BLKG = din('BLKG', [128, 256])
    k.s5_lre = din('s5_lre', [128, DEPTH, 16]); k.s5_lim = din('s5_lim', [128, DEPTH, 16]); k.s5_ldt = din('s5_ldt', [128, DEPTH, 16])
    k.s5_WB = din('s5_WB', [DEPTH, 128, 8, 2, 128]); k.s5_WC = din('s5_WC', [DEPTH, 128, 8, 2, 128])
    k.s5_dFM = din('s5_dFM', [128, DEPTH, 2]); k.s5_gbFM = din('s5_gbFM', [128, DEPTH, 2])
    k.s5_gw = din('s5_gw', [DEPTH, 128, 2, 256])
    k.convFM = din('convFM', [128, DEPTH, 6, 5]); k.dn_par = din('dn_par', [128, DEPTH, 2])
    k.MH = din('MH', [128, 4]); k.OH = din('OH', [128, 2, 512]); k.HM2 = din('HM2', [128, 2]); k.SELR = din('SELR', [128, 4, 128]); k.SELP = din('SELP', [128, 2, 128]); k.MB = din('MB', [128, 2, 4, 128])
    k.gkw = din('gkw', [DEPTH, 128, 128]); k.gkbFM = din('gkbFM', [128, DEPTH, 2])
    if 'mix_in' in dbg:
        k.mix_in = din('mix_in', [NB, D, T])
    k.out = nc.dram_tensor('out', [NB, L, D], F32, kind="ExternalOutput").ap()

    k.pFM = dscr('pFM', [NB, NFM, T])
    k.pTM = dscr('pTM', [NB, T, NTM])
    k.mixFM = dscr('mixFM', [NB, D, T])
    k.Xs = dscr('Xs', [NB, L, D])
    k.Zs = dscr('Zs', [NB, LC, D])
    k.gateD = dscr('gateD', [DEPTH, 2, 4, D])
    k.dbg_out = {}
    for name, shape in dbg.get('outs', {}).items():
        k.dbg_out[name] = nc.dram_tensor('dbg_' + name, list(shape), F32, kind="ExternalOutput").ap()

    with ExitStack() as es:
        P = Prog(nc, es)
        k.P = P
        sb = lambda name, shape, dt=F32: es.enter_context(_sbt(nc, name, list(shape), dt))
        k.identf = sb('identf', [128, 128])
        k.identb = sb('identb', [128, 128], BF16)
        k.silu_c = sb('silu_c', [128, 8, 4])
        k.modFM = sb('modFM', [128, DEPTH, 48, 4])
        k.ngFM = sb('ngFM', [128, DEPTH, 2, 8])
        k.Amod = sb('Amod', [128, DEPTH, 2, 8, 4])
        k.lnq8 = sb('lnq8', [128, 1])

        phase_init(k)
        stages = dbg.get('stages', None)
        for l in range(DEPTH):
            last = (l == DEPTH - 1)
            if stages is None or ('mod', l) in stages:
                phase_mod(k, l)
            for b in range(NB):
                if stages is None or ('inproj', l) in stages:
                    phase_inproj(k, l, b)
                if stages is None or ('mixers', l) in stages:
                    phase_mixers(k, l, b)
                if stages is None or ('outproj', l) in stages:
                    phase_outproj_moe(k, l, b, last)
        phase_dbg(k)
    return nc


def resid_src(k, l, b, tt):
    if tt < 2:
        src = k.ctx if l == 0 else k.Zs
        return src[b, tt * 128:(tt + 1) * 128, :]
    src = k.x if l == 0 else k.Xs
    return src[b, (tt - 2) * 128:(tt - 1) * 128, :]


def resid_dst(k, b, tt):
    if tt < 2:
        return k.Zs[b, tt * 128:(tt + 1) * 128, :]
    return k.Xs[b, (tt - 2) * 128:(tt - 1) * 128, :]


def phase_init(k):
    P, nc = k.P, k.nc
    P.dma(k.identf[:], k.ident[:, :], writes=['identf'])
    P.dma(k.identb[:], k.ident[:, :], writes=['identb'], eng='gpsimd')
    P.dma(k.silu_c[:], k.cT[:, :, :], writes=['silu_c'])
    P.dma(k.ngFM[:], k.norm_gFM[:, :, :, :], writes=['ngFM'])
    P.act(k.silu_c[:], k.silu_c[:], AF.Silu, reads=['silu_c'], writes=['silu_c'])
    P.memset(k.lnq8[:], math.log(0.125), writes=['lnq8'])
    P.flush()


def phase_mod(k, l):
    P, nc = k.P, k.nc
    with ExitStack() as es:
        wbuf = [es.enter_context(_sbt(nc, 'wada%d' % i, [128, 8, 512], F32)) for i in range(2)]
        bfm = es.enter_context(_sbt(nc, 'bfm', [128, 48], F32))
        b4 = es.enter_context(_sbt(nc, 'b4', [4, 6 * D], F32))
        gtmp = es.enter_context(_sbt(nc, 'gtmp', [4, 512], F32))
        ps = [es.enter_context(_pst(nc, 'psm%d' % i, [128, 512], F32)) for i in range(2)]
        psg = es.enter_context(_pst(nc, 'psg', [4, 512], F32))
        P.dma(bfm[:], k.b_adaFM[:, l, :], writes=['bfm'])
        P.dma(b4[:], k.b_ada4[:, l, :], writes=['b4'])
        for blk in range(12):
            w = wbuf[blk % 2]
            wk = 'wada%d' % (blk % 2)
            P.dma(w[:], k.w_ada[l, :, :, blk * 512:(blk + 1) * 512], writes=[wk])
            pst = ps[blk % 2]
            pk = 'psm%d' % (blk % 2)
            for j in range(4):
                for kc in range(8):
                    P.mm(pst[:, j * 4:(j + 1) * 4], w[:, kc, j * 128:(j + 1) * 128], k.silu_c[:, kc, :],
                         kc == 0, kc == 7, reads=[wk, 'silu_c'], writes=[pk])
            for j in range(4):
                jj = blk * 4 + j
                P.ts(k.modFM[:, l, jj, :], pst[:, j * 4:(j + 1) * 4], bfm[:, jj:jj + 1], None, ALU.add,
                     reads=[pk, 'bfm'], writes=['modFM'])
            m = blk // 2
            if m in (2, 5):
                n = 0 if m == 2 else 1
                c0 = (blk % 2) * 512
                for kc in range(8):
                    P.mm(psg[:, :], k.silu_c[:, kc, :], w[:, kc, :], kc == 0, kc == 7,
                         reads=[wk, 'silu_c'], writes=['psg'])
                P.tt(gtmp[:, :], psg[:, :], b4[:, blk * 512:(blk + 1) * 512], ALU.add,
                     reads=['psg', 'b4'], writes=['gtmp'])
                P.dma(k.gateD[l, n, :, c0:c0 + 512], gtmp[:, :], reads=['gtmp'])
        for n in range(2):
            for fc in range(8):
                P.ts(k.Amod[:, l, n, fc, :], k.modFM[:, l, (1 + 3 * n) * 8 + fc, :], 1.0,
                     k.ngFM[:, l, n, fc:fc + 1], ALU.add, ALU.mult,
                     reads=['modFM', 'ngFM'], writes=['Amod'])
        P.flush()


def norm_modulate_tile(k, P, xt, xk, l, n, col, ssq, rstd, xn, pst, pkeys, hT_slices, hkeys, tag,
                       h32=None, h32key=None):
    P.op('scalar', lambda s: s.activation(out=xn[:], in_=xt, func=AF.Square, accum_out=ssq[:]),
         reads=[xk], writes=['xn' + tag, 'ssq' + tag])
    P.act(rstd[:], ssq[:], AF.Sqrt, reads=['ssq' + tag], writes=['rstd' + tag], scale=1.0 / D, bias=k.eps_col[:])
    P.recip(rstd[:], rstd[:], reads=['rstd' + tag], writes=['rstd' + tag])
    P.ts(xn[:], xt, rstd[:, 0:1], None, ALU.mult, reads=[xk, 'rstd' + tag], writes=['xn' + tag])
    for half in range(2):
        pt = pst[half]
        for j in range(4):
            fc = half * 4 + j
            P.tr(pt[:, j * 128:(j + 1) * 128], xn[:, fc * 128:(fc + 1) * 128], k.identf[:],
                 reads=['xn' + tag, 'identf'], writes=[pkeys[half]])
        for j in range(4):
            fc = half * 4 + j
            P.ts(hT_slices[fc], pt[:, j * 128:(j + 1) * 128], k.Amod[:, l, n, fc, col:col + 1],
                 k.modFM[:, l, (3 * n) * 8 + fc, col:col + 1], ALU.mult, ALU.add,
                 reads=[pkeys[half], 'Amod', 'modFM'], writes=[hkeys[fc]])
            if h32 is not None:
                P.ts(h32[:, fc, :], pt[:, j * 128:(j + 1) * 128], k.Amod[:, l, n, fc, col:col + 1],
                     k.modFM[:, l, (3 * n) * 8 + fc, col:col + 1], ALU.mult, ALU.add,
                     reads=[pkeys[half], 'Amod', 'modFM'], writes=[h32key])


def phase_inproj(k, l, b):
    P, nc = k.P, k.nc
    with ExitStack() as es:
        sbt = lambda name, shape, dt=F32: es.enter_context(_sbt(nc, name, list(shape), dt))
        hT = sbt('hT', [128, 8, T], BF16)
        wfm = sbt('wfm', [128, 8, NFM], BF16)
        wtm = sbt('wtm', [128, 8, NTM], BF16)
        xt = [sbt('xt%d' % i, [128, D]) for i in range(2)]
        xn = [sbt('xn%d' % i, [128, D]) for i in range(2)]
        ssq = [sbt('ssq%d' % i, [128, 1]) for i in range(2)]
        rstd = [sbt('rstd%d' % i, [128, 1]) for i in range(2)]
        k.eps_col = sbt('eps_col', [128, 1])
        stg = [sbt('stg%d' % i, [128, 512]) for i in range(3)]
        ps = [es.enter_context(_pst(nc, 'ps%d' % i, [128, 512], F32)) for i in range(6)]
        P.memset(k.eps_col[:], EPS, writes=['eps'])
        for kc in range(8):
            P.dma(wfm[:, kc, :], k.w_inFM[l, :, kc, :], writes=['wfm'], eng='gpsimd')
            P.dma(wtm[:, kc, :], k.w_inTM[l, :, kc, :], writes=['wtm'], eng='gpsimd')
        for tt in range(NT):
            i = tt % 2
            tag = str(i)
            P.dma(xt[i][:], resid_src(k, l, b, tt), writes=['xt' + tag])
            col = 2 if tt < 2 else b
            norm_modulate_tile(k, P, xt[i][:], 'xt' + tag, l, 0, col, ssq[i], rstd[i], xn[i],
                               [ps[2 * i], ps[2 * i + 1]], ['ps%d' % (2 * i), 'ps%d' % (2 * i + 1)],
                               [hT[:, fc, tt * 128:(tt + 1) * 128] for fc in range(8)],
                               [('hT', tt)] * 8, tag)
        cnt = 0
        for cb in range(NFM // 128):
            for (t0, tn) in TBLK:
                pt = ps[4 + cnt % 2]
                pk = 'ps%d' % (4 + cnt % 2)
                st = stg[cnt % 3]
                sk = 'stg%d' % (cnt % 3)
                cnt += 1
                hk = [('hT', t0 // 128 + j) for j in range(tn // 128)]
                for kc in range(8):
                    P.mm(pt[:, :tn], wfm[:, kc, cb * 128:(cb + 1) * 128], hT[:, kc, t0:t0 + tn], kc == 0, kc == 7,
                         reads=['wfm'] + hk, writes=[pk])
                if cnt % 2:
                    P.copy(st[:, :tn], pt[:, :tn], reads=[pk], writes=[sk])
                else:
                    P.copy(st[:, :tn], pt[:, :tn], reads=[pk], writes=[sk], eng='scalar')
                P.dma(k.pFM[b, cb * 128:(cb + 1) * 128, t0:t0 + tn], st[:, :tn], reads=[sk])
        CB = [(0, 512)]
        for tt in range(NT):
            for (c0, cn) in CB:
                pt = ps[4 + cnt % 2]
                pk = 'ps%d' % (4 + cnt % 2)
                st = stg[cnt % 3]
                sk = 'stg%d' % (cnt % 3)
                cnt += 1
                for kc in range(8):
                    P.mm(pt[:, :cn], hT[:, kc, tt * 128:(tt + 1) * 128], wtm[:, kc, c0:c0 + cn], kc == 0, kc == 7,
                         reads=['wtm', ('hT', tt)], writes=[pk])
                if cnt % 2:
                    P.copy(st[:, :cn], pt[:, :cn], reads=[pk], writes=[sk])
                else:
                    P.copy(st[:, :cn], pt[:, :cn], reads=[pk], writes=[sk], eng='scalar')
                P.dma(k.pTM[b, tt * 128:(tt + 1) * 128, c0:c0 + cn], st[:, :cn], reads=[sk])
        P.flush()


def phase_mixers(k, l, b):
    P, nc = k.P, k.nc
    if 'mix_in' in k.dbg:
        with ExitStack() as es:
            t = es.enter_context(_sbt(nc, 'mixcp', [128, T], F32))
            for fc in range(8):
                P.dma(t[:], k.mix_in[b, fc * 128:(fc + 1) * 128, :], writes=['mixcp'])
                P.dma(k.mixFM[b, fc * 128:(fc + 1) * 128, :], t[:], reads=['mixcp'])
            P.flush()
        return
    which = k.dbg.get('mixers', ['gla', 'ret', 's5', 'dn'])
    if 'ret' in which:
        phase_ret(k, l, b)
    if 'gla' in which:
        phase_gla(k, l, b)
    if 's5' in which:
        phase_s5(k, l, b)
    if 'dn' in which:
        phase_dn(k, l, b)


def chunk_order(d):
    if d == 0:
        return list(range(NT))
    return [1, 0] + list(range(NT - 1, 1, -1))


def norm_gate(k, P, nc, es, b, oacc, okeys, g_blk, mix_row0, center, tagp):
    sbt = lambda name, shape, dt=F32: es.enter_context(_sbt(nc, name + tagp, list(shape), dt))
    g = sbt('ng_g', [128, T])
    blk = sbt('ng_blk', [128, 128])
    xc = [sbt('ng_xc%d' % i, [128, 512]) for i in range(2)]
    sq = [sbt('ng_sq%d' % i, [128, 512]) for i in range(2)]
    eps = sbt('ng_eps', [128, 1])
    psA = [es.enter_context(_pst(nc, 'ng_psA%d' % i, [128, 512], F32)) for i in range(2)]
    P.memset(eps[:], EPS, writes=['ng_eps'])
    P.dma(blk[:], k.BLK64[:, :], writes=['ng_blk'])
    P.dma(g[:], k.pFM[b, g_blk * 128:(g_blk + 1) * 128, :], writes=['ng_g'])
    P.act(g[:], g[:], AF.Silu, reads=['ng_g'], writes=['ng_g'])
    for bi, (t0, tn) in enumerate(TBLK):
        i = bi % 2
        xk, sk, pk = 'ng_xc%d' % i, 'ng_sq%d' % i, 'ng_ps%d' % i
        src = oacc[:, t0:t0 + tn]
        if center:
            P.mm(psA[i][:, :tn], blk[:], src, True, True, reads=['ng_blk'] + okeys, writes=[pk])
            P.tt(xc[i][:, :tn], src, psA[i][:, :tn], ALU.subtract, reads=[pk] + okeys, writes=[xk])
        else:
            P.copy(xc[i][:, :tn], src, reads=okeys, writes=[xk])
        P.tt(sq[i][:, :tn], xc[i][:, :tn], xc[i][:, :tn], ALU.mult, reads=[xk], writes=[sk])
        P.mm(psA[i][:, :tn], blk[:], sq[i][:, :tn], True, True, reads=['ng_blk', sk], writes=[pk])
        P.act(sq[i][:, :tn], psA[i][:, :tn], AF.Sqrt, reads=[pk], writes=[sk], bias=eps[:], scale=1.0)
        P.recip(sq[i][:, :tn], sq[i][:, :tn], reads=[sk], writes=[sk])
        P.tt(xc[i][:, :tn], xc[i][:, :tn], sq[i][:, :tn], ALU.mult, reads=[xk, sk], writes=[xk])
        P.tt(xc[i][:, :tn], xc[i][:, :tn], g[:, t0:t0 + tn], ALU.mult, reads=[xk, 'ng_g'], writes=[xk])
        P.dma(k.mixFM[b, mix_row0:mix_row0 + 128, t0:t0 + tn], xc[i][:, :tn], reads=[xk])


def phase_ret(k, l, b):
    P, nc = k.P, k.nc
    with ExitStack() as es:
        sbt = lambda name, shape, dt=F32: es.enter_context(_sbt(nc, name, list(shape), dt))
        lg = sbt('r_lg', [128, 8])
        lgc = sbt('r_lgc', [128, 4])
        GC = sbt('r_GC', [128, 4])
        EQ = sbt('r_EQ', [128, 4, 128])
        EK = sbt('r_EK', [128, 4, 128])
        GAM = sbt('r_GAM', [128, 8, 128])
        pos = sbt('r_pos', [128, 4, 128])
        dif = sbt('r_dif', [128, 2, 128])
        blk2 = sbt('r_blk2', [128, 128])
        P.dma(lg[:], k.ret_logit_rep[l, :, :], writes=['r_lg'])
        P.dma(pos[:], k.POS[:, :, :], writes=['r_pos'])
        P.dma(dif[:, 0, :], k.DIFFf[:, :], writes=['r_dif'])
        P.dma(dif[:, 1, :], k.DIFFb[:, :], writes=['r_dif'])
        P.dma(blk2[:], k.BLK2[:, :], writes=['r_blk2'])
        P.act(lg[:], lg[:], AF.Exp, reads=['r_lg'], writes=['r_lg'], scale=-1.0)
        P.act(lg[:], lg[:], AF.Ln, reads=['r_lg'], writes=['r_lg'], bias=1.0, scale=1.0)
        P.ts(lg[:], lg[:], -1.0, None, ALU.mult, reads=['r_lg'], writes=['r_lg'])
        for d in range(2):
            for hp in range(2):
                j = d * 2 + hp
                P.copy(lgc[0:64, j:j + 1], lg[0:64, d * 4 + hp * 2:d * 4 + hp * 2 + 1], reads=['r_lg'], writes=['r_lgc'])
                P.copy(lgc[64:128, j:j + 1], lg[64:128, d * 4 + hp * 2 + 1:d * 4 + hp * 2 + 2], reads=['r_lg'], writes=['r_lgc'])
        for d in range(2):
            for hp in range(2):
                j = d * 2 + hp
                P.act(EQ[:, j, :], pos[:, d, :], AF.Exp, reads=['r_pos', 'r_lgc'], writes=['r_EQ'],
                      scale=lgc[:, j:j + 1])
                P.act(EK[:, j, :], pos[:, 2 + d, :], AF.Exp, reads=['r_pos', 'r_lgc'], writes=['r_EK'], scale=lgc[:, j:j + 1])
                P.act(GC[:, j:j + 1], lgc[:, j:j + 1], AF.Exp, reads=['r_lgc'], writes=['r_GC'], scale=128.0)
            for h in range(4):
                P.act(GAM[:, d * 4 + h, :], dif[:, d, :], AF.Exp, reads=['r_dif', 'r_lg'], writes=['r_GAM'],
                      scale=lg[:, d * 4 + h:d * 4 + h + 1])
        qr = sbt('r_q', [128, 2, T], BF16)
        kr = sbt('r_k', [128, 2, T], BF16)
        v = sbt('r_v', [128, NT, 256], BF16)
        vpad = sbt('r_vpad', [128, 2, 2, NT, 128], BF16)
        oacc = sbt('r_oacc', [128, 2, T])
        with ExitStack() as es2:
            sb2 = lambda name, shape, dt=F32: es2.enter_context(_sbt(nc, name, list(shape), dt))
            cos = sb2('r_cos', [128, L])
            sin = sb2('r_sin', [128, L])
            ta = sb2('r_ta', [128, T])
            tb = sb2('r_tb', [128, T])
            P.dma(cos[:], k.COS[:, :], writes=['r_cos'])
            P.dma(sin[:], k.SIN[:, :], writes=['r_sin'])
            P.dma(v[:], k.pTM[b].rearrange("(c p) n -> p c n", p=128)[:, :, 256:512], writes=['r_v'], eng='gpsimd')
            P.memset(vpad[:].rearrange("p a b c d -> p (a b c d)"), 0.0, writes=['r_vpad'], eng='gpsimd')
            for hp in range(2):
                for hh in range(2):
                    P.copy(vpad[:, hp, hh, :, hh * 64:(hh + 1) * 64], v[:, :, hp * 128 + hh * 64:hp * 128 + (hh + 1) * 64],
                           reads=['r_v', 'r_vpad'], writes=['r_vpad'], eng='gpsimd')
            for (dst, dk_, bm, bs) in [(qr, 'r_q', 3, 5), (kr, 'r_k', 7, 9)]:
                for hp in range(2):
                    P.dma(ta[:], k.pFM[b, (bm + hp) * 128:(bm + hp + 1) * 128, :], writes=['r_ta'])
                    P.dma(tb[:], k.pFM[b, (bs + hp) * 128:(bs + hp + 1) * 128, :], writes=['r_tb'])
                    if dk_ == 'r_q':
                        P.ts(ta[:], ta[:], 0.125, None, ALU.mult, reads=['r_ta'], writes=['r_ta'])
                        P.ts(tb[:], tb[:], 0.125, None, ALU.mult, reads=['r_tb'], writes=['r_tb'], eng='gpsimd')
                    P.copy(dst[:, hp, 0:LC], ta[:, 0:LC], reads=['r_ta'], writes=[dk_])
                    P.tt(ta[:, LC:], ta[:, LC:], cos[:], ALU.mult, reads=['r_ta', 'r_cos'], writes=['r_ta'])
                    P.tt(tb[:, LC:], tb[:, LC:], sin[:], ALU.mult, reads=['r_tb', 'r_sin'], writes=['r_tb'], eng='gpsimd')
                    P.tt(dst[:, hp, LC:], ta[:, LC:], tb[:, LC:], ALU.add, reads=['r_ta', 'r_tb'], writes=[dk_])
            P.flush()
        with ExitStack() as es2:
            sb2 = lambda name, shape, dt=F32: es2.enter_context(_sbt(nc, name, list(shape), dt))
            NBUF = 2
            Pm = [[sb2('r_Pm%d_%d' % (i, hh), [128, 128], BF16) for hh in range(2)] for i in range(NBUF)]
            qin = [sb2('r_qin%d' % i, [128, 128], BF16) for i in range(NBUF)]
            kout = [sb2('r_kout%d' % i, [128, 128], BF16) for i in range(NBUF)]
            koT = [sb2('r_koT%d' % i, [128, 128], BF16) for i in range(NBUF)]
            tmp = [sb2('r_tmp%d' % i, [128, 128]) for i in range(NBUF)]
            S32 = [sb2('r_S32_%d' % i, [128, 128]) for i in range(4)]
            Sb = [sb2('r_Sb_%d' % i, [128, 128], BF16) for i in range(4)]
            psS = [es2.enter_context(_pst(nc, 'r_psS%d' % i, [128, 128], F32)) for i in range(4)]
            psO = [es2.enter_context(_pst(nc, 'r_psO%d' % i, [128, 128], F32)) for i in range(2)]
            psT = [es2.enter_context(_pst(nc, 'r_psT%d' % i, [128, 128], BF16)) for i in range(1)]
            psU = [es2.enter_context(_pst(nc, 'r_psU%d' % i, [128, 128], F32)) for i in range(1)]
            step = 0
            seen = set()
            for j in range(4):
                P.memset(S32[j][:], 0.0, writes=['r_S32_%d' % j])
                P.memset(Sb[j][:], 0.0, writes=['r_Sb_%d' % j])
            for ci in range(NT):
                for hp in range(2):
                    for d in range(2):
                        j = d * 2 + hp
                        c = chunk_order(d)[ci]
                        cs = slice(c * 128, (c + 1) * 128)
                        i = step % NBUF
                        step += 1
                        si = str(i)
                        Sk, Sbk = 'r_S32_%d' % j, 'r_Sb_%d' % j
                        for hh in range(2):
                            pr = slice(hh * 64, (hh + 1) * 64)
                            pS = psS[2 * i + hh]
                            pSk = 'r_psS%d' % (2 * i + hh)
                            P.mm(pS[:, :], kr[pr, hp, cs], qr[pr, hp, cs], True, True, reads=['r_q', 'r_k'], writes=[pSk])
                            P.tt(Pm[i][hh][:], pS[:, :], GAM[:, d * 4 + hp * 2 + hh, :], ALU.mult,
                                 reads=[pSk, 'r_GAM'], writes=['r_Pm%s_%d' % (si, hh)])
                        P.tt(qin[i][:], qr[:, hp, cs], EQ[:, j, :], ALU.mult, reads=['r_q', 'r_EQ'], writes=['r_qin' + si])
                        P.tt(kout[i][:], kr[:, hp, cs], EK[:, j, :], ALU.mult, reads=['r_k', 'r_EK'], writes=['r_kout' + si])
                        pO = psO[i]
                        pOk = 'r_psO%d' % i
                        P.mm(pO[:, :], vpad[:, hp, 0, c, :], Pm[i][0][:], True, False, reads=['r_vpad', 'r_Pm%s_0' % si], writes=[pOk])
                        P.mm(pO[:, :], vpad[:, hp, 1, c, :], Pm[i][1][:], False, False, reads=['r_vpad', 'r_Pm%s_1' % si], writes=[pOk])
                        P.mm(pO[:, :], Sb[j][:], qin[i][:], False, True, reads=[Sbk, 'r_qin' + si], writes=[pOk])
                        ok = ('r_oacc', hp, c)
                        if (ok, 0) not in seen:
                            seen.add((ok, 0))
                            P.copy(oacc[:, hp, cs], pO[:, :], reads=[pOk], writes=[ok], eng='scalar')
                        else:
                            P.tt(oacc[:, hp, cs], oacc[:, hp, cs], pO[:, :], ALU.add, reads=[pOk, ok], writes=[ok])
                        P.tr(psT[0][:, :], kout[i][:], k.identb[:], reads=['r_kout' + si, 'identb'], writes=['r_psT'])
                        P.copy(koT[i][:], psT[0][:, :], reads=['r_psT'], writes=['r_koT' + si], eng='scalar')
                        P.mm(psU[0][:, :], koT[i][:], v[:, c, hp * 128:(hp + 1) * 128], True, True,
                             reads=['r_koT' + si, 'r_v'], writes=['r_psU'])
                        P.tt(tmp[i][:], psU[0][:, :], blk2[:], ALU.mult, reads=['r_psU', 'r_blk2'], writes=['r_tmp' + si])
                        P.stt(S32[j][:], S32[j][:], GC[:, j:j + 1], tmp[i][:], ALU.mult, ALU.add,
                              reads=[Sk, 'r_GC', 'r_tmp' + si], writes=[Sk])
                        P.copy(Sb[j][:], S32[j][:], reads=[Sk], writes=[Sbk], eng='scalar')
            P.flush()
        for hp in range(2):
            with ExitStack() as es2:
                norm_gate(k, P, nc, es2, b, oacc[:, hp, :], [('r_oacc', hp, c) for c in range(NT)], 22 + hp, 256 + hp * 128, True, 'r%d' % hp)
                P.flush()


def phase_gla(k, l, b):
    P, nc = k.P, k.nc
    with ExitStack() as es:
        sbt = lambda name, shape, dt=F32: es.enter_context(_sbt(nc, name, list(shape), dt))
        qh = sbt('g_qh', [128, 2, 4, T], BF16)
        qt = sbt('g_qt', [128, 2, T], BF16)
        kh = sbt('g_kh', [128, 2, T], BF16)
        elast = sbt('g_el', [128, 2, NT])
        v = sbt('g_v', [128, NT, 256], BF16)
        vpad = sbt('g_vpad', [128, 4, NT, 128], BF16)
        oacc = sbt('g_oacc', [128, 2, T])
        msk = sbt('g_msk', [128, 2, 128])
        blkg = sbt('g_blkg', [128, 256])
        hm = sbt('g_hm', [128, 4])
        P.dma(msk[:, 0, :], k.MSKf[:, :], writes=['g_msk'])
        P.dma(msk[:, 1, :], k.MSKb[:, :], writes=['g_msk'])
        P.dma(blkg[:], k.BLKG[:, :], writes=['g_blkg'])
        P.dma(hm[:], k.HM4[:, :], writes=['g_hm'])
        P.dma(v[:], k.pTM[b].rearrange("(c p) n -> p c n", p=128)[:, :, 0:256], writes=['g_v'], eng='gpsimd')
        P.memset(vpad[:].rearrange("p a c d -> p (a c d)"), 0.0, writes=['g_vpad'], eng='gpsimd')
        for h in range(4):
            hh = h % 2
            P.copy(vpad[:, h, :, hh * 64:(hh + 1) * 64], v[:, :, h * 64:(h + 1) * 64], reads=['g_v', 'g_vpad'], writes=['g_vpad'], eng='gpsimd')
        with ExitStack() as es2:
            sb2 = lambda name, shape, dt=F32: es2.enter_context(_sbt(nc, name, list(shape), dt))
            z = sb2('g_z', [128, T])
            gkw = sb2('g_gkw', [128, 128])
            nb = sb2('g_nb', [128, 2])
            sp = sb2('g_sp', [128, T])
            bc = sb2('g_bc', [128, T])
            ee = sb2('g_ee', [128, T])
            qf = sb2('g_qf', [128, T])
            kf = sb2('g_kf', [128, T])
            ones = sb2('g_ones', [128, 128])
            lnq = sb2('g_lnq', [128, 1])
            ps = [es2.enter_context(_pst(nc, 'g_ps%d' % i, [128, 512], F32)) for i in range(2)]
            P.dma(z[:], k.pFM[b, 2 * 128:3 * 128, :], writes=['g_z'])
            P.dma(gkw[:], k.gkw[l, :, :], writes=['g_gkw'])
            P.dma(nb[:], k.gkbFM[:, l, :], writes=['g_nb'])
            P.dma(qf[:], k.pFM[b, 0:128, :], writes=['g_qf'])
            P.dma(kf[:], k.pFM[b, 128:256, :], writes=['g_kf'])
            P.ts(nb[:], nb[:], -1.0, None, ALU.mult, reads=['g_nb'], writes=['g_nb'])
            P.memset(ones[:], 1.0, writes=['g_ones'])
            P.memset(lnq[:], math.log(32.0 ** -0.5), writes=['g_lnq'])
            for d in range(2):
                pr = slice(d * 32, d * 32 + 16)
                for bi, (t0, tn) in enumerate(TBLK):
                    i = bi % 2
                    P.mm(ps[i][:, :tn], gkw[pr, :], z[pr, t0:t0 + tn], True, True, reads=['g_gkw', 'g_z'], writes=['g_ps%d' % i])
                    P.act(sp[:, t0:t0 + tn], ps[i][:, :tn], AF.Exp, reads=['g_ps%d' % i, 'g_nb'], writes=['g_sp'],
                          scale=-1.0, bias=nb[:, d:d + 1])
                P.act(sp[:], sp[:], AF.Ln, reads=['g_sp'], writes=['g_sp'], bias=1.0, scale=1.0)
                for c in range(NT):
                    cs = slice(c * 128, (c + 1) * 128)
                    if d == 0:
                        P.scan(bc[:, cs], ones[:], sp[:, cs], 0.0, reads=['g_ones', 'g_sp'], writes=['g_bc'])
                    else:
                        P.scan(bc[:, cs][:, ::-1], ones[:], sp[:, cs][:, ::-1], 0.0, reads=['g_ones', 'g_sp'], writes=['g_bc'])
                P.act(ee[:], bc[:], AF.Exp, reads=['g_bc'], writes=['g_ee'], scale=-1.0 / 16.0, bias=lnq[:])
                P.tt(qt[:, d, :], qf[:], ee[:], ALU.mult, reads=['g_qf', 'g_ee'], writes=['g_qt'])
                for h in range(4):
                    P.ts(qh[:, d, h, :], qt[:, d, :], hm[:, h:h + 1], None, ALU.mult, reads=['g_qt', 'g_hm'], writes=['g_qh'],
                         eng='gpsimd' if h % 2 else 'vector')
                lastv = bc[:, 127::128] if d == 0 else bc[:, 0::128]
                P.act(elast[:, d, :], lastv, AF.Exp, reads=['g_bc'], writes=['g_el'], scale=-1.0 / 16.0)
                P.act(ee[:], bc[:], AF.Exp, reads=['g_bc', 'g_qt'], writes=['g_ee'], scale=1.0 / 16.0)
                P.tt(kh[:, d, :], kf[:], ee[:], ALU.mult, reads=['g_kf', 'g_ee'], writes=['g_kh'])
            P.flush()
        with ExitStack() as es2:
            sb2 = lambda name, shape, dt=F32: es2.enter_context(_sbt(nc, name, list(shape), dt))
            NBUF = 2
            Pm = [[sb2('g_Pm%d_%d' % (i, h), [128, 128], BF16) for h in range(4)] for i in range(NBUF)]
            koT = [sb2('g_koT%d' % i, [128, 128], BF16) for i in range(NBUF)]
            tmp = [sb2('g_tmp%d' % i, [128, 256]) for i in range(NBUF)]
            S32 = [sb2('g_S32_%d' % i, [128, 256]) for i in range(2)]
            Sb = [sb2('g_Sb_%d' % i, [128, 256], BF16) for i in range(2)]
            psS = [es2.enter_context(_pst(nc, 'g_psS%d' % i, [128, 128], F32)) for i in range(4)]
            psO = [es2.enter_context(_pst(nc, 'g_psO%d' % i, [128, 128], F32)) for i in range(2)]
            psT = es2.enter_context(_pst(nc, 'g_psT', [128, 128], BF16))
            psU = es2.enter_context(_pst(nc, 'g_psU', [128, 256], F32))
            for j in range(2):
                P.memset(S32[j][:], 0.0, writes=['g_S32_%d' % j])
                P.memset(Sb[j][:], 0.0, writes=['g_Sb_%d' % j])
            step = 0
            seen = set()
            for ci in range(NT):
                for d in range(2):
                    c = chunk_order(d)[ci]
                    cs = slice(c * 128, (c + 1) * 128)
                    i = step % NBUF
                    step += 1
                    si = str(i)
                    Sk, Sbk = 'g_S32_%d' % d, 'g_Sb_%d' % d
                    for h in range(4):
                        P.mm(psS[h][:, :], kh[:, d, cs], qh[:, d, h, cs], True, True, reads=['g_kh', 'g_qh'], writes=['g_psS%d' % h])
                        P.tt(Pm[i][h][:], psS[h][:, :], msk[:, d, :], ALU.mult, reads=['g_psS%d' % h, 'g_msk'],
                             writes=['g_Pm%s_%d' % (si, h)])
                    for vp in range(2):
                        pO = psO[vp]
                        pOk = 'g_psO%d' % vp
                        P.mm(pO[:, :], vpad[:, 2 * vp, c, :], Pm[i][2 * vp][:], True, False,
                             reads=['g_vpad', 'g_Pm%s_%d' % (si, 2 * vp)], writes=[pOk])
                        P.mm(pO[:, :], vpad[:, 2 * vp + 1, c, :], Pm[i][2 * vp + 1][:], False, False,
                             reads=['g_vpad', 'g_Pm%s_%d' % (si, 2 * vp + 1)], writes=[pOk])
                        P.mm(pO[:, :], Sb[d][:, vp * 128:(vp + 1) * 128], qt[:, d, cs], False, True, reads=[Sbk, 'g_qt'], writes=[pOk])
                        ok = ('g_oacc', vp, c)
                        if ok not in seen:
                            seen.add(ok)
                            P.copy(oacc[:, vp, cs], pO[:, :], reads=[pOk], writes=[ok], eng='scalar')
                        else:
                            P.tt(oacc[:, vp, cs], oacc[:, vp, cs], pO[:, :], ALU.add, reads=[pOk, ok], writes=[ok])
                    P.tr(psT[:, :], kh[:, d, cs], k.identb[:], reads=['g_kh', 'identb'], writes=['g_psT'])
                    P.copy(koT[i][:], psT[:, :], reads=['g_psT'], writes=['g_koT' + si], eng='scalar')
                    P.mm(psU[:, :], koT[i][:], v[:, c, :], True, True, reads=['g_koT' + si, 'g_v'], writes=['g_psU'])
                    P.tt(tmp[i][:], psU[:, :], blkg[:], ALU.mult, reads=['g_psU', 'g_blkg'], writes=['g_tmp' + si])
                    P.tt(S32[d][:], S32[d][:], tmp[i][:], ALU.add, reads=[Sk, 'g_tmp' + si], writes=[Sk])
                    P.ts(S32[d][:], S32[d][:], elast[:, d, c:c + 1], None, ALU.mult, reads=[Sk, 'g_el'], writes=[Sk])
                    P.copy(Sb[d][:], S32[d][:], reads=[Sk], writes=[Sbk], eng='scalar')
            P.flush()
        for vp in range(2):
            with ExitStack() as es2:
                norm_gate(k, P, nc, es2, b, oacc[:, vp, :], [('g_oacc', vp, c) for c in range(NT)], 20 + vp, vp * 128, False, 'g%d' % vp)
                P.flush()


TWO_PI = 2.0 * math.pi


def sincos(P, ang, sn, cs, ki, t1, keys):
    R = dict(reads=keys, writes=keys)
    P.ts(t1, ang, 1.0 / TWO_PI, None, ALU.mult, **R)
    P.copy(ki, t1, **R)
    P.copy(t1, ki, **R)
    P.stt(ang, t1, -TWO_PI, ang, ALU.mult, ALU.add, **R)
    P.ts(t1, ang, math.pi, None, ALU.is_gt, **R)
    P.stt(ang, t1, -TWO_PI, ang, ALU.mult, ALU.add, **R)
    P.ts(t1, ang, -math.pi, None, ALU.is_lt, **R)
    P.stt(ang, t1, TWO_PI, ang, ALU.mult, ALU.add, **R)
    P.act(sn, ang, AF.Sin, **R)
    P.ts(ang, ang, math.pi / 2, None, ALU.add, **R)
    P.ts(t1, ang, math.pi, None, ALU.is_gt, **R)
    P.stt(ang, t1, -TWO_PI, ang, ALU.mult, ALU.add, **R)
    P.act(cs, ang, AF.Sin, **R)


def phase_s5(k, l, b):
    P, nc = k.P, k.nc
    with ExitStack() as es:
        sbt = lambda name, shape, dt=F32: es.enter_context(_sbt(nc, name, list(shape), dt))
        TAB = sbt('s_tab', [128, 16, 4, 128])
        rr = sbt('s_r', [128, 16])
        cb = sbt('s_cb', [128, 2, 16])
        WB = sbt('s_WB', [128, 8, 2, 128], BF16)
        WC = sbt('s_WC', [128, 8, 2, 128], BF16)
        u32 = sbt('s_u32', [128, 2, T])
        ub = sbt('s_ub', [128, 2, T], BF16)
        yacc = sbt('s_yacc', [128, 2, T])
        pos = sbt('s_pos', [128, 2, 128])
        P.dma(pos[:], k.POS[:, 0:2, :], writes=['s_pos'])
        P.dma(WB[:], k.s5_WB[l, :, :, :, :], writes=['s_WB'], eng='gpsimd')
        P.dma(WC[:], k.s5_WC[l, :, :, :, :], writes=['s_WC'], eng='gpsimd')
        for ct in range(2):
            P.dma(u32[:, ct, :], k.pFM[b, (11 + ct) * 128:(12 + ct) * 128, :], writes=['s_u32'])
        P.copy(ub[:], u32[:], reads=['s_u32'], writes=['s_ub'], eng='gpsimd')
        with ExitStack() as es2:
            sb2 = lambda name, shape, dt=F32: es2.enter_context(_sbt(nc, name, list(shape), dt))
            lre = sb2('s_lre', [128, 16]); lim = sb2('s_lim', [128, 16]); dt_ = sb2('s_dt', [128, 16])
            th = sb2('s_th', [128, 16]); ang = sb2('s_ang', [128, 16]); sn = sb2('s_sn', [128, 16]); cs = sb2('s_cs', [128, 16])
            ki = sb2('s_ki', [128, 16], I32); t1 = sb2('s_t1', [128, 16]); t2 = sb2('s_t2', [128, 16])
            bre = sb2('s_bre', [128, 16]); bim = sb2('s_bim', [128, 16]); den = sb2('s_den', [128, 16])
            A = sb2('s_A', [128, 16, 128]); K2 = sb2('s_K2', [128, 16, 128], I32); T1 = sb2('s_T1', [128, 16, 128])
            pk = ['s_par']
            R = dict(reads=pk, writes=pk)
            P.dma(lre[:], k.s5_lre[:, l, :], writes=pk)
            P.dma(lim[:], k.s5_lim[:, l, :], writes=pk)
            P.dma(dt_[:], k.s5_ldt[:, l, :], writes=pk)
            P.act(dt_[:], dt_[:], AF.Exp, **R)
            P.tt(th[:], lim[:], dt_[:], ALU.mult, **R)
            P.tt(t2[:], lre[:], dt_[:], ALU.mult, **R)
            P.act(rr[:], t2[:], AF.Exp, reads=pk, writes=pk + ['s_r'])
            P.copy(ang[:], th[:], **R)
            sincos(P, ang[:], sn[:], cs[:], ki[:], t1[:], pk)
            P.tt(t1[:], rr[:], cs[:], ALU.mult, **R)
            P.ts(t1[:], t1[:], -1.0, None, ALU.add, **R)
            P.tt(t2[:], rr[:], sn[:], ALU.mult, **R)
            P.tt(den[:], lre[:], lre[:], ALU.mult, **R)
            P.tt(bre[:], lim[:], lim[:], ALU.mult, **R)
            P.tt(den[:], den[:], bre[:], ALU.add, **R)
            P.recip(den[:], den[:], **R)
            P.tt(bre[:], t1[:], lre[:], ALU.mult, **R)
            P.tt(bim[:], t2[:], lim[:], ALU.mult, **R)
            P.tt(bre[:], bre[:], bim[:], ALU.add, **R)
            P.tt(bre[:], bre[:], den[:], ALU.mult, **R)
            P.tt(bim[:], t2[:], lre[:], ALU.mult, **R)
            P.tt(t2[:], t1[:], lim[:], ALU.mult, **R)
            P.tt(bim[:], bim[:], t2[:], ALU.subtract, **R)
            P.tt(bim[:], bim[:], den[:], ALU.mult, **R)
            tks = [('s_tab', dj) for dj in range(16)]
            for dj in range(16):
                d = dj // 8
                P.ts(A[:, dj, :], pos[:, d, :], th[:, dj:dj + 1], None, ALU.mult, reads=pk + ['s_pos'], writes=['s_A'])
            sincos(P, A[:], TAB[:, :, 3, :], TAB[:, :, 2, :], K2[:], T1[:], tks + ['s_A'])
            for dj in range(16):
                d = dj // 8
                tk = ('s_tab', dj)
                Rt = dict(reads=pk + [tk, 's_T1'], writes=[tk, 's_T1'])
                P.ts(T1[:, dj, :], TAB[:, dj, 3, :], bim[:, dj:dj + 1], None, ALU.mult, **Rt)
                P.stt(TAB[:, dj, 0, :], TAB[:, dj, 2, :], bre[:, dj:dj + 1], T1[:, dj, :], ALU.mult, ALU.add, **Rt)
                P.ts(T1[:, dj, :], TAB[:, dj, 3, :], bre[:, dj:dj + 1], None, ALU.mult, **Rt)
                P.stt(TAB[:, dj, 1, :], TAB[:, dj, 2, :], bim[:, dj:dj + 1], T1[:, dj, :], ALU.mult, ALU.subtract, **Rt)
                li = 127 if d == 0 else 0
                P.copy(cb[:, 0, dj:dj + 1], TAB[:, dj, 2, li:li + 1], reads=[tk], writes=['s_cb'])
                P.copy(cb[:, 1, dj:dj + 1], TAB[:, dj, 3, li:li + 1], reads=[tk], writes=['s_cb'])
            P.flush()
        with ExitStack() as es2:
            sb2 = lambda name, shape, dt=F32: es2.enter_context(_sbt(nc, name, list(shape), dt))
            BUs = [sb2('s_BU%d' % d, [128, 8, 2, 128]) for d in range(2)]
            M1s = [sb2('s_M1%d' % d, [128, 8, 2, 128]) for d in range(2)]
            M2s = [sb2('s_M2%d' % d, [128, 8, 2, 128]) for d in range(2)]
            G = [sb2('s_G%d' % d, [128, 8, 2, 128]) for d in range(2)]
            H = [sb2('s_H%d' % d, [128, 8, 2, 128], BF16) for d in range(2)]
            G0 = [sb2('s_G0%d' % d, [128, 2, 8]) for d in range(2)]
            GL = [sb2('s_GL%d' % d, [128, 2, 8]) for d in range(2)]
            rfull = sb2('s_rfull', [128, 16, 128])
            psB = [es2.enter_context(_pst(nc, 's_psB%d' % i, [128, 2, 2, 128], F32)) for i in range(4)]
            psY = [es2.enter_context(_pst(nc, 's_psY%d' % i, [128, 2, 128], F32)) for i in range(2)]
            for d in range(2):
                P.memset(G0[d][:].rearrange("p a b -> p (a b)"), 0.0, writes=['s_G0%d' % d])
            for dj in range(16):
                P.ts(rfull[:, dj, :], pos[:, 0, :], 0.0, rr[:, dj:dj + 1], ALU.mult, ALU.add, reads=['s_pos', 's_r'], writes=['s_rfull'])
            seen = set()
            tabk = [('s_tab', dj) for dj in range(16)]
            for ci in range(NT):
                for d in range(2):
                    c = chunk_order(d)[ci]
                    cs_ = slice(c * 128, (c + 1) * 128)
                    pY = psY[d]
                    pYk = 's_psY%d' % d
                    tk = tabk[d * 8:(d + 1) * 8]
                    gk = 's_G%d' % d
                    gks = [(gk, j_, r_) for j_ in range(8) for r_ in range(2)]
                    hk = 's_H%d' % d
                    bc4 = lambda q_: TAB[:, d * 8:(d + 1) * 8, q_, :].unsqueeze(2).to_broadcast([128, 8, 2, 128])
                    BU, M1, M2 = BUs[d], M1s[d], M2s[d]
                    kBU, kM1, kM2 = 's_BU%d' % d, 's_M1%d' % d, 's_M2%d' % d
                    for j in range(8):
                        ct = j // 4
                        pB = psB[j // 2]
                        pBk = 's_psB%d' % (j // 2)
                        P.mm(pB[:, j % 2, 0, :], WB[:, j, 0, :], ub[:, ct, cs_], True, True, reads=['s_WB', 's_ub'], writes=[pBk])
                        P.mm(pB[:, j % 2, 1, :], WB[:, j, 1, :], ub[:, ct, cs_], True, True, reads=['s_WB', 's_ub'], writes=[pBk])
                    for q_ in range(4):
                        P.copy(BU[:, 2 * q_:2 * q_ + 2, :, :], psB[q_][:], reads=['s_psB%d' % q_], writes=[kBU], eng='scalar')
                    P.tt(M1[:], BU[:], bc4(0), ALU.mult, reads=[kBU] + tk, writes=[kM1])
                    P.tt(M2[:], BU[:], bc4(1), ALU.mult, reads=[kBU] + tk, writes=[kM2])
                    P.tt(M1[:, :, 0, :], M1[:, :, 0, :], M2[:, :, 1, :], ALU.subtract, reads=[kM1, kM2], writes=[kM1])
                    P.tt(M1[:, :, 1, :], M1[:, :, 1, :], M2[:, :, 0, :], ALU.add, reads=[kM1, kM2], writes=[kM1])
                    for j in range(8):
                        dj = d * 8 + j
                        for ri in range(2):
                            o_ = G[d][:, j, ri, :]
                            d1 = M1[:, j, ri, :]
                            if d == 1:
                                o_, d1 = o_[:, ::-1], d1[:, ::-1]
                            P.scan(o_, rfull[:, dj, :], d1, G0[d][:, ri, j:j + 1], reads=['s_rfull', kM1, 's_G0%d' % d], writes=[(gk, j, ri)])
                    P.tt(M1[:], G[d][:], bc4(2), ALU.mult, reads=gks + tk, writes=[kM1])
                    P.tt(M2[:], G[d][:], bc4(3), ALU.mult, reads=gks + tk, writes=[kM2])
                    P.tt(H[d][:, :, 0, :], M1[:, :, 0, :], M2[:, :, 1, :], ALU.subtract, reads=[kM1, kM2], writes=[hk])
                    P.stt(H[d][:, :, 1, :], M1[:, :, 1, :], -1.0, M2[:, :, 0, :], ALU.mult, ALU.subtract, reads=[kM1, kM2], writes=[hk])
                    for j in range(8):
                        ct = j // 4
                        jj = j % 4
                        P.mm(pY[:, ct, :], WC[:, j, 0, :], H[d][:, j, 0, :], jj == 0, False, reads=['s_WC', hk], writes=[pYk])
                        P.mm(pY[:, ct, :], WC[:, j, 1, :], H[d][:, j, 1, :], False, jj == 3, reads=['s_WC', hk], writes=[pYk])
                    ok = ('s_yacc', c)
                    if ok not in seen:
                        seen.add(ok)
                        P.copy(yacc[:, :, cs_], pY[:], reads=[pYk], writes=[ok], eng='scalar')
                    else:
                        P.tt(yacc[:, :, cs_], yacc[:, :, cs_], pY[:], ALU.add, reads=[pYk, ok], writes=[ok])
                    li = 127 if d == 0 else 0
                    g0k = 's_G0%d' % d
                    glk = 's_GL%d' % d
                    P.copy(GL[d][:], G[d][:, :, :, li].rearrange("p j r -> p r j"), reads=gks, writes=[glk])
                    cbc = cb[:, 0, d * 8:(d + 1) * 8]
                    cbs = cb[:, 1, d * 8:(d + 1) * 8]
                    Rg = dict(reads=[glk, 's_cb', g0k], writes=[g0k])
                    P.tt(G0[d][:, 0, :], GL[d][:, 1, :], cbs, ALU.mult, **Rg)
                    P.tt(G0[d][:, 1, :], GL[d][:, 0, :], cbs, ALU.mult, **Rg)
                    P.tt(GL[d][:, 0, :], GL[d][:, 0, :], cbc, ALU.mult, reads=[glk, 's_cb', g0k], writes=[glk])
                    P.tt(GL[d][:, 1, :], GL[d][:, 1, :], cbc, ALU.mult, reads=[glk, 's_cb', g0k], writes=[glk])
                    P.tt(G0[d][:, 0, :], GL[d][:, 0, :], G0[d][:, 0, :], ALU.subtract, reads=[glk, g0k], writes=[g0k])
                    P.tt(G0[d][:, 1, :], GL[d][:, 1, :], G0[d][:, 1, :], ALU.add, reads=[glk, g0k], writes=[g0k])
            P.flush()
        with ExitStack() as es2:
            sb2 = lambda name, shape, dt=F32: es2.enter_context(_sbt(nc, name, list(shape), dt))
            dsk = sb2('s_dsk', [128, 2])
            gb = sb2('s_gb', [128, 2])
            gw = sb2('s_gw', [128, 2, 256], BF16)
            yb = sb2('s_yb', [128, 2, T], BF16)
            t1 = sb2('s_e1', [128, T])
            zt = [sb2('s_zt%d' % i, [128, 512]) for i in range(2)]
            ps = [es2.enter_context(_pst(nc, 's_psz%d' % i, [128, 512], F32)) for i in range(2)]
            P.dma(dsk[:], k.s5_dFM[:, l, :], writes=['s_dsk'])
            P.dma(gb[:], k.s5_gbFM[:, l, :], writes=['s_gb'])
            P.dma(gw[:], k.s5_gw[l, :, :, :], writes=['s_gw'], eng='gpsimd')
            yk = [('s_yacc', c) for c in range(NT)]
            for ct in range(2):
                y = yacc[:, ct, :]
                P.stt(y, u32[:, ct, :], dsk[:, ct:ct + 1], y, ALU.mult, ALU.add, reads=yk + ['s_u32', 's_dsk'], writes=yk)
                P.tt(t1[:], y, y, ALU.mult, reads=yk, writes=['s_e1'])
                P.ts(t1[:], t1[:], 0.044715, 1.0, ALU.mult, ALU.add, reads=['s_e1'], writes=['s_e1'])
                P.tt(t1[:], t1[:], y, ALU.mult, reads=yk + ['s_e1'], writes=['s_e1'])
                P.act(t1[:], t1[:], AF.Sigmoid, reads=['s_e1'], writes=['s_e1'], scale=2.0 * math.sqrt(2.0 / math.pi))
                P.tt(y, y, t1[:], ALU.mult, reads=yk + ['s_e1'], writes=yk)
                P.copy(yb[:, ct, :], y, reads=yk, writes=['s_yb'], eng='gpsimd')
            cnt = 0
            for nt in range(2):
                for (t0, tn) in TBLK:
                    i = cnt % 2
                    cnt += 1
                    P.mm(ps[i][:, :tn], gw[:, 0, nt * 128:(nt + 1) * 128], yb[:, 0, t0:t0 + tn], True, False, reads=['s_gw', 's_yb'], writes=['s_psz%d' % i])
                    P.mm(ps[i][:, :tn], gw[:, 1, nt * 128:(nt + 1) * 128], yb[:, 1, t0:t0 + tn], False, True, reads=['s_gw', 's_yb'], writes=['s_psz%d' % i])
                    P.act(zt[i][:, :tn], ps[i][:, :tn], AF.Sigmoid, reads=['s_psz%d' % i, 's_gb'], writes=['s_zt%d' % i], bias=gb[:, nt:nt + 1], scale=1.0)
                    P.tt(zt[i][:, :tn], zt[i][:, :tn], yacc[:, nt, t0:t0 + tn], ALU.mult, reads=['s_zt%d' % i] + yk, writes=['s_zt%d' % i])
                    P.dma(k.mixFM[b, 512 + nt * 128:512 + (nt + 1) * 128, t0:t0 + tn], zt[i][:, :tn], reads=['s_zt%d' % i])
            P.flush()


def phase_dn(k, l, b):
    P, nc = k.P, k.nc
    with ExitStack() as es:
        sbt = lambda name, shape, dt=F32: es.enter_context(_sbt(nc, name, list(shape), dt))
        qb = sbt('d_qb', [128, 2, T], BF16)
        kb = sbt('d_kb', [128, 2, T], BF16)
        kn = sbt('d_kn', [128, 2, T])
        vTM = sbt('d_vTM', [128, NT, 256])
        bTM = sbt('d_bTM', [128, NT, 64])
        gall = sbt('d_gall', [128, NT, 3, 128])
        ngc = sbt('d_ngc', [128, T])
        mh = sbt('d_mh', [128, 4])
        oh = sbt('d_oh', [128, 2, 512])
        ones4 = sbt('d_ones4', [128, 128])
        P.dma(mh[:], k.MH[:, :], writes=['d_mh'])
        P.dma(oh[:], k.OH[:, :, :], writes=['d_oh'])
        P.memset(ones4[:], 1.0, writes=['d_ones4'])
        oacc = sbt('d_oacc', [128, 2, T])
        selr = sbt('d_selr', [128, 4, 128])
        selp = sbt('d_selp', [128, 2, 128])
        mb = sbt('d_mb', [128, 2, 4, 128])
        blk2 = sbt('d_blk2', [128, 128])
        P.dma(selr[:], k.SELR[:, :, :], writes=['d_selr'])
        P.dma(selp[:], k.SELP[:, :, :], writes=['d_selp'])
        P.dma(mb[:], k.MB[:, :, :, :], writes=['d_mb'])
        P.dma(blk2[:], k.BLK2[:, :], writes=['d_blk2'])
        hm2 = sbt('d_hm2', [128, 2])
        P.dma(hm2[:], k.HM2[:, :], writes=['d_hm2'])
        with ExitStack() as es2:
            sb2 = lambda name, shape, dt=F32: es2.enter_context(_sbt(nc, name, list(shape), dt))
            x = [sb2('d_x%d' % i, [128, T]) for i in range(2)]
            acc = [sb2('d_acc%d' % i, [128, T]) for i in range(2)]
            sq = sb2('d_sq', [128, T])
            cw = sb2('d_cw', [128, 6, 5])
            eps = sb2('d_eps', [128, 1])
            ps = [es2.enter_context(_pst(nc, 'd_psA%d' % i, [128, 512], F32)) for i in range(2)]
            pst = [es2.enter_context(_pst(nc, 'd_psT%d' % i, [128, 128], F32)) for i in range(2)]
            P.dma(cw[:], k.convFM[:, l, :, :], writes=['d_cw'])
            P.memset(eps[:], EPS, writes=['d_eps'])
            cnt = 0
            for ti in range(6):
                i = ti % 2
                xk, ak = 'd_x%d' % i, 'd_acc%d' % i
                P.dma(x[i][:], k.pFM[b, (13 + ti) * 128:(14 + ti) * 128, :], writes=[xk])
                P.ts(acc[i][:], x[i][:], cw[:, ti, 2:3], None, ALU.mult, reads=[xk, 'd_cw'], writes=[ak])
                for (s0, s1) in [(0, LC), (LC, T)]:
                    for j in (0, 1, 3, 4):
                        sft = j - 2
                        lo, hi = max(s0, s0 - sft), min(s1, s1 - sft)
                        P.stt(acc[i][:, lo:hi], x[i][:, lo + sft:hi + sft], cw[:, ti, j:j + 1], acc[i][:, lo:hi], ALU.mult, ALU.add,
                              reads=[xk, 'd_cw', ak], writes=[ak])
                P.act(acc[i][:], acc[i][:], AF.Silu, reads=[ak], writes=[ak])
                if ti < 4:
                    hp = ti % 2
                    P.tt(sq[:], acc[i][:], acc[i][:], ALU.mult, reads=[ak], writes=['d_sq'], eng='gpsimd')
                    for (t0, tn) in TBLK:
                        pp = ps[cnt % 2]
                        ppk = 'd_psA%d' % (cnt % 2)
                        cnt += 1
                        P.mm(pp[:, :tn], blk2[:], sq[:, t0:t0 + tn], True, True, reads=['d_blk2', 'd_sq'], writes=[ppk])
                        P.act(x[i][:, t0:t0 + tn], pp[:, :tn], AF.Sqrt, reads=[ppk, 'd_eps', xk], writes=[xk], bias=eps[:], scale=1.0)
                    P.recip(x[i][:], x[i][:], reads=[xk], writes=[xk])
                    if ti < 2:
                        P.stt(qb[:, hp, :], acc[i][:], 0.125, x[i][:], ALU.mult, ALU.mult, reads=[ak, xk], writes=['d_qb'])
                    else:
                        P.tt(kn[:, hp, :], acc[i][:], x[i][:], ALU.mult, reads=[ak, xk], writes=['d_kn'])
                        P.copy(kb[:, hp, :], kn[:, hp, :], reads=['d_kn'], writes=['d_kb'], eng='gpsimd')
                else:
                    vt = ti - 4
                    for c in range(NT):
                        pt = pst[c % 2]
                        ptk = 'd_psT%d' % (c % 2)
                        P.tr(pt[:, :], acc[i][:, c * 128:(c + 1) * 128], k.identf[:], reads=[ak, 'identf'], writes=[ptk])
                        P.copy(vTM[:, c, vt * 128:(vt + 1) * 128], pt[:, :], reads=[ptk], writes=['d_vTM'], eng='scalar' if c % 2 else 'vector')
            P.flush()
        if k.dbg.get('dn_stop') == 'A':
            return
        with ExitStack() as es2:
            sb2 = lambda name, shape, dt=F32: es2.enter_context(_sbt(nc, name, list(shape), dt))
            ga = sb2('d_ga', [128, T])
            lb = sb2('d_lb', [128, T])
            bt = sb2('d_bt', [128, T])
            par = sb2('d_par', [128, 2])
            nA = sb2('d_nA', [128, 1])
            ones = sb2('d_ones', [128, 128])
            pst = [es2.enter_context(_pst(nc, 'd_psB%d' % i, [128, 128], F32)) for i in range(2)]
            P.dma(ga[:], k.pFM[b, 19 * 128:20 * 128, :], writes=['d_ga'])
            P.dma(lb[:], k.pFM[b, 26 * 128:27 * 128, :], writes=['d_lb'])
            P.dma(par[:], k.dn_par[:, l, :], writes=['d_par'])
            P.memset(ones[:], 1.0, writes=['d_ones'])
            P.memset(gall[:].rearrange("p a b c -> p (a b c)"), 0.0, writes=['d_gall'], eng='gpsimd')
            P.act(nA[:], par[:, 0:1], AF.Exp, reads=['d_par'], writes=['d_nA'])
            P.ts(nA[:], nA[:], -1.0, None, ALU.mult, reads=['d_nA'], writes=['d_nA'])
            P.act(ga[:], ga[:], AF.Exp, reads=['d_ga', 'd_par'], writes=['d_ga'], bias=par[:, 1:2], scale=1.0)
            P.act(ga[:], ga[:], AF.Ln, reads=['d_ga'], writes=['d_ga'], bias=1.0, scale=1.0)
            P.ts(ga[:], ga[:], nA[:, 0:1], None, ALU.mult, reads=['d_ga', 'd_nA'], writes=['d_ga'])
            P.act(lb[:], lb[:], AF.Exp, reads=['d_lb'], writes=['d_lb'], scale=-1.0)
            P.act(lb[:], lb[:], AF.Ln, reads=['d_lb'], writes=['d_lb'], bias=1.0, scale=1.0)
            P.ts(lb[:], lb[:], -1.0, None, ALU.mult, reads=['d_lb'], writes=['d_lb'])
            P.act(bt[:], lb[:], AF.Exp, reads=['d_lb'], writes=['d_bt'])
            for c in range(NT):
                cs = slice(c * 128, (c + 1) * 128)
                P.scan(gall[0:32, c, 0, :], ones[0:32, :], ga[0:32, cs], 0.0, reads=['d_ones', 'd_ga'], writes=['d_gall'])
                P.scan(gall[32:64, c, 0, :][:, ::-1], ones[32:64, :], ga[32:64, cs][:, ::-1], 0.0, reads=['d_ones', 'd_ga'], writes=['d_gall'])
            for c in range(NT):
                cs = slice(c * 128, (c + 1) * 128)
                P.tt(gall[:, c, 1, :], gall[:, c, 0, :], lb[:, cs], ALU.add, reads=['d_gall', 'd_lb'], writes=['d_gall'])
                P.ts(ngc[:, cs], gall[:, c, 0, :], -1.0, None, ALU.mult, reads=['d_gall'], writes=['d_ngc'], eng='gpsimd')
                P.ts(gall[0:32, c, 2, :], gall[0:32, c, 0, :], -1.0, gall[0:32, c, 0, 127:128], ALU.mult, ALU.add, reads=['d_gall'], writes=['d_gall'])
                P.ts(gall[32:64, c, 2, :], gall[32:64, c, 0, :], -1.0, gall[32:64, c, 0, 0:1], ALU.mult, ALU.add, reads=['d_gall'], writes=['d_gall'])
                pt = pst[c % 2]
                ptk = 'd_psB%d' % (c % 2)
                P.tr(pt[:, :], bt[:, cs], k.identf[:], reads=['d_bt', 'identf'], writes=[ptk])
                P.copy(bTM[:, c, :], pt[:, 0:64], reads=[ptk], writes=['d_bTM'], eng='scalar')
            P.flush()
        if k.dbg.get('dn_stop') == 'B':
            return
        with ExitStack() as es2:
            sb2 = lambda name, shape, dt=F32: es2.enter_context(_sbt(nc, name, list(shape), dt))
            CH = []
            for ch in range(4):
                B_ = {}
                t = 'd%d_' % ch
                B_['E'] = sb2(t + 'E', [128, 3, 128])
                B_['Qin'] = sb2(t + 'Qin', [128, 128], BF16)
                B_['KE2'] = sb2(t + 'KE2', [128, 128])
                B_['KE3'] = sb2(t + 'KE3', [128, 128], BF16)
                B_['RWpad'] = sb2(t + 'RWpad', [128, 2, 128])
                B_['RUpad'] = sb2(t + 'RUpad', [128, 2, 128])
                B_['Upad'] = sb2(t + 'Upad', [128, 2, 128], BF16)
                B_['koT'] = sb2(t + 'koT', [128, 128], BF16)
                B_['Gm'] = sb2(t + 'Gm', [128, 4, 128])
                B_['AT'] = sb2(t + 'AT', [128, 2, 128])
                B_['QKm'] = sb2(t + 'QKm', [128, 2, 128], BF16)
                B_['X'] = sb2(t + 'X', [128, 2, 2, 128])
                B_['Y'] = sb2(t + 'Y', [128, 2, 2, 128])
                B_['TT'] = sb2(t + 'TT', [128, 2, 128])
                B_['WTb'] = sb2(t + 'WTb', [128, 128])
                B_['Ub'] = sb2(t + 'Ub', [128, 128], BF16)
                B_['tmp'] = sb2(t + 'tmp', [128, 128])
                B_['S32'] = sb2(t + 'S32', [128, 128])
                B_['Sb'] = sb2(t + 'Sb', [128, 128], BF16)
                B_['Sn'] = sb2(t + 'Sn', [128, 128])
                B_['knm'] = sb2(t + 'knm', [128, 2, 128])
                B_['Rsel'] = sb2(t + 'Rsel', [128, 2, 2, 128])
                for nm in ['RWpad', 'RUpad', 'Upad']:
                    P.memset(B_[nm][:].rearrange("p a c -> p (a c)"), 0.0, writes=[t + nm], eng='gpsimd')
                for nm in ['S32', 'Sb', 'Sn']:
                    P.memset(B_[nm][:], 0.0, writes=[t + nm])
                CH.append(B_)
            bk = [es2.enter_context(_pst(nc, 'd_bank%d' % i, [128, 512], F32)) for i in range(7)]
            bA, bB, bC, bD, bE, bF, bG = bk
            psTb = es2.enter_context(_pst(nc, 'd_psTb', [128, 128], BF16))
            kk0, W0 = bA[:, 0:128], bA[:, 128:256]
            kk1, W1 = bB[:, 0:128], bB[:, 128:256]
            N0, T32, W2 = bC[:, 0:128], bC[:, 128:256], bC[:, 256:384]
            psS = bE[:, 256:384]
            N1, qk0, qk1 = bD[:, 0:128], bD[:, 128:256], bD[:, 256:384]
            N2 = bE[:, 0:128]
            N0b, N1b, N2b = bA[:, 256:384], bB[:, 256:384], bF[:, 384:512]
            psE = bF[:, 0:384]
            psD = bG[:, :]
            psKK = [kk0, kk1]
            psQK = [qk0, qk1]
            seen = set()
            for ci in range(NT):
                for d in range(2):
                    c = chunk_order(d)[ci]
                    cs = slice(c * 128, (c + 1) * 128)
                    rows = slice(32 * d, 32 * d + 4)
                    lloc = 127 if d == 0 else 0
                    for hp in range(2):
                        ch = d * 2 + hp
                        B_ = CH[ch]
                        t = 'd%d_' % ch
                        kk_ = lambda nm: t + nm
                        P.mm(psE, selp[rows, hp, :], gall[rows, c, :, :].rearrange("p a b -> p (a b)"), True, True,
                             reads=['d_selp', 'd_gall'], writes=['d_psE'])
                        P.act(B_['E'][:].rearrange("p a b -> p (a b)"), psE, AF.Exp, reads=['d_psE'], writes=[kk_('E')])
                        P.tt(B_['Qin'][:], qb[:, hp, cs], B_['E'][:, 0, :], ALU.mult, reads=['d_qb', kk_('E')], writes=[kk_('Qin')])
                        P.tt(B_['KE2'][:], kn[:, hp, cs], B_['E'][:, 1, :], ALU.mult, reads=['d_kn', kk_('E')], writes=[kk_('KE2')])
                        P.tt(B_['KE3'][:], kn[:, hp, cs], B_['E'][:, 2, :], ALU.mult, reads=['d_kn', kk_('E')], writes=[kk_('KE3')])
                        if k.dbg.get('dn_lvl', 99) < 1:
                            continue
                        P.tr(T32, B_['KE2'][:], k.identf[:], reads=[kk_('KE2'), 'identf'], writes=['d_psT32'])
                        for hh in range(2):
                            P.copy(B_['RWpad'][:, hh, hh * 64:(hh + 1) * 64], T32[:, hh * 64:(hh + 1) * 64], reads=['d_psT32'],
                                   writes=[kk_('RWpad')], eng='scalar' if hh else 'vector')
                        P.tr(psTb[:, :], B_['KE3'][:], k.identb[:], reads=[kk_('KE3'), 'identb'], writes=['d_psTb'])
                        P.copy(B_['koT'][:], psTb[:, :], reads=['d_psTb'], writes=[kk_('koT')], eng='scalar')
                        if k.dbg.get('dn_lvl', 99) < 2:
                            continue
                        for hh in range(2):
                            h = 2 * hp + hh
                            P.ts(B_['Rsel'][rows, hh, :, :], gall[rows, c, 0:2, :], mh[rows, h:h + 1], None, ALU.mult,
                                 reads=['d_gall', 'd_mh'], writes=[kk_('Rsel')])
                        P.mm(psD, ones4[rows, :], B_['Rsel'][rows, :, :, :].rearrange("p a b c -> p (a b c)"), True, False,
                             reads=['d_ones4', kk_('Rsel')], writes=['d_psD'])
                        P.mm(psD, ngc[rows, cs], oh[rows, hp, :], False, True, reads=['d_ngc', 'd_oh'], writes=['d_psD'])
                        P.tt(B_['Gm'][:].rearrange("p a b -> p (a b)"), psD, mb[:, d, :, :].rearrange("p a b -> p (a b)"), ALU.add,
                             reads=['d_psD', 'd_mb'], writes=[kk_('Gm')])
                        P.act(B_['Gm'][:], B_['Gm'][:], AF.Exp, reads=[kk_('Gm')], writes=[kk_('Gm')])
                        if k.dbg.get('dn_lvl', 99) < 3:
                            continue
                        for hh in range(2):
                            pr = slice(hh * 64, (hh + 1) * 64)
                            P.ts(B_['knm'][:, hh, :], kn[:, hp, cs], hm2[:, hh:hh + 1], None, ALU.mult, reads=['d_kn', 'd_hm2'], writes=[kk_('knm')],
                                 eng='gpsimd')
                            P.mm(psKK[hh], B_['knm'][:, hh, :], kn[:, hp, cs], True, True, reads=['d_kn', kk_('knm')], writes=['d_psKK%d' % hh])
                            P.mm(psQK[hh], kb[pr, hp, cs], qb[pr, hp, cs], True, True, reads=['d_kb', 'd_qb'], writes=['d_psQK%d' % hh])
                        for hh in range(2):
                            P.tt(B_['AT'][:, hh, :], psKK[hh], B_['Gm'][:, 2 * hh + 1, :], ALU.mult, reads=['d_psKK%d' % hh, kk_('Gm')], writes=[kk_('AT')])
                            P.tt(B_['QKm'][:, hh, :], psQK[hh], B_['Gm'][:, 2 * hh, :], ALU.mult, reads=['d_psQK%d' % hh, kk_('Gm')], writes=[kk_('QKm')])
                        if k.dbg.get('dn_lvl', 99) < 4:
                            continue
                        NB_ = [(N0, N1, N2), (N0b, N1b, N2b)]
                        Xc, Yc = [None, None], [None, None]
                        for hh in range(2):
                            n0, n1, n2 = NB_[hh]
                            X0 = B_['AT'][:, hh, :]
                            P.tr(n0, X0, k.identf[:], reads=[kk_('AT'), 'identf'], writes=['d_psN0_%d' % hh])
                            P.copy(B_['Y'][:, hh, 0, :], n0, reads=['d_psN0_%d' % hh], writes=[kk_('Y%d' % hh)], eng='scalar')
                            P.tt(B_['TT'][:, hh, :], k.identf[:], X0, ALU.subtract, reads=['identf', kk_('AT')], writes=[kk_('TT%d' % hh)])
                            Xc[hh], Yc[hh] = X0, B_['Y'][:, hh, 0, :]
                        for lv in range(1, 7):
                            for hh in range(2):
                                n0, n1, n2 = NB_[hh]
                                ks = [kk_('X%d' % hh), kk_('Y%d' % hh), kk_('AT')]
                                if lv < 6:
                                    P.mm(n0, Yc[hh], Xc[hh], True, True, reads=ks, writes=['d_psN0_%d' % hh])
                                P.mm(n1, Xc[hh], Yc[hh], True, True, reads=ks, writes=['d_psN1_%d' % hh])
                            for hh in range(2):
                                n0, n1, n2 = NB_[hh]
                                Yn = B_['Y'][:, hh, lv % 2, :]
                                if lv < 6:
                                    Xn = B_['X'][:, hh, lv % 2, :]
                                    P.copy(Xn, n0, reads=['d_psN0_%d' % hh], writes=[kk_('X%d' % hh)], eng='scalar')
                                    Xc[hh] = Xn
                                P.copy(Yn, n1, reads=['d_psN1_%d' % hh], writes=[kk_('Y%d' % hh)])
                                Yc[hh] = Yn
                            for hh in range(2):
                                n0, n1, n2 = NB_[hh]
                                P.mm(n2, Yc[hh], B_['TT'][:, hh, :], True, True, reads=[kk_('Y%d' % hh), kk_('TT%d' % hh)], writes=['d_psN2_%d' % hh])
                            for hh in range(2):
                                n0, n1, n2 = NB_[hh]
                                P.tt(B_['TT'][:, hh, :], B_['TT'][:, hh, :], n2, ALU.add, reads=['d_psN2_%d' % hh, kk_('TT%d' % hh)],
                                     writes=[kk_('TT%d' % hh)], eng='vector')
                        if k.dbg.get('dn_lvl', 99) < 5:
                            continue
                        for hh in range(2):
                            P.mm(W0, B_['RWpad'][:, hh, :], B_['TT'][:, hh, :], hh == 0, hh == 1, reads=[kk_('RWpad'), kk_('TT0'), kk_('TT1')], writes=['d_psW0'])
                        if k.dbg.get('dn_sub', 9) < 1:
                            continue
                        P.copy(B_['WTb'][:], W0, reads=['d_psW0'], writes=[kk_('WTb')], eng='scalar')
                        for hh in range(2):
                            h = 2 * hp + hh
                            P.ts(B_['RUpad'][:, hh, hh * 64:(hh + 1) * 64], vTM[:, c, hp * 128 + hh * 64:hp * 128 + (hh + 1) * 64],
                                 bTM[:, c, 32 * d + h:32 * d + h + 1], None, ALU.mult, reads=['d_vTM', 'd_bTM'], writes=[kk_('RUpad')], eng='gpsimd')
                        if k.dbg.get('dn_sub', 9) < 2:
                            continue
                        P.mm(W1, B_['TT'][:, 0, :], B_['RUpad'][:, 0, :], True, False, reads=[kk_('TT0'), kk_('RUpad')], writes=['d_psW1'])
                        P.mm(W1, B_['TT'][:, 1, :], B_['RUpad'][:, 1, :], False, False, reads=[kk_('TT1'), kk_('RUpad')], writes=['d_psW1'])
                        P.mm(W1, B_['WTb'][:], B_['Sn'][:], False, True, reads=[kk_('WTb'), kk_('Sn')], writes=['d_psW1'])
                        if k.dbg.get('dn_sub', 9) < 3:
                            continue
                        P.ts(B_['Ub'][:], W1, 1.0, None, ALU.mult, reads=['d_psW1'], writes=[kk_('Ub')])
                        for hh in range(2):
                            P.ts(B_['Upad'][:, hh, hh * 64:(hh + 1) * 64], W1[:, hh * 64:(hh + 1) * 64], 1.0, None, ALU.mult, reads=['d_psW1'], writes=[kk_('Upad')])
                        if k.dbg.get('dn_lvl', 99) < 6:
                            continue
                        P.mm(W2, B_['Upad'][:, 0, :], B_['QKm'][:, 0, :], True, False, reads=[kk_('Upad'), kk_('QKm')], writes=['d_psW2'])
                        P.mm(W2, B_['Upad'][:, 1, :], B_['QKm'][:, 1, :], False, False, reads=[kk_('Upad'), kk_('QKm')], writes=['d_psW2'])
                        P.mm(W2, B_['Sb'][:], B_['Qin'][:], False, True, reads=[kk_('Sb'), kk_('Qin')], writes=['d_psW2'])
                        ok = ('d_oacc', hp, c)
                        if ok not in seen:
                            seen.add(ok)
                            P.copy(oacc[:, hp, cs], W2, reads=['d_psW2'], writes=[ok], eng='scalar')
                        else:
                            P.tt(oacc[:, hp, cs], oacc[:, hp, cs], W2, ALU.add, reads=['d_psW2', ok], writes=[ok])
                        if k.dbg.get('dn_lvl', 99) < 7:
                            continue
                        P.mm(psS, B_['koT'][:], B_['Ub'][:], True, True, reads=[kk_('koT'), kk_('Ub')], writes=['d_psS'])
                        if k.dbg.get('dn_sub8', 9) < 1:
                            continue
                        P.tt(B_['tmp'][:], psS, blk2[:], ALU.mult, reads=['d_psS', 'd_blk2'], writes=[kk_('tmp')])
                        if k.dbg.get('dn_sub8', 9) < 2:
                            continue
                        P.stt(B_['S32'][:], B_['S32'][:], B_['E'][:, 0, lloc:lloc + 1], B_['tmp'][:], ALU.mult, ALU.add,
                              reads=[kk_('S32'), kk_('E'), kk_('tmp')], writes=[kk_('S32')])
                        if k.dbg.get('dn_sub8', 9) < 3:
                            continue
                        P.copy(B_['Sb'][:], B_['S32'][:], reads=[kk_('S32')], writes=[kk_('Sb')], eng='scalar')
                        if k.dbg.get('dn_sub8', 9) < 4:
                            continue
                        P.ts(B_['Sn'][:], B_['S32'][:], -1.0, None, ALU.mult, reads=[kk_('S32')], writes=[kk_('Sn')])
            P.flush()
        for hp in range(2):
            with ExitStack() as es2:
                norm_gate(k, P, nc, es2, b, oacc[:, hp, :], [('d_oacc', hp, c) for c in range(NT)], 24 + hp, 768 + hp * 128, False, 'd%d' % hp)
                P.flush()


def phase_outproj_moe(k, l, b, last):
    P, nc = k.P, k.nc
    t_first = 2 if last else 0
    with ExitStack() as es:
        sbt = lambda name, shape, dt=F32: es.enter_context(_sbt(nc, name, list(shape), dt))
        h2T = sbt('h2T', [128, 8, T], BF16)
        comb = sbt('comb', [128, NT, NE])
        k.eps_col = sbt('eps_col', [128, 1])
        rw = sbt('rw', [128, 8, NE])
        rb = sbt('rb', [128, NE])
        fg = sbt('fg', [128, D])
        P.memset(k.eps_col[:], EPS, writes=['eps'])
        P.dma(rw[:], k.router_w[:, :, :], writes=['rw'])
        P.dma(rb[:], k.router_b_rep[:, :], writes=['rb'])
        P.dma(fg[:], k.final_g_rep[:, :], writes=['fg'])
        ps = [es.enter_context(_pst(nc, 'ps%d' % i, [128, 512], F32)) for i in range(8)]
        with ExitStack() as es2:
            sb2 = lambda name, shape, dt=F32: es2.enter_context(_sbt(nc, name, list(shape), dt))
            mixT = sb2('mixT', [128, 8, T], BF16)
            wo = sb2('wo', [128, 8, D], BF16)
            grep_ = sb2('grep', [128, 2, D])
            xt = [sb2('xt%d' % i, [128, D]) for i in range(2)]
            xn = [sb2('xn%d' % i, [128, D]) for i in range(2)]
            h32 = [sb2('h32_%d' % i, [128, 8, 128]) for i in range(2)]
            ssq = [sb2('ssq%d' % i, [128, 1]) for i in range(2)]
            rstd = [sb2('rstd%d' % i, [128, 1]) for i in range(2)]
            sc = [sb2('rsc%d' % i, [128, 8, NE]) for i in range(2)]
            for kc in range(8):
                P.dma(wo[:, kc, :], k.w_out[l, :, kc, :], writes=['wo'], eng='gpsimd')
                P.dma(mixT[:, kc, :], k.mixFM[b, kc * 128:(kc + 1) * 128, :], writes=['mixT'], eng='gpsimd')
            for gi, col in enumerate([b, 2]):
                P.dma(grep_[:, gi, :], k.gateD[l, 0, col, :].partition_broadcast(128), writes=['grep'])
            for tt in range(t_first, NT):
                i = tt % 2
                tag = str(i)
                gi = 1 if tt < 2 else 0
                col = 2 if tt < 2 else b
                P.dma(xt[i][:], resid_src(k, l, b, tt), writes=['xt' + tag])
                for half in range(2):
                    pt = ps[4 + half]
                    pk = 'ps%d' % (4 + half)
                    for kc in range(8):
                        P.mm(pt[:, :], mixT[:, kc, tt * 128:(tt + 1) * 128], wo[:, kc, half * 512:(half + 1) * 512],
                             kc == 0, kc == 7, reads=['mixT', 'wo'], writes=[pk])
                    P.tt(xn[i][:, half * 512:(half + 1) * 512], pt[:, :], grep_[:, gi, half * 512:(half + 1) * 512], ALU.mult,
                         reads=[pk, 'grep'], writes=['xn' + tag])
                P.tt(xt[i][:], xt[i][:], xn[i][:], ALU.add, reads=['xt' + tag, 'xn' + tag], writes=['xt' + tag])
                P.dma(resid_dst(k, b, tt), xt[i][:], reads=['xt' + tag])
                if 'x_mid' in k.dbg_out and l == k.dbg.get('l', 0) and b == 0:
                    P.dma(k.dbg_out['x_mid'][tt * 128:(tt + 1) * 128, :], xt[i][:], reads=['xt' + tag])
                norm_modulate_tile(k, P, xt[i][:], 'xt' + tag, l, 1, col, ssq[i], rstd[i], xn[i],
                                   [ps[2 * i], ps[2 * i + 1]], ['ps%d' % (2 * i), 'ps%d' % (2 * i + 1)],
                                   [h2T[:, fc, tt * 128:(tt + 1) * 128] for fc in range(8)],
                                   [('h2T', tt)] * 8, tag, h32=None if k.dbg.get('norouter') else h32[i], h32key='h32_' + tag)
                pr = ps[6 + i]
                prk = 'ps%d' % (6 + i)
                for kc in range(0 if k.dbg.get('norouter') else 8):
                    P.mm(pr[:, 0:NE], h32[i][:, kc, :], rw[:, kc, :], kc == 0, kc == 7,
                         reads=['h32_' + tag, 'rw'], writes=[prk])
                if not k.dbg.get('norouter'):
                    router_tile(k, P, pr[:, 0:NE], prk, rb, sc[i], 'rsc' + tag, comb[:, tt, :], ('comb', tt))
            P.flush()
        if k.dbg.get('stopA'):
            return
        with ExitStack() as es2:
            sb2 = lambda name, shape, dt=F32: es2.enter_context(_sbt(nc, name, list(shape), dt))
            facc = sb2('facc', [128, NT, D])
            for tt in range(NT):
                P.memset(facc[:, tt, :], 0.0, writes=[('facc', tt, 0), ('facc', tt, 1)], eng='gpsimd')
            blks = [(t0, tn) for (t0, tn) in TBLK]
            if last:
                blks = [(256, 512), (768, 512), (1280, 512), (1792, 512)]
            with ExitStack() as es3:
                sb3 = lambda name, shape, dt=F32: es3.enter_context(_sbt(nc, name, list(shape), dt))
                wg = [sb3('wg%d' % i, [128, 8, DFF], BF16) for i in range(2)]
                wu = [sb3('wu%d' % i, [128, 8, DFF], BF16) for i in range(2)]
                wd = [sb3('wd%d' % i, [128, 4, D], BF16) for i in range(2)]
                actT = [sb3('actT%d' % i, [128, 4, 512], BF16) for i in range(2)]
                sg = [sb3('sg%d' % i, [128, 512]) for i in range(2)]
                cnt = 0
                for e in range(NE):
                    i = e % 2
                    si = str(i)
                    for kc in range(8):
                        P.dma(wg[i][:, kc, :], k.moe_wg[l, e, :, kc, :], writes=['wg' + si], eng='gpsimd')
                        P.dma(wu[i][:, kc, :], k.moe_wu[l, e, :, kc, :], writes=['wu' + si], eng='gpsimd')
                    for fc in range(4):
                        P.dma(wd[i][:, fc, :], k.moe_wd[l, e, :, fc, :], writes=['wd' + si], eng='gpsimd')
                    for (t0, tn) in blks:
                        a = actT[cnt % 2]
                        ak = 'actT%d' % (cnt % 2)
                        cnt += 1
                        hk = [('h2T', t0 // 128 + j) for j in range(tn // 128)]
                        for fc in range(4):
                            pg = ps[fc % 2]
                            pu = ps[2 + fc % 2]
                            pgk, puk = 'ps%d' % (fc % 2), 'ps%d' % (2 + fc % 2)
                            s_ = sg[fc % 2]
                            sk = 'sg%d' % (fc % 2)
                            for kc in range(8):
                                P.mm(pg[:, :tn], wg[i][:, kc, fc * 128:(fc + 1) * 128], h2T[:, kc, t0:t0 + tn], kc == 0, kc == 7,
                                     reads=['wg' + si] + hk, writes=[pgk])
                            for kc in range(8):
                                P.mm(pu[:, :tn], wu[i][:, kc, fc * 128:(fc + 1) * 128], h2T[:, kc, t0:t0 + tn], kc == 0, kc == 7,
                                     reads=['wu' + si] + hk, writes=[puk])
                            P.act(s_[:, :tn], pg[:, :tn], AF.Silu, reads=[pgk], writes=[sk])
                            P.tt(a[:, fc, :tn], s_[:, :tn], pu[:, :tn], ALU.mult, reads=[sk, puk], writes=[ak])
                        for j in range(tn // 128):
                            tt = t0 // 128 + j
                            for half in range(2):
                                pd = ps[4 + (2 * j + half) % 4]
                                pdk = 'ps%d' % (4 + (2 * j + half) % 4)
                                for fc in range(4):
                                    P.mm(pd[:, :], a[:, fc, j * 128:(j + 1) * 128], wd[i][:, fc, half * 512:(half + 1) * 512],
                                         fc == 0, fc == 3, reads=[ak, 'wd' + si], writes=[pdk])
                                fs = facc[:, tt, half * 512:(half + 1) * 512]
                                P.stt(fs, pd[:, :], comb[:, tt, e:e + 1], fs, ALU.mult, ALU.add,
                                      reads=[pdk, ('comb', tt), ('facc', tt, half)], writes=[('facc', tt, half)])
                P.flush()
            grep2 = sb2('grep2', [128, 2, D])
            xm = [sb2('xm%d' % i, [128, D]) for i in range(2)]
            ssq = sb2('ssqf', [128, 1])
            rstd = sb2('rstdf', [128, 1])
            junk = sb2('junkf', [128, D])
            for gi, col in enumerate([b, 2]):
                P.dma(grep2[:, gi, :], k.gateD[l, 1, col, :].partition_broadcast(128), writes=['grep2'])
            for tt in range(t_first, NT):
                gi = 1 if tt < 2 else 0
                i = tt % 2
                xk = 'xm%d' % i
                fk = [('facc', tt, 0), ('facc', tt, 1)]
                P.dma(xm[i][:], resid_dst(k, b, tt), writes=[xk])
                P.tt(facc[:, tt, :], facc[:, tt, :], grep2[:, gi, :], ALU.mult, reads=fk + ['grep2'], writes=fk)
                P.tt(xm[i][:], xm[i][:], facc[:, tt, :], ALU.add, reads=fk + [xk], writes=[xk])
                if 'x_end' in k.dbg_out and l == k.dbg.get('l', 0) and b == 0:
                    P.dma(k.dbg_out['x_end'][tt * 128:(tt + 1) * 128, :], xm[i][:], reads=[xk])
                if 'f_out' in k.dbg_out and l == k.dbg.get('l', 0) and b == 0:
                    P.dma(k.dbg_out['f_out'][tt * 128:(tt + 1) * 128, :], facc[:, tt, :], reads=fk)
                if not last:
                    P.dma(resid_dst(k, b, tt), xm[i][:], reads=[xk])
                else:
                    P.op('scalar', lambda s, i=i: s.activation(out=junk[:], in_=xm[i][:], func=AF.Square, accum_out=ssq[:]),
                         reads=[xk], writes=['junkf', 'ssqf'])
                    P.act(rstd[:], ssq[:], AF.Sqrt, reads=['ssqf'], writes=['rstdf'], scale=1.0 / D, bias=k.eps_col[:])
                    P.recip(rstd[:], rstd[:], reads=['rstdf'], writes=['rstdf'])
                    P.stt(xm[i][:], xm[i][:], rstd[:, 0:1], fg[:], ALU.mult, ALU.mult,
                          reads=[xk, 'rstdf', 'fg'], writes=[xk])
                    P.dma(k.out[b, (tt - 2) * 128:(tt - 1) * 128, :], xm[i][:], reads=[xk])
            P.flush()


def router_tile(k, P, logits, lk, rb, sc, sck, comb_out, ck):
    BIG = 1.0e4
    s = sc[:, 0, :]
    sel = sc[:, 1, :]
    t1 = sc[:, 2, :]
    t2 = sc[:, 3, :]
    m1 = sc[:, 4, 0:4]
    m2 = sc[:, 4, 4:8]
    gs = sc[:, 4, 8:12]
    gm = sc[:, 4, 12:13]
    ing = sc[:, 5, 0:4]
    e1 = sc[:, 6, :]
    mx = sc[:, 5, 4:5]
    mx2 = sc[:, 5, 5:6]
    ws = sc[:, 5, 6:7]
    R = dict(reads=[sck], writes=[sck])
    P.act(s, logits, AF.Sigmoid, reads=[lk], writes=[sck])
    P.tt(sel, s, rb[:, :], ALU.add, reads=[sck, 'rb'], writes=[sck])
    sel3 = sc[:, 1, :].rearrange("p (g e) -> p g e", g=4)
    t13 = sc[:, 2, :].rearrange("p (g e) -> p g e", g=4)
    P.red(m1, sel3, ALU.max, **R)
    P.tt(t13, sel3, m1.unsqueeze(2).to_broadcast([128, 4, 4]), ALU.is_equal, **R)
    P.stt(t1, t1, -BIG, sel, ALU.mult, ALU.add, **R)
    P.red(m2, t13, ALU.max, **R)
    P.tt(gs, m1, m2, ALU.add, **R)
    P.red(gm, gs, ALU.max, **R)
    P.ts(ing, gs, gm, None, ALU.is_equal, **R)
    P.ts(ing, ing, -1.0, BIG, ALU.add, ALU.mult, **R)
    P.tt(t13, sel3, ing.unsqueeze(2).to_broadcast([128, 4, 4]), ALU.add, **R)
    P.red(mx, t1, ALU.max, **R)
    P.ts(e1, t1, mx, None, ALU.is_equal, **R)
    P.stt(t2, e1, -BIG, t1, ALU.mult, ALU.add, **R)
    P.red(mx2, t2, ALU.max, **R)
    P.ts(t2, t2, mx2, None, ALU.is_equal, **R)
    P.tt(e1, e1, t2, ALU.add, **R)
    P.tt(e1, e1, s, ALU.mult, **R)
    P.red(ws, e1, ALU.add, **R)
    P.recip(ws, ws, **R)
    P.ts(comb_out, e1, ws, None, ALU.mult, reads=[sck], writes=[ck])


def phase_dbg(k):
    P, nc = k.P, k.nc
    outs = k.dbg_out
    if not outs:
        return
    with ExitStack() as es:
        t = es.enter_context(_sbt(nc, 'dbgt', [128, T], F32))
        if 'pFM' in outs:
            for cb in range(NFM // 128):
                P.dma(t[:], k.pFM[0, cb * 128:(cb + 1) * 128, :], writes=['dbgt'])
                P.dma(outs['pFM'][cb * 128:(cb + 1) * 128, :], t[:], reads=['dbgt'])
        if 'pTM' in outs:
            for tt in range(NT):
                P.dma(t[:, :NTM], k.pTM[0, tt * 128:(tt + 1) * 128, :], writes=['dbgt'])
                P.dma(outs['pTM'][tt * 128:(tt + 1) * 128, :], t[:, :NTM], reads=['dbgt'])
        if 'mixFM' in outs:
            for cb in range(8):
                P.dma(t[:], k.mixFM[0, cb * 128:(cb + 1) * 128, :], writes=['dbgt'])
                P.dma(outs['mixFM'][cb * 128:(cb + 1) * 128, :], t[:], reads=['dbgt'])
        if 'modFM' in outs:
            P.dma(outs['modFM'][:, :], k.modFM[:].rearrange("p a b c -> p (a b c)"), reads=['modFM'])
        P.flush()


_CACHE = {}


def kernel(**inputs):
    inp = {kk: np.asarray(v) for kk, v in inputs.items()}
    sh = host_prep(inp)
    if 'nc' not in _CACHE:
        _CACHE['nc'] = build()
    nc = _CACHE['nc']
    in_maps = []
    for c in range(8):
        m = dict(sh)
        m.update(core_inputs(inp, c))
        in_maps.append(m)
    res = run_bass_kernel_spmd(nc, in_maps, core_ids=list(range(8)))
    out = np.concatenate([r['out'] for r in res.results], axis=0)
    return out.astype(np.float32)
```
